# Optimizing a Trainium2 kernel written in Bass

```python
import jax, jax.numpy as jnp
from jax import lax
import numpy as np

D_MODEL = 1024
BATCH = 16
SEQ = 2048
DEPTH = 2

CTX_LEN = 256
GRID_W = 64
RET_DK = 128
RET_DV = 128
RET_HEADS = D_MODEL // RET_DK
RET_CHUNK = 128
NA_DH = 64
NA_HEADS = D_MODEL // NA_DH
NA_KH = 8
NA_KW = 16
LRU_W = D_MODEL
LRU_BLOCKS = 8
LRU_BS = LRU_W // LRU_BLOCKS
LRU_CONV = 4
LRU_C = 8.0
N_EXPERTS = 32
TOP_K = 4
D_EXPERT = D_MODEL
SWIGLU_LIMIT = 7.0
SWIGLU_ALPHA = 1.702
ROPE_BASE = 10000.0
NORM_EPS = 1e-6
N_BRANCH = 3
RET_W = RET_HEADS * RET_DK
RET_VW = RET_HEADS * RET_DV
NA_W = NA_HEADS * NA_DH
IN_SPLITS = (RET_W, RET_W, RET_VW, RET_VW, NA_W, NA_W, NA_W, LRU_W, LRU_W, D_MODEL, D_MODEL, D_MODEL)
IN_W = sum(IN_SPLITS)

kernel_name = "hybrid_retention_natten_rglru_moe_dit"


def _rmsnorm(t, g):
    tf = t.astype(jnp.float32)
    y = tf * lax.rsqrt(jnp.mean(tf * tf, axis=-1, keepdims=True) + NORM_EPS)
    return (y * g.astype(jnp.float32)).astype(t.dtype)


def _modulate(t, g, shift, scale):
    return _rmsnorm(t, g) * (1.0 + scale) + shift


def _split_cols(p):
    return jnp.split(p, np.cumsum(IN_SPLITS)[:-1].tolist(), axis=-1)


def _heads(t, n):
    b, tl, _ = t.shape
    return t.reshape(b, tl, n, -1).transpose(0, 2, 1, 3)


def _merge_heads(t):
    b, n, tl, d = t.shape
    return t.transpose(0, 2, 1, 3).reshape(b, tl, n * d)


def _rope_1d(u, pos):
    nf = u.shape[-1] // 2
    inv = ROPE_BASE ** (-jnp.arange(nf, dtype=jnp.float32) / nf)
    ang = pos.astype(jnp.float32)[:, None] * inv
    cos, sin = jnp.cos(ang).astype(u.dtype), jnp.sin(ang).astype(u.dtype)
    u1, u2 = u[..., :nf], u[..., nf:]
    return jnp.concatenate([u1 * cos - u2 * sin, u1 * sin + u2 * cos], axis=-1)


def _axial_rope(t, rows, cols):
    half = t.shape[-1] // 2
    return jnp.concatenate([_rope_1d(t[..., :half], rows), _rope_1d(t[..., half:], cols)], axis=-1)


def _retention_chunks(q, k, v, log_g, state0, include_diag):
    b, h, tl, dk = q.shape
    dv = v.shape[-1]
    n = tl // RET_CHUNK
    qc = q.reshape(b, h, n, RET_CHUNK, dk)
    kc = k.reshape(b, h, n, RET_CHUNK, dk)
    vc = v.reshape(b, h, n, RET_CHUNK, dv)
    pos = jnp.arange(RET_CHUNK, dtype=jnp.float32)
    diff = pos[:, None] - pos[None, :]
    mask = (diff >= 0) if include_diag else (diff > 0)
    decay = jnp.where(mask[None], jnp.exp(jnp.maximum(diff, 0.0)[None] * log_g[:, None, None]), 0.0)
    s = jnp.einsum('bhnid,bhnjd->bhnij', qc, kc) * decay[:, None]
    intra = jnp.einsum('bhnij,bhnje->bhnie', s, vc)
    zeta = jnp.exp((RET_CHUNK - 1.0 - pos)[None] * log_g[:, None])
    u = jnp.einsum('bhnjd,bhnje->nbhde', kc * zeta[:, None, :, None], vc)
    g_chunk = jnp.exp(RET_CHUNK * log_g)[:, None, None]

    def step(carry, u_n):
        return g_chunk * carry + u_n, carry

    _, s_prev = lax.scan(step, state0, u)
    xi = jnp.exp((pos + 1.0)[None] * log_g[:, None])
    cross = jnp.einsum('bhnid,nbhde->bhnie', qc * xi[:, None, :, None], s_prev)
    return (intra + cross).reshape(b, h, tl, dv)


def _retention_state(k, v, log_g):
    tl = k.shape[2]
    w = jnp.exp((tl - 1.0 - jnp.arange(tl, dtype=jnp.float32))[None] * log_g[:, None])
    return jnp.einsum('bhtd,bhte->bhde', k * w[None, :, :, None], v)


def _retention(qc, kc, vc, ql, kl, vl, logit_f, logit_b, ctx_out):
    lg_f = jax.nn.log_sigmoid(logit_f.astype(jnp.float32))
    lg_b = jax.nn.log_sigmoid(logit_b.astype(jnp.float32))
    fl = lambda t: jnp.flip(t, axis=2)
    s_f = _retention_state(kc, vc, lg_f)
    s_b = _retention_state(fl(kc), fl(vc), lg_b)
    yl = (_retention_chunks(ql, kl, vl, lg_f, s_f, True)
          + fl(_retention_chunks(fl(ql), fl(kl), fl(vl), lg_b, s_b, False)))
    yc = None
    if ctx_out:
        z = jnp.zeros_like(s_f)
        yc = (_retention_chunks(qc, kc, vc, lg_f, z, True)
              + fl(_retention_chunks(fl(qc), fl(kc), fl(vc), lg_b, z, False)))
    return yl, yc


def _head_rms(t):
    return t * lax.rsqrt(jnp.mean(t * t, axis=-1, keepdims=True) + NORM_EPS)


def _neighbourhood_attention(q, k, v, kc, vc, rpb, n_rows):
    b, h, tl, dh = q.shape
    kh, kw = min(NA_KH, n_rows), NA_KW
    grid = lambda t: t.reshape(b, h, n_rows, GRID_W, dh)
    qg, kg, vg = grid(q), grid(k), grid(v)
    cols = jnp.arange(GRID_W)
    col_idx = jnp.clip(cols - kw // 2, 0, GRID_W - kw)[:, None] + jnp.arange(kw)
    dc = col_idx - cols[:, None] + (NA_KW - 1)

    def row_block(r):
        r0 = jnp.clip(r - kh // 2, 0, n_rows - kh)
        kb = lax.dynamic_slice_in_dim(kg, r0, kh, axis=2)[:, :, :, col_idx]
        vb = lax.dynamic_slice_in_dim(vg, r0, kh, axis=2)[:, :, :, col_idx]
        qr = lax.dynamic_index_in_dim(qg, r, axis=2, keepdims=False)
        dr = r0 + jnp.arange(kh) - r + (NA_KH - 1)
        bias = rpb[:, dr[:, None, None], dc[None]].transpose(0, 2, 1, 3)
        s_loc = (jnp.einsum('bhwd,bhrwkd->bhwrk', qr, kb) + bias[None]).reshape(b, h, GRID_W, kh * kw)
        s_ctx = jnp.einsum('bhwd,bhmd->bhwm', qr, kc)
        p = jax.nn.softmax(jnp.concatenate([s_loc, s_ctx], axis=-1).astype(jnp.float32), axis=-1).astype(v.dtype)
        p_loc = p[..., :kh * kw].reshape(b, h, GRID_W, kh, kw)
        return (jnp.einsum('bhwrk,bhrwkd->bhwd', p_loc, vb)
                + jnp.einsum('bhwm,bhmd->bhwd', p[..., kh * kw:], vc))

    o = lax.map(row_block, jnp.arange(n_rows))
    return o.transpose(1, 2, 0, 3, 4).reshape(b, h, tl, dh)


def _ctx_attention(q, k, v):
    s = jnp.einsum('bhqd,bhkd->bhqk', q, k).astype(jnp.float32)
    p = jax.nn.softmax(s, axis=-1).astype(v.dtype)
    return jnp.einsum('bhqk,bhkd->bhqd', p, v)


def _centred_dwconv(t, w, bias):
    out = lax.conv_general_dilated(t, w[:, None, :], window_strides=(1,),
                                   padding=[((LRU_CONV - 1) // 2, LRU_CONV // 2)],
                                   dimension_numbers=('NWC', 'WIO', 'NWC'),
                                   feature_group_count=t.shape[-1])
    return out + bias


def _lin_combine(lhs, rhs):
    a1, b1 = lhs
    a2, b2 = rhs
    return a1 * a2, a2 * b1 + b2


def _rglru_scan(u, wa, ba, wx, bx, lam, h0):
    b, tl, _ = u.shape
    ub = u.reshape(b, tl, LRU_BLOCKS, LRU_BS)
    r = jax.nn.sigmoid(jnp.einsum('btkc,kcd->btkd', ub, wa).reshape(b, tl, LRU_W) + ba)
    i = jax.nn.sigmoid(jnp.einsum('btkc,kcd->btkd', ub, wx).reshape(b, tl, LRU_W) + bx)
    log_a = -LRU_C * r * jax.nn.softplus(-lam)
    a = jnp.exp(log_a)
    bterm = jnp.sqrt(-jnp.expm1(2.0 * log_a)) * (i * u)
    a_cum, h = lax.associative_scan(_lin_combine, (a, bterm), axis=1)
    return h + a_cum * h0[:, None, :]


def _rglru_mixer(xc, xl, conv_w, conv_b, wa, ba, wx, bx, lam, ctx_out):
    f32 = jnp.float32
    uc = _centred_dwconv(xc, conv_w, conv_b).astype(f32)
    ul = _centred_dwconv(xl, conv_w, conv_b).astype(f32)
    wa, ba, wx, bx, lam = (t.astype(f32) for t in (wa, ba, wx, bx, lam))
    fl = lambda t: jnp.flip(t, axis=1)
    z = jnp.zeros((xl.shape[0], LRU_W), f32)
    hc_f = _rglru_scan(uc, wa[0], ba[0], wx[0], bx[0], lam[0], z)
    hc_b = _rglru_scan(fl(uc), wa[1], ba[1], wx[1], bx[1], lam[1], z)
    hl = (_rglru_scan(ul, wa[0], ba[0], wx[0], bx[0], lam[0], hc_f[:, -1])
          + fl(_rglru_scan(fl(ul), wa[1], ba[1], wx[1], bx[1], lam[1], hc_b[:, -1])))
    hc = (hc_f + fl(hc_b)) if ctx_out else None
    return hl, hc


def _moe(t, w_router, b_router, w_up, b_up, w_down, b_down):
    logits = (t @ w_router + b_router).astype(jnp.float32)
    top_v, top_i = lax.top_k(logits, TOP_K)
    top_w = jax.nn.softmax(top_v, axis=-1)
    gates = jnp.einsum('nk,nke->ne', top_w, jax.nn.one_hot(top_i, N_EXPERTS, dtype=jnp.float32)).astype(t.dtype)
    out = jnp.zeros_like(t)
    for e in range(N_EXPERTS):
        hu = t @ w_up[e] + b_up[e]
        glu = jnp.minimum(hu[:, 0::2], SWIGLU_LIMIT)
        lin = jnp.clip(hu[:, 1::2], -SWIGLU_LIMIT, SWIGLU_LIMIT)
        act = glu * jax.nn.sigmoid(SWIGLU_ALPHA * glu) * (lin + 1.0)
        out = out + gates[:, e:e + 1] * (act @ w_down[e] + b_down[e])
    return out


def _layer(x, xc, c, c_ctx, w_mod, b_mod, norm1_g, norm2_g, w_mix_in, ret_decay_fwd, ret_decay_bwd,
           na_rel_bias, lru_conv_w, lru_conv_b, lru_gate_a_w, lru_gate_a_b, lru_gate_x_w, lru_gate_x_b,
           lru_lambda, w_branch, w_mix_out, w_router, b_router, w_expert_up, b_expert_up,
           w_expert_down, b_expert_down, last):
    f32 = jnp.float32
    b, tl, d = x.shape
    n_rows = tl // GRID_W
    tok = jnp.arange(tl)
    rows, cols = tok // GRID_W, tok % GRID_W
    need_ctx = not last

    mod = (jax.nn.silu(c) @ w_mod + b_mod)[:, None, :]
    mod_c = (jax.nn.silu(c_ctx) @ w_mod + b_mod)[None, None, :]
    sh1, sc1, g1, sh2, sc2, g2 = jnp.split(mod, 6, axis=-1)
    sh1c, sc1c, g1c, sh2c, sc2c, g2c = jnp.split(mod_c, 6, axis=-1)

    rq, rk, rv, rg, nq, nk, nv, lx, ly, ga, gb, gc = _split_cols(_modulate(x, norm1_g, sh1, sc1) @ w_mix_in)
    rqc, rkc, rvc, rgc, nqc, nkc, nvc, lxc, lyc, gac, gbc, gcc = _split_cols(
        _modulate(xc, norm1_g, sh1c, sc1c) @ w_mix_in)

    k_scale = RET_DK ** -0.5
    ret_l, ret_c = _retention(
        _heads(rqc, RET_HEADS).astype(f32), (_heads(rkc, RET_HEADS) * k_scale).astype(f32),
        _heads(rvc, RET_HEADS).astype(f32),
        _axial_rope(_heads(rq, RET_HEADS), rows, cols).astype(f32),
        (_axial_rope(_heads(rk, RET_HEADS), rows, cols) * k_scale).astype(f32),
        _heads(rv, RET_HEADS).astype(f32),
        ret_decay_fwd, ret_decay_bwd, need_ctx)

    q_scale = NA_DH ** -0.5
    kc_na, vc_na = _heads(nkc, NA_HEADS), _heads(nvc, NA_HEADS)
    na_l = _neighbourhood_attention(_heads(nq, NA_HEADS) * q_scale, _heads(nk, NA_HEADS), _heads(nv, NA_HEADS),
                                    kc_na, vc_na, na_rel_bias, n_rows)

    lru_l, lru_c = _rglru_mixer(lxc, lx, lru_conv_w, lru_conv_b, lru_gate_a_w, lru_gate_a_b,
                                lru_gate_x_w, lru_gate_x_b, lru_lambda, need_ctx)

    def merge(ret, rgate, na, lru, ygate, gate_a, gate_b, gate_c):
        out_a = (_merge_heads(_head_rms(ret)).astype(rgate.dtype) * jax.nn.silu(rgate)) @ w_branch[0]
        out_b = _merge_heads(na) @ w_branch[1]
        out_c = (lru.astype(ygate.dtype) * jax.nn.gelu(ygate)) @ w_branch[2]
        mixed = (jax.nn.sigmoid(gate_a) * out_a + jax.nn.sigmoid(gate_b) * out_b
                 + jax.nn.sigmoid(gate_c) * out_c)
        return mixed @ w_mix_out

    x = x + g1 * merge(ret_l, rg, na_l, lru_l, ly, ga, gb, gc)
    if need_ctx:
        na_c = _ctx_attention(_heads(nqc, NA_HEADS) * q_scale, kc_na, vc_na)
        xc = xc + g1c * merge(ret_c, rgc, _merge_heads(na_c).reshape(na_c.shape[0], NA_HEADS, -1, NA_DH) if False else na_c,
                              lru_c, lyc, gac, gbc, gcc)

    moe = lambda t: _moe(t, w_router, b_router, w_expert_up, b_expert_up, w_expert_down, b_expert_down)
    h2 = _modulate(x, norm2_g, sh2, sc2)
    if not need_ctx:
        return x + g2 * moe(h2.reshape(b * tl, d)).reshape(b, tl, d), xc
    h2c = _modulate(xc, norm2_g, sh2c, sc2c)
    n_ctx = xc.shape[1]
    y = moe(jnp.concatenate([h2c, h2], axis=1).reshape(-1, d)).reshape(b, n_ctx + tl, d)
    return x + g2 * y[:, n_ctx:], xc + g2c * y[:, :n_ctx]


def setup_inputs(seed: int = 0) -> dict:
    key = jax.random.key(seed)
    ks = jax.random.split(key, 32)
    f32 = jnp.float32
    nrm = lambda k, shape, scale: jax.random.normal(k, shape, f32) * scale
    d = D_MODEL
    ret_init = jnp.log(2.0 ** (5.0 + jnp.arange(RET_HEADS, dtype=f32)) - 1.0)
    u = jax.random.uniform(ks[18], (DEPTH, 2, LRU_W), f32, 0.9, 0.999)
    s = u ** (1.0 / LRU_C)
    return {
        "x": nrm(ks[0], (BATCH, SEQ, d), 1.0),
        "c": nrm(ks[1], (BATCH, d), 1.0),
        "ctx": nrm(ks[2], (BATCH, CTX_LEN, d), 1.0),
        "c_ctx": nrm(ks[3], (d,), 1.0),
        "w_mod": nrm(ks[4], (DEPTH, d, 6 * d), 0.5 * d ** -0.5),
        "b_mod": nrm(ks[5], (DEPTH, 6 * d), 0.02),
        "norm1_g": 1.0 + nrm(ks[6], (DEPTH, d), 0.01),
        "norm2_g": 1.0 + nrm(ks[7], (DEPTH, d), 0.01),
        "w_mix_in": nrm(ks[8], (DEPTH, d, IN_W), d ** -0.5),
        "ret_decay_fwd": ret_init + nrm(ks[9], (DEPTH, RET_HEADS), 0.01),
        "ret_decay_bwd": ret_init + nrm(ks[10], (DEPTH, RET_HEADS), 0.01),
        "na_rel_bias": nrm(ks[11], (DEPTH, NA_HEADS, 2 * NA_KH - 1, 2 * NA_KW - 1), 0.02),
        "lru_conv_w": nrm(ks[12], (DEPTH, LRU_CONV, LRU_W), LRU_CONV ** -0.5),
        "lru_conv_b": nrm(ks[13], (DEPTH, LRU_W), 0.01),
        "lru_gate_a_w": nrm(ks[14], (DEPTH, 2, LRU_BLOCKS, LRU_BS, LRU_BS), LRU_BS ** -0.5),
        "lru_gate_a_b": nrm(ks[15], (DEPTH, 2, LRU_W), 0.01),
        "lru_gate_x_w": nrm(ks[16], (DEPTH, 2, LRU_BLOCKS, LRU_BS, LRU_BS), LRU_BS ** -0.5),
        "lru_gate_x_b": nrm(ks[17], (DEPTH, 2, LRU_W), 0.01),
        "lru_lambda": jnp.log(s) - jnp.log1p(-s),
        "w_branch": nrm(ks[19], (DEPTH, N_BRANCH, d, d), d ** -0.5),
        "w_mix_out": nrm(ks[20], (DEPTH, d, d), d ** -0.5),
        "w_router": nrm(ks[21], (DEPTH, d, N_EXPERTS), d ** -0.5),
        "b_router": nrm(ks[22], (DEPTH, N_EXPERTS), 0.01),
        "w_expert_up": nrm(ks[23], (DEPTH, N_EXPERTS, d, 2 * D_EXPERT), d ** -0.5),
        "b_expert_up": nrm(ks[24], (DEPTH, N_EXPERTS, 2 * D_EXPERT), 0.01),
        "w_expert_down": nrm(ks[25], (DEPTH, N_EXPERTS, D_EXPERT, d), D_EXPERT ** -0.5),
        "b_expert_down": nrm(ks[26], (DEPTH, N_EXPERTS, d), 0.01),
        "final_norm_g": 1.0 + nrm(ks[27], (d,), 0.01),
    }


def reference(x, c, ctx, c_ctx, w_mod, b_mod, norm1_g, norm2_g, w_mix_in, ret_decay_fwd, ret_decay_bwd,
              na_rel_bias, lru_conv_w, lru_conv_b, lru_gate_a_w, lru_gate_a_b, lru_gate_x_w, lru_gate_x_b,
              lru_lambda, w_branch, w_mix_out, w_router, b_router, w_expert_up, b_expert_up,
              w_expert_down, b_expert_down, final_norm_g):
    xc = ctx
    for l in range(DEPTH):
        x, xc = _layer(x, xc, c, c_ctx, w_mod[l], b_mod[l], norm1_g[l], norm2_g[l], w_mix_in[l],
                       ret_decay_fwd[l], ret_decay_bwd[l], na_rel_bias[l], lru_conv_w[l], lru_conv_b[l],
                       lru_gate_a_w[l], lru_gate_a_b[l], lru_gate_x_w[l], lru_gate_x_b[l], lru_lambda[l],
                       w_branch[l], w_mix_out[l], w_router[l], b_router[l], w_expert_up[l], b_expert_up[l],
                       w_expert_down[l], b_expert_down[l], l == DEPTH - 1)
    return _rmsnorm(x, final_norm_g)
```

```python
import numpy as np
from contextlib import ExitStack
import concourse.bass as bass
import concourse.mybir as mybir
from concourse.bass_utils import run_bass_kernel_spmd

F32 = mybir.dt.float32
BF16 = mybir.dt.bfloat16
AF = mybir.ActivationFunctionType
ALU = mybir.AluOpType
AX = mybir.AxisListType

NCORES = 8
NBC = 2
D = 1024
TC = 256
TL = 2048
T = TC + TL
NE = 32
EPS = 1e-6
BLOCKS = [(0, 256), (256, 512), (768, 512), (1280, 512), (1792, 512)]
BLOCKS256 = [(i * 256, 256) for i in range(9)]
NDSEM = 12
GRP = 1152
MB = 384
DBG_NSEC = 12
SPARSE = True
MT = 512
NTILE = 68
NSLOT = NTILE * MT
NTOK = 4608
I32 = mybir.dt.int32
DBG_RET = 16
DBG_IB = 4


class FW:
    def __init__(self, nc):
        self.nc = nc
        self.eng = {"pe": nc.tensor, "act": nc.scalar, "dve": nc.vector,
                    "pool": nc.gpsimd, "sp": nc.sync}
        self.sem = {}
        self.cnt = {}
        for e in ("pe", "act", "dve", "pool"):
            self.sem[e] = nc.alloc_semaphore("s_" + e)
            self.cnt[e] = 0
        self.dsem = {}
        self.dcnt = {}
        for q in ("sp", "pool"):
            self.dsem[q] = [nc.alloc_semaphore("d_%s_%d" % (q, i)) for i in range(NDSEM)]
            self.dcnt[q] = 0
        self.waited = {e: {} for e in self.eng}
        self.lastw = {}
        self.readers = {}
        self.ninst = 0
        self.nwait = 0

    def _wait(self, e, tok):
        sem, val, src = tok
        if src == e and e == "pe":
            return
        w = self.waited[e]
        k = id(sem)
        if w.get(k, 0) >= val:
            return
        w[k] = val
        self.eng[e].wait_ge(sem, val)
        self.nwait += 1

    def _deps(self, e, reads, writes):
        for r in reads:
            t = self.lastw.get(r)
            if t is not None:
                self._wait(e, t)
            if (isinstance(r, tuple) and r[0] in ("ps", "psb", "O")) or r in ("ps0", "psm"):
                for t in self.readers.get(r, ()):
                    if t[2] != e:
                        self._wait(e, t)
        for w in writes:
            t = self.lastw.get(w)
            if t is not None:
                self._wait(e, t)
            for t in self.readers.get(w, ()):
                self._wait(e, t)

    def _record(self, tok, reads, writes):
        for r in reads:
            self.readers.setdefault(r, []).append(tok)
        for w in writes:
            self.lastw[w] = tok
            self.readers[w] = []

    def op(self, e, fn, reads=(), writes=()):
        self._deps(e, reads, writes)
        ins = fn(self.eng[e])
        self.cnt[e] += 1
        ins.then_inc(self.sem[e], 1)
        tok = (self.sem[e], self.cnt[e], e)
        self._record(tok, reads, writes)
        self.ninst += 1
        return tok

    def dma(self, q, out, in_, reads=(), writes=(), **kw):
        j = self.dcnt[q]
        sem = self.dsem[q][j % NDSEM]
        gen = j // NDSEM
        if gen > 0:
            self._wait(q, (sem, 16 * gen, "dma"))
        self._deps(q, reads, writes)
        ins = self.eng[q].dma_start(out=out, in_=in_, **kw)
        ins.then_inc(sem, 16)
        self.dcnt[q] = j + 1
        tok = (sem, 16 * (gen + 1), "dma")
        self._record(tok, reads, writes)
        self.ninst += 1
        return tok

    def idma(self, out, out_off, in_, in_off, reads=(), writes=(), **kw):
        q = "pool"
        j = self.dcnt[q]
        sem = self.dsem[q][j % NDSEM]
        gen = j // NDSEM
        if gen > 0:
            self._wait(q, (sem, 16 * gen, "dma"))
        self._deps(q, reads, writes)
        ins = self.eng[q].indirect_dma_start(out=out, out_offset=out_off, in_=in_, in_offset=in_off, **kw)
        ins.then_inc(sem, 16)
        self.dcnt[q] = j + 1
        tok = (sem, 16 * (gen + 1), "dma")
        self._record(tok, reads, writes)
        self.ninst += 1
        return tok

    def barrier(self):
        toks = []
        for e in ("pe", "act", "dve", "pool"):
            if self.cnt[e] > 0:
                toks.append((self.sem[e], self.cnt[e], e))
        for q in ("sp", "pool"):
            j = self.dcnt[q]
            for i in range(NDSEM):
                n = (j - i + NDSEM - 1) // NDSEM if j > i else 0
                if n > 0:
                    toks.append((self.dsem[q][i], 16 * n, "dma"))
        for e in self.eng:
            for t in toks:
                if t[2] == e and e != "pe":
                    pass
                sem, val, src = t
                w = self.waited[e]
                if w.get(id(sem), 0) >= val:
                    continue
                w[id(sem)] = val
                self.eng[e].wait_ge(sem, val)
                self.nwait += 1
        self.lastw = {}
        self.readers = {}


def build(nlayers=2, dbg=(), stop_after=None):
    nc = bass.Bass("TRN2", target_bir_lowering=False)
    fw = FW(nc)
    I = {}

    def din(name, shape):
        I[name] = nc.dram_tensor(name, list(shape), F32, kind="ExternalInput").ap()

    def scr(name, shape, dt):
        kind = "ExternalOutput" if name in dbg else "Internal"
        return nc.dram_tensor(name, list(shape), dt, kind=kind).ap()

    L = 2
    din("xin", [NBC, 8, 128, T])
    din("cT", [128, 8, 3])
    din("w_mod", [L, D, 6 * D])
    din("b_modT", [L, 128, 48])
    din("n1gT", [L, 128, 8])
    din("n2gT", [L, 128, 8])
    din("fngT", [128, 8])
    din("w_mix_in", [L, D, 12 * D])
    din("decf", [L, 8])
    din("decb", [L, 8])
    din("na_bias", [L, 8, 128, 8, 512])
    din("convwT", [L, 128, 8, 4])
    din("convbT", [L, 128, 8])
    din("lru_wa", [L, 2, 8, 128, 128])
    din("lru_wx", [L, 2, 8, 128, 128])
    din("lru_baT", [L, 128, 2, 8])
    din("lru_bxT", [L, 128, 2, 8])
    din("lru_lamT", [L, 128, 2, 8])
    din("w_branch", [L, 3, D, D])
    din("w_mix_out", [L, D, D])
    din("w_router", [L, D, NE])
    din("b_router", [L, NE])
    din("w_up", [L, NE, D, 2 * D])
    din("bupT", [L, 128, NE, 2, 8])
    din("w_down", [L, NE, D, D])
    din("b_down", [L, NE, D])
    din("ropeC", [128, T])
    din("ropeS", [128, T])
    din("ropeCk", [128, T])
    din("ropeSk", [128, T])
    din("Rp", [128, 4, 512])
    din("Rn", [128, 4, 512])
    din("EA", [128, 15])
    din("EB", [128, 14])
    din("I512", [128, 512])
    din("I511r", [128, 512])
    din("Lmat", [128, 128])
    din("pj", [128, 8])
    din("iota32", [128, NE])
    din("bup2", [L, NE, 128, 16])
    I["tokid"] = nc.dram_tensor("tokid", [128, 36, 2], I32, kind="ExternalInput").ap()
    outT = nc.dram_tensor("outT", [NBC, 8, 128, TL], F32, kind="ExternalOutput").ap()

    xres = scr("xres", [NBC, 8, 128, T], F32)
    fmaj = {}
    for nm in ("rqT", "rkT", "rgT", "nqT", "nkT", "lyT", "gaT", "gbT", "gcT", "AinT", "BinT", "CinT", "hT2"):
        fmaj[nm] = scr(nm, [NBC, 8, 128, T], BF16)
    lxT = scr("lxT", [NBC, 8, 128, T], F32)
    rv = scr("rv", [NBC, T, D], BF16)
    nv = scr("nv", [NBC, T, D], BF16)
    gTd = scr("gTd", [NE, NBC * T], F32)
    yT = scr("yT", [NBC, 8, 128, T], F32)
    h2tok = scr("h2tok", [NTOK + 128, D], BF16)
    gtab = scr("gtab", [NTOK + 128, NE], F32)
    slot_tok = scr("slot_tok", [NSLOT, 2], I32)
    ypairs = scr("ypairs", [NSLOT, D], F32)
    wupb = scr("wupb", [NE * D, 2 * D], BF16)
    wdnb = scr("wdnb", [NE * D, D], BF16)

    def fm(ap_b):
        return ap_b.rearrange("c p t -> p c t")

    PS = [nc.alloc_psum_tensor("ps%d" % i, [128, 512], F32).ap() for i in range(8)]
    PSB = [PS[6].bitcast(BF16), PS[7].bitcast(BF16)]

    def gsb(name, shape, dt):
        return nc.alloc_sbuf_tensor(name, list(shape), dt).ap()

    ones_f = gsb("ones_f", [128, 128], F32)
    ones_b = gsb("ones_b", [128, 128], BF16)
    ident_f = gsb("ident_f", [128, 128], F32)
    ident_b = gsb("ident_b", [128, 128], BF16)
    modT = gsb("modT", [128, 48, 3], F32)
    mul1 = gsb("mul1", [128, 8, 3], F32)
    mul2 = gsb("mul2", [128, 8, 3], F32)
    fng = gsb("fng", [128, 8], F32)
    widx = gsb("widx", [128, NTILE, 8], I32)
    bidx = gsb("bidx", [128, NTILE], I32)
    eidx = gsb("eidx", [128, NTILE], I32)
    ETf = gsb("ETf", [128, NTILE], F32)
    S4_all = gsb("S4_all", [128, 36, 4], I32)
    fw.op("pool", lambda e: e.memset(ones_f, 1.0), writes=["ones_f"])
    fw.op("pool", lambda e: e.memset(ones_b, 1.0), writes=["ones_b"])
    fw.op("pool", lambda e: e.memset(ident_f, 0.0), writes=["ident_f"])
    fw.op("pool", lambda e: e.affine_select(out=ident_f, in_=ident_f, pattern=[[-1, 128]],
                                            compare_op=ALU.not_equal, fill=1.0, base=0, channel_multiplier=1),
          reads=["ident_f"], writes=["ident_f"])
    fw.op("dve", lambda e: e.tensor_copy(out=ident_b, in_=ident_f), reads=["ident_f"], writes=["ident_b"])
    fw.dma("sp", fng, I["fngT"], writes=["fng"])
    fw.barrier()

    class Phase:
        cnt = 0

        def __init__(self, name):
            self.name = name
            self.es = ExitStack()
            self.n = 0

        def sb(self, shape, dt):
            self.n += 1
            Phase.cnt += 1
            t = self.es.enter_context(nc.sbuf_tensor("%s_%d_%d" % (self.name, Phase.cnt, self.n), list(shape), dt))
            return t.ap()

        def end(self):
            fw.barrier()
            self.es.close()

    def mm(out, lhsT, rhs, start, stop, reads, writes):
        fw.op("pe", lambda e: e.matmul(out, lhsT=lhsT, rhs=rhs, start=start, stop=stop), reads, writes)

    def phase_mod(l):
        P = Phase("mod")
        cs = P.sb([128, 8, 4], F32)
        bm = P.sb([128, 48], F32)
        fw.op("pool", lambda e: e.memset(cs, 0.0), writes=["cs"])
        n1 = P.sb([128, 8], F32)
        n2 = P.sb([128, 8], F32)
        fw.dma("sp", cs[:, :, 0:3], I["cT"], writes=["cs"])
        fw.dma("sp", bm, I["b_modT"][l], writes=["bm"])
        fw.dma("sp", n1, I["n1gT"][l], writes=["n1"])
        fw.dma("sp", n2, I["n2gT"][l], writes=["n2"])
        fw.op("act", lambda e: e.activation(out=cs, in_=cs, func=AF.Silu), reads=["cs"], writes=["cs"])
        wbuf = [P.sb([128, 8, 1024], F32) for _ in range(2)]
        psm = PS[0]
        for s in range(6):
            wb = wbuf[s % 2]
            fw.dma("sp", wb, I["w_mod"][l, :, s * 1024:(s + 1) * 1024].rearrange("(j p) n -> p j n", p=128),
                   writes=[("wm", s % 2)])
            for j in range(8):
                n = s * 8 + j
                for k in range(8):
                    mm(psm[:, n * 4:n * 4 + 4], wb[:, k, j * 128:(j + 1) * 128], cs[:, k, :], k == 0, k == 7,
                       [("wm", s % 2), "cs"], ["psm"])
        psv = psm[:, 0:192].rearrange("p (n r) -> p n r", r=4)
        for r in range(3):
            fw.op("dve", lambda e: e.tensor_tensor(out=modT[:, :, r], in0=psv[:, :, r], in1=bm, op=ALU.add),
                  reads=["psm", "bm"], writes=["modT"])
        for r in range(3):
            fw.op("dve", lambda e: e.scalar_tensor_tensor(out=mul1[:, :, r], in0=modT[:, 8:16, r], scalar=1.0, in1=n1,
                                                          op0=ALU.add, op1=ALU.mult),
                  reads=["modT", "n1"], writes=["mul1"])
            fw.op("dve", lambda e: e.scalar_tensor_tensor(out=mul2[:, :, r], in0=modT[:, 32:40, r], scalar=1.0, in1=n2,
                                                          op0=ALU.add, op1=ALU.mult),
                  reads=["modT", "n2"], writes=["mul2"])
        P.end()

    def emit_norm(xt, N, xkey, sq, rstd, mulT, r, out_fn):
        fw.op("act", lambda e: e.activation(out=sq[:, :, :N], in_=xt[:, :, :N], func=AF.Square),
              reads=[xkey], writes=["sq"])
        for j in range(8):
            mm(PS[0][:, :N], ones_f, sq[:, j, :N], j == 0, j == 7, ["sq", "ones_f"], ["ps0"])
        fw.op("act", lambda e: e.activation(out=rstd[:, :N], in_=PS[0][:, :N], func=AF.Sqrt, scale=1.0 / D, bias=EPS),
              reads=["ps0"], writes=["rstd"])
        fw.op("dve", lambda e: e.reciprocal(out=rstd[:, :N], in_=rstd[:, :N]), reads=["rstd"], writes=["rstd"])
        for j in range(8):
            out_fn(j)

    def phase_mixin(l, xsrc):
        es_h = ExitStack()
        hT = es_h.enter_context(nc.sbuf_tensor("hT_%d" % l, [128, 8, NBC * T], BF16)).ap()
        P = Phase("n1")
        xt = [P.sb([128, 8, 512], F32) for _ in range(2)]
        sq = P.sb([128, 8, 512], F32)
        rstd = P.sb([128, 512], F32)
        tmp = [P.sb([128, 512], F32) for _ in range(2)]
        i = 0
        for b in range(NBC):
            for (t0, N) in BLOCKS:
                x_ = xt[i % 2]
                xk = ("xt", i % 2)
                fw.dma("sp", x_[:, :, :N], fm(xsrc[b])[:, :, t0:t0 + N], writes=[xk])
                r = 2 if t0 < TC else b

                def out_fn(j, x_=x_, xk=xk, N=N, r=r, b=b, t0=t0):
                    tm = tmp[j % 2]
                    fw.op("dve", lambda e: e.scalar_tensor_tensor(out=tm[:, :N], in0=x_[:, j, :N], scalar=mul1[:, j, r:r + 1],
                                                                  in1=rstd[:, :N], op0=ALU.mult, op1=ALU.mult),
                          reads=[xk, "rstd", "mul1"], writes=[("tmp", j % 2)])
                    fw.op("act", lambda e: e.activation(out=hT[:, j, b * T + t0:b * T + t0 + N], in_=tm[:, :N],
                                                        func=AF.Identity, bias=modT[:, j, r:r + 1]),
                          reads=[("tmp", j % 2), "modT"], writes=["hT"])
                emit_norm(x_, N, xk, sq, rstd, mul1, r, out_fn)
                i += 1
        P.end()

        P = Phase("mix")
        wb = [P.sb([128, 8, 1024], BF16) for _ in range(2)]
        wp = P.sb([128, 8, 1024], BF16)
        stage = [P.sb([128, 8, 512], BF16) for _ in range(2)]
        stage32 = P.sb([128, 8, 512], F32)
        stT = [P.sb([128, 1024], BF16) for _ in range(2)]
        rC = P.sb([128, T], F32)
        rS = P.sb([128, T], F32)
        rt = [P.sb([128, 512], F32) for _ in range(4)]
        names = ["rqT", "rkT", None, "rgT", "nqT", "nkT", None, None, "lyT", "gaT", "gbT", "gcT"]

        def loadw(s):
            fw.dma("pool", wb[s % 2], I["w_mix_in"][l, :, s * 1024:(s + 1) * 1024].rearrange("(j p) n -> p j n", p=128),
                   writes=[("wb", s % 2)])
        loadw(0)
        psi = 0
        sti = 0
        ev = 0
        for s in range(DBG_NSEC):
            if s + 1 < DBG_NSEC:
                loadw(s + 1)
            w = wb[s % 2]
            wk = ("wb", s % 2)
            if s < 2:
                Wv = w.rearrange("p j (h q r) -> p (j h) q r", q=4, r=32)
                Pv = wp.rearrange("p j (h q r) -> p (j h) q r", q=4, r=32)
                for q in range(4):
                    eng = "dve" if q % 2 == 0 else "act"
                    if eng == "dve":
                        fw.op("dve", lambda e: e.tensor_copy(out=Pv[:, :, q, :], in_=Wv[:, :, q ^ 1, :]), reads=[wk], writes=["wp"])
                    else:
                        fw.op("act", lambda e: e.copy(out=Pv[:, :, q, :], in_=Wv[:, :, q ^ 1, :]), reads=[wk], writes=["wp"])
                fw.dma("sp", rC, I["ropeC" if s == 0 else "ropeCk"], writes=["rC"])
                fw.dma("sp", rS, I["ropeS" if s == 0 else "ropeSk"], writes=["rS"])
            if s in (2, 6):
                dst = rv if s == 2 else nv
                for b in range(NBC):
                    for tt in range(18):
                        st = stT[sti % 2]
                        sk = ("stT", sti % 2)
                        for half in range(2):
                            ps = PS[psi % 4]
                            pk = ("ps", psi % 4)
                            psi += 1
                            for k in range(8):
                                mm(ps, hT[:, k, b * T + tt * 128:b * T + (tt + 1) * 128], w[:, k, half * 512:(half + 1) * 512],
                                   k == 0, k == 7, ["hT", wk], [pk])
                            if ev % 2 == 0:
                                fw.op("act", lambda e: e.copy(out=st[:, half * 512:(half + 1) * 512], in_=ps), reads=[pk], writes=[sk])
                            else:
                                fw.op("dve", lambda e: e.tensor_copy(out=st[:, half * 512:(half + 1) * 512], in_=ps), reads=[pk], writes=[sk])
                            ev += 1
                        fw.dma("sp", dst[b, tt * 128:(tt + 1) * 128, :], st, reads=[sk], writes=[("dst", s, b, tt)])
                        sti += 1
                continue
            for b in range(NBC):
                for (t0, N) in BLOCKS:
                    if s == 7:
                        st = stage32
                        sk = "stage32"
                    else:
                        st = stage[sti % 2]
                        sk = ("stage", sti % 2)
                        sti += 1
                    hsl = slice(b * T + t0, b * T + t0 + N)
                    for c in range(8):
                        ps = PS[psi % 4]
                        pk = ("ps", psi % 4)
                        psi += 1
                        for k in range(8):
                            mm(ps[:, :N], w[:, k, c * 128:(c + 1) * 128], hT[:, k, hsl], k == 0, k == 7, ["hT", wk], [pk])
                        o = st[:, c, :N]
                        if s < 2:
                            ps2 = PS[4 + psi % 2]
                            pk2 = ("ps", 4 + psi % 2)
                            for k in range(8):
                                mm(ps2[:, :N], wp[:, k, c * 128:(c + 1) * 128], hT[:, k, hsl], k == 0, k == 7, ["hT", "wp"], [pk2])
                            ta = rt[(psi % 2) * 2]
                            tb = rt[(psi % 2) * 2 + 1]
                            ka = ("rt", (psi % 2) * 2)
                            kb = ("rt", (psi % 2) * 2 + 1)
                            fw.op("dve", lambda e: e.tensor_tensor(out=ta[:, :N], in0=ps[:, :N], in1=rC[:, t0:t0 + N], op=ALU.mult),
                                  reads=[pk, "rC"], writes=[ka])
                            fw.op("dve", lambda e: e.tensor_tensor(out=tb[:, :N], in0=ps2[:, :N], in1=rS[:, t0:t0 + N], op=ALU.mult),
                                  reads=[pk2, "rS"], writes=[kb])
                            fw.op("pool", lambda e: e.tensor_tensor(out=o, in0=ta[:, :N], in1=tb[:, :N], op=ALU.add),
                                  reads=[ka, kb], writes=[sk])
                        elif s == 3:
                            fw.op("act", lambda e: e.activation(out=o, in_=ps[:, :N], func=AF.Silu), reads=[pk], writes=[sk])
                        elif s == 8:
                            fw.op("act", lambda e: e.activation(out=o, in_=ps[:, :N], func=AF.Gelu), reads=[pk], writes=[sk])
                        elif s >= 9:
                            fw.op("act", lambda e: e.activation(out=o, in_=ps[:, :N], func=AF.Sigmoid), reads=[pk], writes=[sk])
                        elif s == 4:
                            if ev % 2 == 0:
                                fw.op("act", lambda e: e.activation(out=o, in_=ps[:, :N], func=AF.Copy, scale=0.125), reads=[pk], writes=[sk])
                            else:
                                fw.op("dve", lambda e: e.tensor_scalar(out=o, in0=ps[:, :N], scalar1=0.125, scalar2=None, op0=ALU.mult),
                                      reads=[pk], writes=[sk])
                            ev += 1
                        else:
                            if ev % 2 == 0:
                                fw.op("act", lambda e: e.copy(out=o, in_=ps[:, :N]), reads=[pk], writes=[sk])
                            else:
                                fw.op("dve", lambda e: e.tensor_copy(out=o, in_=ps[:, :N]), reads=[pk], writes=[sk])
                            ev += 1
                    dst = lxT if s == 7 else fmaj[names[s]]
                    fw.dma("sp", fm(dst[b])[:, :, t0:t0 + N], st[:, :, :N], reads=[sk], writes=[("dst", s, b, t0)])
        P.end()
        es_h.close()

    def phase_ret(l):
        P = Phase("ret")
        lg = P.sb([128, 16], F32)
        fw.dma("sp", lg[:, 0:8], I["decf"][l].partition_broadcast(128), writes=["lg"])
        fw.dma("sp", lg[:, 8:16], I["decb"][l].partition_broadcast(128), writes=["lg"])
        fw.op("act", lambda e: e.activation(out=lg, in_=lg, func=AF.Exp, scale=-1.0), reads=["lg"], writes=["lg"])
        fw.op("act", lambda e: e.activation(out=lg, in_=lg, func=AF.Ln, bias=1.0), reads=["lg"], writes=["lg"])
        fw.op("dve", lambda e: e.tensor_scalar(out=lg, in0=lg, scalar1=-1.0, scalar2=None, op0=ALU.mult), reads=["lg"], writes=["lg"])
        Rp = P.sb([128, 4, 512], F32)
        Rn = P.sb([128, 4, 512], F32)
        EA = P.sb([128, 15], F32)
        EB = P.sb([128, 14], F32)
        I512 = P.sb([128, 512], F32)
        I511r = P.sb([128, 512], F32)
        for nm, t in (("Rp", Rp), ("Rn", Rn), ("EA", EA), ("EB", EB), ("I512", I512), ("I511r", I511r)):
            fw.dma("sp", t, I[nm], writes=[nm])
        Dm = P.sb([128, 8, 4, 512], BF16)
        Af = P.sb([128, 8, 15], F32)
        Ab = P.sb([128, 8, 14], F32)
        bf = P.sb([128, 8, 512], F32)
        bb = P.sb([128, 8, 512], F32)
        t1 = P.sb([128, 512], F32)
        for h in range(8):
            for rel in range(4):
                fw.op("dve", lambda e: e.tensor_scalar(out=t1, in0=Rp[:, rel, :], scalar1=lg[:, h:h + 1], scalar2=None, op0=ALU.mult),
                      reads=["Rp", "lg"], writes=["t1"])
                fw.op("dve", lambda e: e.scalar_tensor_tensor(out=t1, in0=Rn[:, rel, :], scalar=lg[:, 8 + h:9 + h], in1=t1,
                                                              op0=ALU.mult, op1=ALU.add),
                      reads=["Rn", "lg", "t1"], writes=["t1"])
                fw.op("act", lambda e: e.activation(out=Dm[:, h, rel, :], in_=t1, func=AF.Exp), reads=["t1"], writes=["Dm"])
            fw.op("act", lambda e: e.activation(out=Af[:, h, :], in_=EA, func=AF.Exp, scale=lg[:, h:h + 1]), reads=["EA", "lg"], writes=["Af"])
            fw.op("act", lambda e: e.activation(out=Ab[:, h, :], in_=EB, func=AF.Exp, scale=lg[:, 8 + h:9 + h]), reads=["EB", "lg"], writes=["Ab"])
            fw.op("act", lambda e: e.activation(out=bf[:, h, :], in_=I512, func=AF.Exp, scale=lg[:, h:h + 1]), reads=["I512", "lg"], writes=["bf"])
            fw.op("act", lambda e: e.activation(out=bb[:, h, :], in_=I511r, func=AF.Exp, scale=lg[:, 8 + h:9 + h]), reads=["I511r", "lg"], writes=["bb"])
        qT = [P.sb([128, T], BF16) for _ in range(2)]
        kT = [P.sb([128, T], BF16) for _ in range(2)]
        V = [P.sb([128, 18, 128], BF16) for _ in range(2)]
        sg = [P.sb([128, T], BF16) for _ in range(2)]
        ost = [P.sb([128, T], BF16) for _ in range(2)]
        pb = [P.sb([128, 512], BF16) for _ in range(4)]
        y32 = P.sb([128, 512], F32)
        e1 = P.sb([128, 512], F32)
        e2 = P.sb([128, 512], F32)
        sqy = P.sb([128, 512], F32)
        rsy = P.sb([128, 512], F32)
        it = 0
        pbi = 0
        evc = 0
        for b in range(NBC):
            for h in range(8):
                if it >= DBG_RET:
                    continue
                p = it % 2
                it += 1
                q_, k_, v_, s_, o_ = qT[p], kT[p], V[p], sg[p], ost[p]
                fw.dma("sp", q_, fmaj["rqT"][b, h], writes=[("q", p)])
                fw.dma("sp", k_, fmaj["rkT"][b, h], writes=[("k", p)])
                fw.dma("sp", v_, rv[b].rearrange("(t p) (h d) -> h p t d", p=128, d=128)[h], writes=[("v", p)])
                fw.dma("sp", s_, fmaj["rgT"][b, h], writes=[("sg", p)])
                SBK = [0, 1, 6, 7]
                steps = []
                blkinfo = {}
                for ib in range(-1, DBG_IB):
                    if ib < 0:
                        q0, N = 0, 256
                        kks = [(kk, [("d", kk)]) for kk in range(2)]
                    else:
                        q0, N = TC + 512 * ib, 512
                        kks = []
                        for kk in range(18):
                            if kk < 2:
                                kks.append((kk, [("f", 2 + 4 * ib - kk), ("b", 12 + kk - 4 * ib)]))
                            else:
                                rel = kk - 2 - 4 * ib
                                if rel < 0:
                                    kks.append((kk, [("f", -rel)]))
                                elif rel < 4:
                                    kks.append((kk, [("d", rel)]))
                                else:
                                    kks.append((kk, [("b", rel - 4)]))
                    tot = {"d": 0, "f": 0, "b": 0}
                    for (_, its) in kks:
                        for (ty, _) in its:
                            tot[ty] += 1
                    blkinfo[ib] = (q0, N, tot)
                    for j, (kk, its) in enumerate(kks):
                        steps.append((ib, kk, its, j == 0, j == len(kks) - 1))

                def emitS(i):
                    ib, kk, _, _, _ = steps[i]
                    q0, N, _ = blkinfo[ib]
                    bk = SBK[i % 4]
                    mm(PS[bk][:, :N], k_[:, kk * 128:(kk + 1) * 128], q_[:, q0:q0 + N], True, True, [("q", p), ("k", p)], [("ps", bk)])

                def epilogue(ib):
                    q0, N, _ = blkinfo[ib]
                    if ib < 0:
                        fw.op("dve", lambda e: e.tensor_copy(out=y32[:, :N], in_=PS[2][:, :N]), reads=[("ps", 2)], writes=["y32"])
                    else:
                        fw.op("dve", lambda e: e.tensor_tensor(out=e1, in0=PS[3], in1=bf[:, h, :], op=ALU.mult), reads=[("ps", 3), "bf"], writes=["e1"])
                        fw.op("dve", lambda e: e.tensor_tensor(out=e2, in0=PS[4], in1=bb[:, h, :], op=ALU.mult), reads=[("ps", 4), "bb"], writes=["e2"])
                        fw.op("pool", lambda e: e.tensor_tensor(out=e1, in0=e1, in1=e2, op=ALU.add), reads=["e1", "e2"], writes=["e1"])
                        fw.op("dve", lambda e: e.tensor_tensor(out=y32, in0=PS[2], in1=e1, op=ALU.add), reads=[("ps", 2), "e1"], writes=["y32"])
                    fw.op("act", lambda e: e.activation(out=sqy[:, :N], in_=y32[:, :N], func=AF.Square), reads=["y32"], writes=["sqy"])
                    mm(PS[5][:, :N], ones_f, sqy[:, :N], True, True, ["sqy", "ones_f"], [("ps", 5)])
                    fw.op("act", lambda e: e.activation(out=rsy[:, :N], in_=PS[5][:, :N], func=AF.Sqrt, scale=1.0 / 128, bias=EPS),
                          reads=[("ps", 5)], writes=["rsy"])
                    fw.op("dve", lambda e: e.reciprocal(out=rsy[:, :N], in_=rsy[:, :N]), reads=["rsy"], writes=["rsy"])
                    fw.op("pool", lambda e: e.tensor_tensor(out=y32[:, :N], in0=y32[:, :N], in1=rsy[:, :N], op=ALU.mult),
                          reads=["y32", "rsy"], writes=["y32"])
                    fw.op("dve", lambda e: e.tensor_tensor(out=o_[:, q0:q0 + N], in0=y32[:, :N], in1=s_[:, q0:q0 + N], op=ALU.mult),
                          reads=["y32", ("sg", p)], writes=[("ost", p)])

                LA = 3
                for i in range(min(LA, len(steps))):
                    emitS(i)
                accb = {"d": 2, "f": 3, "b": 4}
                cnts = None
                for i, (ib, kk, its, first, last) in enumerate(steps):
                    q0, N, tot = blkinfo[ib]
                    if first:
                        cnts = {"d": 0, "f": 0, "b": 0}
                    bk = SBK[i % 4]
                    sps = PS[bk]
                    spk = ("ps", bk)
                    for (ty, idx) in its:
                        pt = pb[pbi % 4]
                        pk = ("pb", pbi % 4)
                        pbi += 1
                        if ty == "d":
                            fw.op("dve", lambda e: e.tensor_tensor(out=pt[:, :N], in0=sps[:, :N], in1=Dm[:, h, idx, :N], op=ALU.mult),
                                  reads=[spk, "Dm"], writes=[pk])
                        else:
                            sc = Af[:, h, idx:idx + 1] if ty == "f" else Ab[:, h, idx:idx + 1]
                            if evc % 3 != 0:
                                fw.op("act", lambda e: e.activation(out=pt[:, :N], in_=sps[:, :N], func=AF.Identity, scale=sc),
                                      reads=[spk, "Af", "Ab"], writes=[pk])
                            else:
                                fw.op("dve", lambda e: e.tensor_scalar(out=pt[:, :N], in0=sps[:, :N], scalar1=sc, scalar2=None, op0=ALU.mult),
                                      reads=[spk, "Af", "Ab"], writes=[pk])
                            evc += 1
                        ab = accb[ty]
                        mm(PS[ab][:, :N], v_[:, kk, :], pt[:, :N], cnts[ty] == 0, cnts[ty] == tot[ty] - 1, [("v", p), pk], [("ps", ab)])
                        cnts[ty] += 1
                    if i + LA < len(steps):
                        emitS(i + LA)
                    if last:
                        epilogue(ib)
                fw.dma("sp", fmaj["AinT"][b, h], o_, reads=[("ost", p)], writes=[("Ain", b, h)])
        P.end()

    def phase_na(l):
        P = Phase("na")
        qbd = [P.sb([128, 36, 128], BF16) for _ in range(2)]
        kT = [P.sb([128, T], BF16) for _ in range(2)]
        Ve = [P.sb([128, 18, 128], BF16) for _ in range(2)]
        Vo = [P.sb([128, 15, 128], BF16) for _ in range(2)]
        Bt = [P.sb([128, 8, 512], F32) for _ in range(2)]
        Ssb = [P.sb([128, 768], F32) for _ in range(3)]
        Pb = [P.sb([128, 768], BF16) for _ in range(3)]
        PT = [P.sb([128, 6, 128], BF16) for _ in range(3)]
        mx = [P.sb([128, 1], F32) for _ in range(3)]
        rec = [P.sb([128, 128], F32) for _ in range(2)]
        ost = [P.sb([128, T], BF16) for _ in range(2)]
        for i in range(2):
            fw.op("pool", lambda e: e.memset(qbd[i], 0.0), writes=[("qbd", i)])
        it = 0
        ri = 0
        for b in range(NBC):
            nvv = nv[b].rearrange("(t p) (g d) -> g p t d", p=128, d=128)
            nvo = nv[b, TC + 64:TC + 64 + 15 * 128, :].rearrange("(t p) (g d) -> g p t d", p=128, d=128)
            for g in range(8):
                p = it % 2
                it += 1
                qb, k_, ve, vo, bt, o_ = qbd[p], kT[p], Ve[p], Vo[p], Bt[p], ost[p]
                nq = fmaj["nqT"][b, g]
                fw.dma("sp", qb[0:64, :, 0:64], nq[0:64, :].rearrange("p (r w) -> p r w", w=64), writes=[("qbd", p)])
                fw.dma("sp", qb[64:128, :, 64:128], nq[64:128, :].rearrange("p (r w) -> p r w", w=64), writes=[("qbd", p)])
                fw.dma("sp", k_, fmaj["nkT"][b, g], writes=[("k", p)])
                fw.dma("sp", ve, nvv[g], writes=[("ve", p)])
                fw.dma("sp", vo, nvo[g], writes=[("vo", p)])
                fw.dma("sp", bt, I["na_bias"][l, g], writes=[("bt", p)])
                def rowinfo(rr):
                    if rr < 4:
                        return True, 256, 2, 0, 0
                    r = rr - 4
                    r0 = min(max(r - 4, 0), 24)
                    return False, 768, 6, r0, r - r0

                def stA(rr):
                    ctxrow, W, nkt, r0, dl = rowinfo(rr)
                    a = rr % 2
                    a3 = rr % 3
                    X = PS[2 * a]
                    Y = PS[2 * a + 1]
                    xk = ("ps", 2 * a)
                    yk = ("ps", 2 * a + 1)
                    S_ = Ssb[a3]
                    sk = ("Ssb", a3)
                    mm(Y[:, :256], qb[:, rr, :], k_[:, 0:256], True, True, [("qbd", p), ("k", p)], [yk])
                    if ctxrow:
                        fw.op("act", lambda e: e.copy(out=S_[:, 0:256], in_=Y[:, :256]), reads=[yk], writes=[sk])
                    else:
                        mm(X, qb[:, rr, :], k_[:, TC + r0 * 64:TC + r0 * 64 + 512], True, True, [("qbd", p), ("k", p)], [xk])
                        fw.op("dve", lambda e: e.tensor_tensor(out=S_[:, 0:512], in0=X, in1=bt[:, dl, :], op=ALU.add),
                              reads=[xk, ("bt", p)], writes=[sk])
                        fw.op("act", lambda e: e.copy(out=S_[:, 512:768], in_=Y[:, :256]), reads=[yk], writes=[sk])
                    fw.op("dve", lambda e: e.tensor_reduce(out=mx[a3], in_=S_[:, :W], axis=AX.X, op=ALU.max, negate=True),
                          reads=[sk], writes=[("mx", a3)])
                    fw.op("act", lambda e: e.activation(out=Pb[a3][:, :W], in_=S_[:, :W], func=AF.Exp, bias=mx[a3]),
                          reads=[sk, ("mx", a3)], writes=[("Pb", a3)])

                def stB(rr):
                    ctxrow, W, nkt, r0, dl = rowinfo(rr)
                    a = rr % 2
                    a3 = rr % 3
                    psb = PSB[a]
                    pbk = ("psb", a)
                    for kt in range(nkt):
                        fw.op("pe", lambda e: e.transpose(out=psb[:, kt * 128:(kt + 1) * 128], in_=Pb[a3][:, kt * 128:(kt + 1) * 128], identity=ident_b),
                              reads=[("Pb", a3), "ident_b"], writes=[pbk])
                    ptv = PT[a3].rearrange("p k q -> p (k q)")
                    ptk = ("PT", a3)
                    if rr % 2 == 0:
                        fw.op("act", lambda e: e.copy(out=ptv[:, :nkt * 128], in_=psb[:, :nkt * 128]), reads=[pbk], writes=[ptk])
                    else:
                        fw.op("dve", lambda e: e.tensor_copy(out=ptv[:, :nkt * 128], in_=psb[:, :nkt * 128]), reads=[pbk], writes=[ptk])

                def stC(rr):
                    ctxrow, W, nkt, r0, dl = rowinfo(rr)
                    a = rr % 2
                    a3 = rr % 3
                    pt = PT[a3]
                    ptk = ("PT", a3)
                    if ctxrow:
                        vts = [ve[:, 0, :], ve[:, 1, :]]
                        vks = [("ve", p)]
                    else:
                        if r0 % 2 == 0:
                            vts = [ve[:, 2 + r0 // 2 + kt, :] for kt in range(4)]
                        else:
                            vts = [vo[:, (r0 - 1) // 2 + kt, :] for kt in range(4)]
                        vts += [ve[:, 0, :], ve[:, 1, :]]
                        vks = [("ve", p), ("vo", p)]
                    O = PS[4 + a][:, 0:128]
                    Dn = PS[4 + a][:, 128:256]
                    ok = ("O", a)
                    for kt in range(nkt):
                        mm(O, vts[kt], pt[:, kt, :], kt == 0, kt == nkt - 1, vks + [ptk], [ok])
                    for kt in range(nkt):
                        mm(Dn, ones_b, pt[:, kt, :], kt == 0, kt == nkt - 1, ["ones_b", ptk], [ok])
                    fw.op("dve", lambda e: e.reciprocal(out=rec[a], in_=Dn), reads=[ok], writes=[("rec", a)])
                    fw.op("dve", lambda e: e.tensor_tensor(out=o_[0:64, rr * 64:(rr + 1) * 64], in0=O[0:64, 0:64], in1=rec[a][0:64, 0:64], op=ALU.mult),
                          reads=[ok, ("rec", a)], writes=[("ost", p)])
                    fw.op("dve", lambda e: e.tensor_tensor(out=o_[64:128, rr * 64:(rr + 1) * 64], in0=O[64:128, 64:128], in1=rec[a][64:128, 64:128], op=ALU.mult),
                          reads=[ok, ("rec", a)], writes=[("ost", p)])

                for t in range(36 + 2):
                    if t < 36:
                        stA(t)
                    if 0 <= t - 1 < 36:
                        stB(t - 1)
                    if 0 <= t - 2 < 36:
                        stC(t - 2)
                fw.dma("sp", fmaj["BinT"][b, g], o_, reads=[("ost", p)], writes=[("Bin", b, g)])
        P.end()

    def phase_lru(l):
        P = Phase("lru")
        wg = P.sb([128, 32, 128], BF16)
        fw.dma("pool", wg[:, 0:16, :], I["lru_wa"][l].rearrange("r k c d -> c (r k) d"), writes=["wg"])
        fw.dma("pool", wg[:, 16:32, :], I["lru_wx"][l].rearrange("r k c d -> c (r k) d"), writes=["wg"])
        cw = P.sb([128, 8, 4], F32)
        cb = P.sb([128, 8], F32)
        ba = P.sb([128, 2, 8], F32)
        bx = P.sb([128, 2, 8], F32)
        lam = P.sb([128, 2, 8], F32)
        fw.dma("sp", cw, I["convwT"][l], writes=["cw"])
        fw.dma("sp", cb, I["convbT"][l], writes=["cb"])
        fw.dma("sp", ba, I["lru_baT"][l], writes=["ba"])
        fw.dma("sp", bx, I["lru_bxT"][l], writes=["bx"])
        fw.dma("sp", lam, I["lru_lamT"][l], writes=["lam"])
        fw.op("act", lambda e: e.activation(out=lam, in_=lam, func=AF.Exp, scale=-1.0), reads=["lam"], writes=["lam"])
        fw.op("act", lambda e: e.activation(out=lam, in_=lam, func=AF.Ln, bias=1.0), reads=["lam"], writes=["lam"])
        fw.op("dve", lambda e: e.tensor_scalar(out=lam, in0=lam, scalar1=-8.0, scalar2=None, op0=ALU.mult), reads=["lam"], writes=["lam"])
        xs = [P.sb([128, T], F32) for _ in range(2)]
        gy = [P.sb([128, T], BF16) for _ in range(2)]
        u = P.sb([128, T], F32)
        ub = P.sb([128, T], BF16)
        r_ = P.sb([128, T], F32)
        i_ = P.sb([128, T], F32)
        a_ = P.sb([128, T], F32)
        s_ = P.sb([128, T], F32)
        hf = P.sb([128, T], F32)
        hb = P.sb([128, T], F32)
        oc = [P.sb([128, T], BF16) for _ in range(2)]
        it = 0
        psi = 0
        segs = [(0, TC), (TC, T)]
        for b in range(NBC):
            for k in range(8):
                p = it % 2
                it += 1
                x_ = xs[p]
                fw.dma("sp", x_, lxT[b, k], writes=[("x", p)])
                fw.dma("sp", gy[p], fmaj["lyT"][b, k], writes=[("gy", p)])
                fw.op("dve", lambda e: e.tensor_scalar(out=u, in0=x_, scalar1=cw[:, k, 1:2], scalar2=cb[:, k:k + 1], op0=ALU.mult, op1=ALU.add),
                      reads=[("x", p), "cw", "cb"], writes=["u"])
                for (s0, s1) in segs:
                    fw.op("dve", lambda e: e.scalar_tensor_tensor(out=u[:, s0 + 1:s1], in0=x_[:, s0:s1 - 1], scalar=cw[:, k, 0:1], in1=u[:, s0 + 1:s1],
                                                                  op0=ALU.mult, op1=ALU.add), reads=[("x", p), "u", "cw"], writes=["u"])
                    fw.op("dve", lambda e: e.scalar_tensor_tensor(out=u[:, s0:s1 - 1], in0=x_[:, s0 + 1:s1], scalar=cw[:, k, 2:3], in1=u[:, s0:s1 - 1],
                                                                  op0=ALU.mult, op1=ALU.add), reads=[("x", p), "u", "cw"], writes=["u"])
                    fw.op("dve", lambda e: e.scalar_tensor_tensor(out=u[:, s0:s1 - 2], in0=x_[:, s0 + 2:s1], scalar=cw[:, k, 3:4], in1=u[:, s0:s1 - 2],
                                                                  op0=ALU.mult, op1=ALU.add), reads=[("x", p), "u", "cw"], writes=["u"])
                fw.op("act", lambda e: e.copy(out=ub, in_=u), reads=["u"], writes=["ub"])
                for dr in range(2):
                    for (t0, N) in BLOCKS:
                        ps = PS[psi % 4]
                        pk = ("ps", psi % 4)
                        psi += 1
                        mm(ps[:, :N], wg[:, dr * 8 + k, :], ub[:, t0:t0 + N], True, True, ["wg", "ub"], [pk])
                        fw.op("act", lambda e: e.activation(out=r_[:, t0:t0 + N], in_=ps[:, :N], func=AF.Sigmoid, bias=ba[:, dr, k:k + 1]),
                              reads=[pk, "ba"], writes=["r_"])
                        ps = PS[psi % 4]
                        pk = ("ps", psi % 4)
                        psi += 1
                        mm(ps[:, :N], wg[:, 16 + dr * 8 + k, :], ub[:, t0:t0 + N], True, True, ["wg", "ub"], [pk])
                        fw.op("act", lambda e: e.activation(out=i_[:, t0:t0 + N], in_=ps[:, :N], func=AF.Sigmoid, bias=bx[:, dr, k:k + 1]),
                              reads=[pk, "bx"], writes=["i_"])
                    fw.op("act", lambda e: e.activation(out=a_, in_=r_, func=AF.Exp, scale=lam[:, dr, k:k + 1]), reads=["r_", "lam"], writes=["a_"])
                    fw.op("act", lambda e: e.activation(out=s_, in_=a_, func=AF.Square), reads=["a_"], writes=["s_"])
                    fw.op("act", lambda e: e.activation(out=s_, in_=s_, func=AF.Sqrt, scale=-1.0, bias=1.0), reads=["s_"], writes=["s_"])
                    fw.op("pool", lambda e: e.tensor_tensor(out=i_, in0=i_, in1=u, op=ALU.mult), reads=["i_", "u"], writes=["i_"])
                    fw.op("dve", lambda e: e.tensor_tensor(out=s_, in0=s_, in1=i_, op=ALU.mult), reads=["s_", "i_"], writes=["s_"])
                    if dr == 0:
                        fw.op("dve", lambda e: e.tensor_tensor_scan(out=hf, data0=a_, data1=s_, initial=0.0, op0=ALU.mult, op1=ALU.add),
                              reads=["a_", "s_"], writes=["hf"])
                    else:
                        fw.op("dve", lambda e: e.tensor_tensor_scan(out=hb[:, 0:TC][:, ::-1], data0=a_[:, 0:TC][:, ::-1], data1=s_[:, 0:TC][:, ::-1],
                                                                    initial=0.0, op0=ALU.mult, op1=ALU.add),
                              reads=["a_", "s_"], writes=["hb"])
                        fw.op("dve", lambda e: e.tensor_tensor_scan(out=hb[:, TC:T][:, ::-1], data0=a_[:, TC:T][:, ::-1], data1=s_[:, TC:T][:, ::-1],
                                                                    initial=hb[:, 0:1], op0=ALU.mult, op1=ALU.add),
                              reads=["a_", "s_", "hb"], writes=["hb"])
                fw.op("pool", lambda e: e.tensor_tensor(out=hf, in0=hf, in1=hb, op=ALU.add), reads=["hf", "hb"], writes=["hf"])
                fw.op("dve", lambda e: e.tensor_tensor(out=oc[p], in0=hf, in1=gy[p], op=ALU.mult), reads=["hf", ("gy", p)], writes=[("oc", p)])
                fw.dma("sp", fmaj["CinT"][b, k], oc[p], reads=[("oc", p)], writes=[("Cin", b, k)])
        P.end()

    def phase_merge(l, xsrc):
        P = Phase("mrg")
        wbr = P.sb([128, 3, 8, 1024], BF16)
        wmo = P.sb([128, 8, 1024], BF16)
        for x in range(3):
            fw.dma("pool", wbr[:, x], I["w_branch"][l, x].rearrange("(j p) n -> p j n", p=128), writes=["wbr"])
        fw.dma("pool", wmo, I["w_mix_out"][l].rearrange("(j p) n -> p j n", p=128), writes=["wmo"])
        NN = 256
        ins = [[P.sb([128, 8, NN], BF16) for _ in range(6)] for _ in range(2)]
        xt = [P.sb([128, 8, NN], F32) for _ in range(2)]
        mixed = P.sb([128, 8, NN], BF16)
        xo = [P.sb([128, 8, NN], F32) for _ in range(2)]
        tA = [P.sb([128, NN], F32) for _ in range(2)]
        tB = [P.sb([128, NN], F32) for _ in range(2)]
        tC = [P.sb([128, NN], F32) for _ in range(2)]
        srcs = ["AinT", "BinT", "CinT", "gaT", "gbT", "gcT"]
        it = 0
        for b in range(NBC):
            for (t0, N) in BLOCKS256:
                p = it % 2
                it += 1
                r = 2 if t0 < TC else b
                for si, nm in enumerate(srcs):
                    fw.dma("sp", ins[p][si], fm(fmaj[nm][b])[:, :, t0:t0 + N], writes=[("in", p, si)])
                fw.dma("sp", xt[p], fm(xsrc[b])[:, :, t0:t0 + N], writes=[("xt", p)])
                for c in range(8):
                    q = c % 2
                    pss = [PS[3 * q + x] for x in range(3)]
                    for x in range(3):
                        for k in range(8):
                            mm(pss[x][:, :N], wbr[:, x, k, c * 128:(c + 1) * 128], ins[p][x][:, k, :], k == 0, k == 7,
                               ["wbr", ("in", p, x)], [("ps", 3 * q + x)])
                    tt = [tA[q], tB[q], tC[q]]
                    for x in range(3):
                        fw.op("dve", lambda e: e.tensor_tensor(out=tt[x], in0=pss[x][:, :N], in1=ins[p][3 + x][:, c, :], op=ALU.mult),
                              reads=[("ps", 3 * q + x), ("in", p, 3 + x)], writes=[("tt", q, x)])
                    fw.op("pool", lambda e: e.tensor_tensor(out=tt[0], in0=tt[0], in1=tt[1], op=ALU.add),
                          reads=[("tt", q, 0), ("tt", q, 1)], writes=[("tt", q, 0)])
                    fw.op("pool", lambda e: e.tensor_tensor(out=mixed[:, c, :], in0=tt[0], in1=tt[2], op=ALU.add),
                          reads=[("tt", q, 0), ("tt", q, 2)], writes=["mixed"])
                for c in range(8):
                    ps = PS[6 + c % 2]
                    pk = ("ps", 6 + c % 2)
                    for k in range(8):
                        mm(ps[:, :N], wmo[:, k, c * 128:(c + 1) * 128], mixed[:, k, :], k == 0, k == 7, ["wmo", "mixed"], [pk])
                    fw.op("dve", lambda e: e.scalar_tensor_tensor(out=xo[p][:, c, :], in0=ps[:, :N], scalar=modT[:, 16 + c, r:r + 1], in1=xt[p][:, c, :],
                                                                  op0=ALU.mult, op1=ALU.add),
                          reads=[pk, "modT", ("xt", p)], writes=[("xo", p)])
                fw.dma("sp", fm(xres[b])[:, :, t0:t0 + N], xo[p], reads=[("xo", p)], writes=[("xres", b, t0)])
        P.end()

    def phase_moepre(l):
        P = Phase("mpre")
        xt = [P.sb([128, 8, 512], F32) for _ in range(2)]
        sq = P.sb([128, 8, 512], F32)
        rstd = P.sb([128, 512], F32)
        tmp = [P.sb([128, 512], F32) for _ in range(2)]
        hf = P.sb([128, 8, 512], F32)
        hb = [P.sb([128, 8, 512], BF16) for _ in range(2)]
        wr = P.sb([128, 8, NE], F32)
        brt = P.sb([128, NE], F32)
        fw.dma("sp", wr, I["w_router"][l].rearrange("(j p) n -> p j n", p=128), writes=["wr"])
        fw.dma("sp", brt, I["b_router"][l].partition_broadcast(128), writes=["brt"])
        lgs = P.sb([128, NE], F32)
        top8 = P.sb([128, 8], F32)
        msk = P.sb([128, NE], F32)
        nmx = P.sb([128, 1], F32)
        ex = P.sb([128, NE], F32)
        ssum = P.sb([128, 1], F32)
        gts = P.sb([128, NE], F32)
        gTs = [P.sb([NE, 512], F32) for _ in range(2)]
        i = 0
        for b in range(NBC):
            for (t0, N) in BLOCKS:
                x_ = xt[i % 2]
                xk = ("xt", i % 2)
                hb_ = hb[i % 2]
                hbk = ("hb", i % 2)
                gt_ = gTs[i % 2]
                gtk = ("gTs", i % 2)
                i += 1
                fw.dma("sp", x_[:, :, :N], fm(xres[b])[:, :, t0:t0 + N], writes=[xk])
                r = 2 if t0 < TC else b

                def out_fn(j, x_=x_, xk=xk, N=N, r=r):
                    tm = tmp[j % 2]
                    fw.op("dve", lambda e: e.scalar_tensor_tensor(out=tm[:, :N], in0=x_[:, j, :N], scalar=mul2[:, j, r:r + 1],
                                                                  in1=rstd[:, :N], op0=ALU.mult, op1=ALU.mult),
                          reads=[xk, "rstd", "mul2"], writes=[("tmp", j % 2)])
                    fw.op("act", lambda e: e.activation(out=hf[:, j, :N], in_=tm[:, :N], func=AF.Identity, bias=modT[:, 24 + j, r:r + 1]),
                          reads=[("tmp", j % 2), "modT"], writes=["hf"])
                emit_norm(x_, N, xk, sq, rstd, mul2, r, out_fn)
                fw.op("pool", lambda e: e.tensor_copy(out=hb_[:, :, :N], in_=hf[:, :, :N]), reads=["hf"], writes=[hbk])
                fw.dma("sp", fm(fmaj["hT2"][b])[:, :, t0:t0 + N], hb_[:, :, :N], reads=[hbk], writes=[("hT2", b, t0)])
                for tt in range(N // 128):
                    lp = PS[1][:, tt * 32:(tt + 1) * 32]
                    for j in range(8):
                        mm(lp, hf[:, j, tt * 128:(tt + 1) * 128], wr[:, j, :], j == 0, j == 7, ["hf", "wr"], [("ps", 1)])
                    fw.op("dve", lambda e: e.tensor_tensor(out=lgs, in0=lp, in1=brt, op=ALU.add), reads=[("ps", 1), "brt"], writes=["lgs"])
                    fw.op("dve", lambda e: e.max(out=top8, in_=lgs), reads=["lgs"], writes=["top8"])
                    fw.op("dve", lambda e: e.tensor_scalar(out=msk, in0=lgs, scalar1=top8[:, 3:4], scalar2=None, op0=ALU.is_ge),
                          reads=["lgs", "top8"], writes=["msk"])
                    fw.op("dve", lambda e: e.tensor_scalar(out=nmx, in0=top8[:, 0:1], scalar1=-1.0, scalar2=None, op0=ALU.mult),
                          reads=["top8"], writes=["nmx"])
                    fw.op("act", lambda e: e.activation(out=ex, in_=lgs, func=AF.Exp, bias=nmx), reads=["lgs", "nmx"], writes=["ex"])
                    fw.op("dve", lambda e: e.tensor_tensor(out=ex, in0=ex, in1=msk, op=ALU.mult), reads=["ex", "msk"], writes=["ex"])
                    fw.op("dve", lambda e: e.tensor_reduce(out=ssum, in_=ex, axis=AX.X, op=ALU.add), reads=["ex"], writes=["ssum"])
                    fw.op("dve", lambda e: e.reciprocal(out=ssum, in_=ssum), reads=["ssum"], writes=["ssum"])
                    fw.op("dve", lambda e: e.tensor_scalar(out=gts, in0=ex, scalar1=ssum, scalar2=None, op0=ALU.mult),
                          reads=["ex", "ssum"], writes=["gts"])
                    fw.op("pe", lambda e: e.transpose(out=PS[2][0:NE, tt * 128:(tt + 1) * 128], in_=gts, identity=ident_f),
                          reads=["gts", "ident_f"], writes=[("ps", 2)])
                fw.op("act", lambda e: e.copy(out=gt_[:, :N], in_=PS[2][0:NE, :N]), reads=[("ps", 2)], writes=[gtk])
                fw.dma("sp", gTd[:, b * T + t0:b * T + t0 + N], gt_[:, :N], reads=[gtk], writes=[("gTd", b, t0)])
        P.end()

    def phase_moe(l):
        P = Phase("moe")
        wup = [P.sb([128, 8, 2048], BF16) for _ in range(2)]
        wdn = [P.sb([128, 8, 1024], BF16) for _ in range(2)]
        hT2 = P.sb([128, 8, GRP], BF16)
        yacc = P.sb([128, 8, GRP], F32)
        gTs = P.sb([NE, GRP], F32)
        sel = P.sb([NE, NE, 128], F32)
        bup = P.sb([128, NE, 2, 8], F32)
        bdn = P.sb([NE, D], F32)
        Ge = [P.sb([128, MB], F32) for _ in range(2)]
        actT = [P.sb([128, 8, MB], BF16) for _ in range(2)]
        tg = [P.sb([128, MB], F32) for _ in range(2)]
        tsg = [P.sb([128, MB], F32) for _ in range(2)]
        tl = [P.sb([128, MB], F32) for _ in range(2)]
        fw.dma("sp", bup, I["bupT"][l], writes=["bup"])
        fw.dma("sp", bdn, I["b_down"][l], writes=["bdn"])
        fw.op("dve", lambda e: e.tensor_scalar(out=bup[:, :, 1, :], in0=bup[:, :, 1, :], scalar1=1.0, scalar2=None, op0=ALU.add),
              reads=["bup"], writes=["bup"])
        fw.op("pool", lambda e: e.memset(sel, 0.0), writes=["sel"])
        fw.op("pool", lambda e: e.affine_select(out=sel, in_=sel, pattern=[[-1, NE], [0, 128]],
                                                compare_op=ALU.not_equal, fill=1.0, base=0, channel_multiplier=1),
              reads=["sel"], writes=["sel"])
        ngrp = NBC * T // GRP
        nblk = GRP // MB

        def loadw(i):
            e_ = i % NE
            fw.dma("pool", wup[i % 2], I["w_up"][l, e_].rearrange("(j p) n -> p j n", p=128), writes=[("wup", i % 2)])

        def loadwd(i):
            e_ = i % NE
            fw.dma("pool", wdn[i % 2], I["w_down"][l, e_].rearrange("(j p) n -> p j n", p=128), writes=[("wdn", i % 2)])
        loadw(0)
        loadwd(0)
        ci = 0
        for g in range(ngrp):
            b = g // 2
            g0 = (g % 2) * GRP
            fw.dma("sp", hT2, fm(fmaj["hT2"][b])[:, :, g0:g0 + GRP], writes=["hT2"])
            fw.dma("sp", gTs, gTd[:, b * T + g0:b * T + g0 + GRP], writes=["gTs"])
            for blk in range(nblk):
                for co in range(8):
                    ps = PS[4 + co % 2]
                    pk = ("ps", 4 + co % 2)
                    mm(ps[:, :MB], bdn[:, co * 128:(co + 1) * 128], gTs[:, blk * MB:(blk + 1) * MB], True, True, ["bdn", "gTs"], [pk])
                    fw.op("act", lambda e: e.copy(out=yacc[:, co, blk * MB:(blk + 1) * MB], in_=ps[:, :MB]), reads=[pk], writes=[("yacc", blk)])
            tasks = [(ex, blk) for ex in range(NE) for blk in range(nblk)]

            def up(ti):
                nonlocal ci
                ex, blk = tasks[ti]
                wix = wbase + ex
                wu = wup[wix % 2]
                wuk = ("wup", wix % 2)
                if blk == 0 and wix + 1 < ngrp * NE:
                    loadw(wix + 1)
                tsl = slice(blk * MB, (blk + 1) * MB)
                ge = Ge[ti % 2]
                gek = ("Ge", ti % 2)
                at = actT[ti % 2]
                atk = ("actT", ti % 2)
                mm(PS[6][:, :MB], sel[:, ex, :], gTs[:, tsl], True, True, ["sel", "gTs"], [("ps", 6)])
                fw.op("act", lambda e: e.copy(out=ge, in_=PS[6][:, :MB]), reads=[("ps", 6)], writes=[gek])
                for c in range(8):
                    q = ci % 2
                    ci += 1
                    pg = PS[2 * q]
                    pl = PS[2 * q + 1]
                    pgk = ("ps", 2 * q)
                    plk = ("ps", 2 * q + 1)
                    for k in range(8):
                        mm(pg[:, :MB], wu[:, k, c * 256:(c + 1) * 256:2], hT2[:, k, tsl], k == 0, k == 7, [wuk, "hT2"], [pgk])
                    for k in range(8):
                        mm(pl[:, :MB], wu[:, k, c * 256 + 1:(c + 1) * 256:2], hT2[:, k, tsl], k == 0, k == 7, [wuk, "hT2"], [plk])
                    g_ = tg[q]
                    s_ = tsg[q]
                    l_ = tl[q]
                    fw.op("dve", lambda e: e.tensor_scalar(out=g_, in0=pg[:, :MB], scalar1=bup[:, ex, 0, c:c + 1], scalar2=7.0, op0=ALU.add, op1=ALU.min),
                          reads=[pgk, "bup"], writes=[("tg", q)])
                    fw.op("act", lambda e: e.activation(out=s_, in_=g_, func=AF.Sigmoid, scale=1.702), reads=[("tg", q)], writes=[("tsg", q)])
                    fw.op("dve", lambda e: e.tensor_scalar(out=l_, in0=pl[:, :MB], scalar1=bup[:, ex, 1, c:c + 1], scalar2=8.0, op0=ALU.add, op1=ALU.min),
                          reads=[plk, "bup"], writes=[("tl", q)])
                    fw.op("dve", lambda e: e.scalar_tensor_tensor(out=l_, in0=l_, scalar=-6.0, in1=ge, op0=ALU.max, op1=ALU.mult),
                          reads=[("tl", q), gek], writes=[("tl", q)])
                    fw.op("pool", lambda e: e.tensor_tensor(out=g_, in0=g_, in1=s_, op=ALU.mult), reads=[("tg", q), ("tsg", q)], writes=[("tg", q)])
                    fw.op("pool", lambda e: e.tensor_tensor(out=at[:, c, :], in0=g_, in1=l_, op=ALU.mult), reads=[("tg", q), ("tl", q)], writes=[atk])

            def down(ti):
                ex, blk = tasks[ti]
                wix = wbase + ex
                wd = wdn[wix % 2]
                wdk = ("wdn", wix % 2)
                if blk == 0 and wix + 1 < ngrp * NE:
                    loadwd(wix + 1)
                tsl = slice(blk * MB, (blk + 1) * MB)
                at = actT[ti % 2]
                atk = ("actT", ti % 2)
                for co in range(8):
                    ps = PS[4 + co % 2]
                    pk = ("ps", 4 + co % 2)
                    for c in range(8):
                        mm(ps[:, :MB], wd[:, c, co * 128:(co + 1) * 128], at[:, c, :], c == 0, c == 7, [wdk, atk], [pk])
                    fw.op("dve", lambda e: e.tensor_tensor(out=yacc[:, co, tsl], in0=ps[:, :MB], in1=yacc[:, co, tsl], op=ALU.add),
                          reads=[pk, ("yacc", blk)], writes=[("yacc", blk)])

            wbase = g * NE
            up(0)
            for ti in range(len(tasks)):
                if ti + 1 < len(tasks):
                    up(ti + 1)
                down(ti)
            fw.dma("sp", fm(yT[b])[:, :, g0:g0 + GRP], yacc, reads=[("yacc", blk) for blk in range(nblk)], writes=[("yT", g)])
        P.end()

    def phase_moepost(l, last):
        P = Phase("mpost")
        xt = [P.sb([128, 8, 512], F32) for _ in range(2)]
        yt = [P.sb([128, 8, 512], F32) for _ in range(2)]
        sq = P.sb([128, 8, 512], F32)
        rstd = P.sb([128, 512], F32)
        oo = [P.sb([128, 8, 512], F32) for _ in range(2)]
        i = 0
        for b in range(NBC):
            for (t0, N) in BLOCKS:
                if last and t0 < TC:
                    continue
                p = i % 2
                i += 1
                r = 2 if t0 < TC else b
                fw.dma("sp", xt[p][:, :, :N], fm(xres[b])[:, :, t0:t0 + N], writes=[("xt", p)])
                fw.dma("sp", yt[p][:, :, :N], fm(yT[b])[:, :, t0:t0 + N], writes=[("yt", p)])
                for c in range(8):
                    fw.op("dve", lambda e: e.scalar_tensor_tensor(out=xt[p][:, c, :N], in0=yt[p][:, c, :N], scalar=modT[:, 40 + c, r:r + 1],
                                                                  in1=xt[p][:, c, :N], op0=ALU.mult, op1=ALU.add),
                          reads=[("xt", p), ("yt", p), "modT"], writes=[("xt", p)])
                if not last:
                    fw.dma("sp", fm(xres[b])[:, :, t0:t0 + N], xt[p][:, :, :N], reads=[("xt", p)], writes=[("xres", b, t0)])
                else:
                    def out_fn(j, p=p, N=N):
                        fw.op("dve", lambda e: e.scalar_tensor_tensor(out=oo[p][:, j, :N], in0=xt[p][:, j, :N], scalar=fng[:, j:j + 1],
                                                                      in1=rstd[:, :N], op0=ALU.mult, op1=ALU.mult),
                              reads=[("xt", p), "rstd", "fng"], writes=[("oo", p)])
                    emit_norm(xt[p], N, ("xt", p), sq, rstd, None, r, out_fn)
                    fw.dma("sp", fm(outT[b])[:, :, t0 - TC:t0 - TC + N], oo[p][:, :, :N], reads=[("oo", p)], writes=[("out", b, t0)])
        P.end()


    IOA = bass.IndirectOffsetOnAxis

    def phase_moepre_sparse(l):
        P = Phase("spre")
        xt = [P.sb([128, 8, 512], F32) for _ in range(2)]
        sq = P.sb([128, 8, 512], F32)
        rstd = P.sb([128, 512], F32)
        tmp = [P.sb([128, 512], F32) for _ in range(2)]
        hf = P.sb([128, 8, 512], F32)
        hb = P.sb([128, 8, 512], BF16)
        htk = [P.sb([128, 1024], BF16) for _ in range(2)]
        wr = P.sb([128, 8, NE], F32)
        brt = P.sb([128, NE], F32)
        fw.dma("sp", wr, I["w_router"][l].rearrange("(j p) n -> p j n", p=128), writes=["wr"])
        fw.dma("sp", brt, I["b_router"][l].partition_broadcast(128), writes=["brt"])
        G_all = P.sb([128, 36, NE], F32)
        M_all = P.sb([128, 36, NE], F32)
        lgs = P.sb([128, NE], F32)
        top8 = P.sb([128, 8], F32)
        nmx = P.sb([128, 1], F32)
        ex = P.sb([128, NE], F32)
        ssum = P.sb([128, 1], F32)
        zt = P.sb([128, 1024], BF16)
        zf = P.sb([128, NE], F32)
        fill = P.sb([128, NSLOT * 2 // 128], I32)
        Lm = P.sb([128, 128], F32)
        pj = P.sb([128, 8], F32)
        tokid = P.sb([128, 36, 2], I32)
        fw.dma("sp", Lm, I["Lmat"], writes=["Lm"])
        fw.dma("sp", pj, I["pj"], writes=["pj"])
        fw.dma("sp", tokid, I["tokid"], writes=["tokid"])
        fw.op("pool", lambda e: e.memset(zt, 0.0), writes=["zt"])
        fw.op("pool", lambda e: e.memset(zf, 0.0), writes=["zf"])
        fw.op("pool", lambda e: e.memset(fill, NTOK), writes=["fill"])
        fw.dma("sp", h2tok[NTOK:NTOK + 128, :], zt, reads=["zt"], writes=["h2z"])
        fw.dma("sp", gtab[NTOK:NTOK + 128, :], zf, reads=["zf"], writes=["gtz"])
        fw.dma("sp", slot_tok.rearrange("(p a) c -> p (a c)", p=128), fill, reads=["fill"], writes=["stfill"])
        i = 0
        for b in range(NBC):
            for (t0, N) in BLOCKS:
                x_ = xt[i % 2]
                xk = ("xt", i % 2)
                i += 1
                fw.dma("sp", x_[:, :, :N], fm(xres[b])[:, :, t0:t0 + N], writes=[xk])
                r = 2 if t0 < TC else b

                def out_fn(j, x_=x_, xk=xk, N=N, r=r):
                    tm = tmp[j % 2]
                    fw.op("dve", lambda e: e.scalar_tensor_tensor(out=tm[:, :N], in0=x_[:, j, :N], scalar=mul2[:, j, r:r + 1],
                                                                  in1=rstd[:, :N], op0=ALU.mult, op1=ALU.mult),
                          reads=[xk, "rstd", "mul2"], writes=[("tmp", j % 2)])
                    fw.op("act", lambda e: e.activation(out=hf[:, j, :N], in_=tm[:, :N], func=AF.Identity, bias=modT[:, 24 + j, r:r + 1]),
                          reads=[("tmp", j % 2), "modT"], writes=["hf"])
                emit_norm(x_, N, xk, sq, rstd, mul2, r, out_fn)
                fw.op("pool", lambda e: e.tensor_copy(out=hb[:, :, :N], in_=hf[:, :, :N]), reads=["hf"], writes=["hb"])
                for tt in range(N // 128):
                    gi = b * 18 + t0 // 128 + tt
                    a = gi % 2
                    for j in range(8):
                        fw.op("pe", lambda e: e.transpose(out=PSB[a][:, j * 128:(j + 1) * 128], in_=hb[:, j, tt * 128:(tt + 1) * 128], identity=ident_b),
                              reads=["hb", "ident_b"], writes=[("psb", a)])
                    fw.op("act", lambda e: e.copy(out=htk[a], in_=PSB[a]), reads=[("psb", a)], writes=[("htk", a)])
                    fw.dma("sp", h2tok[gi * 128:(gi + 1) * 128, :], htk[a], reads=[("htk", a)], writes=[("h2tok", gi)])
                    lp = PS[1][:, (tt % 4) * 32:(tt % 4) * 32 + 32]
                    for j in range(8):
                        mm(lp, hf[:, j, tt * 128:(tt + 1) * 128], wr[:, j, :], j == 0, j == 7, ["hf", "wr"], [("ps", 1)])
                    fw.op("dve", lambda e: e.tensor_tensor(out=lgs, in0=lp, in1=brt, op=ALU.add), reads=[("ps", 1), "brt"], writes=["lgs"])
                    fw.op("dve", lambda e: e.max(out=top8, in_=lgs), reads=["lgs"], writes=["top8"])
                    fw.op("dve", lambda e: e.tensor_scalar(out=M_all[:, gi, :], in0=lgs, scalar1=top8[:, 3:4], scalar2=None, op0=ALU.is_ge),
                          reads=["lgs", "top8"], writes=[("M", gi)])
                    fw.op("dve", lambda e: e.tensor_scalar(out=nmx, in0=top8[:, 0:1], scalar1=-1.0, scalar2=None, op0=ALU.mult),
                          reads=["top8"], writes=["nmx"])
                    fw.op("act", lambda e: e.activation(out=ex, in_=lgs, func=AF.Exp, bias=nmx), reads=["lgs", "nmx"], writes=["ex"])
                    fw.op("dve", lambda e: e.tensor_tensor(out=ex, in0=ex, in1=M_all[:, gi, :], op=ALU.mult), reads=["ex", ("M", gi)], writes=["ex"])
                    fw.op("dve", lambda e: e.tensor_reduce(out=ssum, in_=ex, axis=AX.X, op=ALU.add), reads=["ex"], writes=["ssum"])
                    fw.op("dve", lambda e: e.reciprocal(out=ssum, in_=ssum), reads=["ssum"], writes=["ssum"])
                    fw.op("dve", lambda e: e.tensor_scalar(out=G_all[:, gi, :], in0=ex, scalar1=ssum, scalar2=None, op0=ALU.mult),
                          reads=["ex", "ssum"], writes=[("G", gi)])
                    fw.dma("sp", gtab[gi * 128:(gi + 1) * 128, :], G_all[:, gi, :], reads=[("G", gi)], writes=[("gtab", gi)])
        cnt = P.sb([128, NE], F32)
        ntl = P.sb([128, NE], F32)
        cume = P.sb([128, NE], F32)
        cb = P.sb([128, NE], F32)
        sf = P.sb([128, NE], F32)
        t8 = P.sb([128, 8], F32)
        cm = P.sb([128, NE], F32)
        wf = P.sb([128, NTILE, 8], F32)
        bf_ = P.sb([128, NTILE], F32)
        for gi in range(36):
            mm(PS[3][:, 0:NE], ones_f, M_all[:, gi, :], gi == 0, gi == 35, ["ones_f", ("M", gi)], [("ps", 3)])
        fw.op("dve", lambda e: e.tensor_copy(out=cnt, in_=PS[3][:, 0:NE]), reads=[("ps", 3)], writes=["cnt"])
        fw.op("dve", lambda e: e.tensor_scalar(out=ntl, in0=cnt, scalar1=0.0, scalar2=None, op0=ALU.is_gt), reads=["cnt"], writes=["ntl"])
        for m in range(1, NTOK // MT + 1):
            fw.op("dve", lambda e: e.scalar_tensor_tensor(out=ntl, in0=cnt, scalar=float(m * MT), in1=ntl, op0=ALU.is_gt, op1=ALU.add),
                  reads=["cnt", "ntl"], writes=["ntl"])
        fw.op("dve", lambda e: e.tensor_tensor_scan(out=cume, data0=ones_f[:, 0:NE], data1=ntl, initial=0.0, op0=ALU.mult, op1=ALU.add),
              reads=["ntl", "ones_f"], writes=["cume"])
        fw.op("dve", lambda e: e.tensor_tensor(out=cb, in0=cume, in1=ntl, op=ALU.subtract), reads=["cume", "ntl"], writes=["cb"])
        fw.op("dve", lambda e: e.tensor_scalar(out=cb, in0=cb, scalar1=float(MT), scalar2=None, op0=ALU.mult), reads=["cb"], writes=["cb"])
        for gi in range(36):
            mm(PS[4][:, 0:NE], Lm, M_all[:, gi, :], True, True, ["Lm", ("M", gi)], [("ps", 4)])
            mm(PS[5][:, 0:NE], ones_f, M_all[:, gi, :], True, True, ["ones_f", ("M", gi)], [("ps", 5)])
            fw.op("dve", lambda e: e.tensor_tensor(out=sf, in0=PS[4][:, 0:NE], in1=cb, op=ALU.add), reads=[("ps", 4), "cb"], writes=["sf"])
            fw.op("dve", lambda e: e.scalar_tensor_tensor(out=sf, in0=sf, scalar=1.0, in1=M_all[:, gi, :], op0=ALU.add, op1=ALU.mult),
                  reads=["sf", ("M", gi)], writes=["sf"])
            fw.op("dve", lambda e: e.tensor_scalar(out=sf, in0=sf, scalar1=-1.0, scalar2=None, op0=ALU.add), reads=["sf"], writes=["sf"])
            fw.op("dve", lambda e: e.max(out=t8, in_=sf), reads=["sf"], writes=["t8"])
            fw.op("dve", lambda e: e.tensor_copy(out=S4_all[:, gi, :], in_=t8[:, 0:4]), reads=["t8"], writes=[("S4", gi)])
            fw.op("dve", lambda e: e.tensor_tensor(out=cb, in0=cb, in1=PS[5][:, 0:NE], op=ALU.add), reads=["cb", ("ps", 5)], writes=["cb"])
            for k in range(4):
                fw.idma(slot_tok, IOA(ap=S4_all[:, gi, k:k + 1], axis=0), tokid[:, gi, :], None,
                        reads=[("S4", gi), "tokid", "stfill"], writes=[("st", gi, k)])
        for t in range(NTILE):
            fw.op("dve", lambda e: e.tensor_scalar(out=cm, in0=cume, scalar1=float(t), scalar2=None, op0=ALU.is_le), reads=["cume"], writes=["cm"])
            fw.op("dve", lambda e: e.tensor_reduce(out=ETf[:, t:t + 1], in_=cm, axis=AX.X, op=ALU.add), reads=["cm"], writes=["ETf"])
        fw.op("dve", lambda e: e.tensor_scalar(out=ETf, in0=ETf, scalar1=float(NE - 1), scalar2=None, op0=ALU.min), reads=["ETf"], writes=["ETf"])
        fw.op("dve", lambda e: e.tensor_scalar(out=bf_, in0=ETf, scalar1=float(l * NE), scalar2=None, op0=ALU.add), reads=["ETf"], writes=["bf_"])
        fw.op("dve", lambda e: e.tensor_copy(out=eidx, in_=bf_), reads=["bf_"], writes=["eidx"])
        fw.op("dve", lambda e: e.tensor_scalar(out=bf_, in0=ETf, scalar1=128.0, scalar2=pj[:, 0:1], op0=ALU.mult, op1=ALU.add),
              reads=["ETf", "pj"], writes=["bf_"])
        fw.op("dve", lambda e: e.tensor_scalar(out=bf_, in0=bf_, scalar1=float(l * NE * 128), scalar2=None, op0=ALU.add), reads=["bf_"], writes=["bf_"])
        fw.op("dve", lambda e: e.tensor_copy(out=bidx, in_=bf_), reads=["bf_"], writes=["bidx"])
        for j in range(8):
            fw.op("dve", lambda e: e.tensor_scalar(out=wf[:, :, j], in0=ETf, scalar1=1024.0, scalar2=pj[:, j:j + 1], op0=ALU.mult, op1=ALU.add),
                  reads=["ETf", "pj"], writes=["wf"])
        fw.op("dve", lambda e: e.tensor_scalar(out=wf, in0=wf, scalar1=float(l * NE * 1024), scalar2=None, op0=ALU.add), reads=["wf"], writes=["wf"])
        fw.op("dve", lambda e: e.tensor_copy(out=widx, in_=wf), reads=["wf"], writes=["widx"])
        P.end()

    def phase_wcast(l):
        P = Phase("wcast")
        bu = [P.sb([128, 8, 2048], BF16) for _ in range(3)]
        bd = [P.sb([128, 8, 1024], BF16) for _ in range(3)]
        for ex in range(NE):
            s_ = ex % 3
            fw.dma("pool", bu[s_], I["w_up"][l, ex].rearrange("(j p) n -> p j n", p=128), writes=[("bu", s_)])
            fw.dma("sp", wupb[ex * D:(ex + 1) * D, :].rearrange("(j p) n -> p j n", p=128), bu[s_], reads=[("bu", s_)], writes=[("wupb", ex)])
            fw.dma("pool", bd[s_], I["w_down"][l, ex].rearrange("(j p) n -> p j n", p=128), writes=[("bd", s_)])
            fw.dma("sp", wdnb[ex * D:(ex + 1) * D, :].rearrange("(j p) n -> p j n", p=128), bd[s_], reads=[("bd", s_)], writes=[("wdnb", ex)])
        P.end()

    def phase_moe_sparse(l):
        P = Phase("smoe")
        wup = [P.sb([128, 8, 2048], BF16) for _ in range(2)]
        wdn = [P.sb([128, 8, 1024], BF16) for _ in range(2)]
        bupg = [P.sb([128, 16], F32) for _ in range(2)]
        bdng = [P.sb([128, 1024], F32) for _ in range(2)]
        stok = [P.sb([128, 4, 2], I32) for _ in range(2)]
        htk = [[P.sb([128, 1024], BF16) for _ in range(4)] for _ in range(2)]
        grow = [P.sb([128, 4, NE], F32) for _ in range(2)]
        hsT = [P.sb([128, 8, MT], BF16) for _ in range(2)]
        at = [P.sb([128, 8, MT], BF16) for _ in range(2)]
        oh = P.sb([128, NE], F32)
        gtmp = P.sb([128, 4, NE], F32)
        gsl = [P.sb([128, 4], F32) for _ in range(2)]
        tg = [P.sb([128, MT], F32) for _ in range(2)]
        tsg = [P.sb([128, MT], F32) for _ in range(2)]
        tl = [P.sb([128, MT], F32) for _ in range(2)]
        ytmp = [P.sb([128, 512], F32) for _ in range(2)]
        yrow = [P.sb([128, 1024], F32) for _ in range(2)]
        iota = P.sb([128, NE], F32)
        fw.dma("sp", iota, I["iota32"], writes=["iota"])
        wv = I["w_up"].rearrange("l e r n -> (l e r) n")
        wdv = I["w_down"].rearrange("l e r n -> (l e r) n")
        bupv = I["bup2"].rearrange("l e p n -> (l e p) n")
        bdv = I["b_down"].rearrange("l e n -> (l e) n")

        def gather(t):
            s_ = t % 2
            fw.dma("sp", stok[s_], slot_tok[t * MT:(t + 1) * MT, :].rearrange("(a p) c -> p a c", p=128), writes=[("stok", s_)])
            for j in range(8):
                fw.idma(wup[s_][:, j, :], None, wv, IOA(ap=widx[:, t, j:j + 1], axis=0), writes=[("wup", s_)])
            for a in range(4):
                fw.idma(htk[s_][a], None, h2tok, IOA(ap=stok[s_][:, a, 0:1], axis=0), reads=[("stok", s_)], writes=[("htk", s_, a)])
                fw.idma(grow[s_][:, a, :], None, gtab, IOA(ap=stok[s_][:, a, 0:1], axis=0), reads=[("stok", s_)], writes=[("grow", s_)])
            fw.idma(bupg[s_], None, bupv, IOA(ap=bidx[:, t:t + 1], axis=0), writes=[("bupg", s_)])
            fw.idma(bdng[s_], None, bdv, IOA(ap=eidx[:, t:t + 1], axis=0), writes=[("bdng", s_)])
            for j in range(8):
                fw.idma(wdn[s_][:, j, :], None, wdv, IOA(ap=widx[:, t, j:j + 1], axis=0), writes=[("wdn", s_)])

        gather(0)
        ci = 0
        yi = 0
        ev = 0
        for t in range(NTILE):
            s_ = t % 2
            if t + 1 < NTILE:
                gather(t + 1)
            fw.op("dve", lambda e: e.tensor_scalar(out=oh, in0=iota, scalar1=ETf[:, t:t + 1], scalar2=None, op0=ALU.is_equal),
                  reads=["iota"], writes=["oh"])
            for a in range(4):
                fw.op("dve", lambda e: e.tensor_tensor(out=gtmp[:, a, :], in0=grow[s_][:, a, :], in1=oh, op=ALU.mult),
                      reads=[("grow", s_), "oh"], writes=["gtmp"])
            fw.op("dve", lambda e: e.tensor_reduce(out=gsl[s_], in_=gtmp, axis=AX.X, op=ALU.add), reads=["gtmp"], writes=[("gsl", s_)])
            fw.op("dve", lambda e: e.tensor_scalar(out=bupg[s_][:, 8:16], in0=bupg[s_][:, 8:16], scalar1=1.0, scalar2=None, op0=ALU.add),
                  reads=[("bupg", s_)], writes=[("bupg", s_)])
            for jp in range(4):
                bank = PSB[jp % 2]
                bk = ("psb", jp % 2)
                for jj in range(2):
                    j = 2 * jp + jj
                    for a in range(4):
                        fw.op("pe", lambda e: e.transpose(out=bank[:, jj * 512 + a * 128:jj * 512 + (a + 1) * 128],
                                                          in_=htk[s_][a][:, j * 128:(j + 1) * 128], identity=ident_b),
                              reads=[("htk", s_, a), "ident_b"], writes=[bk])
                dst = hsT[s_][:, 2 * jp:2 * jp + 2, :].rearrange("p j n -> p (j n)")
                if jp % 2 == 0:
                    fw.op("act", lambda e: e.copy(out=dst, in_=bank), reads=[bk], writes=[("hsT", s_)])
                else:
                    fw.op("dve", lambda e: e.tensor_copy(out=dst, in_=bank), reads=[bk], writes=[("hsT", s_)])
            wu = wup[s_]
            wuk = ("wup", s_)
            for c in range(8):
                q = ci % 2
                ci += 1
                pg = PS[2 * q]
                pl = PS[2 * q + 1]
                pgk = ("ps", 2 * q)
                plk = ("ps", 2 * q + 1)
                for k in range(8):
                    mm(pg, wu[:, k, c * 256:(c + 1) * 256:2], hsT[s_][:, k, :], k == 0, k == 7, [wuk, ("hsT", s_)], [pgk])
                for k in range(8):
                    mm(pl, wu[:, k, c * 256 + 1:(c + 1) * 256:2], hsT[s_][:, k, :], k == 0, k == 7, [wuk, ("hsT", s_)], [plk])
                g_ = tg[q]
                sg_ = tsg[q]
                l_ = tl[q]
                fw.op("dve", lambda e: e.tensor_scalar(out=g_, in0=pg, scalar1=bupg[s_][:, c:c + 1], scalar2=7.0, op0=ALU.add, op1=ALU.min),
                      reads=[pgk, ("bupg", s_)], writes=[("tg", q)])
                fw.op("act", lambda e: e.activation(out=sg_, in_=g_, func=AF.Sigmoid, scale=1.702), reads=[("tg", q)], writes=[("tsg", q)])
                fw.op("act", lambda e: e.activation(out=l_, in_=pl, func=AF.Identity, bias=bupg[s_][:, 8 + c:9 + c]),
                      reads=[plk, ("bupg", s_)], writes=[("tl", q)])
                fw.op("dve", lambda e: e.tensor_scalar(out=l_, in0=l_, scalar1=8.0, scalar2=-6.0, op0=ALU.min, op1=ALU.max),
                      reads=[("tl", q)], writes=[("tl", q)])
                fw.op("dve", lambda e: e.tensor_tensor(out=g_, in0=g_, in1=sg_, op=ALU.mult), reads=[("tg", q), ("tsg", q)], writes=[("tg", q)])
                fw.op("dve", lambda e: e.tensor_tensor(out=at[s_][:, c, :], in0=g_, in1=l_, op=ALU.mult), reads=[("tg", q), ("tl", q)], writes=[("at", s_)])
            wd = wdn[s_]
            wdk = ("wdn", s_)
            for a in range(4):
                yr = yrow[yi % 2]
                yk = ("yrow", yi % 2)
                yi += 1
                for half in range(2):
                    ps = PS[4 + half]
                    pk = ("ps", 4 + half)
                    for c in range(8):
                        mm(ps, at[s_][:, c, a * 128:(a + 1) * 128], wd[:, c, half * 512:(half + 1) * 512], c == 0, c == 7, [("at", s_), wdk], [pk])
                    yt_ = ytmp[half]
                    fw.op("dve", lambda e: e.tensor_tensor(out=yt_, in0=ps, in1=bdng[s_][:, half * 512:(half + 1) * 512], op=ALU.add),
                          reads=[pk, ("bdng", s_)], writes=[("ytmp", half)])
                    fw.op("act", lambda e: e.activation(out=yr[:, half * 512:(half + 1) * 512], in_=yt_, func=AF.Identity, scale=gsl[s_][:, a:a + 1]),
                          reads=[("ytmp", half), ("gsl", s_)], writes=[yk])
                fw.dma("sp", ypairs[t * MT + a * 128:t * MT + (a + 1) * 128, :], yr, reads=[yk], writes=[("yp", t, a)])
        P.end()

    def phase_moepost_sparse(l, last):
        P = Phase("spost")
        xt = [P.sb([128, 8, 512], F32) for _ in range(2)]
        sq = P.sb([128, 8, 512], F32)
        rstd = P.sb([128, 512], F32)
        oo = [P.sb([128, 8, 512], F32) for _ in range(2)]
        yk = [[P.sb([128, 1024], F32) for _ in range(4)] for _ in range(2)]
        i = 0
        si = 0
        for b in range(NBC):
            for (t0, N) in BLOCKS:
                if last and t0 < TC:
                    continue
                p = i % 2
                i += 1
                r = 2 if t0 < TC else b
                fw.dma("sp", xt[p][:, :, :N], fm(xres[b])[:, :, t0:t0 + N], writes=[("xt", p)])
                for sub in range(N // 128):
                    gi = b * 18 + t0 // 128 + sub
                    u = si % 2
                    si += 1
                    for k in range(4):
                        fw.idma(yk[u][k], None, ypairs, IOA(ap=S4_all[:, gi, k:k + 1], axis=0), writes=[("yk", u, k)])
                    fw.op("dve", lambda e: e.tensor_tensor(out=yk[u][0], in0=yk[u][0], in1=yk[u][1], op=ALU.add),
                          reads=[("yk", u, 0), ("yk", u, 1)], writes=[("yk", u, 0)])
                    fw.op("pool", lambda e: e.tensor_tensor(out=yk[u][2], in0=yk[u][2], in1=yk[u][3], op=ALU.add),
                          reads=[("yk", u, 2), ("yk", u, 3)], writes=[("yk", u, 2)])
                    fw.op("dve", lambda e: e.tensor_tensor(out=yk[u][0], in0=yk[u][0], in1=yk[u][2], op=ALU.add),
                          reads=[("yk", u, 0), ("yk", u, 2)], writes=[("yk", u, 0)])
                    for c in range(8):
                        bank = PS[1 + 2 * u + c // 4]
                        fw.op("pe", lambda e: e.transpose(out=bank[:, (c % 4) * 128:(c % 4 + 1) * 128], in_=yk[u][0][:, c * 128:(c + 1) * 128], identity=ident_f),
                              reads=[("yk", u, 0), "ident_f"], writes=[("ps", 1 + 2 * u + c // 4)])
                    for c in range(8):
                        bank = PS[1 + 2 * u + c // 4]
                        fw.op("dve", lambda e: e.scalar_tensor_tensor(out=xt[p][:, c, sub * 128:(sub + 1) * 128], in0=bank[:, (c % 4) * 128:(c % 4 + 1) * 128],
                                                                      scalar=modT[:, 40 + c, r:r + 1], in1=xt[p][:, c, sub * 128:(sub + 1) * 128],
                                                                      op0=ALU.mult, op1=ALU.add),
                              reads=[("ps", 1 + 2 * u + c // 4), ("xt", p), "modT"], writes=[("xt", p)])
                if not last:
                    fw.dma("sp", fm(xres[b])[:, :, t0:t0 + N], xt[p][:, :, :N], reads=[("xt", p)], writes=[("xres", b, t0)])
                else:
                    def out_fn(j, p=p, N=N):
                        fw.op("dve", lambda e: e.scalar_tensor_tensor(out=oo[p][:, j, :N], in0=xt[p][:, j, :N], scalar=fng[:, j:j + 1],
                                                                      in1=rstd[:, :N], op0=ALU.mult, op1=ALU.mult),
                              reads=[("xt", p), "rstd", "fng"], writes=[("oo", p)])
                    emit_norm(xt[p], N, ("xt", p), sq, rstd, None, r, out_fn)
                    fw.dma("sp", fm(outT[b])[:, :, t0 - TC:t0 - TC + N], oo[p][:, :, :N], reads=[("oo", p)], writes=[("out", b, t0)])
        P.end()

    seq = []
    for l in range(nlayers):
        xsrc = I["xin"] if l == 0 else xres
        last = (l == nlayers - 1)
        seq += [("mod", lambda l=l: phase_mod(l)),
                ("mixin", lambda l=l, xsrc=xsrc: phase_mixin(l, xsrc)),
                ("ret", lambda l=l: phase_ret(l)),
                ("na", lambda l=l: phase_na(l)),
                ("lru", lambda l=l: phase_lru(l)),
                ("merge", lambda l=l, xsrc=xsrc: phase_merge(l, xsrc)),
                ("moepre", lambda l=l: (phase_moepre_sparse(l) if SPARSE else phase_moepre(l))),
                ("moe", lambda l=l: (phase_moe_sparse(l) if SPARSE else phase_moe(l))),
                ("moepost", lambda l=l, last=last: (phase_moepost_sparse(l, last) if SPARSE else phase_moepost(l, last)))]
    for name, fn in seq:
        fn()
        if stop_after is not None and name == stop_after:
            break
    fw.barrier()
    return nc, fw


def _consts():
    c = {}
    inv = (10000.0 ** (-np.arange(32, dtype=np.float32) / 32)).astype(np.float32)
    tok = np.arange(TL)
    rows = (tok // 64).astype(np.float32)
    cols = (tok % 64).astype(np.float32)
    ar = rows[None, :] * inv[:, None]
    ac = cols[None, :] * inv[:, None]
    C = np.ones((128, T), np.float32)
    S = np.zeros((128, T), np.float32)
    C[0:32, TC:] = np.cos(ar); C[32:64, TC:] = np.cos(ar); C[64:96, TC:] = np.cos(ac); C[96:128, TC:] = np.cos(ac)
    S[0:32, TC:] = -np.sin(ar); S[32:64, TC:] = np.sin(ar); S[64:96, TC:] = -np.sin(ac); S[96:128, TC:] = np.sin(ac)
    ks = np.float32(128.0 ** -0.5)
    c["ropeC"] = C; c["ropeS"] = S; c["ropeCk"] = C * ks; c["ropeSk"] = S * ks
    j = np.arange(128)[:, None, None]
    rel = np.arange(4)[None, :, None]
    i = np.arange(512)[None, None, :]
    diff = (i - (rel * 128 + j)).astype(np.float32)
    c["Rp"] = np.maximum(diff, 0).astype(np.float32)
    c["Rn"] = np.maximum(-diff, 0).astype(np.float32)
    jj = np.arange(128)[:, None]
    c["EA"] = (128 * np.arange(15)[None, :] - jj).astype(np.float32)
    c["EB"] = (128 * np.arange(14)[None, :] + jj + 1).astype(np.float32)
    c["I512"] = np.broadcast_to(np.arange(512, dtype=np.float32)[None, :], (128, 512)).copy()
    c["I511r"] = np.broadcast_to((511 - np.arange(512)).astype(np.float32)[None, :], (128, 512)).copy()
    c["Lmat"] = (np.arange(128)[:, None] < np.arange(128)[None, :]).astype(np.float32)
    c["pj"] = (np.arange(8)[None, :] * 128 + np.arange(128)[:, None]).astype(np.float32)
    c["iota32"] = np.broadcast_to(np.arange(NE, dtype=np.float32)[None, :], (128, NE)).copy()
    tk = (np.arange(36)[None, :] * 128 + np.arange(128)[:, None]).astype(np.int32)
    c["tokid"] = np.ascontiguousarray(np.stack([tk, tk], axis=-1))
    return c


def _na_bias_gather(rpb):
    L_ = rpb.shape[0]
    w = np.arange(64)[:, None, None]
    rr = np.arange(8)[None, :, None]
    kc = np.arange(64)[None, None, :]
    c0 = np.clip(w - 8, 0, 48)
    valid = (kc >= c0) & (kc < c0 + 16)
    dc = np.clip(kc - w + 15, 0, 30)
    out = np.empty((L_, 8, 128, 8, 512), np.float32)
    for dl in range(8):
        dr = np.clip(rr + 7 - dl, 0, 14)
        drb = np.broadcast_to(dr, (64, 8, 64))
        dcb = np.broadcast_to(dc, (64, 8, 64))
        vb = np.broadcast_to(valid, (64, 8, 64))
        g = rpb[:, :, drb, dcb]
        g = np.where(vb[None, None], g, np.float32(-30000.0)).reshape(L_, 8, 2, 64, 512)
        out[:, :, :, dl, :] = g.reshape(L_, 8, 128, 512)
    return out


def _pT(a):
    sh = a.shape[:-1]
    return np.ascontiguousarray(np.swapaxes(a.reshape(sh + (8, 128)), -1, -2))


def prep_inputs(inp):
    f = lambda k: np.asarray(inp[k], dtype=np.float32)
    x, c, ctx, c_ctx = f("x"), f("c"), f("ctx"), f("c_ctx")
    shared = {}
    shared["w_mod"] = f("w_mod")
    shared["b_modT"] = np.ascontiguousarray(f("b_mod").reshape(2, 48, 128).transpose(0, 2, 1))
    shared["n1gT"] = _pT(f("norm1_g"))
    shared["n2gT"] = _pT(f("norm2_g"))
    shared["fngT"] = _pT(f("final_norm_g"))
    shared["w_mix_in"] = f("w_mix_in")
    shared["decf"] = f("ret_decay_fwd")
    shared["decb"] = f("ret_decay_bwd")
    shared["na_bias"] = _na_bias_gather(f("na_rel_bias"))
    shared["convwT"] = np.ascontiguousarray(f("lru_conv_w").reshape(2, 4, 8, 128).transpose(0, 3, 2, 1))
    shared["convbT"] = _pT(f("lru_conv_b"))
    shared["lru_wa"] = f("lru_gate_a_w")
    shared["lru_wx"] = f("lru_gate_x_w")
    shared["lru_baT"] = np.ascontiguousarray(f("lru_gate_a_b").reshape(2, 2, 8, 128).transpose(0, 3, 1, 2))
    shared["lru_bxT"] = np.ascontiguousarray(f("lru_gate_x_b").reshape(2, 2, 8, 128).transpose(0, 3, 1, 2))
    shared["lru_lamT"] = np.ascontiguousarray(f("lru_lambda").reshape(2, 2, 8, 128).transpose(0, 3, 1, 2))
    shared["w_branch"] = f("w_branch")
    shared["w_mix_out"] = f("w_mix_out")
    shared["w_router"] = f("w_router")
    shared["b_router"] = f("b_router")
    shared["w_up"] = f("w_expert_up")
    shared["bupT"] = np.ascontiguousarray(f("b_expert_up").reshape(2, NE, 8, 128, 2).transpose(0, 3, 1, 4, 2))
    shared["bup2"] = np.ascontiguousarray(f("b_expert_up").reshape(2, NE, 8, 128, 2).transpose(0, 1, 3, 4, 2).reshape(2, NE, 128, 16))
    shared["w_down"] = f("w_expert_down")
    shared["b_down"] = f("b_expert_down")
    shared.update(_consts())
    in_maps = []
    for core in range(NCORES):
        bs = slice(core * NBC, (core + 1) * NBC)
        xa = np.concatenate([ctx[bs], x[bs]], axis=1)
        xin = np.ascontiguousarray(xa.transpose(0, 2, 1).reshape(NBC, 8, 128, T))
        crow = np.stack([c[core * NBC], c[core * NBC + 1], c_ctx], axis=0)
        cT = np.ascontiguousarray(crow.reshape(3, 8, 128).transpose(2, 1, 0))
        m = dict(shared)
        m["xin"] = xin
        m["cT"] = cT
        in_maps.append(m)
    return in_maps


def kernel(**inputs):
    in_maps = prep_inputs(inputs)
    nc, fw = build()
    res = run_bass_kernel_spmd(nc, in_maps, core_ids=list(range(NCORES)))
    outs = []
    for core in range(NCORES):
        o = res.results[core]["outT"]
        outs.append(o.reshape(NBC, D, TL).transpose(0, 2, 1))
    return np.ascontiguousarray(np.concatenate(outs, axis=0)).astype(np.float32)
```

```python
import numpy as np
from contextlib import ExitStack
import concourse.bass as bass
import concourse.mybir as mybir
from concourse.bass_utils import run_bass_kernel_spmd

F32 = mybir.dt.float32
BF16 = mybir.dt.bfloat16
AF = mybir.ActivationFunctionType
ALU = mybir.AluOpType
AX = mybir.AxisListType

NCORES = 8
NBC = 2
D = 1024
TC = 256
TL = 2048
T = TC + TL
NE = 32
EPS = 1e-6
BLOCKS = [(0, 256), (256, 512), (768, 512), (1280, 512), (1792, 512)]
BLOCKS256 = [(i * 256, 256) for i in range(9)]
NDSEM = 12
GRP = 1152
MB = 384
DBG_NSEC = 12
SPARSE = True
MT = 512
NTILE = 68
NSLOT = NTILE * MT
NTOK = 4608
I32 = mybir.dt.int32
DBG_RET = 16
DBG_IB = 4


class FW:
    def __init__(self, nc):
        self.nc = nc
        self.eng = {"pe": nc.tensor, "act": nc.scalar, "dve": nc.vector,
                    "pool": nc.gpsimd, "sp": nc.sync}
        self.sem = {}
        self.cnt = {}
        for e in ("pe", "act", "dve", "pool"):
            self.sem[e] = nc.alloc_semaphore("s_" + e)
            self.cnt[e] = 0
        self.dsem = {}
        self.dcnt = {}
        for q in ("sp", "pool"):
            self.dsem[q] = [nc.alloc_semaphore("d_%s_%d" % (q, i)) for i in range(NDSEM)]
            self.dcnt[q] = 0
        self.waited = {e: {} for e in self.eng}
        self.lastw = {}
        self.readers = {}
        self.ninst = 0
        self.nwait = 0

    def _wait(self, e, tok):
        sem, val, src = tok
        if src == e and e == "pe":
            return
        w = self.waited[e]
        k = id(sem)
        if w.get(k, 0) >= val:
            return
        w[k] = val
        self.eng[e].wait_ge(sem, val)
        self.nwait += 1

    def _deps(self, e, reads, writes):
        for r in reads:
            t = self.lastw.get(r)
            if t is not None:
                self._wait(e, t)
            if (isinstance(r, tuple) and r[0] in ("ps", "psb", "O")) or r in ("ps0", "psm"):
                for t in self.readers.get(r, ()):
                    if t[2] != e:
                        self._wait(e, t)
        for w in writes:
            t = self.lastw.get(w)
            if t is not None:
                self._wait(e, t)
            for t in self.readers.get(w, ()):
                self._wait(e, t)

    def _record(self, tok, reads, writes):
        for r in reads:
            self.readers.setdefault(r, []).append(tok)
        for w in writes:
            self.lastw[w] = tok
            self.readers[w] = []

    def op(self, e, fn, reads=(), writes=()):
        self._deps(e, reads, writes)
        ins = fn(self.eng[e])
        self.cnt[e] += 1
        ins.then_inc(self.sem[e], 1)
        tok = (self.sem[e], self.cnt[e], e)
        self._record(tok, reads, writes)
        self.ninst += 1
        return tok

    def dma(self, q, out, in_, reads=(), writes=(), **kw):
        j = self.dcnt[q]
        sem = self.dsem[q][j % NDSEM]
        gen = j // NDSEM
        if gen > 0:
            self._wait(q, (sem, 16 * gen, "dma"))
        self._deps(q, reads, writes)
        ins = self.eng[q].dma_start(out=out, in_=in_, **kw)
        ins.then_inc(sem, 16)
        self.dcnt[q] = j + 1
        tok = (sem, 16 * (gen + 1), "dma")
        self._record(tok, reads, writes)
        self.ninst += 1
        return tok

    def idma(self, out, out_off, in_, in_off, reads=(), writes=(), **kw):
        q = "pool"
        j = self.dcnt[q]
        sem = self.dsem[q][j % NDSEM]
        gen = j // NDSEM
        if gen > 0:
            self._wait(q, (sem, 16 * gen, "dma"))
        self._deps(q, reads, writes)
        ins = self.eng[q].indirect_dma_start(out=out, out_offset=out_off, in_=in_, in_offset=in_off, **kw)
        ins.then_inc(sem, 16)
        self.dcnt[q] = j + 1
        tok = (sem, 16 * (gen + 1), "dma")
        self._record(tok, reads, writes)
        self.ninst += 1
        return tok

    def barrier(self):
        toks = []
        for e in ("pe", "act", "dve", "pool"):
            if self.cnt[e] > 0:
                toks.append((self.sem[e], self.cnt[e], e))
        for q in ("sp", "pool"):
            j = self.dcnt[q]
            for i in range(NDSEM):
                n = (j - i + NDSEM - 1) // NDSEM if j > i else 0
                if n > 0:
                    toks.append((self.dsem[q][i], 16 * n, "dma"))
        for e in self.eng:
            for t in toks:
                if t[2] == e and e != "pe":
                    pass
                sem, val, src = t
                w = self.waited[e]
                if w.get(id(sem), 0) >= val:
                    continue
                w[id(sem)] = val
                self.eng[e].wait_ge(sem, val)
                self.nwait += 1
        self.lastw = {}
        self.readers = {}


def build(nlayers=2, dbg=(), stop_after=None):
    nc = bass.Bass("TRN2", target_bir_lowering=False)
    fw = FW(nc)
    I = {}

    def din(name, shape):
        I[name] = nc.dram_tensor(name, list(shape), F32, kind="ExternalInput").ap()

    def scr(name, shape, dt):
        kind = "ExternalOutput" if name in dbg else "Internal"
        return nc.dram_tensor(name, list(shape), dt, kind=kind).ap()

    L = 2
    din("xin", [NBC, 8, 128, T])
    din("cT", [128, 8, 3])
    din("w_mod", [L, D, 6 * D])
    din("b_modT", [L, 128, 48])
    din("n1gT", [L, 128, 8])
    din("n2gT", [L, 128, 8])
    din("fngT", [128, 8])
    din("w_mix_in", [L, D, 12 * D])
    din("decf", [L, 8])
    din("decb", [L, 8])
    din("na_bias", [L, 8, 128, 8, 512])
    din("convwT", [L, 128, 8, 4])
    din("convbT", [L, 128, 8])
    din("lru_wa", [L, 2, 8, 128, 128])
    din("lru_wx", [L, 2, 8, 128, 128])
    din("lru_baT", [L, 128, 2, 8])
    din("lru_bxT", [L, 128, 2, 8])
    din("lru_lamT", [L, 128, 2, 8])
    din("w_branch", [L, 3, D, D])
    din("w_mix_out", [L, D, D])
    din("w_router", [L, D, NE])
    din("b_router", [L, NE])
    din("w_up", [L, NE, D, 2 * D])
    din("bupT", [L, 128, NE, 2, 8])
    din("w_down", [L, NE, D, D])
    din("b_down", [L, NE, D])
    din("ropeC", [128, T])
    din("ropeS", [128, T])
    din("ropeCk", [128, T])
    din("ropeSk", [128, T])
    din("Rp", [128, 4, 512])
    din("Rn", [128, 4, 512])
    din("EA", [128, 15])
    din("EB", [128, 14])
    din("I512", [128, 512])
    din("I511r", [128, 512])
    din("Lmat", [128, 128])
    din("pj", [128, 8])
    din("iota32", [128, NE])
    din("bup2", [L, NE, 128, 16])
    I["tokid"] = nc.dram_tensor("tokid", [128, 36, 2], I32, kind="ExternalInput").ap()
    outT = nc.dram_tensor("outT", [NBC, 8, 128, TL], F32, kind="ExternalOutput").ap()

    xres = scr("xres", [NBC, 8, 128, T], F32)
    fmaj = {}
    for nm in ("rqT", "rkT", "rgT", "nqT", "nkT", "lyT", "gaT", "gbT", "gcT", "AinT", "BinT", "CinT", "hT2"):
        fmaj[nm] = scr(nm, [NBC, 8, 128, T], BF16)
    lxT = scr("lxT", [NBC, 8, 128, T], F32)
    rv = scr("rv", [NBC, T, D], BF16)
    nv = scr("nv", [NBC, T, D], BF16)
    gTd = scr("gTd", [NE, NBC * T], F32)
    yT = scr("yT", [NBC, 8, 128, T], F32)
    h2tok = scr("h2tok", [NTOK + 128, D], BF16)
    gtab = scr("gtab", [NTOK + 128, NE], F32)
    slot_tok = scr("slot_tok", [NSLOT, 2], I32)
    ypairs = scr("ypairs", [NSLOT, D], F32)
    wupb = scr("wupb", [NE * 128, 8 * 2 * D], BF16)
    wdnb = scr("wdnb", [NE * 128, 8 * D], BF16)

    def fm(ap_b):
        return ap_b.rearrange("c p t -> p c t")

    PS = [nc.alloc_psum_tensor("ps%d" % i, [128, 512], F32).ap() for i in range(8)]
    PSB = [PS[6].bitcast(BF16), PS[7].bitcast(BF16)]

    def gsb(name, shape, dt):
        return nc.alloc_sbuf_tensor(name, list(shape), dt).ap()

    ones_f = gsb("ones_f", [128, 128], F32)
    ones_b = gsb("ones_b", [128, 128], BF16)
    ident_f = gsb("ident_f", [128, 128], F32)
    ident_b = gsb("ident_b", [128, 128], BF16)
    modT = gsb("modT", [128, 48, 3], F32)
    mul1 = gsb("mul1", [128, 8, 3], F32)
    mul2 = gsb("mul2", [128, 8, 3], F32)
    fng = gsb("fng", [128, 8], F32)
    widx = gsb("widx", [128, NTILE, 8], I32)
    bidx = gsb("bidx", [128, NTILE], I32)
    eidx = gsb("eidx", [128, NTILE], I32)
    ETf = gsb("ETf", [128, NTILE], F32)
    S4_all = gsb("S4_all", [128, 36, 4], I32)
    pidx = gsb("pidx", [128, NTILE], I32)
    fw.op("pool", lambda e: e.memset(ones_f, 1.0), writes=["ones_f"])
    fw.op("pool", lambda e: e.memset(ones_b, 1.0), writes=["ones_b"])
    fw.op("pool", lambda e: e.memset(ident_f, 0.0), writes=["ident_f"])
    fw.op("pool", lambda e: e.affine_select(out=ident_f, in_=ident_f, pattern=[[-1, 128]],
                                            compare_op=ALU.not_equal, fill=1.0, base=0, channel_multiplier=1),
          reads=["ident_f"], writes=["ident_f"])
    fw.op("dve", lambda e: e.tensor_copy(out=ident_b, in_=ident_f), reads=["ident_f"], writes=["ident_b"])
    fw.dma("sp", fng, I["fngT"], writes=["fng"])
    fw.barrier()

    class Phase:
        cnt = 0

        def __init__(self, name):
            self.name = name
            self.es = ExitStack()
            self.n = 0

        def sb(self, shape, dt):
            self.n += 1
            Phase.cnt += 1
            t = self.es.enter_context(nc.sbuf_tensor("%s_%d_%d" % (self.name, Phase.cnt, self.n), list(shape), dt))
            return t.ap()

        def end(self):
            fw.barrier()
            self.es.close()

    def mm(out, lhsT, rhs, start, stop, reads, writes):
        fw.op("pe", lambda e: e.matmul(out, lhsT=lhsT, rhs=rhs, start=start, stop=stop), reads, writes)

    def phase_mod(l):
        P = Phase("mod")
        cs = P.sb([128, 8, 4], F32)
        bm = P.sb([128, 48], F32)
        fw.op("pool", lambda e: e.memset(cs, 0.0), writes=["cs"])
        n1 = P.sb([128, 8], F32)
        n2 = P.sb([128, 8], F32)
        fw.dma("sp", cs[:, :, 0:3], I["cT"], writes=["cs"])
        fw.dma("sp", bm, I["b_modT"][l], writes=["bm"])
        fw.dma("sp", n1, I["n1gT"][l], writes=["n1"])
        fw.dma("sp", n2, I["n2gT"][l], writes=["n2"])
        fw.op("act", lambda e: e.activation(out=cs, in_=cs, func=AF.Silu), reads=["cs"], writes=["cs"])
        wbuf = [P.sb([128, 8, 1024], F32) for _ in range(2)]
        psm = PS[0]
        for s in range(6):
            wb = wbuf[s % 2]
            fw.dma("sp", wb, I["w_mod"][l, :, s * 1024:(s + 1) * 1024].rearrange("(j p) n -> p j n", p=128),
                   writes=[("wm", s % 2)])
            for j in range(8):
                n = s * 8 + j
                for k in range(8):
                    mm(psm[:, n * 4:n * 4 + 4], wb[:, k, j * 128:(j + 1) * 128], cs[:, k, :], k == 0, k == 7,
                       [("wm", s % 2), "cs"], ["psm"])
        psv = psm[:, 0:192].rearrange("p (n r) -> p n r", r=4)
        for r in range(3):
            fw.op("dve", lambda e: e.tensor_tensor(out=modT[:, :, r], in0=psv[:, :, r], in1=bm, op=ALU.add),
                  reads=["psm", "bm"], writes=["modT"])
        for r in range(3):
            fw.op("dve", lambda e: e.scalar_tensor_tensor(out=mul1[:, :, r], in0=modT[:, 8:16, r], scalar=1.0, in1=n1,
                                                          op0=ALU.add, op1=ALU.mult),
                  reads=["modT", "n1"], writes=["mul1"])
            fw.op("dve", lambda e: e.scalar_tensor_tensor(out=mul2[:, :, r], in0=modT[:, 32:40, r], scalar=1.0, in1=n2,
                                                          op0=ALU.add, op1=ALU.mult),
                  reads=["modT", "n2"], writes=["mul2"])
        P.end()

    def emit_norm(xt, N, xkey, sq, rstd, mulT, r, out_fn):
        fw.op("act", lambda e: e.activation(out=sq[:, :, :N], in_=xt[:, :, :N], func=AF.Square),
              reads=[xkey], writes=["sq"])
        for j in range(8):
            mm(PS[0][:, :N], ones_f, sq[:, j, :N], j == 0, j == 7, ["sq", "ones_f"], ["ps0"])
        fw.op("act", lambda e: e.activation(out=rstd[:, :N], in_=PS[0][:, :N], func=AF.Sqrt, scale=1.0 / D, bias=EPS),
              reads=["ps0"], writes=["rstd"])
        fw.op("dve", lambda e: e.reciprocal(out=rstd[:, :N], in_=rstd[:, :N]), reads=["rstd"], writes=["rstd"])
        for j in range(8):
            out_fn(j)

    def phase_mixin(l, xsrc):
        es_h = ExitStack()
        hT = es_h.enter_context(nc.sbuf_tensor("hT_%d" % l, [128, 8, NBC * T], BF16)).ap()
        P = Phase("n1")
        xt = [P.sb([128, 8, 512], F32) for _ in range(2)]
        sq = P.sb([128, 8, 512], F32)
        rstd = P.sb([128, 512], F32)
        tmp = [P.sb([128, 512], F32) for _ in range(2)]
        i = 0
        for b in range(NBC):
            for (t0, N) in BLOCKS:
                x_ = xt[i % 2]
                xk = ("xt", i % 2)
                fw.dma("sp", x_[:, :, :N], fm(xsrc[b])[:, :, t0:t0 + N], writes=[xk])
                r = 2 if t0 < TC else b

                def out_fn(j, x_=x_, xk=xk, N=N, r=r, b=b, t0=t0):
                    tm = tmp[j % 2]
                    fw.op("dve", lambda e: e.scalar_tensor_tensor(out=tm[:, :N], in0=x_[:, j, :N], scalar=mul1[:, j, r:r + 1],
                                                                  in1=rstd[:, :N], op0=ALU.mult, op1=ALU.mult),
                          reads=[xk, "rstd", "mul1"], writes=[("tmp", j % 2)])
                    fw.op("act", lambda e: e.activation(out=hT[:, j, b * T + t0:b * T + t0 + N], in_=tm[:, :N],
                                                        func=AF.Identity, bias=modT[:, j, r:r + 1]),
                          reads=[("tmp", j % 2), "modT"], writes=["hT"])
                emit_norm(x_, N, xk, sq, rstd, mul1, r, out_fn)
                i += 1
        P.end()

        P = Phase("mix")
        wb = [P.sb([128, 8, 1024], BF16) for _ in range(2)]
        wp = P.sb([128, 8, 1024], BF16)
        stage = [P.sb([128, 8, 512], BF16) for _ in range(2)]
        stage32 = P.sb([128, 8, 512], F32)
        stT = [P.sb([128, 1024], BF16) for _ in range(2)]
        rC = P.sb([128, T], F32)
        rS = P.sb([128, T], F32)
        rt = [P.sb([128, 512], F32) for _ in range(4)]
        names = ["rqT", "rkT", None, "rgT", "nqT", "nkT", None, None, "lyT", "gaT", "gbT", "gcT"]

        def loadw(s):
            fw.dma("pool", wb[s % 2], I["w_mix_in"][l, :, s * 1024:(s + 1) * 1024].rearrange("(j p) n -> p j n", p=128),
                   writes=[("wb", s % 2)])
        loadw(0)
        psi = 0
        sti = 0
        ev = 0
        for s in range(DBG_NSEC):
            if s + 1 < DBG_NSEC:
                loadw(s + 1)
            w = wb[s % 2]
            wk = ("wb", s % 2)
            if s < 2:
                Wv = w.rearrange("p j (h q r) -> p (j h) q r", q=4, r=32)
                Pv = wp.rearrange("p j (h q r) -> p (j h) q r", q=4, r=32)
                for q in range(4):
                    eng = "dve" if q % 2 == 0 else "act"
                    if eng == "dve":
                        fw.op("dve", lambda e: e.tensor_copy(out=Pv[:, :, q, :], in_=Wv[:, :, q ^ 1, :]), reads=[wk], writes=["wp"])
                    else:
                        fw.op("act", lambda e: e.copy(out=Pv[:, :, q, :], in_=Wv[:, :, q ^ 1, :]), reads=[wk], writes=["wp"])
                fw.dma("sp", rC, I["ropeC" if s == 0 else "ropeCk"], writes=["rC"])
                fw.dma("sp", rS, I["ropeS" if s == 0 else "ropeSk"], writes=["rS"])
            if s in (2, 6):
                dst = rv if s == 2 else nv
                for b in range(NBC):
                    for tt in range(18):
                        st = stT[sti % 2]
                        sk = ("stT", sti % 2)
                        for half in range(2):
                            ps = PS[psi % 4]
                            pk = ("ps", psi % 4)
                            psi += 1
                            for k in range(8):
                                mm(ps, hT[:, k, b * T + tt * 128:b * T + (tt + 1) * 128], w[:, k, half * 512:(half + 1) * 512],
                                   k == 0, k == 7, ["hT", wk], [pk])
                            if ev % 2 == 0:
                                fw.op("act", lambda e: e.copy(out=st[:, half * 512:(half + 1) * 512], in_=ps), reads=[pk], writes=[sk])
                            else:
                                fw.op("dve", lambda e: e.tensor_copy(out=st[:, half * 512:(half + 1) * 512], in_=ps), reads=[pk], writes=[sk])
                            ev += 1
                        fw.dma("sp", dst[b, tt * 128:(tt + 1) * 128, :], st, reads=[sk], writes=[("dst", s, b, tt)])
                        sti += 1
                continue
            for b in range(NBC):
                for (t0, N) in BLOCKS:
                    if s == 7:
                        st = stage32
                        sk = "stage32"
                    else:
                        st = stage[sti % 2]
                        sk = ("stage", sti % 2)
                        sti += 1
                    hsl = slice(b * T + t0, b * T + t0 + N)
                    for c in range(8):
                        ps = PS[psi % 4]
                        pk = ("ps", psi % 4)
                        psi += 1
                        for k in range(8):
                            mm(ps[:, :N], w[:, k, c * 128:(c + 1) * 128], hT[:, k, hsl], k == 0, k == 7, ["hT", wk], [pk])
                        o = st[:, c, :N]
                        if s < 2:
                            ps2 = PS[4 + psi % 2]
                            pk2 = ("ps", 4 + psi % 2)
                            for k in range(8):
                                mm(ps2[:, :N], wp[:, k, c * 128:(c + 1) * 128], hT[:, k, hsl], k == 0, k == 7, ["hT", "wp"], [pk2])
                            ta = rt[(psi % 2) * 2]
                            tb = rt[(psi % 2) * 2 + 1]
                            ka = ("rt", (psi % 2) * 2)
                            kb = ("rt", (psi % 2) * 2 + 1)
                            fw.op("dve", lambda e: e.tensor_tensor(out=ta[:, :N], in0=ps[:, :N], in1=rC[:, t0:t0 + N], op=ALU.mult),
                                  reads=[pk, "rC"], writes=[ka])
                            fw.op("dve", lambda e: e.tensor_tensor(out=tb[:, :N], in0=ps2[:, :N], in1=rS[:, t0:t0 + N], op=ALU.mult),
                                  reads=[pk2, "rS"], writes=[kb])
                            fw.op("pool", lambda e: e.tensor_tensor(out=o, in0=ta[:, :N], in1=tb[:, :N], op=ALU.add),
                                  reads=[ka, kb], writes=[sk])
                        elif s == 3:
                            fw.op("act", lambda e: e.activation(out=o, in_=ps[:, :N], func=AF.Silu), reads=[pk], writes=[sk])
                        elif s == 8:
                            fw.op("act", lambda e: e.activation(out=o, in_=ps[:, :N], func=AF.Gelu), reads=[pk], writes=[sk])
                        elif s >= 9:
                            fw.op("act", lambda e: e.activation(out=o, in_=ps[:, :N], func=AF.Sigmoid), reads=[pk], writes=[sk])
                        elif s == 4:
                            if ev % 2 == 0:
                                fw.op("act", lambda e: e.activation(out=o, in_=ps[:, :N], func=AF.Copy, scale=0.125), reads=[pk], writes=[sk])
                            else:
                                fw.op("dve", lambda e: e.tensor_scalar(out=o, in0=ps[:, :N], scalar1=0.125, scalar2=None, op0=ALU.mult),
                                      reads=[pk], writes=[sk])
                            ev += 1
                        else:
                            if ev % 2 == 0:
                                fw.op("act", lambda e: e.copy(out=o, in_=ps[:, :N]), reads=[pk], writes=[sk])
                            else:
                                fw.op("dve", lambda e: e.tensor_copy(out=o, in_=ps[:, :N]), reads=[pk], writes=[sk])
                            ev += 1
                    dst = lxT if s == 7 else fmaj[names[s]]
                    fw.dma("sp", fm(dst[b])[:, :, t0:t0 + N], st[:, :, :N], reads=[sk], writes=[("dst", s, b, t0)])
        P.end()
        es_h.close()

    def phase_ret(l):
        P = Phase("ret")
        lg = P.sb([128, 16], F32)
        fw.dma("sp", lg[:, 0:8], I["decf"][l].partition_broadcast(128), writes=["lg"])
        fw.dma("sp", lg[:, 8:16], I["decb"][l].partition_broadcast(128), writes=["lg"])
        fw.op("act", lambda e: e.activation(out=lg, in_=lg, func=AF.Exp, scale=-1.0), reads=["lg"], writes=["lg"])
        fw.op("act", lambda e: e.activation(out=lg, in_=lg, func=AF.Ln, bias=1.0), reads=["lg"], writes=["lg"])
        fw.op("dve", lambda e: e.tensor_scalar(out=lg, in0=lg, scalar1=-1.0, scalar2=None, op0=ALU.mult), reads=["lg"], writes=["lg"])
        Rp = P.sb([128, 4, 512], F32)
        Rn = P.sb([128, 4, 512], F32)
        EA = P.sb([128, 15], F32)
        EB = P.sb([128, 14], F32)
        I512 = P.sb([128, 512], F32)
        I511r = P.sb([128, 512], F32)
        for nm, t in (("Rp", Rp), ("Rn", Rn), ("EA", EA), ("EB", EB), ("I512", I512), ("I511r", I511r)):
            fw.dma("sp", t, I[nm], writes=[nm])
        Dm = P.sb([128, 8, 4, 512], BF16)
        Af = P.sb([128, 8, 15], F32)
        Ab = P.sb([128, 8, 14], F32)
        bf = P.sb([128, 8, 512], F32)
        bb = P.sb([128, 8, 512], F32)
        t1 = P.sb([128, 512], F32)
        for h in range(8):
            for rel in range(4):
                fw.op("dve", lambda e: e.tensor_scalar(out=t1, in0=Rp[:, rel, :], scalar1=lg[:, h:h + 1], scalar2=None, op0=ALU.mult),
                      reads=["Rp", "lg"], writes=["t1"])
                fw.op("dve", lambda e: e.scalar_tensor_tensor(out=t1, in0=Rn[:, rel, :], scalar=lg[:, 8 + h:9 + h], in1=t1,
                                                              op0=ALU.mult, op1=ALU.add),
                      reads=["Rn", "lg", "t1"], writes=["t1"])
                fw.op("act", lambda e: e.activation(out=Dm[:, h, rel, :], in_=t1, func=AF.Exp), reads=["t1"], writes=["Dm"])
            fw.op("act", lambda e: e.activation(out=Af[:, h, :], in_=EA, func=AF.Exp, scale=lg[:, h:h + 1]), reads=["EA", "lg"], writes=["Af"])
            fw.op("act", lambda e: e.activation(out=Ab[:, h, :], in_=EB, func=AF.Exp, scale=lg[:, 8 + h:9 + h]), reads=["EB", "lg"], writes=["Ab"])
            fw.op("act", lambda e: e.activation(out=bf[:, h, :], in_=I512, func=AF.Exp, scale=lg[:, h:h + 1]), reads=["I512", "lg"], writes=["bf"])
            fw.op("act", lambda e: e.activation(out=bb[:, h, :], in_=I511r, func=AF.Exp, scale=lg[:, 8 + h:9 + h]), reads=["I511r", "lg"], writes=["bb"])
        qT = [P.sb([128, T], BF16) for _ in range(2)]
        kT = [P.sb([128, T], BF16) for _ in range(2)]
        V = [P.sb([128, 18, 128], BF16) for _ in range(2)]
        sg = [P.sb([128, T], BF16) for _ in range(2)]
        ost = [P.sb([128, T], BF16) for _ in range(2)]
        pb = [P.sb([128, 512], BF16) for _ in range(4)]
        y32 = P.sb([128, 512], F32)
        e1 = P.sb([128, 512], F32)
        e2 = P.sb([128, 512], F32)
        sqy = P.sb([128, 512], F32)
        rsy = P.sb([128, 512], F32)
        it = 0
        pbi = 0
        evc = 0
        for b in range(NBC):
            for h in range(8):
                if it >= DBG_RET:
                    continue
                p = it % 2
                it += 1
                q_, k_, v_, s_, o_ = qT[p], kT[p], V[p], sg[p], ost[p]
                fw.dma("sp", q_, fmaj["rqT"][b, h], writes=[("q", p)])
                fw.dma("sp", k_, fmaj["rkT"][b, h], writes=[("k", p)])
                fw.dma("sp", v_, rv[b].rearrange("(t p) (h d) -> h p t d", p=128, d=128)[h], writes=[("v", p)])
                fw.dma("sp", s_, fmaj["rgT"][b, h], writes=[("sg", p)])
                SBK = [0, 1, 6, 7]
                steps = []
                blkinfo = {}
                for ib in range(-1, DBG_IB):
                    if ib < 0:
                        q0, N = 0, 256
                        kks = [(kk, [("d", kk)]) for kk in range(2)]
                    else:
                        q0, N = TC + 512 * ib, 512
                        kks = []
                        for kk in range(18):
                            if kk < 2:
                                kks.append((kk, [("f", 2 + 4 * ib - kk), ("b", 12 + kk - 4 * ib)]))
                            else:
                                rel = kk - 2 - 4 * ib
                                if rel < 0:
                                    kks.append((kk, [("f", -rel)]))
                                elif rel < 4:
                                    kks.append((kk, [("d", rel)]))
                                else:
                                    kks.append((kk, [("b", rel - 4)]))
                    tot = {"d": 0, "f": 0, "b": 0}
                    for (_, its) in kks:
                        for (ty, _) in its:
                            tot[ty] += 1
                    blkinfo[ib] = (q0, N, tot)
                    for j, (kk, its) in enumerate(kks):
                        steps.append((ib, kk, its, j == 0, j == len(kks) - 1))

                def emitS(i):
                    ib, kk, _, _, _ = steps[i]
                    q0, N, _ = blkinfo[ib]
                    bk = SBK[i % 4]
                    mm(PS[bk][:, :N], k_[:, kk * 128:(kk + 1) * 128], q_[:, q0:q0 + N], True, True, [("q", p), ("k", p)], [("ps", bk)])

                def epilogue(ib):
                    q0, N, _ = blkinfo[ib]
                    if ib < 0:
                        fw.op("dve", lambda e: e.tensor_copy(out=y32[:, :N], in_=PS[2][:, :N]), reads=[("ps", 2)], writes=["y32"])
                    else:
                        fw.op("dve", lambda e: e.tensor_tensor(out=e1, in0=PS[3], in1=bf[:, h, :], op=ALU.mult), reads=[("ps", 3), "bf"], writes=["e1"])
                        fw.op("dve", lambda e: e.tensor_tensor(out=e2, in0=PS[4], in1=bb[:, h, :], op=ALU.mult), reads=[("ps", 4), "bb"], writes=["e2"])
                        fw.op("pool", lambda e: e.tensor_tensor(out=e1, in0=e1, in1=e2, op=ALU.add), reads=["e1", "e2"], writes=["e1"])
                        fw.op("dve", lambda e: e.tensor_tensor(out=y32, in0=PS[2], in1=e1, op=ALU.add), reads=[("ps", 2), "e1"], writes=["y32"])
                    fw.op("act", lambda e: e.activation(out=sqy[:, :N], in_=y32[:, :N], func=AF.Square), reads=["y32"], writes=["sqy"])
                    mm(PS[5][:, :N], ones_f, sqy[:, :N], True, True, ["sqy", "ones_f"], [("ps", 5)])
                    fw.op("act", lambda e: e.activation(out=rsy[:, :N], in_=PS[5][:, :N], func=AF.Sqrt, scale=1.0 / 128, bias=EPS),
                          reads=[("ps", 5)], writes=["rsy"])
                    fw.op("dve", lambda e: e.reciprocal(out=rsy[:, :N], in_=rsy[:, :N]), reads=["rsy"], writes=["rsy"])
                    fw.op("pool", lambda e: e.tensor_tensor(out=y32[:, :N], in0=y32[:, :N], in1=rsy[:, :N], op=ALU.mult),
                          reads=["y32", "rsy"], writes=["y32"])
                    fw.op("dve", lambda e: e.tensor_tensor(out=o_[:, q0:q0 + N], in0=y32[:, :N], in1=s_[:, q0:q0 + N], op=ALU.mult),
                          reads=["y32", ("sg", p)], writes=[("ost", p)])

                LA = 3
                for i in range(min(LA, len(steps))):
                    emitS(i)
                accb = {"d": 2, "f": 3, "b": 4}
                cnts = None
                for i, (ib, kk, its, first, last) in enumerate(steps):
                    q0, N, tot = blkinfo[ib]
                    if first:
                        cnts = {"d": 0, "f": 0, "b": 0}
                    bk = SBK[i % 4]
                    sps = PS[bk]
                    spk = ("ps", bk)
                    for (ty, idx) in its:
                        pt = pb[pbi % 4]
                        pk = ("pb", pbi % 4)
                        pbi += 1
                        if ty == "d":
                            fw.op("dve", lambda e: e.tensor_tensor(out=pt[:, :N], in0=sps[:, :N], in1=Dm[:, h, idx, :N], op=ALU.mult),
                                  reads=[spk, "Dm"], writes=[pk])
                        else:
                            sc = Af[:, h, idx:idx + 1] if ty == "f" else Ab[:, h, idx:idx + 1]
                            if evc % 3 != 0:
                                fw.op("act", lambda e: e.activation(out=pt[:, :N], in_=sps[:, :N], func=AF.Identity, scale=sc),
                                      reads=[spk, "Af", "Ab"], writes=[pk])
                            else:
                                fw.op("dve", lambda e: e.tensor_scalar(out=pt[:, :N], in0=sps[:, :N], scalar1=sc, scalar2=None, op0=ALU.mult),
                                      reads=[spk, "Af", "Ab"], writes=[pk])
                            evc += 1
                        ab = accb[ty]
                        mm(PS[ab][:, :N], v_[:, kk, :], pt[:, :N], cnts[ty] == 0, cnts[ty] == tot[ty] - 1, [("v", p), pk], [("ps", ab)])
                        cnts[ty] += 1
                    if i + LA < len(steps):
                        emitS(i + LA)
                    if last:
                        epilogue(ib)
                fw.dma("sp", fmaj["AinT"][b, h], o_, reads=[("ost", p)], writes=[("Ain", b, h)])
        P.end()

    def phase_na(l):
        P = Phase("na")
        qbd = [P.sb([128, 36, 128], BF16) for _ in range(2)]
        kT = [P.sb([128, T], BF16) for _ in range(2)]
        Ve = [P.sb([128, 18, 128], BF16) for _ in range(2)]
        Vo = [P.sb([128, 15, 128], BF16) for _ in range(2)]
        Bt = [P.sb([128, 8, 512], F32) for _ in range(2)]
        Ssb = [P.sb([128, 768], F32) for _ in range(3)]
        Pb = [P.sb([128, 768], BF16) for _ in range(4)]
        PT = [P.sb([128, 6, 128], BF16) for _ in range(4)]
        mx = [P.sb([128, 1], F32) for _ in range(3)]
        rec = [P.sb([128, 128], F32) for _ in range(2)]
        ost = [P.sb([128, T], BF16) for _ in range(2)]
        for i in range(2):
            fw.op("pool", lambda e: e.memset(qbd[i], 0.0), writes=[("qbd", i)])
        it = 0
        ri = 0
        for b in range(NBC):
            nvv = nv[b].rearrange("(t p) (g d) -> g p t d", p=128, d=128)
            nvo = nv[b, TC + 64:TC + 64 + 15 * 128, :].rearrange("(t p) (g d) -> g p t d", p=128, d=128)
            for g in range(8):
                p = it % 2
                it += 1
                qb, k_, ve, vo, bt, o_ = qbd[p], kT[p], Ve[p], Vo[p], Bt[p], ost[p]
                nq = fmaj["nqT"][b, g]
                fw.dma("sp", qb[0:64, :, 0:64], nq[0:64, :].rearrange("p (r w) -> p r w", w=64), writes=[("qbd", p)])
                fw.dma("sp", qb[64:128, :, 64:128], nq[64:128, :].rearrange("p (r w) -> p r w", w=64), writes=[("qbd", p)])
                fw.dma("sp", k_, fmaj["nkT"][b, g], writes=[("k", p)])
                fw.dma("sp", ve, nvv[g], writes=[("ve", p)])
                fw.dma("sp", vo, nvo[g], writes=[("vo", p)])
                fw.dma("sp", bt, I["na_bias"][l, g], writes=[("bt", p)])
                def rowinfo(rr):
                    if rr < 4:
                        return True, 256, 2, 0, 0
                    r = rr - 4
                    r0 = min(max(r - 4, 0), 24)
                    return False, 768, 6, r0, r - r0

                def stA(rr):
                    ctxrow, W, nkt, r0, dl = rowinfo(rr)
                    a = rr % 2
                    a3 = rr % 3
                    X = PS[2 * a]
                    Y = PS[2 * a + 1]
                    xk = ("ps", 2 * a)
                    yk = ("ps", 2 * a + 1)
                    S_ = Ssb[a3]
                    sk = ("Ssb", a3)
                    mm(Y[:, :256], qb[:, rr, :], k_[:, 0:256], True, True, [("qbd", p), ("k", p)], [yk])
                    if ctxrow:
                        fw.op("act", lambda e: e.copy(out=S_[:, 0:256], in_=Y[:, :256]), reads=[yk], writes=[sk])
                    else:
                        mm(X, qb[:, rr, :], k_[:, TC + r0 * 64:TC + r0 * 64 + 512], True, True, [("qbd", p), ("k", p)], [xk])
                        fw.op("dve", lambda e: e.tensor_tensor(out=S_[:, 0:512], in0=X, in1=bt[:, dl, :], op=ALU.add),
                              reads=[xk, ("bt", p)], writes=[sk])
                        fw.op("act", lambda e: e.copy(out=S_[:, 512:768], in_=Y[:, :256]), reads=[yk], writes=[sk])
                    fw.op("dve", lambda e: e.tensor_reduce(out=mx[a3], in_=S_[:, :W], axis=AX.X, op=ALU.max, negate=True),
                          reads=[sk], writes=[("mx", a3)])
                    fw.op("act", lambda e: e.activation(out=Pb[rr % 4][:, :W], in_=S_[:, :W], func=AF.Exp, bias=mx[a3]),
                          reads=[sk, ("mx", a3)], writes=[("Pb", rr % 4)])

                def stB(rr):
                    ctxrow, W, nkt, r0, dl = rowinfo(rr)
                    a = rr % 2
                    a3 = rr % 3
                    psb = PSB[a]
                    pbk = ("psb", a)
                    for kt in range(nkt):
                        fw.op("pe", lambda e: e.transpose(out=psb[:, kt * 128:(kt + 1) * 128], in_=Pb[rr % 4][:, kt * 128:(kt + 1) * 128], identity=ident_b),
                              reads=[("Pb", rr % 4), "ident_b"], writes=[pbk])
                    ptv = PT[rr % 4].rearrange("p k q -> p (k q)")
                    ptk = ("PT", rr % 4)
                    if rr % 2 == 0:
                        fw.op("act", lambda e: e.copy(out=ptv[:, :nkt * 128], in_=psb[:, :nkt * 128]), reads=[pbk], writes=[ptk])
                    else:
                        fw.op("dve", lambda e: e.tensor_copy(out=ptv[:, :nkt * 128], in_=psb[:, :nkt * 128]), reads=[pbk], writes=[ptk])

                def stC(rr):
                    ctxrow, W, nkt, r0, dl = rowinfo(rr)
                    a = rr % 2
                    a3 = rr % 3
                    pt = PT[rr % 4]
                    ptk = ("PT", rr % 4)
                    if ctxrow:
                        vts = [ve[:, 0, :], ve[:, 1, :]]
                        vks = [("ve", p)]
                    else:
                        if r0 % 2 == 0:
                            vts = [ve[:, 2 + r0 // 2 + kt, :] for kt in range(4)]
                        else:
                            vts = [vo[:, (r0 - 1) // 2 + kt, :] for kt in range(4)]
                        vts += [ve[:, 0, :], ve[:, 1, :]]
                        vks = [("ve", p), ("vo", p)]
                    O = PS[4 + a][:, 0:128]
                    Dn = PS[4 + a][:, 128:256]
                    ok = ("O", a)
                    for kt in range(nkt):
                        mm(O, vts[kt], pt[:, kt, :], kt == 0, kt == nkt - 1, vks + [ptk], [ok])
                    for kt in range(nkt):
                        mm(Dn, ones_b, pt[:, kt, :], kt == 0, kt == nkt - 1, ["ones_b", ptk], [ok])
                    fw.op("dve", lambda e: e.reciprocal(out=rec[a], in_=Dn), reads=[ok], writes=[("rec", a)])
                    fw.op("dve", lambda e: e.tensor_tensor(out=o_[0:64, rr * 64:(rr + 1) * 64], in0=O[0:64, 0:64], in1=rec[a][0:64, 0:64], op=ALU.mult),
                          reads=[ok, ("rec", a)], writes=[("ost", p)])
                    fw.op("dve", lambda e: e.tensor_tensor(out=o_[64:128, rr * 64:(rr + 1) * 64], in0=O[64:128, 64:128], in1=rec[a][64:128, 64:128], op=ALU.mult),
                          reads=[ok, ("rec", a)], writes=[("ost", p)])

                for t in range(36 + 4):
                    if t < 36:
                        stA(t)
                    if 0 <= t - 2 < 36:
                        stB(t - 2)
                    if 0 <= t - 4 < 36:
                        stC(t - 4)
                fw.dma("sp", fmaj["BinT"][b, g], o_, reads=[("ost", p)], writes=[("Bin", b, g)])
        P.end()

    def phase_lru(l):
        P = Phase("lru")
        wg = P.sb([128, 32, 128], BF16)
        fw.dma("pool", wg[:, 0:16, :], I["lru_wa"][l].rearrange("r k c d -> c (r k) d"), writes=["wg"])
        fw.dma("pool", wg[:, 16:32, :], I["lru_wx"][l].rearrange("r k c d -> c (r k) d"), writes=["wg"])
        cw = P.sb([128, 8, 4], F32)
        cb = P.sb([128, 8], F32)
        ba = P.sb([128, 2, 8], F32)
        bx = P.sb([128, 2, 8], F32)
        lam = P.sb([128, 2, 8], F32)
        fw.dma("sp", cw, I["convwT"][l], writes=["cw"])
        fw.dma("sp", cb, I["convbT"][l], writes=["cb"])
        fw.dma("sp", ba, I["lru_baT"][l], writes=["ba"])
        fw.dma("sp", bx, I["lru_bxT"][l], writes=["bx"])
        fw.dma("sp", lam, I["lru_lamT"][l], writes=["lam"])
        fw.op("act", lambda e: e.activation(out=lam, in_=lam, func=AF.Exp, scale=-1.0), reads=["lam"], writes=["lam"])
        fw.op("act", lambda e: e.activation(out=lam, in_=lam, func=AF.Ln, bias=1.0), reads=["lam"], writes=["lam"])
        fw.op("dve", lambda e: e.tensor_scalar(out=lam, in0=lam, scalar1=-8.0, scalar2=None, op0=ALU.mult), reads=["lam"], writes=["lam"])
        xs = [P.sb([128, T], F32) for _ in range(2)]
        gy = [P.sb([128, T], BF16) for _ in range(2)]
        u = P.sb([128, T], F32)
        ub = P.sb([128, T], BF16)
        r_ = P.sb([128, T], F32)
        i_ = P.sb([128, T], F32)
        a_ = P.sb([128, T], F32)
        s_ = P.sb([128, T], F32)
        hf = P.sb([128, T], F32)
        hb = P.sb([128, T], F32)
        oc = [P.sb([128, T], BF16) for _ in range(2)]
        it = 0
        psi = 0
        segs = [(0, TC), (TC, T)]
        for b in range(NBC):
            for k in range(8):
                p = it % 2
                it += 1
                x_ = xs[p]
                fw.dma("sp", x_, lxT[b, k], writes=[("x", p)])
                fw.dma("sp", gy[p], fmaj["lyT"][b, k], writes=[("gy", p)])
                fw.op("dve", lambda e: e.tensor_scalar(out=u, in0=x_, scalar1=cw[:, k, 1:2], scalar2=cb[:, k:k + 1], op0=ALU.mult, op1=ALU.add),
                      reads=[("x", p), "cw", "cb"], writes=["u"])
                for (s0, s1) in segs:
                    fw.op("dve", lambda e: e.scalar_tensor_tensor(out=u[:, s0 + 1:s1], in0=x_[:, s0:s1 - 1], scalar=cw[:, k, 0:1], in1=u[:, s0 + 1:s1],
                                                                  op0=ALU.mult, op1=ALU.add), reads=[("x", p), "u", "cw"], writes=["u"])
                    fw.op("dve", lambda e: e.scalar_tensor_tensor(out=u[:, s0:s1 - 1], in0=x_[:, s0 + 1:s1], scalar=cw[:, k, 2:3], in1=u[:, s0:s1 - 1],
                                                                  op0=ALU.mult, op1=ALU.add), reads=[("x", p), "u", "cw"], writes=["u"])
                    fw.op("dve", lambda e: e.scalar_tensor_tensor(out=u[:, s0:s1 - 2], in0=x_[:, s0 + 2:s1], scalar=cw[:, k, 3:4], in1=u[:, s0:s1 - 2],
                                                                  op0=ALU.mult, op1=ALU.add), reads=[("x", p), "u", "cw"], writes=["u"])
                fw.op("act", lambda e: e.copy(out=ub, in_=u), reads=["u"], writes=["ub"])
                for dr in range(2):
                    for (t0, N) in BLOCKS:
                        ps = PS[psi % 4]
                        pk = ("ps", psi % 4)
                        psi += 1
                        mm(ps[:, :N], wg[:, dr * 8 + k, :], ub[:, t0:t0 + N], True, True, ["wg", "ub"], [pk])
                        fw.op("act", lambda e: e.activation(out=r_[:, t0:t0 + N], in_=ps[:, :N], func=AF.Sigmoid, bias=ba[:, dr, k:k + 1]),
                              reads=[pk, "ba"], writes=["r_"])
                        ps = PS[psi % 4]
                        pk = ("ps", psi % 4)
                        psi += 1
                        mm(ps[:, :N], wg[:, 16 + dr * 8 + k, :], ub[:, t0:t0 + N], True, True, ["wg", "ub"], [pk])
                        fw.op("act", lambda e: e.activation(out=i_[:, t0:t0 + N], in_=ps[:, :N], func=AF.Sigmoid, bias=bx[:, dr, k:k + 1]),
                              reads=[pk, "bx"], writes=["i_"])
                    fw.op("act", lambda e: e.activation(out=a_, in_=r_, func=AF.Exp, scale=lam[:, dr, k:k + 1]), reads=["r_", "lam"], writes=["a_"])
                    fw.op("act", lambda e: e.activation(out=s_, in_=a_, func=AF.Square), reads=["a_"], writes=["s_"])
                    fw.op("act", lambda e: e.activation(out=s_, in_=s_, func=AF.Sqrt, scale=-1.0, bias=1.0), reads=["s_"], writes=["s_"])
                    fw.op("pool", lambda e: e.tensor_tensor(out=i_, in0=i_, in1=u, op=ALU.mult), reads=["i_", "u"], writes=["i_"])
                    fw.op("dve", lambda e: e.tensor_tensor(out=s_, in0=s_, in1=i_, op=ALU.mult), reads=["s_", "i_"], writes=["s_"])
                    if dr == 0:
                        fw.op("dve", lambda e: e.tensor_tensor_scan(out=hf, data0=a_, data1=s_, initial=0.0, op0=ALU.mult, op1=ALU.add),
                              reads=["a_", "s_"], writes=["hf"])
                    else:
                        fw.op("dve", lambda e: e.tensor_tensor_scan(out=hb[:, 0:TC][:, ::-1], data0=a_[:, 0:TC][:, ::-1], data1=s_[:, 0:TC][:, ::-1],
                                                                    initial=0.0, op0=ALU.mult, op1=ALU.add),
                              reads=["a_", "s_"], writes=["hb"])
                        fw.op("dve", lambda e: e.tensor_tensor_scan(out=hb[:, TC:T][:, ::-1], data0=a_[:, TC:T][:, ::-1], data1=s_[:, TC:T][:, ::-1],
                                                                    initial=hb[:, 0:1], op0=ALU.mult, op1=ALU.add),
                              reads=["a_", "s_", "hb"], writes=["hb"])
                fw.op("pool", lambda e: e.tensor_tensor(out=hf, in0=hf, in1=hb, op=ALU.add), reads=["hf", "hb"], writes=["hf"])
                fw.op("dve", lambda e: e.tensor_tensor(out=oc[p], in0=hf, in1=gy[p], op=ALU.mult), reads=["hf", ("gy", p)], writes=[("oc", p)])
                fw.dma("sp", fmaj["CinT"][b, k], oc[p], reads=[("oc", p)], writes=[("Cin", b, k)])
        P.end()

    def phase_merge(l, xsrc):
        P = Phase("mrg")
        wbr = P.sb([128, 3, 8, 1024], BF16)
        wmo = P.sb([128, 8, 1024], BF16)
        for x in range(3):
            fw.dma("pool", wbr[:, x], I["w_branch"][l, x].rearrange("(j p) n -> p j n", p=128), writes=["wbr"])
        fw.dma("pool", wmo, I["w_mix_out"][l].rearrange("(j p) n -> p j n", p=128), writes=["wmo"])
        NN = 256
        ins = [[P.sb([128, 8, NN], BF16) for _ in range(6)] for _ in range(2)]
        xt = [P.sb([128, 8, NN], F32) for _ in range(2)]
        mixed = P.sb([128, 8, NN], BF16)
        xo = [P.sb([128, 8, NN], F32) for _ in range(2)]
        tA = [P.sb([128, NN], F32) for _ in range(2)]
        tB = [P.sb([128, NN], F32) for _ in range(2)]
        tC = [P.sb([128, NN], F32) for _ in range(2)]
        srcs = ["AinT", "BinT", "CinT", "gaT", "gbT", "gcT"]
        it = 0
        for b in range(NBC):
            for (t0, N) in BLOCKS256:
                p = it % 2
                it += 1
                r = 2 if t0 < TC else b
                for si, nm in enumerate(srcs):
                    fw.dma("sp", ins[p][si], fm(fmaj[nm][b])[:, :, t0:t0 + N], writes=[("in", p, si)])
                fw.dma("sp", xt[p], fm(xsrc[b])[:, :, t0:t0 + N], writes=[("xt", p)])
                for c in range(8):
                    q = c % 2
                    pss = [PS[3 * q + x] for x in range(3)]
                    for x in range(3):
                        for k in range(8):
                            mm(pss[x][:, :N], wbr[:, x, k, c * 128:(c + 1) * 128], ins[p][x][:, k, :], k == 0, k == 7,
                               ["wbr", ("in", p, x)], [("ps", 3 * q + x)])
                    tt = [tA[q], tB[q], tC[q]]
                    for x in range(3):
                        fw.op("dve", lambda e: e.tensor_tensor(out=tt[x], in0=pss[x][:, :N], in1=ins[p][3 + x][:, c, :], op=ALU.mult),
                              reads=[("ps", 3 * q + x), ("in", p, 3 + x)], writes=[("tt", q, x)])
                    fw.op("pool", lambda e: e.tensor_tensor(out=tt[0], in0=tt[0], in1=tt[1], op=ALU.add),
                          reads=[("tt", q, 0), ("tt", q, 1)], writes=[("tt", q, 0)])
                    fw.op("pool", lambda e: e.tensor_tensor(out=mixed[:, c, :], in0=tt[0], in1=tt[2], op=ALU.add),
                          reads=[("tt", q, 0), ("tt", q, 2)], writes=["mixed"])
                for c in range(8):
                    ps = PS[6 + c % 2]
                    pk = ("ps", 6 + c % 2)
                    for k in range(8):
                        mm(ps[:, :N], wmo[:, k, c * 128:(c + 1) * 128], mixed[:, k, :], k == 0, k == 7, ["wmo", "mixed"], [pk])
                    fw.op("dve", lambda e: e.scalar_tensor_tensor(out=xo[p][:, c, :], in0=ps[:, :N], scalar=modT[:, 16 + c, r:r + 1], in1=xt[p][:, c, :],
                                                                  op0=ALU.mult, op1=ALU.add),
                          reads=[pk, "modT", ("xt", p)], writes=[("xo", p)])
                fw.dma("sp", fm(xres[b])[:, :, t0:t0 + N], xo[p], reads=[("xo", p)], writes=[("xres", b, t0)])
        P.end()

    def phase_moepre(l):
        P = Phase("mpre")
        xt = [P.sb([128, 8, 512], F32) for _ in range(2)]
        sq = P.sb([128, 8, 512], F32)
        rstd = P.sb([128, 512], F32)
        tmp = [P.sb([128, 512], F32) for _ in range(2)]
        hf = P.sb([128, 8, 512], F32)
        hb = [P.sb([128, 8, 512], BF16) for _ in range(2)]
        wr = P.sb([128, 8, NE], F32)
        brt = P.sb([128, NE], F32)
        fw.dma("sp", wr, I["w_router"][l].rearrange("(j p) n -> p j n", p=128), writes=["wr"])
        fw.dma("sp", brt, I["b_router"][l].partition_broadcast(128), writes=["brt"])
        lgs = P.sb([128, NE], F32)
        top8 = P.sb([128, 8], F32)
        msk = P.sb([128, NE], F32)
        nmx = P.sb([128, 1], F32)
        ex = P.sb([128, NE], F32)
        ssum = P.sb([128, 1], F32)
        gts = P.sb([128, NE], F32)
        gTs = [P.sb([NE, 512], F32) for _ in range(2)]
        i = 0
        for b in range(NBC):
            for (t0, N) in BLOCKS:
                x_ = xt[i % 2]
                xk = ("xt", i % 2)
                hb_ = hb[i % 2]
                hbk = ("hb", i % 2)
                gt_ = gTs[i % 2]
                gtk = ("gTs", i % 2)
                i += 1
                fw.dma("sp", x_[:, :, :N], fm(xres[b])[:, :, t0:t0 + N], writes=[xk])
                r = 2 if t0 < TC else b

                def out_fn(j, x_=x_, xk=xk, N=N, r=r):
                    tm = tmp[j % 2]
                    fw.op("dve", lambda e: e.scalar_tensor_tensor(out=tm[:, :N], in0=x_[:, j, :N], scalar=mul2[:, j, r:r + 1],
                                                                  in1=rstd[:, :N], op0=ALU.mult, op1=ALU.mult),
                          reads=[xk, "rstd", "mul2"], writes=[("tmp", j % 2)])
                    fw.op("act", lambda e: e.activation(out=hf[:, j, :N], in_=tm[:, :N], func=AF.Identity, bias=modT[:, 24 + j, r:r + 1]),
                          reads=[("tmp", j % 2), "modT"], writes=["hf"])
                emit_norm(x_, N, xk, sq, rstd, mul2, r, out_fn)
                fw.op("pool", lambda e: e.tensor_copy(out=hb_[:, :, :N], in_=hf[:, :, :N]), reads=["hf"], writes=[hbk])
                fw.dma("sp", fm(fmaj["hT2"][b])[:, :, t0:t0 + N], hb_[:, :, :N], reads=[hbk], writes=[("hT2", b, t0)])
                for tt in range(N // 128):
                    lp = PS[1][:, tt * 32:(tt + 1) * 32]
                    for j in range(8):
                        mm(lp, hf[:, j, tt * 128:(tt + 1) * 128], wr[:, j, :], j == 0, j == 7, ["hf", "wr"], [("ps", 1)])
                    fw.op("dve", lambda e: e.tensor_tensor(out=lgs, in0=lp, in1=brt, op=ALU.add), reads=[("ps", 1), "brt"], writes=["lgs"])
                    fw.op("dve", lambda e: e.max(out=top8, in_=lgs), reads=["lgs"], writes=["top8"])
                    fw.op("dve", lambda e: e.tensor_scalar(out=msk, in0=lgs, scalar1=top8[:, 3:4], scalar2=None, op0=ALU.is_ge),
                          reads=["lgs", "top8"], writes=["msk"])
                    fw.op("dve", lambda e: e.tensor_scalar(out=nmx, in0=top8[:, 0:1], scalar1=-1.0, scalar2=None, op0=ALU.mult),
                          reads=["top8"], writes=["nmx"])
                    fw.op("act", lambda e: e.activation(out=ex, in_=lgs, func=AF.Exp, bias=nmx), reads=["lgs", "nmx"], writes=["ex"])
                    fw.op("dve", lambda e: e.tensor_tensor(out=ex, in0=ex, in1=msk, op=ALU.mult), reads=["ex", "msk"], writes=["ex"])
                    fw.op("dve", lambda e: e.tensor_reduce(out=ssum, in_=ex, axis=AX.X, op=ALU.add), reads=["ex"], writes=["ssum"])
                    fw.op("dve", lambda e: e.reciprocal(out=ssum, in_=ssum), reads=["ssum"], writes=["ssum"])
                    fw.op("dve", lambda e: e.tensor_scalar(out=gts, in0=ex, scalar1=ssum, scalar2=None, op0=ALU.mult),
                          reads=["ex", "ssum"], writes=["gts"])
                    fw.op("pe", lambda e: e.transpose(out=PS[2][0:NE, tt * 128:(tt + 1) * 128], in_=gts, identity=ident_f),
                          reads=["gts", "ident_f"], writes=[("ps", 2)])
                fw.op("act", lambda e: e.copy(out=gt_[:, :N], in_=PS[2][0:NE, :N]), reads=[("ps", 2)], writes=[gtk])
                fw.dma("sp", gTd[:, b * T + t0:b * T + t0 + N], gt_[:, :N], reads=[gtk], writes=[("gTd", b, t0)])
        P.end()

    def phase_moe(l):
        P = Phase("moe")
        wup = [P.sb([128, 8, 2048], BF16) for _ in range(2)]
        wdn = [P.sb([128, 8, 1024], BF16) for _ in range(2)]
        hT2 = P.sb([128, 8, GRP], BF16)
        yacc = P.sb([128, 8, GRP], F32)
        gTs = P.sb([NE, GRP], F32)
        sel = P.sb([NE, NE, 128], F32)
        bup = P.sb([128, NE, 2, 8], F32)
        bdn = P.sb([NE, D], F32)
        Ge = [P.sb([128, MB], F32) for _ in range(2)]
        actT = [P.sb([128, 8, MB], BF16) for _ in range(2)]
        tg = [P.sb([128, MB], F32) for _ in range(2)]
        tsg = [P.sb([128, MB], F32) for _ in range(2)]
        tl = [P.sb([128, MB], F32) for _ in range(2)]
        fw.dma("sp", bup, I["bupT"][l], writes=["bup"])
        fw.dma("sp", bdn, I["b_down"][l], writes=["bdn"])
        fw.op("dve", lambda e: e.tensor_scalar(out=bup[:, :, 1, :], in0=bup[:, :, 1, :], scalar1=1.0, scalar2=None, op0=ALU.add),
              reads=["bup"], writes=["bup"])
        fw.op("pool", lambda e: e.memset(sel, 0.0), writes=["sel"])
        fw.op("pool", lambda e: e.affine_select(out=sel, in_=sel, pattern=[[-1, NE], [0, 128]],
                                                compare_op=ALU.not_equal, fill=1.0, base=0, channel_multiplier=1),
              reads=["sel"], writes=["sel"])
        ngrp = NBC * T // GRP
        nblk = GRP // MB

        def loadw(i):
            e_ = i % NE
            fw.dma("pool", wup[i % 2], I["w_up"][l, e_].rearrange("(j p) n -> p j n", p=128), writes=[("wup", i % 2)])

        def loadwd(i):
            e_ = i % NE
            fw.dma("pool", wdn[i % 2], I["w_down"][l, e_].rearrange("(j p) n -> p j n", p=128), writes=[("wdn", i % 2)])
        loadw(0)
        loadwd(0)
        ci = 0
        for g in range(ngrp):
            b = g // 2
            g0 = (g % 2) * GRP
            fw.dma("sp", hT2, fm(fmaj["hT2"][b])[:, :, g0:g0 + GRP], writes=["hT2"])
            fw.dma("sp", gTs, gTd[:, b * T + g0:b * T + g0 + GRP], writes=["gTs"])
            for blk in range(nblk):
                for co in range(8):
                    ps = PS[4 + co % 2]
                    pk = ("ps", 4 + co % 2)
                    mm(ps[:, :MB], bdn[:, co * 128:(co + 1) * 128], gTs[:, blk * MB:(blk + 1) * MB], True, True, ["bdn", "gTs"], [pk])
                    fw.op("act", lambda e: e.copy(out=yacc[:, co, blk * MB:(blk + 1) * MB], in_=ps[:, :MB]), reads=[pk], writes=[("yacc", blk)])
            tasks = [(ex, blk) for ex in range(NE) for blk in range(nblk)]

            def up(ti):
                nonlocal ci
                ex, blk = tasks[ti]
                wix = wbase + ex
                wu = wup[wix % 2]
                wuk = ("wup", wix % 2)
                if blk == 0 and wix + 1 < ngrp * NE:
                    loadw(wix + 1)
                tsl = slice(blk * MB, (blk + 1) * MB)
                ge = Ge[ti % 2]
                gek = ("Ge", ti % 2)
                at = actT[ti % 2]
                atk = ("actT", ti % 2)
                mm(PS[6][:, :MB], sel[:, ex, :], gTs[:, tsl], True, True, ["sel", "gTs"], [("ps", 6)])
                fw.op("act", lambda e: e.copy(out=ge, in_=PS[6][:, :MB]), reads=[("ps", 6)], writes=[gek])
                for c in range(8):
                    q = ci % 2
                    ci += 1
                    pg = PS[2 * q]
                    pl = PS[2 * q + 1]
                    pgk = ("ps", 2 * q)
                    plk = ("ps", 2 * q + 1)
                    for k in range(8):
                        mm(pg[:, :MB], wu[:, k, c * 256:(c + 1) * 256:2], hT2[:, k, tsl], k == 0, k == 7, [wuk, "hT2"], [pgk])
                    for k in range(8):
                        mm(pl[:, :MB], wu[:, k, c * 256 + 1:(c + 1) * 256:2], hT2[:, k, tsl], k == 0, k == 7, [wuk, "hT2"], [plk])
                    g_ = tg[q]
                    s_ = tsg[q]
                    l_ = tl[q]
                    fw.op("dve", lambda e: e.tensor_scalar(out=g_, in0=pg[:, :MB], scalar1=bup[:, ex, 0, c:c + 1], scalar2=7.0, op0=ALU.add, op1=ALU.min),
                          reads=[pgk, "bup"], writes=[("tg", q)])
                    fw.op("act", lambda e: e.activation(out=s_, in_=g_, func=AF.Sigmoid, scale=1.702), reads=[("tg", q)], writes=[("tsg", q)])
                    fw.op("dve", lambda e: e.tensor_scalar(out=l_, in0=pl[:, :MB], scalar1=bup[:, ex, 1, c:c + 1], scalar2=8.0, op0=ALU.add, op1=ALU.min),
                          reads=[plk, "bup"], writes=[("tl", q)])
                    fw.op("dve", lambda e: e.scalar_tensor_tensor(out=l_, in0=l_, scalar=-6.0, in1=ge, op0=ALU.max, op1=ALU.mult),
                          reads=[("tl", q), gek], writes=[("tl", q)])
                    fw.op("pool", lambda e: e.tensor_tensor(out=g_, in0=g_, in1=s_, op=ALU.mult), reads=[("tg", q), ("tsg", q)], writes=[("tg", q)])
                    fw.op("pool", lambda e: e.tensor_tensor(out=at[:, c, :], in0=g_, in1=l_, op=ALU.mult), reads=[("tg", q), ("tl", q)], writes=[atk])

            def down(ti):
                ex, blk = tasks[ti]
                wix = wbase + ex
                wd = wdn[wix % 2]
                wdk = ("wdn", wix % 2)
                if blk == 0 and wix + 1 < ngrp * NE:
                    loadwd(wix + 1)
                tsl = slice(blk * MB, (blk + 1) * MB)
                at = actT[ti % 2]
                atk = ("actT", ti % 2)
                for co in range(8):
                    ps = PS[4 + co % 2]
                    pk = ("ps", 4 + co % 2)
                    for c in range(8):
                        mm(ps[:, :MB], wd[:, c, co * 128:(co + 1) * 128], at[:, c, :], c == 0, c == 7, [wdk, atk], [pk])
                    fw.op("dve", lambda e: e.tensor_tensor(out=yacc[:, co, tsl], in0=ps[:, :MB], in1=yacc[:, co, tsl], op=ALU.add),
                          reads=[pk, ("yacc", blk)], writes=[("yacc", blk)])

            wbase = g * NE
            up(0)
            for ti in range(len(tasks)):
                if ti + 1 < len(tasks):
                    up(ti + 1)
                down(ti)
            fw.dma("sp", fm(yT[b])[:, :, g0:g0 + GRP], yacc, reads=[("yacc", blk) for blk in range(nblk)], writes=[("yT", g)])
        P.end()

    def phase_moepost(l, last):
        P = Phase("mpost")
        xt = [P.sb([128, 8, 512], F32) for _ in range(2)]
        yt = [P.sb([128, 8, 512], F32) for _ in range(2)]
        sq = P.sb([128, 8, 512], F32)
        rstd = P.sb([128, 512], F32)
        oo = [P.sb([128, 8, 512], F32) for _ in range(2)]
        i = 0
        for b in range(NBC):
            for (t0, N) in BLOCKS:
                if last and t0 < TC:
                    continue
                p = i % 2
                i += 1
                r = 2 if t0 < TC else b
                fw.dma("sp", xt[p][:, :, :N], fm(xres[b])[:, :, t0:t0 + N], writes=[("xt", p)])
                fw.dma("sp", yt[p][:, :, :N], fm(yT[b])[:, :, t0:t0 + N], writes=[("yt", p)])
                for c in range(8):
                    fw.op("dve", lambda e: e.scalar_tensor_tensor(out=xt[p][:, c, :N], in0=yt[p][:, c, :N], scalar=modT[:, 40 + c, r:r + 1],
                                                                  in1=xt[p][:, c, :N], op0=ALU.mult, op1=ALU.add),
                          reads=[("xt", p), ("yt", p), "modT"], writes=[("xt", p)])
                if not last:
                    fw.dma("sp", fm(xres[b])[:, :, t0:t0 + N], xt[p][:, :, :N], reads=[("xt", p)], writes=[("xres", b, t0)])
                else:
                    def out_fn(j, p=p, N=N):
                        fw.op("dve", lambda e: e.scalar_tensor_tensor(out=oo[p][:, j, :N], in0=xt[p][:, j, :N], scalar=fng[:, j:j + 1],
                                                                      in1=rstd[:, :N], op0=ALU.mult, op1=ALU.mult),
                              reads=[("xt", p), "rstd", "fng"], writes=[("oo", p)])
                    emit_norm(xt[p], N, ("xt", p), sq, rstd, None, r, out_fn)
                    fw.dma("sp", fm(outT[b])[:, :, t0 - TC:t0 - TC + N], oo[p][:, :, :N], reads=[("oo", p)], writes=[("out", b, t0)])
        P.end()


    IOA = bass.IndirectOffsetOnAxis

    def phase_moepre_sparse(l):
        P = Phase("spre")
        xt = [P.sb([128, 8, 512], F32) for _ in range(2)]
        sq = P.sb([128, 8, 512], F32)
        rstd = P.sb([128, 512], F32)
        tmp = [P.sb([128, 512], F32) for _ in range(2)]
        hf = P.sb([128, 8, 512], F32)
        hb = P.sb([128, 8, 512], BF16)
        htk = [P.sb([128, 1024], BF16) for _ in range(2)]
        wr = P.sb([128, 8, NE], F32)
        brt = P.sb([128, NE], F32)
        fw.dma("sp", wr, I["w_router"][l].rearrange("(j p) n -> p j n", p=128), writes=["wr"])
        fw.dma("sp", brt, I["b_router"][l].partition_broadcast(128), writes=["brt"])
        G_all = P.sb([128, 36, NE], F32)
        M_all = P.sb([128, 36, NE], F32)
        lgs = P.sb([128, NE], F32)
        top8 = P.sb([128, 8], F32)
        nmx = P.sb([128, 1], F32)
        ex = P.sb([128, NE], F32)
        ssum = P.sb([128, 1], F32)
        zt = P.sb([128, 1024], BF16)
        zf = P.sb([128, NE], F32)
        fill = P.sb([128, NSLOT * 2 // 128], I32)
        Lm = P.sb([128, 128], F32)
        pj = P.sb([128, 8], F32)
        tokid = P.sb([128, 36, 2], I32)
        fw.dma("sp", Lm, I["Lmat"], writes=["Lm"])
        fw.dma("sp", pj, I["pj"], writes=["pj"])
        fw.dma("sp", tokid, I["tokid"], writes=["tokid"])
        fw.op("pool", lambda e: e.memset(zt, 0.0), writes=["zt"])
        fw.op("pool", lambda e: e.memset(zf, 0.0), writes=["zf"])
        fw.op("pool", lambda e: e.memset(fill, NTOK), writes=["fill"])
        fw.dma("sp", h2tok[NTOK:NTOK + 128, :], zt, reads=["zt"], writes=["h2z"])
        fw.dma("sp", gtab[NTOK:NTOK + 128, :], zf, reads=["zf"], writes=["gtz"])
        fw.dma("sp", slot_tok.rearrange("(p a) c -> p (a c)", p=128), fill, reads=["fill"], writes=["stfill"])
        i = 0
        for b in range(NBC):
            for (t0, N) in BLOCKS:
                x_ = xt[i % 2]
                xk = ("xt", i % 2)
                i += 1
                fw.dma("sp", x_[:, :, :N], fm(xres[b])[:, :, t0:t0 + N], writes=[xk])
                r = 2 if t0 < TC else b

                def out_fn(j, x_=x_, xk=xk, N=N, r=r):
                    tm = tmp[j % 2]
                    fw.op("dve", lambda e: e.scalar_tensor_tensor(out=tm[:, :N], in0=x_[:, j, :N], scalar=mul2[:, j, r:r + 1],
                                                                  in1=rstd[:, :N], op0=ALU.mult, op1=ALU.mult),
                          reads=[xk, "rstd", "mul2"], writes=[("tmp", j % 2)])
                    fw.op("act", lambda e: e.activation(out=hf[:, j, :N], in_=tm[:, :N], func=AF.Identity, bias=modT[:, 24 + j, r:r + 1]),
                          reads=[("tmp", j % 2), "modT"], writes=["hf"])
                emit_norm(x_, N, xk, sq, rstd, mul2, r, out_fn)
                fw.op("pool", lambda e: e.tensor_copy(out=hb[:, :, :N], in_=hf[:, :, :N]), reads=["hf"], writes=["hb"])
                for tt in range(N // 128):
                    gi = b * 18 + t0 // 128 + tt
                    a = gi % 2
                    for j in range(8):
                        fw.op("pe", lambda e: e.transpose(out=PSB[a][:, j * 128:(j + 1) * 128], in_=hb[:, j, tt * 128:(tt + 1) * 128], identity=ident_b),
                              reads=["hb", "ident_b"], writes=[("psb", a)])
                    fw.op("act", lambda e: e.copy(out=htk[a], in_=PSB[a]), reads=[("psb", a)], writes=[("htk", a)])
                    fw.dma("sp", h2tok[gi * 128:(gi + 1) * 128, :], htk[a], reads=[("htk", a)], writes=[("h2tok", gi)])
                    lp = PS[1][:, (tt % 4) * 32:(tt % 4) * 32 + 32]
                    for j in range(8):
                        mm(lp, hf[:, j, tt * 128:(tt + 1) * 128], wr[:, j, :], j == 0, j == 7, ["hf", "wr"], [("ps", 1)])
                    fw.op("dve", lambda e: e.tensor_tensor(out=lgs, in0=lp, in1=brt, op=ALU.add), reads=[("ps", 1), "brt"], writes=["lgs"])
                    fw.op("dve", lambda e: e.max(out=top8, in_=lgs), reads=["lgs"], writes=["top8"])
                    fw.op("dve", lambda e: e.tensor_scalar(out=M_all[:, gi, :], in0=lgs, scalar1=top8[:, 3:4], scalar2=None, op0=ALU.is_ge),
                          reads=["lgs", "top8"], writes=[("M", gi)])
                    fw.op("dve", lambda e: e.tensor_scalar(out=nmx, in0=top8[:, 0:1], scalar1=-1.0, scalar2=None, op0=ALU.mult),
                          reads=["top8"], writes=["nmx"])
                    fw.op("act", lambda e: e.activation(out=ex, in_=lgs, func=AF.Exp, bias=nmx), reads=["lgs", "nmx"], writes=["ex"])
                    fw.op("dve", lambda e: e.tensor_tensor(out=ex, in0=ex, in1=M_all[:, gi, :], op=ALU.mult), reads=["ex", ("M", gi)], writes=["ex"])
                    fw.op("dve", lambda e: e.tensor_reduce(out=ssum, in_=ex, axis=AX.X, op=ALU.add), reads=["ex"], writes=["ssum"])
                    fw.op("dve", lambda e: e.reciprocal(out=ssum, in_=ssum), reads=["ssum"], writes=["ssum"])
                    fw.op("dve", lambda e: e.tensor_scalar(out=G_all[:, gi, :], in0=ex, scalar1=ssum, scalar2=None, op0=ALU.mult),
                          reads=["ex", "ssum"], writes=[("G", gi)])
                    fw.dma("sp", gtab[gi * 128:(gi + 1) * 128, :], G_all[:, gi, :], reads=[("G", gi)], writes=[("gtab", gi)])
        cnt = P.sb([128, NE], F32)
        ntl = P.sb([128, NE], F32)
        cume = P.sb([128, NE], F32)
        cb = P.sb([128, NE], F32)
        sf = P.sb([128, NE], F32)
        t8 = P.sb([128, 8], F32)
        cm = P.sb([128, NE], F32)
        wf = P.sb([128, NTILE, 8], F32)
        bf_ = P.sb([128, NTILE], F32)
        for gi in range(36):
            mm(PS[3][:, 0:NE], ones_f, M_all[:, gi, :], gi == 0, gi == 35, ["ones_f", ("M", gi)], [("ps", 3)])
        fw.op("dve", lambda e: e.tensor_copy(out=cnt, in_=PS[3][:, 0:NE]), reads=[("ps", 3)], writes=["cnt"])
        fw.op("dve", lambda e: e.tensor_scalar(out=ntl, in0=cnt, scalar1=0.0, scalar2=None, op0=ALU.is_gt), reads=["cnt"], writes=["ntl"])
        for m in range(1, NTOK // MT + 1):
            fw.op("dve", lambda e: e.scalar_tensor_tensor(out=ntl, in0=cnt, scalar=float(m * MT), in1=ntl, op0=ALU.is_gt, op1=ALU.add),
                  reads=["cnt", "ntl"], writes=["ntl"])
        fw.op("dve", lambda e: e.tensor_tensor_scan(out=cume, data0=ones_f[:, 0:NE], data1=ntl, initial=0.0, op0=ALU.mult, op1=ALU.add),
              reads=["ntl", "ones_f"], writes=["cume"])
        fw.op("dve", lambda e: e.tensor_tensor(out=cb, in0=cume, in1=ntl, op=ALU.subtract), reads=["cume", "ntl"], writes=["cb"])
        fw.op("dve", lambda e: e.tensor_scalar(out=cb, in0=cb, scalar1=float(MT), scalar2=None, op0=ALU.mult), reads=["cb"], writes=["cb"])
        for gi in range(36):
            mm(PS[4][:, 0:NE], Lm, M_all[:, gi, :], True, True, ["Lm", ("M", gi)], [("ps", 4)])
            mm(PS[5][:, 0:NE], ones_f, M_all[:, gi, :], True, True, ["ones_f", ("M", gi)], [("ps", 5)])
            fw.op("dve", lambda e: e.tensor_tensor(out=sf, in0=PS[4][:, 0:NE], in1=cb, op=ALU.add), reads=[("ps", 4), "cb"], writes=["sf"])
            fw.op("dve", lambda e: e.scalar_tensor_tensor(out=sf, in0=sf, scalar=1.0, in1=M_all[:, gi, :], op0=ALU.add, op1=ALU.mult),
                  reads=["sf", ("M", gi)], writes=["sf"])
            fw.op("dve", lambda e: e.tensor_scalar(out=sf, in0=sf, scalar1=-1.0, scalar2=None, op0=ALU.add), reads=["sf"], writes=["sf"])
            fw.op("dve", lambda e: e.max(out=t8, in_=sf), reads=["sf"], writes=["t8"])
            fw.op("dve", lambda e: e.tensor_copy(out=S4_all[:, gi, :], in_=t8[:, 0:4]), reads=["t8"], writes=[("S4", gi)])
            fw.op("dve", lambda e: e.tensor_tensor(out=cb, in0=cb, in1=PS[5][:, 0:NE], op=ALU.add), reads=["cb", ("ps", 5)], writes=["cb"])
            for k in range(4):
                fw.idma(slot_tok, IOA(ap=S4_all[:, gi, k:k + 1], axis=0), tokid[:, gi, :], None,
                        reads=[("S4", gi), "tokid", "stfill"], writes=[("st", gi, k)])
        for t in range(NTILE):
            fw.op("dve", lambda e: e.tensor_scalar(out=cm, in0=cume, scalar1=float(t), scalar2=None, op0=ALU.is_le), reads=["cume"], writes=["cm"])
            fw.op("dve", lambda e: e.tensor_reduce(out=ETf[:, t:t + 1], in_=cm, axis=AX.X, op=ALU.add), reads=["cm"], writes=["ETf"])
        fw.op("dve", lambda e: e.tensor_scalar(out=ETf, in0=ETf, scalar1=float(NE - 1), scalar2=None, op0=ALU.min), reads=["ETf"], writes=["ETf"])
        fw.op("dve", lambda e: e.tensor_scalar(out=bf_, in0=ETf, scalar1=float(l * NE), scalar2=None, op0=ALU.add), reads=["ETf"], writes=["bf_"])
        fw.op("dve", lambda e: e.tensor_copy(out=eidx, in_=bf_), reads=["bf_"], writes=["eidx"])
        fw.op("dve", lambda e: e.tensor_scalar(out=bf_, in0=ETf, scalar1=128.0, scalar2=pj[:, 0:1], op0=ALU.mult, op1=ALU.add),
              reads=["ETf", "pj"], writes=["bf_"])
        fw.op("dve", lambda e: e.tensor_copy(out=pidx, in_=bf_), reads=["bf_"], writes=["pidx"])
        fw.op("dve", lambda e: e.tensor_scalar(out=bf_, in0=bf_, scalar1=float(l * NE * 128), scalar2=None, op0=ALU.add), reads=["bf_"], writes=["bf_"])
        fw.op("dve", lambda e: e.tensor_copy(out=bidx, in_=bf_), reads=["bf_"], writes=["bidx"])
        for j in range(8):
            fw.op("dve", lambda e: e.tensor_scalar(out=wf[:, :, j], in0=ETf, scalar1=1024.0, scalar2=pj[:, j:j + 1], op0=ALU.mult, op1=ALU.add),
                  reads=["ETf", "pj"], writes=["wf"])
        fw.op("dve", lambda e: e.tensor_scalar(out=wf, in0=wf, scalar1=float(l * NE * 1024), scalar2=None, op0=ALU.add), reads=["wf"], writes=["wf"])
        fw.op("dve", lambda e: e.tensor_copy(out=widx, in_=wf), reads=["wf"], writes=["widx"])
        P.end()

    def phase_wcast(l):
        P = Phase("wcast")
        bu = [P.sb([128, 8, 2048], BF16) for _ in range(3)]
        bd = [P.sb([128, 8, 1024], BF16) for _ in range(3)]
        for ex in range(NE):
            s_ = ex % 3
            fw.dma("pool", bu[s_], I["w_up"][l, ex].rearrange("(j p) n -> p j n", p=128), writes=[("bu", s_)])
            fw.dma("sp", wupb[ex * 128:(ex + 1) * 128, :], bu[s_].rearrange("p j n -> p (j n)"), reads=[("bu", s_)], writes=[("wupb", ex)])
            fw.dma("pool", bd[s_], I["w_down"][l, ex].rearrange("(j p) n -> p j n", p=128), writes=[("bd", s_)])
            fw.dma("sp", wdnb[ex * 128:(ex + 1) * 128, :], bd[s_].rearrange("p j n -> p (j n)"), reads=[("bd", s_)], writes=[("wdnb", ex)])
        P.end()

    def phase_moe_sparse(l):
        P = Phase("smoe")
        wup = [P.sb([128, 8, 2048], BF16) for _ in range(2)]
        wdn = [P.sb([128, 8, 1024], BF16) for _ in range(2)]
        bupg = [P.sb([128, 16], F32) for _ in range(2)]
        bdng = [P.sb([128, 1024], F32) for _ in range(2)]
        stok = [P.sb([128, 4, 2], I32) for _ in range(2)]
        htk = [[P.sb([128, 1024], BF16) for _ in range(4)] for _ in range(2)]
        grow = [P.sb([128, 4, NE], F32) for _ in range(2)]
        hsT = [P.sb([128, 8, MT], BF16) for _ in range(2)]
        at = [P.sb([128, 8, MT], BF16) for _ in range(2)]
        oh = P.sb([128, NE], F32)
        gtmp = P.sb([128, 4, NE], F32)
        gsl = [P.sb([128, 4], F32) for _ in range(2)]
        tg = [P.sb([128, MT], F32) for _ in range(2)]
        tsg = [P.sb([128, MT], F32) for _ in range(2)]
        tl = [P.sb([128, MT], F32) for _ in range(2)]
        ytmp = [P.sb([128, 512], F32) for _ in range(2)]
        yrow = [P.sb([128, 1024], F32) for _ in range(2)]
        iota = P.sb([128, NE], F32)
        fw.dma("sp", iota, I["iota32"], writes=["iota"])
        wv = I["w_up"].rearrange("l e r n -> (l e r) n")
        wdv = I["w_down"].rearrange("l e r n -> (l e r) n")
        bupv = I["bup2"].rearrange("l e p n -> (l e p) n")
        bdv = I["b_down"].rearrange("l e n -> (l e) n")

        def gather(t):
            s_ = t % 2
            fw.dma("sp", stok[s_], slot_tok[t * MT:(t + 1) * MT, :].rearrange("(a p) c -> p a c", p=128), writes=[("stok", s_)])
            for a in range(4):
                fw.idma(htk[s_][a], None, h2tok, IOA(ap=stok[s_][:, a, 0:1], axis=0), reads=[("stok", s_)], writes=[("htk", s_, a)])
            for a in range(4):
                fw.idma(grow[s_][:, a, :], None, gtab, IOA(ap=stok[s_][:, a, 0:1], axis=0), reads=[("stok", s_)], writes=[("grow", s_)])
            fw.idma(bupg[s_], None, bupv, IOA(ap=bidx[:, t:t + 1], axis=0), writes=[("bupg", s_)])
            fw.idma(bdng[s_], None, bdv, IOA(ap=eidx[:, t:t + 1], axis=0), writes=[("bdng", s_)])
            fw.idma(wup[s_].rearrange("p j n -> p (j n)"), None, wupb, IOA(ap=pidx[:, t:t + 1], axis=0), writes=[("wup", s_)])
            fw.idma(wdn[s_].rearrange("p j n -> p (j n)"), None, wdnb, IOA(ap=pidx[:, t:t + 1], axis=0), writes=[("wdn", s_)])

        gather(0)
        ci = 0
        yi = 0
        ev = 0
        for t in range(NTILE):
            s_ = t % 2
            if t + 1 < NTILE:
                gather(t + 1)
            fw.op("dve", lambda e: e.tensor_scalar(out=oh, in0=iota, scalar1=ETf[:, t:t + 1], scalar2=None, op0=ALU.is_equal),
                  reads=["iota"], writes=["oh"])
            for a in range(4):
                fw.op("dve", lambda e: e.tensor_tensor(out=gtmp[:, a, :], in0=grow[s_][:, a, :], in1=oh, op=ALU.mult),
                      reads=[("grow", s_), "oh"], writes=["gtmp"])
            fw.op("dve", lambda e: e.tensor_reduce(out=gsl[s_], in_=gtmp, axis=AX.X, op=ALU.add), reads=["gtmp"], writes=[("gsl", s_)])
            fw.op("dve", lambda e: e.tensor_scalar(out=bupg[s_][:, 8:16], in0=bupg[s_][:, 8:16], scalar1=1.0, scalar2=None, op0=ALU.add),
                  reads=[("bupg", s_)], writes=[("bupg", s_)])
            for jp in range(4):
                bank = PSB[jp % 2]
                bk = ("psb", jp % 2)
                for jj in range(2):
                    j = 2 * jp + jj
                    for a in range(4):
                        fw.op("pe", lambda e: e.transpose(out=bank[:, jj * 512 + a * 128:jj * 512 + (a + 1) * 128],
                                                          in_=htk[s_][a][:, j * 128:(j + 1) * 128], identity=ident_b),
                              reads=[("htk", s_, a), "ident_b"], writes=[bk])
                dst = hsT[s_][:, 2 * jp:2 * jp + 2, :].rearrange("p j n -> p (j n)")
                if jp % 2 == 0:
                    fw.op("act", lambda e: e.copy(out=dst, in_=bank), reads=[bk], writes=[("hsT", s_)])
                else:
                    fw.op("dve", lambda e: e.tensor_copy(out=dst, in_=bank), reads=[bk], writes=[("hsT", s_)])
            wu = wup[s_]
            wuk = ("wup", s_)
            for c in range(8):
                q = ci % 2
                ci += 1
                pg = PS[2 * q]
                pl = PS[2 * q + 1]
                pgk = ("ps", 2 * q)
                plk = ("ps", 2 * q + 1)
                for k in range(8):
                    mm(pg, wu[:, k, c * 256:(c + 1) * 256:2], hsT[s_][:, k, :], k == 0, k == 7, [wuk, ("hsT", s_)], [pgk])
                for k in range(8):
                    mm(pl, wu[:, k, c * 256 + 1:(c + 1) * 256:2], hsT[s_][:, k, :], k == 0, k == 7, [wuk, ("hsT", s_)], [plk])
                g_ = tg[q]
                sg_ = tsg[q]
                l_ = tl[q]
                fw.op("dve", lambda e: e.tensor_scalar(out=g_, in0=pg, scalar1=bupg[s_][:, c:c + 1], scalar2=7.0, op0=ALU.add, op1=ALU.min),
                      reads=[pgk, ("bupg", s_)], writes=[("tg", q)])
                fw.op("act", lambda e: e.activation(out=sg_, in_=g_, func=AF.Sigmoid, scale=1.702), reads=[("tg", q)], writes=[("tsg", q)])
                fw.op("act", lambda e: e.activation(out=l_, in_=pl, func=AF.Identity, bias=bupg[s_][:, 8 + c:9 + c]),
                      reads=[plk, ("bupg", s_)], writes=[("tl", q)])
                fw.op("dve", lambda e: e.tensor_scalar(out=l_, in0=l_, scalar1=8.0, scalar2=-6.0, op0=ALU.min, op1=ALU.max),
                      reads=[("tl", q)], writes=[("tl", q)])
                fw.op("dve", lambda e: e.tensor_tensor(out=g_, in0=g_, in1=sg_, op=ALU.mult), reads=[("tg", q), ("tsg", q)], writes=[("tg", q)])
                fw.op("dve", lambda e: e.tensor_tensor(out=at[s_][:, c, :], in0=g_, in1=l_, op=ALU.mult), reads=[("tg", q), ("tl", q)], writes=[("at", s_)])
            wd = wdn[s_]
            wdk = ("wdn", s_)
            for a in range(4):
                yr = yrow[yi % 2]
                yk = ("yrow", yi % 2)
                yi += 1
                for half in range(2):
                    ps = PS[4 + half]
                    pk = ("ps", 4 + half)
                    for c in range(8):
                        mm(ps, at[s_][:, c, a * 128:(a + 1) * 128], wd[:, c, half * 512:(half + 1) * 512], c == 0, c == 7, [("at", s_), wdk], [pk])
                    yt_ = ytmp[half]
                    fw.op("dve", lambda e: e.tensor_tensor(out=yt_, in0=ps, in1=bdng[s_][:, half * 512:(half + 1) * 512], op=ALU.add),
                          reads=[pk, ("bdng", s_)], writes=[("ytmp", half)])
                    fw.op("act", lambda e: e.activation(out=yr[:, half * 512:(half + 1) * 512], in_=yt_, func=AF.Identity, scale=gsl[s_][:, a:a + 1]),
                          reads=[("ytmp", half), ("gsl", s_)], writes=[yk])
                fw.dma("sp", ypairs[t * MT + a * 128:t * MT + (a + 1) * 128, :], yr, reads=[yk], writes=[("yp", t, a)])
        P.end()

    def phase_moepost_sparse(l, last):
        P = Phase("spost")
        xt = [P.sb([128, 8, 512], F32) for _ in range(2)]
        sq = P.sb([128, 8, 512], F32)
        rstd = P.sb([128, 512], F32)
        oo = [P.sb([128, 8, 512], F32) for _ in range(2)]
        yk = [[P.sb([128, 1024], F32) for _ in range(4)] for _ in range(2)]
        i = 0
        si = 0
        for b in range(NBC):
            for (t0, N) in BLOCKS:
                if last and t0 < TC:
                    continue
                p = i % 2
                i += 1
                r = 2 if t0 < TC else b
                fw.dma("sp", xt[p][:, :, :N], fm(xres[b])[:, :, t0:t0 + N], writes=[("xt", p)])
                for sub in range(N // 128):
                    gi = b * 18 + t0 // 128 + sub
                    u = si % 2
                    si += 1
                    for k in range(4):
                        fw.idma(yk[u][k], None, ypairs, IOA(ap=S4_all[:, gi, k:k + 1], axis=0), writes=[("yk", u, k)])
                    fw.op("dve", lambda e: e.tensor_tensor(out=yk[u][0], in0=yk[u][0], in1=yk[u][1], op=ALU.add),
                          reads=[("yk", u, 0), ("yk", u, 1)], writes=[("yk", u, 0)])
                    fw.op("pool", lambda e: e.tensor_tensor(out=yk[u][2], in0=yk[u][2], in1=yk[u][3], op=ALU.add),
                          reads=[("yk", u, 2), ("yk", u, 3)], writes=[("yk", u, 2)])
                    fw.op("dve", lambda e: e.tensor_tensor(out=yk[u][0], in0=yk[u][0], in1=yk[u][2], op=ALU.add),
                          reads=[("yk", u, 0), ("yk", u, 2)], writes=[("yk", u, 0)])
                    for c in range(8):
                        bank = PS[1 + 2 * u + c // 4]
                        fw.op("pe", lambda e: e.transpose(out=bank[:, (c % 4) * 128:(c % 4 + 1) * 128], in_=yk[u][0][:, c * 128:(c + 1) * 128], identity=ident_f),
                              reads=[("yk", u, 0), "ident_f"], writes=[("ps", 1 + 2 * u + c // 4)])
                    for c in range(8):
                        bank = PS[1 + 2 * u + c // 4]
                        fw.op("dve", lambda e: e.scalar_tensor_tensor(out=xt[p][:, c, sub * 128:(sub + 1) * 128], in0=bank[:, (c % 4) * 128:(c % 4 + 1) * 128],
                                                                      scalar=modT[:, 40 + c, r:r + 1], in1=xt[p][:, c, sub * 128:(sub + 1) * 128],
                                                                      op0=ALU.mult, op1=ALU.add),
                              reads=[("ps", 1 + 2 * u + c // 4), ("xt", p), "modT"], writes=[("xt", p)])
                if not last:
                    fw.dma("sp", fm(xres[b])[:, :, t0:t0 + N], xt[p][:, :, :N], reads=[("xt", p)], writes=[("xres", b, t0)])
                else:
                    def out_fn(j, p=p, N=N):
                        fw.op("dve", lambda e: e.scalar_tensor_tensor(out=oo[p][:, j, :N], in0=xt[p][:, j, :N], scalar=fng[:, j:j + 1],
                                                                      in1=rstd[:, :N], op0=ALU.mult, op1=ALU.mult),
                              reads=[("xt", p), "rstd", "fng"], writes=[("oo", p)])
                    emit_norm(xt[p], N, ("xt", p), sq, rstd, None, r, out_fn)
                    fw.dma("sp", fm(outT[b])[:, :, t0 - TC:t0 - TC + N], oo[p][:, :, :N], reads=[("oo", p)], writes=[("out", b, t0)])
        P.end()

    seq = []
    for l in range(nlayers):
        xsrc = I["xin"] if l == 0 else xres
        last = (l == nlayers - 1)
        seq += [("mod", lambda l=l: phase_mod(l)),
                ("mixin", lambda l=l, xsrc=xsrc: phase_mixin(l, xsrc)),
                ("ret", lambda l=l: phase_ret(l)),
                ("na", lambda l=l: phase_na(l)),
                ("lru", lambda l=l: phase_lru(l)),
                ("merge", lambda l=l, xsrc=xsrc: phase_merge(l, xsrc)),
                ("moepre", lambda l=l: (phase_moepre_sparse(l) if SPARSE else phase_moepre(l))),
                ("wcast", lambda l=l: (phase_wcast(l) if SPARSE else None)),
                ("moe", lambda l=l: (phase_moe_sparse(l) if SPARSE else phase_moe(l))),
                ("moepost", lambda l=l, last=last: (phase_moepost_sparse(l, last) if SPARSE else phase_moepost(l, last)))]
    for name, fn in seq:
        fn()
        if stop_after is not None and name == stop_after:
            break
    fw.barrier()
    return nc, fw


def _consts():
    c = {}
    inv = (10000.0 ** (-np.arange(32, dtype=np.float32) / 32)).astype(np.float32)
    tok = np.arange(TL)
    rows = (tok // 64).astype(np.float32)
    cols = (tok % 64).astype(np.float32)
    ar = rows[None, :] * inv[:, None]
    ac = cols[None, :] * inv[:, None]
    C = np.ones((128, T), np.float32)
    S = np.zeros((128, T), np.float32)
    C[0:32, TC:] = np.cos(ar); C[32:64, TC:] = np.cos(ar); C[64:96, TC:] = np.cos(ac); C[96:128, TC:] = np.cos(ac)
    S[0:32, TC:] = -np.sin(ar); S[32:64, TC:] = np.sin(ar); S[64:96, TC:] = -np.sin(ac); S[96:128, TC:] = np.sin(ac)
    ks = np.float32(128.0 ** -0.5)
    c["ropeC"] = C; c["ropeS"] = S; c["ropeCk"] = C * ks; c["ropeSk"] = S * ks
    j = np.arange(128)[:, None, None]
    rel = np.arange(4)[None, :, None]
    i = np.arange(512)[None, None, :]
    diff = (i - (rel * 128 + j)).astype(np.float32)
    c["Rp"] = np.maximum(diff, 0).astype(np.float32)
    c["Rn"] = np.maximum(-diff, 0).astype(np.float32)
    jj = np.arange(128)[:, None]
    c["EA"] = (128 * np.arange(15)[None, :] - jj).astype(np.float32)
    c["EB"] = (128 * np.arange(14)[None, :] + jj + 1).astype(np.float32)
    c["I512"] = np.broadcast_to(np.arange(512, dtype=np.float32)[None, :], (128, 512)).copy()
    c["I511r"] = np.broadcast_to((511 - np.arange(512)).astype(np.float32)[None, :], (128, 512)).copy()
    c["Lmat"] = (np.arange(128)[:, None] < np.arange(128)[None, :]).astype(np.float32)
    c["pj"] = (np.arange(8)[None, :] * 128 + np.arange(128)[:, None]).astype(np.float32)
    c["iota32"] = np.broadcast_to(np.arange(NE, dtype=np.float32)[None, :], (128, NE)).copy()
    tk = (np.arange(36)[None, :] * 128 + np.arange(128)[:, None]).astype(np.int32)
    c["tokid"] = np.ascontiguousarray(np.stack([tk, tk], axis=-1))
    return c


def _na_bias_gather(rpb):
    L_ = rpb.shape[0]
    w = np.arange(64)[:, None, None]
    rr = np.arange(8)[None, :, None]
    kc = np.arange(64)[None, None, :]
    c0 = np.clip(w - 8, 0, 48)
    valid = (kc >= c0) & (kc < c0 + 16)
    dc = np.clip(kc - w + 15, 0, 30)
    out = np.empty((L_, 8, 128, 8, 512), np.float32)
    for dl in range(8):
        dr = np.clip(rr + 7 - dl, 0, 14)
        drb = np.broadcast_to(dr, (64, 8, 64))
        dcb = np.broadcast_to(dc, (64, 8, 64))
        vb = np.broadcast_to(valid, (64, 8, 64))
        g = rpb[:, :, drb, dcb]
        g = np.where(vb[None, None], g, np.float32(-30000.0)).reshape(L_, 8, 2, 64, 512)
        out[:, :, :, dl, :] = g.reshape(L_, 8, 128, 512)
    return out


def _pT(a):
    sh = a.shape[:-1]
    return np.ascontiguousarray(np.swapaxes(a.reshape(sh + (8, 128)), -1, -2))


def prep_inputs(inp):
    f = lambda k: np.asarray(inp[k], dtype=np.float32)
    x, c, ctx, c_ctx = f("x"), f("c"), f("ctx"), f("c_ctx")
    shared = {}
    shared["w_mod"] = f("w_mod")
    shared["b_modT"] = np.ascontiguousarray(f("b_mod").reshape(2, 48, 128).transpose(0, 2, 1))
    shared["n1gT"] = _pT(f("norm1_g"))
    shared["n2gT"] = _pT(f("norm2_g"))
    shared["fngT"] = _pT(f("final_norm_g"))
    shared["w_mix_in"] = f("w_mix_in")
    shared["decf"] = f("ret_decay_fwd")
    shared["decb"] = f("ret_decay_bwd")
    shared["na_bias"] = _na_bias_gather(f("na_rel_bias"))
    shared["convwT"] = np.ascontiguousarray(f("lru_conv_w").reshape(2, 4, 8, 128).transpose(0, 3, 2, 1))
    shared["convbT"] = _pT(f("lru_conv_b"))
    shared["lru_wa"] = f("lru_gate_a_w")
    shared["lru_wx"] = f("lru_gate_x_w")
    shared["lru_baT"] = np.ascontiguousarray(f("lru_gate_a_b").reshape(2, 2, 8, 128).transpose(0, 3, 1, 2))
    shared["lru_bxT"] = np.ascontiguousarray(f("lru_gate_x_b").reshape(2, 2, 8, 128).transpose(0, 3, 1, 2))
    shared["lru_lamT"] = np.ascontiguousarray(f("lru_lambda").reshape(2, 2, 8, 128).transpose(0, 3, 1, 2))
    shared["w_branch"] = f("w_branch")
    shared["w_mix_out"] = f("w_mix_out")
    shared["w_router"] = f("w_router")
    shared["b_router"] = f("b_router")
    shared["w_up"] = f("w_expert_up")
    shared["bupT"] = np.ascontiguousarray(f("b_expert_up").reshape(2, NE, 8, 128, 2).transpose(0, 3, 1, 4, 2))
    shared["bup2"] = np.ascontiguousarray(f("b_expert_up").reshape(2, NE, 8, 128, 2).transpose(0, 1, 3, 4, 2).reshape(2, NE, 128, 16))
    shared["w_down"] = f("w_expert_down")
    shared["b_down"] = f("b_expert_down")
    shared.update(_consts())
    in_maps = []
    for core in range(NCORES):
        bs = slice(core * NBC, (core + 1) * NBC)
        xa = np.concatenate([ctx[bs], x[bs]], axis=1)
        xin = np.ascontiguousarray(xa.transpose(0, 2, 1).reshape(NBC, 8, 128, T))
        crow = np.stack([c[core * NBC], c[core * NBC + 1], c_ctx], axis=0)
        cT = np.ascontiguousarray(crow.reshape(3, 8, 128).transpose(2, 1, 0))
        m = dict(shared)
        m["xin"] = xin
        m["cT"] = cT
        in_maps.append(m)
    return in_maps


def kernel(**inputs):
    in_maps = prep_inputs(inputs)
    nc, fw = build()
    res = run_bass_kernel_spmd(nc, in_maps, core_ids=list(range(NCORES)))
    outs = []
    for core in range(NCORES):
        o = res.results[core]["outT"]
        outs.append(o.reshape(NBC, D, TL).transpose(0, 2, 1))
    return np.ascontiguousarray(np.concatenate(outs, axis=0)).astype(np.float32)
```

```python
import numpy as np
from contextlib import ExitStack
import concourse.bass as bass
import concourse.mybir as mybir
from concourse.bass_utils import run_bass_kernel_spmd

F32 = mybir.dt.float32
BF16 = mybir.dt.bfloat16
AF = mybir.ActivationFunctionType
ALU = mybir.AluOpType
AX = mybir.AxisListType

NCORES = 8
NBC = 2
D = 1024
TC = 256
TL = 2048
T = TC + TL
NE = 32
EPS = 1e-6
BLOCKS = [(0, 256), (256, 512), (768, 512), (1280, 512), (1792, 512)]
BLOCKS256 = [(i * 256, 256) for i in range(9)]
NDSEM = 12
GRP = 1152
MB = 384
DBG_NSEC = 12
SPARSE = True
MT = 512
NTILE = 68
NSLOT = NTILE * MT
NTOK = 4608
I32 = mybir.dt.int32
DBG_RET = 16
DBG_IB = 4


class FW:
    def __init__(self, nc):
        self.nc = nc
        self.eng = {"pe": nc.tensor, "act": nc.scalar, "dve": nc.vector,
                    "pool": nc.gpsimd, "sp": nc.sync}
        self.sem = {}
        self.cnt = {}
        for e in ("pe", "act", "dve", "pool"):
            self.sem[e] = nc.alloc_semaphore("s_" + e)
            self.cnt[e] = 0
        self.dsem = {}
        self.dcnt = {}
        for q in ("sp", "pool"):
            self.dsem[q] = [nc.alloc_semaphore("d_%s_%d" % (q, i)) for i in range(NDSEM)]
            self.dcnt[q] = 0
        self.waited = {e: {} for e in self.eng}
        self.lastw = {}
        self.readers = {}
        self.ninst = 0
        self.nwait = 0

    def _wait(self, e, tok):
        sem, val, src = tok
        if src == e and e == "pe":
            return
        w = self.waited[e]
        k = id(sem)
        if w.get(k, 0) >= val:
            return
        w[k] = val
        self.eng[e].wait_ge(sem, val)
        self.nwait += 1

    def _deps(self, e, reads, writes):
        for r in reads:
            t = self.lastw.get(r)
            if t is not None:
                self._wait(e, t)
            if (isinstance(r, tuple) and r[0] in ("ps", "psb", "O")) or r in ("ps0", "psm"):
                for t in self.readers.get(r, ()):
                    if t[2] != e:
                        self._wait(e, t)
        for w in writes:
            t = self.lastw.get(w)
            if t is not None:
                self._wait(e, t)
            for t in self.readers.get(w, ()):
                self._wait(e, t)

    def _record(self, tok, reads, writes):
        for r in reads:
            self.readers.setdefault(r, []).append(tok)
        for w in writes:
            self.lastw[w] = tok
            self.readers[w] = []

    def op(self, e, fn, reads=(), writes=()):
        self._deps(e, reads, writes)
        ins = fn(self.eng[e])
        self.cnt[e] += 1
        ins.then_inc(self.sem[e], 1)
        tok = (self.sem[e], self.cnt[e], e)
        self._record(tok, reads, writes)
        self.ninst += 1
        return tok

    def dma(self, q, out, in_, reads=(), writes=(), **kw):
        j = self.dcnt[q]
        sem = self.dsem[q][j % NDSEM]
        gen = j // NDSEM
        if gen > 0:
            self._wait(q, (sem, 16 * gen, "dma"))
        self._deps(q, reads, writes)
        ins = self.eng[q].dma_start(out=out, in_=in_, **kw)
        ins.then_inc(sem, 16)
        self.dcnt[q] = j + 1
        tok = (sem, 16 * (gen + 1), "dma")
        self._record(tok, reads, writes)
        self.ninst += 1
        return tok

    def idma(self, out, out_off, in_, in_off, reads=(), writes=(), **kw):
        q = "pool"
        j = self.dcnt[q]
        sem = self.dsem[q][j % NDSEM]
        gen = j // NDSEM
        if gen > 0:
            self._wait(q, (sem, 16 * gen, "dma"))
        self._deps(q, reads, writes)
        ins = self.eng[q].indirect_dma_start(out=out, out_offset=out_off, in_=in_, in_offset=in_off, **kw)
        ins.then_inc(sem, 16)
        self.dcnt[q] = j + 1
        tok = (sem, 16 * (gen + 1), "dma")
        self._record(tok, reads, writes)
        self.ninst += 1
        return tok

    def barrier(self):
        toks = []
        for e in ("pe", "act", "dve", "pool"):
            if self.cnt[e] > 0:
                toks.append((self.sem[e], self.cnt[e], e))
        for q in ("sp", "pool"):
            j = self.dcnt[q]
            for i in range(NDSEM):
                n = (j - i + NDSEM - 1) // NDSEM if j > i else 0
                if n > 0:
                    toks.append((self.dsem[q][i], 16 * n, "dma"))
        for e in self.eng:
            for t in toks:
                if t[2] == e and e != "pe":
                    pass
                sem, val, src = t
                w = self.waited[e]
                if w.get(id(sem), 0) >= val:
                    continue
                w[id(sem)] = val
                self.eng[e].wait_ge(sem, val)
                self.nwait += 1
        self.lastw = {}
        self.readers = {}


def build(nlayers=2, dbg=(), stop_after=None):
    nc = bass.Bass("TRN2", target_bir_lowering=False)
    fw = FW(nc)
    I = {}

    def din(name, shape):
        I[name] = nc.dram_tensor(name, list(shape), F32, kind="ExternalInput").ap()

    def scr(name, shape, dt):
        kind = "ExternalOutput" if name in dbg else "Internal"
        return nc.dram_tensor(name, list(shape), dt, kind=kind).ap()

    L = 2
    din("xin", [NBC, 8, 128, T])
    din("cT", [128, 8, 3])
    din("w_mod", [L, D, 6 * D])
    din("b_modT", [L, 128, 48])
    din("n1gT", [L, 128, 8])
    din("n2gT", [L, 128, 8])
    din("fngT", [128, 8])
    din("w_mix_in", [L, D, 12 * D])
    din("decf", [L, 8])
    din("decb", [L, 8])
    din("na_bias", [L, 8, 128, 8, 512])
    din("convwT", [L, 128, 8, 4])
    din("convbT", [L, 128, 8])
    din("lru_wa", [L, 2, 8, 128, 128])
    din("lru_wx", [L, 2, 8, 128, 128])
    din("lru_baT", [L, 128, 2, 8])
    din("lru_bxT", [L, 128, 2, 8])
    din("lru_lamT", [L, 128, 2, 8])
    din("w_branch", [L, 3, D, D])
    din("w_mix_out", [L, D, D])
    din("w_router", [L, D, NE])
    din("b_router", [L, NE])
    din("w_up", [L, NE, D, 2 * D])
    din("bupT", [L, 128, NE, 2, 8])
    din("w_down", [L, NE, D, D])
    din("b_down", [L, NE, D])
    din("ropeC", [128, T])
    din("ropeS", [128, T])
    din("ropeCk", [128, T])
    din("ropeSk", [128, T])
    din("Rp", [128, 4, 512])
    din("Rn", [128, 4, 512])
    din("EA", [128, 15])
    din("EB", [128, 14])
    din("I512", [128, 512])
    din("I511r", [128, 512])
    din("Lmat", [128, 128])
    din("pj", [128, 8])
    din("iota32", [128, NE])
    din("bup2", [L, NE, 128, 16])
    I["tokid"] = nc.dram_tensor("tokid", [128, 36, 2], I32, kind="ExternalInput").ap()
    outT = nc.dram_tensor("outT", [NBC, 8, 128, TL], F32, kind="ExternalOutput").ap()

    xres = scr("xres", [NBC, 8, 128, T], F32)
    fmaj = {}
    for nm in ("rqT", "rkT", "rgT", "nqT", "nkT", "lyT", "gaT", "gbT", "gcT", "AinT", "BinT", "CinT", "hT2"):
        fmaj[nm] = scr(nm, [NBC, 8, 128, T], BF16)
    lxT = scr("lxT", [NBC, 8, 128, T], F32)
    rv = scr("rv", [NBC, T, D], BF16)
    nv = scr("nv", [NBC, T, D], BF16)
    gTd = scr("gTd", [NE, NBC * T], F32)
    yT = scr("yT", [NBC, 8, 128, T], F32)
    h2tok = scr("h2tok", [NTOK + 128, D], BF16)
    gtab = scr("gtab", [NTOK + 128, NE], F32)
    slot_tok = scr("slot_tok", [NSLOT, 2], I32)
    ypairs = scr("ypairs", [NSLOT, D], F32)
    wupb = scr("wupb", [NE * 128, 8 * 2 * D], BF16)
    wdnb = scr("wdnb", [NE * 128, 8 * D], BF16)

    def fm(ap_b):
        return ap_b.rearrange("c p t -> p c t")

    PS = [nc.alloc_psum_tensor("ps%d" % i, [128, 512], F32).ap() for i in range(8)]
    PSB = [PS[6].bitcast(BF16), PS[7].bitcast(BF16)]

    def gsb(name, shape, dt):
        return nc.alloc_sbuf_tensor(name, list(shape), dt).ap()

    ones_f = gsb("ones_f", [128, 128], F32)
    ones_b = gsb("ones_b", [128, 128], BF16)
    ident_f = gsb("ident_f", [128, 128], F32)
    ident_b = gsb("ident_b", [128, 128], BF16)
    modT = gsb("modT", [128, 48, 3], F32)
    mul1 = gsb("mul1", [128, 8, 3], F32)
    mul2 = gsb("mul2", [128, 8, 3], F32)
    fng = gsb("fng", [128, 8], F32)
    widx = gsb("widx", [128, NTILE, 8], I32)
    bidx = gsb("bidx", [128, NTILE], I32)
    eidx = gsb("eidx", [128, NTILE], I32)
    ETf = gsb("ETf", [128, NTILE], F32)
    S4_all = gsb("S4_all", [128, 36, 4], I32)
    pidx = gsb("pidx", [128, NTILE], I32)
    fw.op("pool", lambda e: e.memset(ones_f, 1.0), writes=["ones_f"])
    fw.op("pool", lambda e: e.memset(ones_b, 1.0), writes=["ones_b"])
    fw.op("pool", lambda e: e.memset(ident_f, 0.0), writes=["ident_f"])
    fw.op("pool", lambda e: e.affine_select(out=ident_f, in_=ident_f, pattern=[[-1, 128]],
                                            compare_op=ALU.not_equal, fill=1.0, base=0, channel_multiplier=1),
          reads=["ident_f"], writes=["ident_f"])
    fw.op("dve", lambda e: e.tensor_copy(out=ident_b, in_=ident_f), reads=["ident_f"], writes=["ident_b"])
    fw.dma("sp", fng, I["fngT"], writes=["fng"])
    fw.barrier()

    class Phase:
        cnt = 0

        def __init__(self, name):
            self.name = name
            self.es = ExitStack()
            self.n = 0

        def sb(self, shape, dt):
            self.n += 1
            Phase.cnt += 1
            t = self.es.enter_context(nc.sbuf_tensor("%s_%d_%d" % (self.name, Phase.cnt, self.n), list(shape), dt))
            return t.ap()

        def end(self):
            fw.barrier()
            self.es.close()

    def mm(out, lhsT, rhs, start, stop, reads, writes):
        fw.op("pe", lambda e: e.matmul(out, lhsT=lhsT, rhs=rhs, start=start, stop=stop), reads, writes)

    def phase_mod(l):
        P = Phase("mod")
        cs = P.sb([128, 8, 4], F32)
        bm = P.sb([128, 48], F32)
        fw.op("pool", lambda e: e.memset(cs, 0.0), writes=["cs"])
        n1 = P.sb([128, 8], F32)
        n2 = P.sb([128, 8], F32)
        fw.dma("sp", cs[:, :, 0:3], I["cT"], writes=["cs"])
        fw.dma("sp", bm, I["b_modT"][l], writes=["bm"])
        fw.dma("sp", n1, I["n1gT"][l], writes=["n1"])
        fw.dma("sp", n2, I["n2gT"][l], writes=["n2"])
        fw.op("act", lambda e: e.activation(out=cs, in_=cs, func=AF.Silu), reads=["cs"], writes=["cs"])
        wbuf = [P.sb([128, 8, 1024], F32) for _ in range(2)]
        psm = PS[0]
        for s in range(6):
            wb = wbuf[s % 2]
            fw.dma("sp", wb, I["w_mod"][l, :, s * 1024:(s + 1) * 1024].rearrange("(j p) n -> p j n", p=128),
                   writes=[("wm", s % 2)])
            for j in range(8):
                n = s * 8 + j
                for k in range(8):
                    mm(psm[:, n * 4:n * 4 + 4], wb[:, k, j * 128:(j + 1) * 128], cs[:, k, :], k == 0, k == 7,
                       [("wm", s % 2), "cs"], ["psm"])
        psv = psm[:, 0:192].rearrange("p (n r) -> p n r", r=4)
        for r in range(3):
            fw.op("dve", lambda e: e.tensor_tensor(out=modT[:, :, r], in0=psv[:, :, r], in1=bm, op=ALU.add),
                  reads=["psm", "bm"], writes=["modT"])
        for r in range(3):
            fw.op("dve", lambda e: e.scalar_tensor_tensor(out=mul1[:, :, r], in0=modT[:, 8:16, r], scalar=1.0, in1=n1,
                                                          op0=ALU.add, op1=ALU.mult),
                  reads=["modT", "n1"], writes=["mul1"])
            fw.op("dve", lambda e: e.scalar_tensor_tensor(out=mul2[:, :, r], in0=modT[:, 32:40, r], scalar=1.0, in1=n2,
                                                          op0=ALU.add, op1=ALU.mult),
                  reads=["modT", "n2"], writes=["mul2"])
        P.end()

    def emit_norm(xt, N, xkey, sq, rstd, mulT, r, out_fn):
        fw.op("act", lambda e: e.activation(out=sq[:, :, :N], in_=xt[:, :, :N], func=AF.Square),
              reads=[xkey], writes=["sq"])
        for j in range(8):
            mm(PS[0][:, :N], ones_f, sq[:, j, :N], j == 0, j == 7, ["sq", "ones_f"], ["ps0"])
        fw.op("act", lambda e: e.activation(out=rstd[:, :N], in_=PS[0][:, :N], func=AF.Sqrt, scale=1.0 / D, bias=EPS),
              reads=["ps0"], writes=["rstd"])
        fw.op("dve", lambda e: e.reciprocal(out=rstd[:, :N], in_=rstd[:, :N]), reads=["rstd"], writes=["rstd"])
        for j in range(8):
            out_fn(j)

    def phase_mixin(l, xsrc):
        es_h = ExitStack()
        hT = es_h.enter_context(nc.sbuf_tensor("hT_%d" % l, [128, 8, NBC * T], BF16)).ap()
        P = Phase("n1")
        xt = [P.sb([128, 8, 512], F32) for _ in range(2)]
        sq = P.sb([128, 8, 512], F32)
        rstd = P.sb([128, 512], F32)
        tmp = [P.sb([128, 512], F32) for _ in range(2)]
        i = 0
        for b in range(NBC):
            for (t0, N) in BLOCKS:
                x_ = xt[i % 2]
                xk = ("xt", i % 2)
                fw.dma("sp", x_[:, :, :N], fm(xsrc[b])[:, :, t0:t0 + N], writes=[xk])
                r = 2 if t0 < TC else b

                def out_fn(j, x_=x_, xk=xk, N=N, r=r, b=b, t0=t0):
                    tm = tmp[j % 2]
                    fw.op("dve", lambda e: e.scalar_tensor_tensor(out=tm[:, :N], in0=x_[:, j, :N], scalar=mul1[:, j, r:r + 1],
                                                                  in1=rstd[:, :N], op0=ALU.mult, op1=ALU.mult),
                          reads=[xk, "rstd", "mul1"], writes=[("tmp", j % 2)])
                    fw.op("act", lambda e: e.activation(out=hT[:, j, b * T + t0:b * T + t0 + N], in_=tm[:, :N],
                                                        func=AF.Identity, bias=modT[:, j, r:r + 1]),
                          reads=[("tmp", j % 2), "modT"], writes=["hT"])
                emit_norm(x_, N, xk, sq, rstd, mul1, r, out_fn)
                i += 1
        P.end()

        P = Phase("mix")
        wb = [P.sb([128, 8, 1024], BF16) for _ in range(2)]
        wp = P.sb([128, 8, 1024], BF16)
        stage = [P.sb([128, 8, 512], BF16) for _ in range(2)]
        stage32 = P.sb([128, 8, 512], F32)
        stT = [P.sb([128, 1024], BF16) for _ in range(2)]
        rC = P.sb([128, T], F32)
        rS = P.sb([128, T], F32)
        rt = [P.sb([128, 512], F32) for _ in range(4)]
        names = ["rqT", "rkT", None, "rgT", "nqT", "nkT", None, None, "lyT", "gaT", "gbT", "gcT"]

        def loadw(s):
            fw.dma("pool", wb[s % 2], I["w_mix_in"][l, :, s * 1024:(s + 1) * 1024].rearrange("(j p) n -> p j n", p=128),
                   writes=[("wb", s % 2)])
        loadw(0)
        psi = 0
        sti = 0
        ev = 0
        for s in range(DBG_NSEC):
            if s + 1 < DBG_NSEC:
                loadw(s + 1)
            w = wb[s % 2]
            wk = ("wb", s % 2)
            if s < 2:
                Wv = w.rearrange("p j (h q r) -> p (j h) q r", q=4, r=32)
                Pv = wp.rearrange("p j (h q r) -> p (j h) q r", q=4, r=32)
                for q in range(4):
                    eng = "dve" if q % 2 == 0 else "act"
                    if eng == "dve":
                        fw.op("dve", lambda e: e.tensor_copy(out=Pv[:, :, q, :], in_=Wv[:, :, q ^ 1, :]), reads=[wk], writes=["wp"])
                    else:
                        fw.op("act", lambda e: e.copy(out=Pv[:, :, q, :], in_=Wv[:, :, q ^ 1, :]), reads=[wk], writes=["wp"])
                fw.dma("sp", rC, I["ropeC" if s == 0 else "ropeCk"], writes=["rC"])
                fw.dma("sp", rS, I["ropeS" if s == 0 else "ropeSk"], writes=["rS"])
            if s in (2, 6):
                dst = rv if s == 2 else nv
                for b in range(NBC):
                    for tt in range(18):
                        st = stT[sti % 2]
                        sk = ("stT", sti % 2)
                        for half in range(2):
                            ps = PS[psi % 4]
                            pk = ("ps", psi % 4)
                            psi += 1
                            for k in range(8):
                                mm(ps, hT[:, k, b * T + tt * 128:b * T + (tt + 1) * 128], w[:, k, half * 512:(half + 1) * 512],
                                   k == 0, k == 7, ["hT", wk], [pk])
                            if ev % 2 == 0:
                                fw.op("act", lambda e: e.copy(out=st[:, half * 512:(half + 1) * 512], in_=ps), reads=[pk], writes=[sk])
                            else:
                                fw.op("dve", lambda e: e.tensor_copy(out=st[:, half * 512:(half + 1) * 512], in_=ps), reads=[pk], writes=[sk])
                            ev += 1
                        fw.dma("sp", dst[b, tt * 128:(tt + 1) * 128, :], st, reads=[sk], writes=[("dst", s, b, tt)])
                        sti += 1
                continue
            for b in range(NBC):
                for (t0, N) in BLOCKS:
                    if s == 7:
                        st = stage32
                        sk = "stage32"
                    else:
                        st = stage[sti % 2]
                        sk = ("stage", sti % 2)
                        sti += 1
                    hsl = slice(b * T + t0, b * T + t0 + N)
                    for c in range(8):
                        ps = PS[psi % 4]
                        pk = ("ps", psi % 4)
                        psi += 1
                        for k in range(8):
                            mm(ps[:, :N], w[:, k, c * 128:(c + 1) * 128], hT[:, k, hsl], k == 0, k == 7, ["hT", wk], [pk])
                        o = st[:, c, :N]
                        if s < 2:
                            ps2 = PS[4 + psi % 2]
                            pk2 = ("ps", 4 + psi % 2)
                            for k in range(8):
                                mm(ps2[:, :N], wp[:, k, c * 128:(c + 1) * 128], hT[:, k, hsl], k == 0, k == 7, ["hT", "wp"], [pk2])
                            ta = rt[(psi % 2) * 2]
                            tb = rt[(psi % 2) * 2 + 1]
                            ka = ("rt", (psi % 2) * 2)
                            kb = ("rt", (psi % 2) * 2 + 1)
                            fw.op("dve", lambda e: e.tensor_tensor(out=ta[:, :N], in0=ps[:, :N], in1=rC[:, t0:t0 + N], op=ALU.mult),
                                  reads=[pk, "rC"], writes=[ka])
                            fw.op("dve", lambda e: e.tensor_tensor(out=tb[:, :N], in0=ps2[:, :N], in1=rS[:, t0:t0 + N], op=ALU.mult),
                                  reads=[pk2, "rS"], writes=[kb])
                            fw.op("pool", lambda e: e.tensor_tensor(out=o, in0=ta[:, :N], in1=tb[:, :N], op=ALU.add),
                                  reads=[ka, kb], writes=[sk])
                        elif s == 3:
                            fw.op("act", lambda e: e.activation(out=o, in_=ps[:, :N], func=AF.Silu), reads=[pk], writes=[sk])
                        elif s == 8:
                            fw.op("act", lambda e: e.activation(out=o, in_=ps[:, :N], func=AF.Gelu), reads=[pk], writes=[sk])
                        elif s >= 9:
                            fw.op("act", lambda e: e.activation(out=o, in_=ps[:, :N], func=AF.Sigmoid), reads=[pk], writes=[sk])
                        elif s == 4:
                            if ev % 2 == 0:
                                fw.op("act", lambda e: e.activation(out=o, in_=ps[:, :N], func=AF.Copy, scale=0.125), reads=[pk], writes=[sk])
                            else:
                                fw.op("dve", lambda e: e.tensor_scalar(out=o, in0=ps[:, :N], scalar1=0.125, scalar2=None, op0=ALU.mult),
                                      reads=[pk], writes=[sk])
                            ev += 1
                        else:
                            if ev % 2 == 0:
                                fw.op("act", lambda e: e.copy(out=o, in_=ps[:, :N]), reads=[pk], writes=[sk])
                            else:
                                fw.op("dve", lambda e: e.tensor_copy(out=o, in_=ps[:, :N]), reads=[pk], writes=[sk])
                            ev += 1
                    dst = lxT if s == 7 else fmaj[names[s]]
                    fw.dma("sp", fm(dst[b])[:, :, t0:t0 + N], st[:, :, :N], reads=[sk], writes=[("dst", s, b, t0)])
        P.end()
        es_h.close()

    def phase_ret(l):
        P = Phase("ret")
        lg = P.sb([128, 16], F32)
        fw.dma("sp", lg[:, 0:8], I["decf"][l].partition_broadcast(128), writes=["lg"])
        fw.dma("sp", lg[:, 8:16], I["decb"][l].partition_broadcast(128), writes=["lg"])
        fw.op("act", lambda e: e.activation(out=lg, in_=lg, func=AF.Exp, scale=-1.0), reads=["lg"], writes=["lg"])
        fw.op("act", lambda e: e.activation(out=lg, in_=lg, func=AF.Ln, bias=1.0), reads=["lg"], writes=["lg"])
        fw.op("dve", lambda e: e.tensor_scalar(out=lg, in0=lg, scalar1=-1.0, scalar2=None, op0=ALU.mult), reads=["lg"], writes=["lg"])
        Rp = P.sb([128, 4, 512], F32)
        Rn = P.sb([128, 4, 512], F32)
        EA = P.sb([128, 15], F32)
        EB = P.sb([128, 14], F32)
        I512 = P.sb([128, 512], F32)
        I511r = P.sb([128, 512], F32)
        for nm, t in (("Rp", Rp), ("Rn", Rn), ("EA", EA), ("EB", EB), ("I512", I512), ("I511r", I511r)):
            fw.dma("sp", t, I[nm], writes=[nm])
        Dm = P.sb([128, 8, 4, 512], BF16)
        Af = P.sb([128, 8, 15], F32)
        Ab = P.sb([128, 8, 14], F32)
        bf = P.sb([128, 8, 512], F32)
        bb = P.sb([128, 8, 512], F32)
        t1 = P.sb([128, 512], F32)
        for h in range(8):
            for rel in range(4):
                fw.op("dve", lambda e: e.tensor_scalar(out=t1, in0=Rp[:, rel, :], scalar1=lg[:, h:h + 1], scalar2=None, op0=ALU.mult),
                      reads=["Rp", "lg"], writes=["t1"])
                fw.op("dve", lambda e: e.scalar_tensor_tensor(out=t1, in0=Rn[:, rel, :], scalar=lg[:, 8 + h:9 + h], in1=t1,
                                                              op0=ALU.mult, op1=ALU.add),
                      reads=["Rn", "lg", "t1"], writes=["t1"])
                fw.op("act", lambda e: e.activation(out=Dm[:, h, rel, :], in_=t1, func=AF.Exp), reads=["t1"], writes=["Dm"])
            fw.op("act", lambda e: e.activation(out=Af[:, h, :], in_=EA, func=AF.Exp, scale=lg[:, h:h + 1]), reads=["EA", "lg"], writes=["Af"])
            fw.op("act", lambda e: e.activation(out=Ab[:, h, :], in_=EB, func=AF.Exp, scale=lg[:, 8 + h:9 + h]), reads=["EB", "lg"], writes=["Ab"])
            fw.op("act", lambda e: e.activation(out=bf[:, h, :], in_=I512, func=AF.Exp, scale=lg[:, h:h + 1]), reads=["I512", "lg"], writes=["bf"])
            fw.op("act", lambda e: e.activation(out=bb[:, h, :], in_=I511r, func=AF.Exp, scale=lg[:, 8 + h:9 + h]), reads=["I511r", "lg"], writes=["bb"])
        qT = [P.sb([128, T], BF16) for _ in range(2)]
        kT = [P.sb([128, T], BF16) for _ in range(2)]
        V = [P.sb([128, 18, 128], BF16) for _ in range(2)]
        sg = [P.sb([128, T], BF16) for _ in range(2)]
        ost = [P.sb([128, T], BF16) for _ in range(2)]
        pb = [P.sb([128, 512], BF16) for _ in range(4)]
        y32 = P.sb([128, 512], F32)
        e1 = P.sb([128, 512], F32)
        e2 = P.sb([128, 512], F32)
        sqy = P.sb([128, 512], F32)
        rsy = P.sb([128, 512], F32)
        it = 0
        pbi = 0
        evc = 0
        for b in range(NBC):
            for h in range(8):
                if it >= DBG_RET:
                    continue
                p = it % 2
                it += 1
                q_, k_, v_, s_, o_ = qT[p], kT[p], V[p], sg[p], ost[p]
                fw.dma("sp", q_, fmaj["rqT"][b, h], writes=[("q", p)])
                fw.dma("sp", k_, fmaj["rkT"][b, h], writes=[("k", p)])
                fw.dma("sp", v_, rv[b].rearrange("(t p) (h d) -> h p t d", p=128, d=128)[h], writes=[("v", p)])
                fw.dma("sp", s_, fmaj["rgT"][b, h], writes=[("sg", p)])
                SBK = [0, 1, 6, 7]
                steps = []
                blkinfo = {}
                for ib in range(-1, DBG_IB):
                    if ib < 0:
                        q0, N = 0, 256
                        kks = [(kk, [("d", kk)]) for kk in range(2)]
                    else:
                        q0, N = TC + 512 * ib, 512
                        kks = []
                        for kk in range(18):
                            if kk < 2:
                                kks.append((kk, [("f", 2 + 4 * ib - kk), ("b", 12 + kk - 4 * ib)]))
                            else:
                                rel = kk - 2 - 4 * ib
                                if rel < 0:
                                    kks.append((kk, [("f", -rel)]))
                                elif rel < 4:
                                    kks.append((kk, [("d", rel)]))
                                else:
                                    kks.append((kk, [("b", rel - 4)]))
                    tot = {"d": 0, "f": 0, "b": 0}
                    for (_, its) in kks:
                        for (ty, _) in its:
                            tot[ty] += 1
                    blkinfo[ib] = (q0, N, tot)
                    for j, (kk, its) in enumerate(kks):
                        steps.append((ib, kk, its, j == 0, j == len(kks) - 1))

                def emitS(i):
                    ib, kk, _, _, _ = steps[i]
                    q0, N, _ = blkinfo[ib]
                    bk = SBK[i % 4]
                    mm(PS[bk][:, :N], k_[:, kk * 128:(kk + 1) * 128], q_[:, q0:q0 + N], True, True, [("q", p), ("k", p)], [("ps", bk)])

                def epilogue(ib):
                    q0, N, _ = blkinfo[ib]
                    if ib < 0:
                        fw.op("dve", lambda e: e.tensor_copy(out=y32[:, :N], in_=PS[2][:, :N]), reads=[("ps", 2)], writes=["y32"])
                    else:
                        fw.op("dve", lambda e: e.tensor_tensor(out=e1, in0=PS[3], in1=bf[:, h, :], op=ALU.mult), reads=[("ps", 3), "bf"], writes=["e1"])
                        fw.op("dve", lambda e: e.tensor_tensor(out=e2, in0=PS[4], in1=bb[:, h, :], op=ALU.mult), reads=[("ps", 4), "bb"], writes=["e2"])
                        fw.op("pool", lambda e: e.tensor_tensor(out=e1, in0=e1, in1=e2, op=ALU.add), reads=["e1", "e2"], writes=["e1"])
                        fw.op("dve", lambda e: e.tensor_tensor(out=y32, in0=PS[2], in1=e1, op=ALU.add), reads=[("ps", 2), "e1"], writes=["y32"])
                    fw.op("act", lambda e: e.activation(out=sqy[:, :N], in_=y32[:, :N], func=AF.Square), reads=["y32"], writes=["sqy"])
                    mm(PS[5][:, :N], ones_f, sqy[:, :N], True, True, ["sqy", "ones_f"], [("ps", 5)])
                    fw.op("act", lambda e: e.activation(out=rsy[:, :N], in_=PS[5][:, :N], func=AF.Sqrt, scale=1.0 / 128, bias=EPS),
                          reads=[("ps", 5)], writes=["rsy"])
                    fw.op("dve", lambda e: e.reciprocal(out=rsy[:, :N], in_=rsy[:, :N]), reads=["rsy"], writes=["rsy"])
                    fw.op("pool", lambda e: e.tensor_tensor(out=y32[:, :N], in0=y32[:, :N], in1=rsy[:, :N], op=ALU.mult),
                          reads=["y32", "rsy"], writes=["y32"])
                    fw.op("dve", lambda e: e.tensor_tensor(out=o_[:, q0:q0 + N], in0=y32[:, :N], in1=s_[:, q0:q0 + N], op=ALU.mult),
                          reads=["y32", ("sg", p)], writes=[("ost", p)])

                LA = 3
                for i in range(min(LA, len(steps))):
                    emitS(i)
                accb = {"d": 2, "f": 3, "b": 4}
                cnts = None
                for i, (ib, kk, its, first, last) in enumerate(steps):
                    q0, N, tot = blkinfo[ib]
                    if first:
                        cnts = {"d": 0, "f": 0, "b": 0}
                    bk = SBK[i % 4]
                    sps = PS[bk]
                    spk = ("ps", bk)
                    for (ty, idx) in its:
                        pt = pb[pbi % 4]
                        pk = ("pb", pbi % 4)
                        pbi += 1
                        if ty == "d":
                            fw.op("dve", lambda e: e.tensor_tensor(out=pt[:, :N], in0=sps[:, :N], in1=Dm[:, h, idx, :N], op=ALU.mult),
                                  reads=[spk, "Dm"], writes=[pk])
                        else:
                            sc = Af[:, h, idx:idx + 1] if ty == "f" else Ab[:, h, idx:idx + 1]
                            if evc % 3 != 0:
                                fw.op("act", lambda e: e.activation(out=pt[:, :N], in_=sps[:, :N], func=AF.Identity, scale=sc),
                                      reads=[spk, "Af", "Ab"], writes=[pk])
                            else:
                                fw.op("dve", lambda e: e.tensor_scalar(out=pt[:, :N], in0=sps[:, :N], scalar1=sc, scalar2=None, op0=ALU.mult),
                                      reads=[spk, "Af", "Ab"], writes=[pk])
                            evc += 1
                        ab = accb[ty]
                        mm(PS[ab][:, :N], v_[:, kk, :], pt[:, :N], cnts[ty] == 0, cnts[ty] == tot[ty] - 1, [("v", p), pk], [("ps", ab)])
                        cnts[ty] += 1
                    if i + LA < len(steps):
                        emitS(i + LA)
                    if last:
                        epilogue(ib)
                fw.dma("pool", fmaj["AinT"][b, h], o_, reads=[("ost", p)], writes=[("Ain", b, h)])
        P.end()

    def phase_na(l):
        P = Phase("na")
        qbd = [P.sb([128, 36, 128], BF16) for _ in range(2)]
        kT = [P.sb([128, T], BF16) for _ in range(2)]
        Ve = [P.sb([128, 18, 128], BF16) for _ in range(2)]
        Vo = [P.sb([128, 15, 128], BF16) for _ in range(2)]
        Bt = [P.sb([128, 8, 512], F32) for _ in range(2)]
        Ssb = [P.sb([128, 768], F32) for _ in range(3)]
        Pb = [P.sb([128, 768], BF16) for _ in range(3)]
        PT = [P.sb([128, 6, 128], BF16) for _ in range(3)]
        mx = [P.sb([128, 1], F32) for _ in range(3)]
        rec = [P.sb([128, 128], F32) for _ in range(2)]
        ost = [P.sb([128, T], BF16) for _ in range(2)]
        for i in range(2):
            fw.op("pool", lambda e: e.memset(qbd[i], 0.0), writes=[("qbd", i)])
        wcb = [P.sb([128, 8, 1024], BF16) for _ in range(2)]
        wcu = [0]

        def wc_src_dst(u):
            ex, k = u // 3, u % 3
            if k < 2:
                src = I["w_up"][l, ex][:, k * 1024:(k + 1) * 1024].rearrange("(j p) n -> p j n", p=128)
                dst = wupb[ex * 128:(ex + 1) * 128, :].rearrange("p (j n) -> p j n", j=8)[:, :, k * 1024:(k + 1) * 1024]
            else:
                src = I["w_down"][l, ex].rearrange("(j p) n -> p j n", p=128)
                dst = wdnb[ex * 128:(ex + 1) * 128, :].rearrange("p (j n) -> p j n", j=8)
            return src, dst

        def wc_load(u):
            src, _ = wc_src_dst(u)
            fw.dma("pool", wcb[u % 2], src, writes=[("wcb", u % 2)])

        def wc_store(u):
            _, dst = wc_src_dst(u)
            fw.dma("sp", dst, wcb[u % 2], reads=[("wcb", u % 2)], writes=[("wcd", u)])
        it = 0
        ri = 0
        for b in range(NBC):
            nvv = nv[b].rearrange("(t p) (g d) -> g p t d", p=128, d=128)
            nvo = nv[b, TC + 64:TC + 64 + 15 * 128, :].rearrange("(t p) (g d) -> g p t d", p=128, d=128)
            for g in range(8):
                p = it % 2
                it += 1
                qb, k_, ve, vo, bt, o_ = qbd[p], kT[p], Ve[p], Vo[p], Bt[p], ost[p]
                nq = fmaj["nqT"][b, g]
                fw.dma("sp", qb[0:64, :, 0:64], nq[0:64, :].rearrange("p (r w) -> p r w", w=64), writes=[("qbd", p)])
                fw.dma("sp", qb[64:128, :, 64:128], nq[64:128, :].rearrange("p (r w) -> p r w", w=64), writes=[("qbd", p)])
                fw.dma("sp", k_, fmaj["nkT"][b, g], writes=[("k", p)])
                fw.dma("sp", ve, nvv[g], writes=[("ve", p)])
                fw.dma("sp", vo, nvo[g], writes=[("vo", p)])
                fw.dma("sp", bt, I["na_bias"][l, g], writes=[("bt", p)])
                def rowinfo(rr):
                    if rr < 4:
                        return True, 256, 2, 0, 0
                    r = rr - 4
                    r0 = min(max(r - 4, 0), 24)
                    return False, 768, 6, r0, r - r0

                def stA(rr):
                    ctxrow, W, nkt, r0, dl = rowinfo(rr)
                    a = rr % 2
                    a3 = rr % 3
                    X = PS[2 * a]
                    Y = PS[2 * a + 1]
                    xk = ("ps", 2 * a)
                    yk = ("ps", 2 * a + 1)
                    S_ = Ssb[a3]
                    sk = ("Ssb", a3)
                    mm(Y[:, :256], qb[:, rr, :], k_[:, 0:256], True, True, [("qbd", p), ("k", p)], [yk])
                    if ctxrow:
                        fw.op("act", lambda e: e.copy(out=S_[:, 0:256], in_=Y[:, :256]), reads=[yk], writes=[sk])
                    else:
                        mm(X, qb[:, rr, :], k_[:, TC + r0 * 64:TC + r0 * 64 + 512], True, True, [("qbd", p), ("k", p)], [xk])
                        fw.op("dve", lambda e: e.tensor_tensor(out=S_[:, 0:512], in0=X, in1=bt[:, dl, :], op=ALU.add),
                              reads=[xk, ("bt", p)], writes=[sk])
                        fw.op("act", lambda e: e.copy(out=S_[:, 512:768], in_=Y[:, :256]), reads=[yk], writes=[sk])
                    fw.op("dve", lambda e: e.tensor_reduce(out=mx[a3], in_=S_[:, :W], axis=AX.X, op=ALU.max, negate=True),
                          reads=[sk], writes=[("mx", a3)])
                    fw.op("act", lambda e: e.activation(out=Pb[a3][:, :W], in_=S_[:, :W], func=AF.Exp, bias=mx[a3]),
                          reads=[sk, ("mx", a3)], writes=[("Pb", a3)])

                def stB(rr):
                    ctxrow, W, nkt, r0, dl = rowinfo(rr)
                    a = rr % 2
                    a3 = rr % 3
                    psb = PSB[a]
                    pbk = ("psb", a)
                    for kt in range(nkt):
                        fw.op("pe", lambda e: e.transpose(out=psb[:, kt * 128:(kt + 1) * 128], in_=Pb[a3][:, kt * 128:(kt + 1) * 128], identity=ident_b),
                              reads=[("Pb", a3), "ident_b"], writes=[pbk])
                    ptv = PT[a3].rearrange("p k q -> p (k q)")
                    ptk = ("PT", a3)
                    if rr % 2 == 0:
                        fw.op("act", lambda e: e.copy(out=ptv[:, :nkt * 128], in_=psb[:, :nkt * 128]), reads=[pbk], writes=[ptk])
                    else:
                        fw.op("dve", lambda e: e.tensor_copy(out=ptv[:, :nkt * 128], in_=psb[:, :nkt * 128]), reads=[pbk], writes=[ptk])

                def stC(rr):
                    ctxrow, W, nkt, r0, dl = rowinfo(rr)
                    a = rr % 2
                    a3 = rr % 3
                    pt = PT[a3]
                    ptk = ("PT", a3)
                    if ctxrow:
                        vts = [ve[:, 0, :], ve[:, 1, :]]
                        vks = [("ve", p)]
                    else:
                        if r0 % 2 == 0:
                            vts = [ve[:, 2 + r0 // 2 + kt, :] for kt in range(4)]
                        else:
                            vts = [vo[:, (r0 - 1) // 2 + kt, :] for kt in range(4)]
                        vts += [ve[:, 0, :], ve[:, 1, :]]
                        vks = [("ve", p), ("vo", p)]
                    O = PS[4 + a][:, 0:128]
                    Dn = PS[4 + a][:, 128:256]
                    ok = ("O", a)
                    for kt in range(nkt):
                        mm(O, vts[kt], pt[:, kt, :], kt == 0, kt == nkt - 1, vks + [ptk], [ok])
                    for kt in range(nkt):
                        mm(Dn, ones_b, pt[:, kt, :], kt == 0, kt == nkt - 1, ["ones_b", ptk], [ok])
                    fw.op("dve", lambda e: e.reciprocal(out=rec[a], in_=Dn), reads=[ok], writes=[("rec", a)])
                    fw.op("dve", lambda e: e.tensor_tensor(out=o_[0:64, rr * 64:(rr + 1) * 64], in0=O[0:64, 0:64], in1=rec[a][0:64, 0:64], op=ALU.mult),
                          reads=[ok, ("rec", a)], writes=[("ost", p)])
                    fw.op("dve", lambda e: e.tensor_tensor(out=o_[64:128, rr * 64:(rr + 1) * 64], in0=O[64:128, 64:128], in1=rec[a][64:128, 64:128], op=ALU.mult),
                          reads=[ok, ("rec", a)], writes=[("ost", p)])

                for t in range(36 + 2):
                    if SPARSE and t % 6 == 0 and t < 36:
                        wc_load(wcu[0])
                    if SPARSE and t % 6 == 3 and t < 36:
                        wc_store(wcu[0])
                        wcu[0] += 1
                    if t < 36:
                        stA(t)
                    if 0 <= t - 1 < 36:
                        stB(t - 1)
                    if 0 <= t - 2 < 36:
                        stC(t - 2)
                fw.dma("pool", fmaj["BinT"][b, g], o_, reads=[("ost", p)], writes=[("Bin", b, g)])
        P.end()

    def phase_lru(l):
        P = Phase("lru")
        wg = P.sb([128, 32, 128], BF16)
        fw.dma("pool", wg[:, 0:16, :], I["lru_wa"][l].rearrange("r k c d -> c (r k) d"), writes=["wg"])
        fw.dma("pool", wg[:, 16:32, :], I["lru_wx"][l].rearrange("r k c d -> c (r k) d"), writes=["wg"])
        cw = P.sb([128, 8, 4], F32)
        cb = P.sb([128, 8], F32)
        ba = P.sb([128, 2, 8], F32)
        bx = P.sb([128, 2, 8], F32)
        lam = P.sb([128, 2, 8], F32)
        fw.dma("sp", cw, I["convwT"][l], writes=["cw"])
        fw.dma("sp", cb, I["convbT"][l], writes=["cb"])
        fw.dma("sp", ba, I["lru_baT"][l], writes=["ba"])
        fw.dma("sp", bx, I["lru_bxT"][l], writes=["bx"])
        fw.dma("sp", lam, I["lru_lamT"][l], writes=["lam"])
        fw.op("act", lambda e: e.activation(out=lam, in_=lam, func=AF.Exp, scale=-1.0), reads=["lam"], writes=["lam"])
        fw.op("act", lambda e: e.activation(out=lam, in_=lam, func=AF.Ln, bias=1.0), reads=["lam"], writes=["lam"])
        fw.op("dve", lambda e: e.tensor_scalar(out=lam, in0=lam, scalar1=-8.0, scalar2=None, op0=ALU.mult), reads=["lam"], writes=["lam"])
        xs = [P.sb([128, T], F32) for _ in range(2)]
        gy = [P.sb([128, T], BF16) for _ in range(2)]
        u = P.sb([128, T], F32)
        ub = P.sb([128, T], BF16)
        r_ = P.sb([128, T], F32)
        i_ = P.sb([128, T], F32)
        a_ = P.sb([128, T], F32)
        s_ = P.sb([128, T], F32)
        hf = P.sb([128, T], F32)
        hb = P.sb([128, T], F32)
        oc = [P.sb([128, T], BF16) for _ in range(2)]
        it = 0
        psi = 0
        segs = [(0, TC), (TC, T)]
        for b in range(NBC):
            for k in range(8):
                p = it % 2
                it += 1
                x_ = xs[p]
                fw.dma("sp", x_, lxT[b, k], writes=[("x", p)])
                fw.dma("sp", gy[p], fmaj["lyT"][b, k], writes=[("gy", p)])
                fw.op("dve", lambda e: e.tensor_scalar(out=u, in0=x_, scalar1=cw[:, k, 1:2], scalar2=cb[:, k:k + 1], op0=ALU.mult, op1=ALU.add),
                      reads=[("x", p), "cw", "cb"], writes=["u"])
                for (s0, s1) in segs:
                    fw.op("dve", lambda e: e.scalar_tensor_tensor(out=u[:, s0 + 1:s1], in0=x_[:, s0:s1 - 1], scalar=cw[:, k, 0:1], in1=u[:, s0 + 1:s1],
                                                                  op0=ALU.mult, op1=ALU.add), reads=[("x", p), "u", "cw"], writes=["u"])
                    fw.op("dve", lambda e: e.scalar_tensor_tensor(out=u[:, s0:s1 - 1], in0=x_[:, s0 + 1:s1], scalar=cw[:, k, 2:3], in1=u[:, s0:s1 - 1],
                                                                  op0=ALU.mult, op1=ALU.add), reads=[("x", p), "u", "cw"], writes=["u"])
                    fw.op("dve", lambda e: e.scalar_tensor_tensor(out=u[:, s0:s1 - 2], in0=x_[:, s0 + 2:s1], scalar=cw[:, k, 3:4], in1=u[:, s0:s1 - 2],
                                                                  op0=ALU.mult, op1=ALU.add), reads=[("x", p), "u", "cw"], writes=["u"])
                fw.op("act", lambda e: e.copy(out=ub, in_=u), reads=["u"], writes=["ub"])
                for dr in range(2):
                    for (t0, N) in BLOCKS:
                        ps = PS[psi % 4]
                        pk = ("ps", psi % 4)
                        psi += 1
                        mm(ps[:, :N], wg[:, dr * 8 + k, :], ub[:, t0:t0 + N], True, True, ["wg", "ub"], [pk])
                        fw.op("act", lambda e: e.activation(out=r_[:, t0:t0 + N], in_=ps[:, :N], func=AF.Sigmoid, bias=ba[:, dr, k:k + 1]),
                              reads=[pk, "ba"], writes=["r_"])
                        ps = PS[psi % 4]
                        pk = ("ps", psi % 4)
                        psi += 1
                        mm(ps[:, :N], wg[:, 16 + dr * 8 + k, :], ub[:, t0:t0 + N], True, True, ["wg", "ub"], [pk])
                        fw.op("act", lambda e: e.activation(out=i_[:, t0:t0 + N], in_=ps[:, :N], func=AF.Sigmoid, bias=bx[:, dr, k:k + 1]),
                              reads=[pk, "bx"], writes=["i_"])
                    fw.op("act", lambda e: e.activation(out=a_, in_=r_, func=AF.Exp, scale=lam[:, dr, k:k + 1]), reads=["r_", "lam"], writes=["a_"])
                    fw.op("act", lambda e: e.activation(out=s_, in_=a_, func=AF.Square), reads=["a_"], writes=["s_"])
                    fw.op("act", lambda e: e.activation(out=s_, in_=s_, func=AF.Sqrt, scale=-1.0, bias=1.0), reads=["s_"], writes=["s_"])
                    fw.op("pool", lambda e: e.tensor_tensor(out=i_, in0=i_, in1=u, op=ALU.mult), reads=["i_", "u"], writes=["i_"])
                    fw.op("dve", lambda e: e.tensor_tensor(out=s_, in0=s_, in1=i_, op=ALU.mult), reads=["s_", "i_"], writes=["s_"])
                    if dr == 0:
                        fw.op("dve", lambda e: e.tensor_tensor_scan(out=hf, data0=a_, data1=s_, initial=0.0, op0=ALU.mult, op1=ALU.add),
                              reads=["a_", "s_"], writes=["hf"])
                    else:
                        fw.op("dve", lambda e: e.tensor_tensor_scan(out=hb[:, 0:TC][:, ::-1], data0=a_[:, 0:TC][:, ::-1], data1=s_[:, 0:TC][:, ::-1],
                                                                    initial=0.0, op0=ALU.mult, op1=ALU.add),
                              reads=["a_", "s_"], writes=["hb"])
                        fw.op("dve", lambda e: e.tensor_tensor_scan(out=hb[:, TC:T][:, ::-1], data0=a_[:, TC:T][:, ::-1], data1=s_[:, TC:T][:, ::-1],
                                                                    initial=hb[:, 0:1], op0=ALU.mult, op1=ALU.add),
                              reads=["a_", "s_", "hb"], writes=["hb"])
                fw.op("pool", lambda e: e.tensor_tensor(out=hf, in0=hf, in1=hb, op=ALU.add), reads=["hf", "hb"], writes=["hf"])
                fw.op("dve", lambda e: e.tensor_tensor(out=oc[p], in0=hf, in1=gy[p], op=ALU.mult), reads=["hf", ("gy", p)], writes=[("oc", p)])
                fw.dma("pool", fmaj["CinT"][b, k], oc[p], reads=[("oc", p)], writes=[("Cin", b, k)])
        P.end()

    def phase_merge(l, xsrc):
        P = Phase("mrg")
        wbr = P.sb([128, 3, 8, 1024], BF16)
        wmo = P.sb([128, 8, 1024], BF16)
        for x in range(3):
            fw.dma("pool", wbr[:, x], I["w_branch"][l, x].rearrange("(j p) n -> p j n", p=128), writes=["wbr"])
        fw.dma("pool", wmo, I["w_mix_out"][l].rearrange("(j p) n -> p j n", p=128), writes=["wmo"])
        NN = 256
        ins = [[P.sb([128, 8, NN], BF16) for _ in range(6)] for _ in range(2)]
        xt = [P.sb([128, 8, NN], F32) for _ in range(2)]
        mixed = P.sb([128, 8, NN], BF16)
        xo = [P.sb([128, 8, NN], F32) for _ in range(2)]
        tA = [P.sb([128, NN], F32) for _ in range(2)]
        tB = [P.sb([128, NN], F32) for _ in range(2)]
        tC = [P.sb([128, NN], F32) for _ in range(2)]
        srcs = ["AinT", "BinT", "CinT", "gaT", "gbT", "gcT"]
        it = 0
        for b in range(NBC):
            for (t0, N) in BLOCKS256:
                p = it % 2
                it += 1
                r = 2 if t0 < TC else b
                for si, nm in enumerate(srcs):
                    fw.dma("sp", ins[p][si], fm(fmaj[nm][b])[:, :, t0:t0 + N], writes=[("in", p, si)])
                fw.dma("sp", xt[p], fm(xsrc[b])[:, :, t0:t0 + N], writes=[("xt", p)])
                for c in range(8):
                    q = c % 2
                    pss = [PS[3 * q + x] for x in range(3)]
                    for x in range(3):
                        for k in range(8):
                            mm(pss[x][:, :N], wbr[:, x, k, c * 128:(c + 1) * 128], ins[p][x][:, k, :], k == 0, k == 7,
                               ["wbr", ("in", p, x)], [("ps", 3 * q + x)])
                    tt = [tA[q], tB[q], tC[q]]
                    for x in range(3):
                        fw.op("dve", lambda e: e.tensor_tensor(out=tt[x], in0=pss[x][:, :N], in1=ins[p][3 + x][:, c, :], op=ALU.mult),
                              reads=[("ps", 3 * q + x), ("in", p, 3 + x)], writes=[("tt", q, x)])
                    fw.op("pool", lambda e: e.tensor_tensor(out=tt[0], in0=tt[0], in1=tt[1], op=ALU.add),
                          reads=[("tt", q, 0), ("tt", q, 1)], writes=[("tt", q, 0)])
                    fw.op("pool", lambda e: e.tensor_tensor(out=mixed[:, c, :], in0=tt[0], in1=tt[2], op=ALU.add),
                          reads=[("tt", q, 0), ("tt", q, 2)], writes=["mixed"])
                for c in range(8):
                    ps = PS[6 + c % 2]
                    pk = ("ps", 6 + c % 2)
                    for k in range(8):
                        mm(ps[:, :N], wmo[:, k, c * 128:(c + 1) * 128], mixed[:, k, :], k == 0, k == 7, ["wmo", "mixed"], [pk])
                    fw.op("dve", lambda e: e.scalar_tensor_tensor(out=xo[p][:, c, :], in0=ps[:, :N], scalar=modT[:, 16 + c, r:r + 1], in1=xt[p][:, c, :],
                                                                  op0=ALU.mult, op1=ALU.add),
                          reads=[pk, "modT", ("xt", p)], writes=[("xo", p)])
                fw.dma("pool", fm(xres[b])[:, :, t0:t0 + N], xo[p], reads=[("xo", p)], writes=[("xres", b, t0)])
        P.end()

    def phase_moepre(l):
        P = Phase("mpre")
        xt = [P.sb([128, 8, 512], F32) for _ in range(2)]
        sq = P.sb([128, 8, 512], F32)
        rstd = P.sb([128, 512], F32)
        tmp = [P.sb([128, 512], F32) for _ in range(2)]
        hf = P.sb([128, 8, 512], F32)
        hb = [P.sb([128, 8, 512], BF16) for _ in range(2)]
        wr = P.sb([128, 8, NE], F32)
        brt = P.sb([128, NE], F32)
        fw.dma("sp", wr, I["w_router"][l].rearrange("(j p) n -> p j n", p=128), writes=["wr"])
        fw.dma("sp", brt, I["b_router"][l].partition_broadcast(128), writes=["brt"])
        lgs = P.sb([128, NE], F32)
        top8 = P.sb([128, 8], F32)
        msk = P.sb([128, NE], F32)
        nmx = P.sb([128, 1], F32)
        ex = P.sb([128, NE], F32)
        ssum = P.sb([128, 1], F32)
        gts = P.sb([128, NE], F32)
        gTs = [P.sb([NE, 512], F32) for _ in range(2)]
        i = 0
        for b in range(NBC):
            for (t0, N) in BLOCKS:
                x_ = xt[i % 2]
                xk = ("xt", i % 2)
                hb_ = hb[i % 2]
                hbk = ("hb", i % 2)
                gt_ = gTs[i % 2]
                gtk = ("gTs", i % 2)
                i += 1
                fw.dma("sp", x_[:, :, :N], fm(xres[b])[:, :, t0:t0 + N], writes=[xk])
                r = 2 if t0 < TC else b

                def out_fn(j, x_=x_, xk=xk, N=N, r=r):
                    tm = tmp[j % 2]
                    fw.op("dve", lambda e: e.scalar_tensor_tensor(out=tm[:, :N], in0=x_[:, j, :N], scalar=mul2[:, j, r:r + 1],
                                                                  in1=rstd[:, :N], op0=ALU.mult, op1=ALU.mult),
                          reads=[xk, "rstd", "mul2"], writes=[("tmp", j % 2)])
                    fw.op("act", lambda e: e.activation(out=hf[:, j, :N], in_=tm[:, :N], func=AF.Identity, bias=modT[:, 24 + j, r:r + 1]),
                          reads=[("tmp", j % 2), "modT"], writes=["hf"])
                emit_norm(x_, N, xk, sq, rstd, mul2, r, out_fn)
                fw.op("pool", lambda e: e.tensor_copy(out=hb_[:, :, :N], in_=hf[:, :, :N]), reads=["hf"], writes=[hbk])
                fw.dma("sp", fm(fmaj["hT2"][b])[:, :, t0:t0 + N], hb_[:, :, :N], reads=[hbk], writes=[("hT2", b, t0)])
                for tt in range(N // 128):
                    lp = PS[1][:, tt * 32:(tt + 1) * 32]
                    for j in range(8):
                        mm(lp, hf[:, j, tt * 128:(tt + 1) * 128], wr[:, j, :], j == 0, j == 7, ["hf", "wr"], [("ps", 1)])
                    fw.op("dve", lambda e: e.tensor_tensor(out=lgs, in0=lp, in1=brt, op=ALU.add), reads=[("ps", 1), "brt"], writes=["lgs"])
                    fw.op("dve", lambda e: e.max(out=top8, in_=lgs), reads=["lgs"], writes=["top8"])
                    fw.op("dve", lambda e: e.tensor_scalar(out=msk, in0=lgs, scalar1=top8[:, 3:4], scalar2=None, op0=ALU.is_ge),
                          reads=["lgs", "top8"], writes=["msk"])
                    fw.op("dve", lambda e: e.tensor_scalar(out=nmx, in0=top8[:, 0:1], scalar1=-1.0, scalar2=None, op0=ALU.mult),
                          reads=["top8"], writes=["nmx"])
                    fw.op("act", lambda e: e.activation(out=ex, in_=lgs, func=AF.Exp, bias=nmx), reads=["lgs", "nmx"], writes=["ex"])
                    fw.op("dve", lambda e: e.tensor_tensor(out=ex, in0=ex, in1=msk, op=ALU.mult), reads=["ex", "msk"], writes=["ex"])
                    fw.op("dve", lambda e: e.tensor_reduce(out=ssum, in_=ex, axis=AX.X, op=ALU.add), reads=["ex"], writes=["ssum"])
                    fw.op("dve", lambda e: e.reciprocal(out=ssum, in_=ssum), reads=["ssum"], writes=["ssum"])
                    fw.op("dve", lambda e: e.tensor_scalar(out=gts, in0=ex, scalar1=ssum, scalar2=None, op0=ALU.mult),
                          reads=["ex", "ssum"], writes=["gts"])
                    fw.op("pe", lambda e: e.transpose(out=PS[2][0:NE, tt * 128:(tt + 1) * 128], in_=gts, identity=ident_f),
                          reads=["gts", "ident_f"], writes=[("ps", 2)])
                fw.op("act", lambda e: e.copy(out=gt_[:, :N], in_=PS[2][0:NE, :N]), reads=[("ps", 2)], writes=[gtk])
                fw.dma("sp", gTd[:, b * T + t0:b * T + t0 + N], gt_[:, :N], reads=[gtk], writes=[("gTd", b, t0)])
        P.end()

    def phase_moe(l):
        P = Phase("moe")
        wup = [P.sb([128, 8, 2048], BF16) for _ in range(2)]
        wdn = [P.sb([128, 8, 1024], BF16) for _ in range(2)]
        hT2 = P.sb([128, 8, GRP], BF16)
        yacc = P.sb([128, 8, GRP], F32)
        gTs = P.sb([NE, GRP], F32)
        sel = P.sb([NE, NE, 128], F32)
        bup = P.sb([128, NE, 2, 8], F32)
        bdn = P.sb([NE, D], F32)
        Ge = [P.sb([128, MB], F32) for _ in range(2)]
        actT = [P.sb([128, 8, MB], BF16) for _ in range(2)]
        tg = [P.sb([128, MB], F32) for _ in range(2)]
        tsg = [P.sb([128, MB], F32) for _ in range(2)]
        tl = [P.sb([128, MB], F32) for _ in range(2)]
        fw.dma("sp", bup, I["bupT"][l], writes=["bup"])
        fw.dma("sp", bdn, I["b_down"][l], writes=["bdn"])
        fw.op("dve", lambda e: e.tensor_scalar(out=bup[:, :, 1, :], in0=bup[:, :, 1, :], scalar1=1.0, scalar2=None, op0=ALU.add),
              reads=["bup"], writes=["bup"])
        fw.op("pool", lambda e: e.memset(sel, 0.0), writes=["sel"])
        fw.op("pool", lambda e: e.affine_select(out=sel, in_=sel, pattern=[[-1, NE], [0, 128]],
                                                compare_op=ALU.not_equal, fill=1.0, base=0, channel_multiplier=1),
              reads=["sel"], writes=["sel"])
        ngrp = NBC * T // GRP
        nblk = GRP // MB

        def loadw(i):
            e_ = i % NE
            fw.dma("pool", wup[i % 2], I["w_up"][l, e_].rearrange("(j p) n -> p j n", p=128), writes=[("wup", i % 2)])

        def loadwd(i):
            e_ = i % NE
            fw.dma("pool", wdn[i % 2], I["w_down"][l, e_].rearrange("(j p) n -> p j n", p=128), writes=[("wdn", i % 2)])
        loadw(0)
        loadwd(0)
        ci = 0
        for g in range(ngrp):
            b = g // 2
            g0 = (g % 2) * GRP
            fw.dma("sp", hT2, fm(fmaj["hT2"][b])[:, :, g0:g0 + GRP], writes=["hT2"])
            fw.dma("sp", gTs, gTd[:, b * T + g0:b * T + g0 + GRP], writes=["gTs"])
            for blk in range(nblk):
                for co in range(8):
                    ps = PS[4 + co % 2]
                    pk = ("ps", 4 + co % 2)
                    mm(ps[:, :MB], bdn[:, co * 128:(co + 1) * 128], gTs[:, blk * MB:(blk + 1) * MB], True, True, ["bdn", "gTs"], [pk])
                    fw.op("act", lambda e: e.copy(out=yacc[:, co, blk * MB:(blk + 1) * MB], in_=ps[:, :MB]), reads=[pk], writes=[("yacc", blk)])
            tasks = [(ex, blk) for ex in range(NE) for blk in range(nblk)]

            def up(ti):
                nonlocal ci
                ex, blk = tasks[ti]
                wix = wbase + ex
                wu = wup[wix % 2]
                wuk = ("wup", wix % 2)
                if blk == 0 and wix + 1 < ngrp * NE:
                    loadw(wix + 1)
                tsl = slice(blk * MB, (blk + 1) * MB)
                ge = Ge[ti % 2]
                gek = ("Ge", ti % 2)
                at = actT[ti % 2]
                atk = ("actT", ti % 2)
                mm(PS[6][:, :MB], sel[:, ex, :], gTs[:, tsl], True, True, ["sel", "gTs"], [("ps", 6)])
                fw.op("act", lambda e: e.copy(out=ge, in_=PS[6][:, :MB]), reads=[("ps", 6)], writes=[gek])
                for c in range(8):
                    q = ci % 2
                    ci += 1
                    pg = PS[2 * q]
                    pl = PS[2 * q + 1]
                    pgk = ("ps", 2 * q)
                    plk = ("ps", 2 * q + 1)
                    for k in range(8):
                        mm(pg[:, :MB], wu[:, k, c * 256:(c + 1) * 256:2], hT2[:, k, tsl], k == 0, k == 7, [wuk, "hT2"], [pgk])
                    for k in range(8):
                        mm(pl[:, :MB], wu[:, k, c * 256 + 1:(c + 1) * 256:2], hT2[:, k, tsl], k == 0, k == 7, [wuk, "hT2"], [plk])
                    g_ = tg[q]
                    s_ = tsg[q]
                    l_ = tl[q]
                    fw.op("dve", lambda e: e.tensor_scalar(out=g_, in0=pg[:, :MB], scalar1=bup[:, ex, 0, c:c + 1], scalar2=7.0, op0=ALU.add, op1=ALU.min),
                          reads=[pgk, "bup"], writes=[("tg", q)])
                    fw.op("act", lambda e: e.activation(out=s_, in_=g_, func=AF.Sigmoid, scale=1.702), reads=[("tg", q)], writes=[("tsg", q)])
                    fw.op("dve", lambda e: e.tensor_scalar(out=l_, in0=pl[:, :MB], scalar1=bup[:, ex, 1, c:c + 1], scalar2=8.0, op0=ALU.add, op1=ALU.min),
                          reads=[plk, "bup"], writes=[("tl", q)])
                    fw.op("dve", lambda e: e.scalar_tensor_tensor(out=l_, in0=l_, scalar=-6.0, in1=ge, op0=ALU.max, op1=ALU.mult),
                          reads=[("tl", q), gek], writes=[("tl", q)])
                    fw.op("pool", lambda e: e.tensor_tensor(out=g_, in0=g_, in1=s_, op=ALU.mult), reads=[("tg", q), ("tsg", q)], writes=[("tg", q)])
                    fw.op("pool", lambda e: e.tensor_tensor(out=at[:, c, :], in0=g_, in1=l_, op=ALU.mult), reads=[("tg", q), ("tl", q)], writes=[atk])

            def down(ti):
                ex, blk = tasks[ti]
                wix = wbase + ex
                wd = wdn[wix % 2]
                wdk = ("wdn", wix % 2)
                if blk == 0 and wix + 1 < ngrp * NE:
                    loadwd(wix + 1)
                tsl = slice(blk * MB, (blk + 1) * MB)
                at = actT[ti % 2]
                atk = ("actT", ti % 2)
                for co in range(8):
                    ps = PS[4 + co % 2]
                    pk = ("ps", 4 + co % 2)
                    for c in range(8):
                        mm(ps[:, :MB], wd[:, c, co * 128:(co + 1) * 128], at[:, c, :], c == 0, c == 7, [wdk, atk], [pk])
                    fw.op("dve", lambda e: e.tensor_tensor(out=yacc[:, co, tsl], in0=ps[:, :MB], in1=yacc[:, co, tsl], op=ALU.add),
                          reads=[pk, ("yacc", blk)], writes=[("yacc", blk)])

            wbase = g * NE
            up(0)
            for ti in range(len(tasks)):
                if ti + 1 < len(tasks):
                    up(ti + 1)
                down(ti)
            fw.dma("sp", fm(yT[b])[:, :, g0:g0 + GRP], yacc, reads=[("yacc", blk) for blk in range(nblk)], writes=[("yT", g)])
        P.end()

    def phase_moepost(l, last):
        P = Phase("mpost")
        xt = [P.sb([128, 8, 512], F32) for _ in range(2)]
        yt = [P.sb([128, 8, 512], F32) for _ in range(2)]
        sq = P.sb([128, 8, 512], F32)
        rstd = P.sb([128, 512], F32)
        oo = [P.sb([128, 8, 512], F32) for _ in range(2)]
        i = 0
        for b in range(NBC):
            for (t0, N) in BLOCKS:
                if last and t0 < TC:
                    continue
                p = i % 2
                i += 1
                r = 2 if t0 < TC else b
                fw.dma("sp", xt[p][:, :, :N], fm(xres[b])[:, :, t0:t0 + N], writes=[("xt", p)])
                fw.dma("sp", yt[p][:, :, :N], fm(yT[b])[:, :, t0:t0 + N], writes=[("yt", p)])
                for c in range(8):
                    fw.op("dve", lambda e: e.scalar_tensor_tensor(out=xt[p][:, c, :N], in0=yt[p][:, c, :N], scalar=modT[:, 40 + c, r:r + 1],
                                                                  in1=xt[p][:, c, :N], op0=ALU.mult, op1=ALU.add),
                          reads=[("xt", p), ("yt", p), "modT"], writes=[("xt", p)])
                if not last:
                    fw.dma("sp", fm(xres[b])[:, :, t0:t0 + N], xt[p][:, :, :N], reads=[("xt", p)], writes=[("xres", b, t0)])
                else:
                    def out_fn(j, p=p, N=N):
                        fw.op("dve", lambda e: e.scalar_tensor_tensor(out=oo[p][:, j, :N], in0=xt[p][:, j, :N], scalar=fng[:, j:j + 1],
                                                                      in1=rstd[:, :N], op0=ALU.mult, op1=ALU.mult),
                              reads=[("xt", p), "rstd", "fng"], writes=[("oo", p)])
                    emit_norm(xt[p], N, ("xt", p), sq, rstd, None, r, out_fn)
                    fw.dma("sp", fm(outT[b])[:, :, t0 - TC:t0 - TC + N], oo[p][:, :, :N], reads=[("oo", p)], writes=[("out", b, t0)])
        P.end()


    IOA = bass.IndirectOffsetOnAxis

    def phase_moepre_sparse(l):
        P = Phase("spre")
        xt = [P.sb([128, 8, 512], F32) for _ in range(2)]
        sq = P.sb([128, 8, 512], F32)
        rstd = P.sb([128, 512], F32)
        tmp = [P.sb([128, 512], F32) for _ in range(2)]
        hf = P.sb([128, 8, 512], F32)
        hb = P.sb([128, 8, 512], BF16)
        htk = [P.sb([128, 1024], BF16) for _ in range(2)]
        wr = P.sb([128, 8, NE], F32)
        brt = P.sb([128, NE], F32)
        fw.dma("sp", wr, I["w_router"][l].rearrange("(j p) n -> p j n", p=128), writes=["wr"])
        fw.dma("sp", brt, I["b_router"][l].partition_broadcast(128), writes=["brt"])
        G_all = P.sb([128, 36, NE], F32)
        M_all = P.sb([128, 36, NE], F32)
        lgs = P.sb([128, NE], F32)
        top8 = P.sb([128, 8], F32)
        nmx = P.sb([128, 1], F32)
        ex = P.sb([128, NE], F32)
        ssum = P.sb([128, 1], F32)
        zt = P.sb([128, 1024], BF16)
        zf = P.sb([128, NE], F32)
        fill = P.sb([128, NSLOT * 2 // 128], I32)
        Lm = P.sb([128, 128], F32)
        pj = P.sb([128, 8], F32)
        tokid = P.sb([128, 36, 2], I32)
        fw.dma("sp", Lm, I["Lmat"], writes=["Lm"])
        fw.dma("sp", pj, I["pj"], writes=["pj"])
        fw.dma("sp", tokid, I["tokid"], writes=["tokid"])
        fw.op("pool", lambda e: e.memset(zt, 0.0), writes=["zt"])
        fw.op("pool", lambda e: e.memset(zf, 0.0), writes=["zf"])
        fw.op("pool", lambda e: e.memset(fill, NTOK), writes=["fill"])
        fw.dma("sp", h2tok[NTOK:NTOK + 128, :], zt, reads=["zt"], writes=["h2z"])
        fw.dma("sp", gtab[NTOK:NTOK + 128, :], zf, reads=["zf"], writes=["gtz"])
        fw.dma("sp", slot_tok.rearrange("(p a) c -> p (a c)", p=128), fill, reads=["fill"], writes=["stfill"])
        i = 0
        for b in range(NBC):
            for (t0, N) in BLOCKS:
                x_ = xt[i % 2]
                xk = ("xt", i % 2)
                i += 1
                fw.dma("sp", x_[:, :, :N], fm(xres[b])[:, :, t0:t0 + N], writes=[xk])
                r = 2 if t0 < TC else b

                def out_fn(j, x_=x_, xk=xk, N=N, r=r):
                    tm = tmp[j % 2]
                    fw.op("dve", lambda e: e.scalar_tensor_tensor(out=tm[:, :N], in0=x_[:, j, :N], scalar=mul2[:, j, r:r + 1],
                                                                  in1=rstd[:, :N], op0=ALU.mult, op1=ALU.mult),
                          reads=[xk, "rstd", "mul2"], writes=[("tmp", j % 2)])
                    fw.op("act", lambda e: e.activation(out=hf[:, j, :N], in_=tm[:, :N], func=AF.Identity, bias=modT[:, 24 + j, r:r + 1]),
                          reads=[("tmp", j % 2), "modT"], writes=["hf"])
                emit_norm(x_, N, xk, sq, rstd, mul2, r, out_fn)
                fw.op("pool", lambda e: e.tensor_copy(out=hb[:, :, :N], in_=hf[:, :, :N]), reads=["hf"], writes=["hb"])
                for tt in range(N // 128):
                    gi = b * 18 + t0 // 128 + tt
                    a = gi % 2
                    for j in range(8):
                        fw.op("pe", lambda e: e.transpose(out=PSB[a][:, j * 128:(j + 1) * 128], in_=hb[:, j, tt * 128:(tt + 1) * 128], identity=ident_b),
                              reads=["hb", "ident_b"], writes=[("psb", a)])
                    fw.op("act", lambda e: e.copy(out=htk[a], in_=PSB[a]), reads=[("psb", a)], writes=[("htk", a)])
                    fw.dma("pool", h2tok[gi * 128:(gi + 1) * 128, :], htk[a], reads=[("htk", a)], writes=[("h2tok", gi)])
                    lp = PS[1][:, (tt % 4) * 32:(tt % 4) * 32 + 32]
                    for j in range(8):
                        mm(lp, hf[:, j, tt * 128:(tt + 1) * 128], wr[:, j, :], j == 0, j == 7, ["hf", "wr"], [("ps", 1)])
                    fw.op("dve", lambda e: e.tensor_tensor(out=lgs, in0=lp, in1=brt, op=ALU.add), reads=[("ps", 1), "brt"], writes=["lgs"])
                    fw.op("dve", lambda e: e.max(out=top8, in_=lgs), reads=["lgs"], writes=["top8"])
                    fw.op("dve", lambda e: e.tensor_scalar(out=M_all[:, gi, :], in0=lgs, scalar1=top8[:, 3:4], scalar2=None, op0=ALU.is_ge),
                          reads=["lgs", "top8"], writes=[("M", gi)])
                    fw.op("dve", lambda e: e.tensor_scalar(out=nmx, in0=top8[:, 0:1], scalar1=-1.0, scalar2=None, op0=ALU.mult),
                          reads=["top8"], writes=["nmx"])
                    fw.op("act", lambda e: e.activation(out=ex, in_=lgs, func=AF.Exp, bias=nmx), reads=["lgs", "nmx"], writes=["ex"])
                    fw.op("dve", lambda e: e.tensor_tensor(out=ex, in0=ex, in1=M_all[:, gi, :], op=ALU.mult), reads=["ex", ("M", gi)], writes=["ex"])
                    fw.op("dve", lambda e: e.tensor_reduce(out=ssum, in_=ex, axis=AX.X, op=ALU.add), reads=["ex"], writes=["ssum"])
                    fw.op("dve", lambda e: e.reciprocal(out=ssum, in_=ssum), reads=["ssum"], writes=["ssum"])
                    fw.op("dve", lambda e: e.tensor_scalar(out=G_all[:, gi, :], in0=ex, scalar1=ssum, scalar2=None, op0=ALU.mult),
                          reads=["ex", "ssum"], writes=[("G", gi)])
                    fw.dma("pool", gtab[gi * 128:(gi + 1) * 128, :], G_all[:, gi, :], reads=[("G", gi)], writes=[("gtab", gi)])
        cnt = P.sb([128, NE], F32)
        ntl = P.sb([128, NE], F32)
        cume = P.sb([128, NE], F32)
        cb = P.sb([128, NE], F32)
        sf = P.sb([128, NE], F32)
        t8 = P.sb([128, 8], F32)
        cm = P.sb([128, NE], F32)
        wf = P.sb([128, NTILE, 8], F32)
        bf_ = P.sb([128, NTILE], F32)
        for gi in range(36):
            mm(PS[3][:, 0:NE], ones_f, M_all[:, gi, :], gi == 0, gi == 35, ["ones_f", ("M", gi)], [("ps", 3)])
        fw.op("dve", lambda e: e.tensor_copy(out=cnt, in_=PS[3][:, 0:NE]), reads=[("ps", 3)], writes=["cnt"])
        fw.op("dve", lambda e: e.tensor_scalar(out=ntl, in0=cnt, scalar1=0.0, scalar2=None, op0=ALU.is_gt), reads=["cnt"], writes=["ntl"])
        for m in range(1, NTOK // MT + 1):
            fw.op("dve", lambda e: e.scalar_tensor_tensor(out=ntl, in0=cnt, scalar=float(m * MT), in1=ntl, op0=ALU.is_gt, op1=ALU.add),
                  reads=["cnt", "ntl"], writes=["ntl"])
        fw.op("dve", lambda e: e.tensor_tensor_scan(out=cume, data0=ones_f[:, 0:NE], data1=ntl, initial=0.0, op0=ALU.mult, op1=ALU.add),
              reads=["ntl", "ones_f"], writes=["cume"])
        fw.op("dve", lambda e: e.tensor_tensor(out=cb, in0=cume, in1=ntl, op=ALU.subtract), reads=["cume", "ntl"], writes=["cb"])
        fw.op("dve", lambda e: e.tensor_scalar(out=cb, in0=cb, scalar1=float(MT), scalar2=None, op0=ALU.mult), reads=["cb"], writes=["cb"])
        for gi in range(36):
            mm(PS[4][:, 0:NE], Lm, M_all[:, gi, :], True, True, ["Lm", ("M", gi)], [("ps", 4)])
            mm(PS[5][:, 0:NE], ones_f, M_all[:, gi, :], True, True, ["ones_f", ("M", gi)], [("ps", 5)])
            fw.op("dve", lambda e: e.tensor_tensor(out=sf, in0=PS[4][:, 0:NE], in1=cb, op=ALU.add), reads=[("ps", 4), "cb"], writes=["sf"])
            fw.op("dve", lambda e: e.scalar_tensor_tensor(out=sf, in0=sf, scalar=1.0, in1=M_all[:, gi, :], op0=ALU.add, op1=ALU.mult),
                  reads=["sf", ("M", gi)], writes=["sf"])
            fw.op("dve", lambda e: e.tensor_scalar(out=sf, in0=sf, scalar1=-1.0, scalar2=None, op0=ALU.add), reads=["sf"], writes=["sf"])
            fw.op("dve", lambda e: e.max(out=t8, in_=sf), reads=["sf"], writes=["t8"])
            fw.op("dve", lambda e: e.tensor_copy(out=S4_all[:, gi, :], in_=t8[:, 0:4]), reads=["t8"], writes=[("S4", gi)])
            fw.op("dve", lambda e: e.tensor_tensor(out=cb, in0=cb, in1=PS[5][:, 0:NE], op=ALU.add), reads=["cb", ("ps", 5)], writes=["cb"])
            for k in range(4):
                fw.idma(slot_tok, IOA(ap=S4_all[:, gi, k:k + 1], axis=0), tokid[:, gi, :], None,
                        reads=[("S4", gi), "tokid", "stfill"], writes=[("st", gi, k)])
        for t in range(NTILE):
            fw.op("dve", lambda e: e.tensor_scalar(out=cm, in0=cume, scalar1=float(t), scalar2=None, op0=ALU.is_le), reads=["cume"], writes=["cm"])
            fw.op("dve", lambda e: e.tensor_reduce(out=ETf[:, t:t + 1], in_=cm, axis=AX.X, op=ALU.add), reads=["cm"], writes=["ETf"])
        fw.op("dve", lambda e: e.tensor_scalar(out=ETf, in0=ETf, scalar1=float(NE - 1), scalar2=None, op0=ALU.min), reads=["ETf"], writes=["ETf"])
        fw.op("dve", lambda e: e.tensor_scalar(out=bf_, in0=ETf, scalar1=float(l * NE), scalar2=None, op0=ALU.add), reads=["ETf"], writes=["bf_"])
        fw.op("dve", lambda e: e.tensor_copy(out=eidx, in_=bf_), reads=["bf_"], writes=["eidx"])
        fw.op("dve", lambda e: e.tensor_scalar(out=bf_, in0=ETf, scalar1=128.0, scalar2=pj[:, 0:1], op0=ALU.mult, op1=ALU.add),
              reads=["ETf", "pj"], writes=["bf_"])
        fw.op("dve", lambda e: e.tensor_copy(out=pidx, in_=bf_), reads=["bf_"], writes=["pidx"])
        fw.op("dve", lambda e: e.tensor_scalar(out=bf_, in0=bf_, scalar1=float(l * NE * 128), scalar2=None, op0=ALU.add), reads=["bf_"], writes=["bf_"])
        fw.op("dve", lambda e: e.tensor_copy(out=bidx, in_=bf_), reads=["bf_"], writes=["bidx"])
        for j in range(8):
            fw.op("dve", lambda e: e.tensor_scalar(out=wf[:, :, j], in0=ETf, scalar1=1024.0, scalar2=pj[:, j:j + 1], op0=ALU.mult, op1=ALU.add),
                  reads=["ETf", "pj"], writes=["wf"])
        fw.op("dve", lambda e: e.tensor_scalar(out=wf, in0=wf, scalar1=float(l * NE * 1024), scalar2=None, op0=ALU.add), reads=["wf"], writes=["wf"])
        fw.op("dve", lambda e: e.tensor_copy(out=widx, in_=wf), reads=["wf"], writes=["widx"])
        P.end()

    def phase_wcast(l):
        P = Phase("wcast")
        bu = [P.sb([128, 8, 2048], BF16) for _ in range(3)]
        bd = [P.sb([128, 8, 1024], BF16) for _ in range(3)]
        for ex in range(NE):
            s_ = ex % 3
            fw.dma("pool", bu[s_], I["w_up"][l, ex].rearrange("(j p) n -> p j n", p=128), writes=[("bu", s_)])
            fw.dma("sp", wupb[ex * 128:(ex + 1) * 128, :], bu[s_].rearrange("p j n -> p (j n)"), reads=[("bu", s_)], writes=[("wupb", ex)])
            fw.dma("pool", bd[s_], I["w_down"][l, ex].rearrange("(j p) n -> p j n", p=128), writes=[("bd", s_)])
            fw.dma("sp", wdnb[ex * 128:(ex + 1) * 128, :], bd[s_].rearrange("p j n -> p (j n)"), reads=[("bd", s_)], writes=[("wdnb", ex)])
        P.end()

    def phase_moe_sparse(l):
        P = Phase("smoe")
        wup = [P.sb([128, 8, 2048], BF16) for _ in range(2)]
        wdn = [P.sb([128, 8, 1024], BF16) for _ in range(2)]
        bupg = [P.sb([128, 16], F32) for _ in range(2)]
        bdng = [P.sb([128, 1024], F32) for _ in range(2)]
        stok = [P.sb([128, 4, 2], I32) for _ in range(2)]
        htk = [[P.sb([128, 1024], BF16) for _ in range(4)] for _ in range(2)]
        grow = [P.sb([128, 4, NE], F32) for _ in range(2)]
        hsT = [P.sb([128, 8, MT], BF16) for _ in range(2)]
        at = [P.sb([128, 8, MT], BF16) for _ in range(2)]
        oh = P.sb([128, NE], F32)
        gtmp = P.sb([128, 4, NE], F32)
        gsl = [P.sb([128, 4], F32) for _ in range(2)]
        tg = [P.sb([128, MT], F32) for _ in range(2)]
        tsg = [P.sb([128, MT], F32) for _ in range(2)]
        tl = [P.sb([128, MT], F32) for _ in range(2)]
        ytmp = [P.sb([128, 512], F32) for _ in range(2)]
        yrow = [P.sb([128, 1024], F32) for _ in range(2)]
        iota = P.sb([128, NE], F32)
        fw.dma("sp", iota, I["iota32"], writes=["iota"])
        wv = I["w_up"].rearrange("l e r n -> (l e r) n")
        wdv = I["w_down"].rearrange("l e r n -> (l e r) n")
        bupv = I["bup2"].rearrange("l e p n -> (l e p) n")
        bdv = I["b_down"].rearrange("l e n -> (l e) n")

        def gather(t):
            s_ = t % 2
            fw.dma("sp", stok[s_], slot_tok[t * MT:(t + 1) * MT, :].rearrange("(a p) c -> p a c", p=128), writes=[("stok", s_)])
            for a in range(4):
                fw.idma(htk[s_][a], None, h2tok, IOA(ap=stok[s_][:, a, 0:1], axis=0), reads=[("stok", s_)], writes=[("htk", s_, a)])
            for a in range(4):
                fw.idma(grow[s_][:, a, :], None, gtab, IOA(ap=stok[s_][:, a, 0:1], axis=0), reads=[("stok", s_)], writes=[("grow", s_)])
            fw.idma(bupg[s_], None, bupv, IOA(ap=bidx[:, t:t + 1], axis=0), writes=[("bupg", s_)])
            fw.idma(bdng[s_], None, bdv, IOA(ap=eidx[:, t:t + 1], axis=0), writes=[("bdng", s_)])
            fw.idma(wup[s_].rearrange("p j n -> p (j n)"), None, wupb, IOA(ap=pidx[:, t:t + 1], axis=0), writes=[("wup", s_)])
            fw.idma(wdn[s_].rearrange("p j n -> p (j n)"), None, wdnb, IOA(ap=pidx[:, t:t + 1], axis=0), writes=[("wdn", s_)])

        gather(0)
        ci = 0
        yi = 0
        ev = 0
        for t in range(NTILE):
            s_ = t % 2
            if t + 1 < NTILE:
                gather(t + 1)
            fw.op("dve", lambda e: e.tensor_scalar(out=oh, in0=iota, scalar1=ETf[:, t:t + 1], scalar2=None, op0=ALU.is_equal),
                  reads=["iota"], writes=["oh"])
            for a in range(4):
                fw.op("dve", lambda e: e.tensor_tensor(out=gtmp[:, a, :], in0=grow[s_][:, a, :], in1=oh, op=ALU.mult),
                      reads=[("grow", s_), "oh"], writes=["gtmp"])
            fw.op("dve", lambda e: e.tensor_reduce(out=gsl[s_], in_=gtmp, axis=AX.X, op=ALU.add), reads=["gtmp"], writes=[("gsl", s_)])
            fw.op("dve", lambda e: e.tensor_scalar(out=bupg[s_][:, 8:16], in0=bupg[s_][:, 8:16], scalar1=1.0, scalar2=None, op0=ALU.add),
                  reads=[("bupg", s_)], writes=[("bupg", s_)])
            for jp in range(4):
                bank = PSB[jp % 2]
                bk = ("psb", jp % 2)
                for jj in range(2):
                    j = 2 * jp + jj
                    for a in range(4):
                        fw.op("pe", lambda e: e.transpose(out=bank[:, jj * 512 + a * 128:jj * 512 + (a + 1) * 128],
                                                          in_=htk[s_][a][:, j * 128:(j + 1) * 128], identity=ident_b),
                              reads=[("htk", s_, a), "ident_b"], writes=[bk])
                dst = hsT[s_][:, 2 * jp:2 * jp + 2, :].rearrange("p j n -> p (j n)")
                if jp % 2 == 0:
                    fw.op("act", lambda e: e.copy(out=dst, in_=bank), reads=[bk], writes=[("hsT", s_)])
                else:
                    fw.op("dve", lambda e: e.tensor_copy(out=dst, in_=bank), reads=[bk], writes=[("hsT", s_)])
            wu = wup[s_]
            wuk = ("wup", s_)
            for c in range(8):
                q = ci % 2
                ci += 1
                pg = PS[2 * q]
                pl = PS[2 * q + 1]
                pgk = ("ps", 2 * q)
                plk = ("ps", 2 * q + 1)
                for k in range(8):
                    mm(pg, wu[:, k, c * 256:(c + 1) * 256:2], hsT[s_][:, k, :], k == 0, k == 7, [wuk, ("hsT", s_)], [pgk])
                for k in range(8):
                    mm(pl, wu[:, k, c * 256 + 1:(c + 1) * 256:2], hsT[s_][:, k, :], k == 0, k == 7, [wuk, ("hsT", s_)], [plk])
                g_ = tg[q]
                sg_ = tsg[q]
                l_ = tl[q]
                fw.op("dve", lambda e: e.tensor_scalar(out=g_, in0=pg, scalar1=bupg[s_][:, c:c + 1], scalar2=7.0, op0=ALU.add, op1=ALU.min),
                      reads=[pgk, ("bupg", s_)], writes=[("tg", q)])
                fw.op("act", lambda e: e.activation(out=sg_, in_=g_, func=AF.Sigmoid, scale=1.702), reads=[("tg", q)], writes=[("tsg", q)])
                fw.op("act", lambda e: e.activation(out=l_, in_=pl, func=AF.Identity, bias=bupg[s_][:, 8 + c:9 + c]),
                      reads=[plk, ("bupg", s_)], writes=[("tl", q)])
                fw.op("dve", lambda e: e.tensor_scalar(out=l_, in0=l_, scalar1=8.0, scalar2=-6.0, op0=ALU.min, op1=ALU.max),
                      reads=[("tl", q)], writes=[("tl", q)])
                fw.op("dve", lambda e: e.tensor_tensor(out=g_, in0=g_, in1=sg_, op=ALU.mult), reads=[("tg", q), ("tsg", q)], writes=[("tg", q)])
                fw.op("dve", lambda e: e.tensor_tensor(out=at[s_][:, c, :], in0=g_, in1=l_, op=ALU.mult), reads=[("tg", q), ("tl", q)], writes=[("at", s_)])
            wd = wdn[s_]
            wdk = ("wdn", s_)
            for a in range(4):
                yr = yrow[yi % 2]
                yk = ("yrow", yi % 2)
                yi += 1
                for half in range(2):
                    ps = PS[4 + half]
                    pk = ("ps", 4 + half)
                    for c in range(8):
                        mm(ps, at[s_][:, c, a * 128:(a + 1) * 128], wd[:, c, half * 512:(half + 1) * 512], c == 0, c == 7, [("at", s_), wdk], [pk])
                    yt_ = ytmp[half]
                    fw.op("dve", lambda e: e.tensor_tensor(out=yt_, in0=ps, in1=bdng[s_][:, half * 512:(half + 1) * 512], op=ALU.add),
                          reads=[pk, ("bdng", s_)], writes=[("ytmp", half)])
                    fw.op("act", lambda e: e.activation(out=yr[:, half * 512:(half + 1) * 512], in_=yt_, func=AF.Identity, scale=gsl[s_][:, a:a + 1]),
                          reads=[("ytmp", half), ("gsl", s_)], writes=[yk])
                fw.dma("sp", ypairs[t * MT + a * 128:t * MT + (a + 1) * 128, :], yr, reads=[yk], writes=[("yp", t, a)])
        P.end()

    def phase_moepost_sparse(l, last):
        P = Phase("spost")
        xt = [P.sb([128, 8, 512], F32) for _ in range(2)]
        sq = P.sb([128, 8, 512], F32)
        rstd = P.sb([128, 512], F32)
        oo = [P.sb([128, 8, 512], F32) for _ in range(2)]
        yk = [[P.sb([128, 1024], F32) for _ in range(4)] for _ in range(2)]
        i = 0
        si = 0
        for b in range(NBC):
            for (t0, N) in BLOCKS:
                if last and t0 < TC:
                    continue
                p = i % 2
                i += 1
                r = 2 if t0 < TC else b
                fw.dma("sp", xt[p][:, :, :N], fm(xres[b])[:, :, t0:t0 + N], writes=[("xt", p)])
                for sub in range(N // 128):
                    gi = b * 18 + t0 // 128 + sub
                    u = si % 2
                    si += 1
                    for k in range(4):
                        fw.idma(yk[u][k], None, ypairs, IOA(ap=S4_all[:, gi, k:k + 1], axis=0), writes=[("yk", u, k)])
                    fw.op("dve", lambda e: e.tensor_tensor(out=yk[u][0], in0=yk[u][0], in1=yk[u][1], op=ALU.add),
                          reads=[("yk", u, 0), ("yk", u, 1)], writes=[("yk", u, 0)])
                    fw.op("pool", lambda e: e.tensor_tensor(out=yk[u][2], in0=yk[u][2], in1=yk[u][3], op=ALU.add),
                          reads=[("yk", u, 2), ("yk", u, 3)], writes=[("yk", u, 2)])
                    fw.op("dve", lambda e: e.tensor_tensor(out=yk[u][0], in0=yk[u][0], in1=yk[u][2], op=ALU.add),
                          reads=[("yk", u, 0), ("yk", u, 2)], writes=[("yk", u, 0)])
                    for c in range(8):
                        bank = PS[1 + 2 * u + c // 4]
                        fw.op("pe", lambda e: e.transpose(out=bank[:, (c % 4) * 128:(c % 4 + 1) * 128], in_=yk[u][0][:, c * 128:(c + 1) * 128], identity=ident_f),
                              reads=[("yk", u, 0), "ident_f"], writes=[("ps", 1 + 2 * u + c // 4)])
                    for c in range(8):
                        bank = PS[1 + 2 * u + c // 4]
                        fw.op("dve", lambda e: e.scalar_tensor_tensor(out=xt[p][:, c, sub * 128:(sub + 1) * 128], in0=bank[:, (c % 4) * 128:(c % 4 + 1) * 128],
                                                                      scalar=modT[:, 40 + c, r:r + 1], in1=xt[p][:, c, sub * 128:(sub + 1) * 128],
                                                                      op0=ALU.mult, op1=ALU.add),
                              reads=[("ps", 1 + 2 * u + c // 4), ("xt", p), "modT"], writes=[("xt", p)])
                if not last:
                    fw.dma("sp", fm(xres[b])[:, :, t0:t0 + N], xt[p][:, :, :N], reads=[("xt", p)], writes=[("xres", b, t0)])
                else:
                    def out_fn(j, p=p, N=N):
                        fw.op("dve", lambda e: e.scalar_tensor_tensor(out=oo[p][:, j, :N], in0=xt[p][:, j, :N], scalar=fng[:, j:j + 1],
                                                                      in1=rstd[:, :N], op0=ALU.mult, op1=ALU.mult),
                              reads=[("xt", p), "rstd", "fng"], writes=[("oo", p)])
                    emit_norm(xt[p], N, ("xt", p), sq, rstd, None, r, out_fn)
                    fw.dma("sp", fm(outT[b])[:, :, t0 - TC:t0 - TC + N], oo[p][:, :, :N], reads=[("oo", p)], writes=[("out", b, t0)])
        P.end()

    seq = []
    for l in range(nlayers):
        xsrc = I["xin"] if l == 0 else xres
        last = (l == nlayers - 1)
        seq += [("mod", lambda l=l: phase_mod(l)),
                ("mixin", lambda l=l, xsrc=xsrc: phase_mixin(l, xsrc)),
                ("ret", lambda l=l: phase_ret(l)),
                ("na", lambda l=l: phase_na(l)),
                ("lru", lambda l=l: phase_lru(l)),
                ("merge", lambda l=l, xsrc=xsrc: phase_merge(l, xsrc)),
                ("moepre", lambda l=l: (phase_moepre_sparse(l) if SPARSE else phase_moepre(l))),
                ("moe", lambda l=l: (phase_moe_sparse(l) if SPARSE else phase_moe(l))),
                ("moepost", lambda l=l, last=last: (phase_moepost_sparse(l, last) if SPARSE else phase_moepost(l, last)))]
    for name, fn in seq:
        fn()
        if stop_after is not None and name == stop_after:
            break
    fw.barrier()
    return nc, fw


def _consts():
    c = {}
    inv = (10000.0 ** (-np.arange(32, dtype=np.float32) / 32)).astype(np.float32)
    tok = np.arange(TL)
    rows = (tok // 64).astype(np.float32)
    cols = (tok % 64).astype(np.float32)
    ar = rows[None, :] * inv[:, None]
    ac = cols[None, :] * inv[:, None]
    C = np.ones((128, T), np.float32)
    S = np.zeros((128, T), np.float32)
    C[0:32, TC:] = np.cos(ar); C[32:64, TC:] = np.cos(ar); C[64:96, TC:] = np.cos(ac); C[96:128, TC:] = np.cos(ac)
    S[0:32, TC:] = -np.sin(ar); S[32:64, TC:] = np.sin(ar); S[64:96, TC:] = -np.sin(ac); S[96:128, TC:] = np.sin(ac)
    ks = np.float32(128.0 ** -0.5)
    c["ropeC"] = C; c["ropeS"] = S; c["ropeCk"] = C * ks; c["ropeSk"] = S * ks
    j = np.arange(128)[:, None, None]
    rel = np.arange(4)[None, :, None]
    i = np.arange(512)[None, None, :]
    diff = (i - (rel * 128 + j)).astype(np.float32)
    c["Rp"] = np.maximum(diff, 0).astype(np.float32)
    c["Rn"] = np.maximum(-diff, 0).astype(np.float32)
    jj = np.arange(128)[:, None]
    c["EA"] = (128 * np.arange(15)[None, :] - jj).astype(np.float32)
    c["EB"] = (128 * np.arange(14)[None, :] + jj + 1).astype(np.float32)
    c["I512"] = np.broadcast_to(np.arange(512, dtype=np.float32)[None, :], (128, 512)).copy()
    c["I511r"] = np.broadcast_to((511 - np.arange(512)).astype(np.float32)[None, :], (128, 512)).copy()
    c["Lmat"] = (np.arange(128)[:, None] < np.arange(128)[None, :]).astype(np.float32)
    c["pj"] = (np.arange(8)[None, :] * 128 + np.arange(128)[:, None]).astype(np.float32)
    c["iota32"] = np.broadcast_to(np.arange(NE, dtype=np.float32)[None, :], (128, NE)).copy()
    tk = (np.arange(36)[None, :] * 128 + np.arange(128)[:, None]).astype(np.int32)
    c["tokid"] = np.ascontiguousarray(np.stack([tk, tk], axis=-1))
    return c


def _na_bias_gather(rpb):
    L_ = rpb.shape[0]
    w = np.arange(64)[:, None, None]
    rr = np.arange(8)[None, :, None]
    kc = np.arange(64)[None, None, :]
    c0 = np.clip(w - 8, 0, 48)
    valid = (kc >= c0) & (kc < c0 + 16)
    dc = np.clip(kc - w + 15, 0, 30)
    out = np.empty((L_, 8, 128, 8, 512), np.float32)
    for dl in range(8):
        dr = np.clip(rr + 7 - dl, 0, 14)
        drb = np.broadcast_to(dr, (64, 8, 64))
        dcb = np.broadcast_to(dc, (64, 8, 64))
        vb = np.broadcast_to(valid, (64, 8, 64))
        g = rpb[:, :, drb, dcb]
        g = np.where(vb[None, None], g, np.float32(-30000.0)).reshape(L_, 8, 2, 64, 512)
        out[:, :, :, dl, :] = g.reshape(L_, 8, 128, 512)
    return out


def _pT(a):
    sh = a.shape[:-1]
    return np.ascontiguousarray(np.swapaxes(a.reshape(sh + (8, 128)), -1, -2))


def prep_inputs(inp):
    f = lambda k: np.asarray(inp[k], dtype=np.float32)
    x, c, ctx, c_ctx = f("x"), f("c"), f("ctx"), f("c_ctx")
    shared = {}
    shared["w_mod"] = f("w_mod")
    shared["b_modT"] = np.ascontiguousarray(f("b_mod").reshape(2, 48, 128).transpose(0, 2, 1))
    shared["n1gT"] = _pT(f("norm1_g"))
    shared["n2gT"] = _pT(f("norm2_g"))
    shared["fngT"] = _pT(f("final_norm_g"))
    shared["w_mix_in"] = f("w_mix_in")
    shared["decf"] = f("ret_decay_fwd")
    shared["decb"] = f("ret_decay_bwd")
    shared["na_bias"] = _na_bias_gather(f("na_rel_bias"))
    shared["convwT"] = np.ascontiguousarray(f("lru_conv_w").reshape(2, 4, 8, 128).transpose(0, 3, 2, 1))
    shared["convbT"] = _pT(f("lru_conv_b"))
    shared["lru_wa"] = f("lru_gate_a_w")
    shared["lru_wx"] = f("lru_gate_x_w")
    shared["lru_baT"] = np.ascontiguousarray(f("lru_gate_a_b").reshape(2, 2, 8, 128).transpose(0, 3, 1, 2))
    shared["lru_bxT"] = np.ascontiguousarray(f("lru_gate_x_b").reshape(2, 2, 8, 128).transpose(0, 3, 1, 2))
    shared["lru_lamT"] = np.ascontiguousarray(f("lru_lambda").reshape(2, 2, 8, 128).transpose(0, 3, 1, 2))
    shared["w_branch"] = f("w_branch")
    shared["w_mix_out"] = f("w_mix_out")
    shared["w_router"] = f("w_router")
    shared["b_router"] = f("b_router")
    shared["w_up"] = f("w_expert_up")
    shared["bupT"] = np.ascontiguousarray(f("b_expert_up").reshape(2, NE, 8, 128, 2).transpose(0, 3, 1, 4, 2))
    shared["bup2"] = np.ascontiguousarray(f("b_expert_up").reshape(2, NE, 8, 128, 2).transpose(0, 1, 3, 4, 2).reshape(2, NE, 128, 16))
    shared["w_down"] = f("w_expert_down")
    shared["b_down"] = f("b_expert_down")
    shared.update(_consts())
    in_maps = []
    for core in range(NCORES):
        bs = slice(core * NBC, (core + 1) * NBC)
        xa = np.concatenate([ctx[bs], x[bs]], axis=1)
        xin = np.ascontiguousarray(xa.transpose(0, 2, 1).reshape(NBC, 8, 128, T))
        crow = np.stack([c[core * NBC], c[core * NBC + 1], c_ctx], axis=0)
        cT = np.ascontiguousarray(crow.reshape(3, 8, 128).transpose(2, 1, 0))
        m = dict(shared)
        m["xin"] = xin
        m["cT"] = cT
        in_maps.append(m)
    return in_maps


def kernel(**inputs):
    in_maps = prep_inputs(inputs)
    nc, fw = build()
    res = run_bass_kernel_spmd(nc, in_maps, core_ids=list(range(NCORES)))
    outs = []
    for core in range(NCORES):
        o = res.results[core]["outT"]
        outs.append(o.reshape(NBC, D, TL).transpose(0, 2, 1))
    return np.ascontiguousarray(np.concatenate(outs, axis=0)).astype(np.float32)
```

```python
import numpy as np
from contextlib import ExitStack
import concourse.bass as bass
import concourse.mybir as mybir
from concourse.bass_utils import run_bass_kernel_spmd

F32 = mybir.dt.float32
BF16 = mybir.dt.bfloat16
AF = mybir.ActivationFunctionType
ALU = mybir.AluOpType
AX = mybir.AxisListType

NCORES = 8
NBC = 2
D = 1024
TC = 256
TL = 2048
T = TC + TL
NE = 32
EPS = 1e-6
BLOCKS = [(0, 256), (256, 512), (768, 512), (1280, 512), (1792, 512)]
BLOCKS256 = [(i * 256, 256) for i in range(9)]
NDSEM = 12
GRP = 1152
MB = 384
DBG_NSEC = 12
SPARSE = True
MT = 512
NTILE = 68
NSLOT = NTILE * MT
NTOK = 4608
I32 = mybir.dt.int32
DBG_RET = 16
DBG_IB = 4


class FW:
    def __init__(self, nc):
        self.nc = nc
        self.eng = {"pe": nc.tensor, "act": nc.scalar, "dve": nc.vector,
                    "pool": nc.gpsimd, "sp": nc.sync}
        self.sem = {}
        self.cnt = {}
        for e in ("pe", "act", "dve", "pool"):
            self.sem[e] = nc.alloc_semaphore("s_" + e)
            self.cnt[e] = 0
        self.dsem = {}
        self.dcnt = {}
        for q in ("sp", "pool"):
            self.dsem[q] = [nc.alloc_semaphore("d_%s_%d" % (q, i)) for i in range(NDSEM)]
            self.dcnt[q] = 0
        self.waited = {e: {} for e in self.eng}
        self.lastw = {}
        self.readers = {}
        self.ninst = 0
        self.nwait = 0

    def _wait(self, e, tok):
        sem, val, src = tok
        if src == e and e == "pe":
            return
        w = self.waited[e]
        k = id(sem)
        if w.get(k, 0) >= val:
            return
        w[k] = val
        self.eng[e].wait_ge(sem, val)
        self.nwait += 1

    def _deps(self, e, reads, writes):
        for r in reads:
            t = self.lastw.get(r)
            if t is not None:
                self._wait(e, t)
            if (isinstance(r, tuple) and r[0] in ("ps", "psb", "O")) or r in ("ps0", "psm"):
                for t in self.readers.get(r, ()):
                    if t[2] != e:
                        self._wait(e, t)
        for w in writes:
            t = self.lastw.get(w)
            if t is not None:
                self._wait(e, t)
            for t in self.readers.get(w, ()):
                self._wait(e, t)

    def _record(self, tok, reads, writes):
        for r in reads:
            self.readers.setdefault(r, []).append(tok)
        for w in writes:
            self.lastw[w] = tok
            self.readers[w] = []

    def op(self, e, fn, reads=(), writes=()):
        self._deps(e, reads, writes)
        ins = fn(self.eng[e])
        self.cnt[e] += 1
        ins.then_inc(self.sem[e], 1)
        tok = (self.sem[e], self.cnt[e], e)
        self._record(tok, reads, writes)
        self.ninst += 1
        return tok

    def dma(self, q, out, in_, reads=(), writes=(), **kw):
        j = self.dcnt[q]
        sem = self.dsem[q][j % NDSEM]
        gen = j // NDSEM
        if gen > 0:
            self._wait(q, (sem, 16 * gen, "dma"))
        self._deps(q, reads, writes)
        ins = self.eng[q].dma_start(out=out, in_=in_, **kw)
        ins.then_inc(sem, 16)
        self.dcnt[q] = j + 1
        tok = (sem, 16 * (gen + 1), "dma")
        self._record(tok, reads, writes)
        self.ninst += 1
        return tok

    def idma(self, out, out_off, in_, in_off, reads=(), writes=(), **kw):
        q = "pool"
        j = self.dcnt[q]
        sem = self.dsem[q][j % NDSEM]
        gen = j // NDSEM
        if gen > 0:
            self._wait(q, (sem, 16 * gen, "dma"))
        self._deps(q, reads, writes)
        ins = self.eng[q].indirect_dma_start(out=out, out_offset=out_off, in_=in_, in_offset=in_off, **kw)
        ins.then_inc(sem, 16)
        self.dcnt[q] = j + 1
        tok = (sem, 16 * (gen + 1), "dma")
        self._record(tok, reads, writes)
        self.ninst += 1
        return tok

    def barrier(self):
        toks = []
        for e in ("pe", "act", "dve", "pool"):
            if self.cnt[e] > 0:
                toks.append((self.sem[e], self.cnt[e], e))
        for q in ("sp", "pool"):
            j = self.dcnt[q]
            for i in range(NDSEM):
                n = (j - i + NDSEM - 1) // NDSEM if j > i else 0
                if n > 0:
                    toks.append((self.dsem[q][i], 16 * n, "dma"))
        for e in self.eng:
            for t in toks:
                if t[2] == e and e != "pe":
                    pass
                sem, val, src = t
                w = self.waited[e]
                if w.get(id(sem), 0) >= val:
                    continue
                w[id(sem)] = val
                self.eng[e].wait_ge(sem, val)
                self.nwait += 1
        self.lastw = {}
        self.readers = {}


def build(nlayers=2, dbg=(), stop_after=None):
    nc = bass.Bass("TRN2", target_bir_lowering=False)
    fw = FW(nc)
    I = {}

    def din(name, shape):
        I[name] = nc.dram_tensor(name, list(shape), F32, kind="ExternalInput").ap()

    def scr(name, shape, dt):
        kind = "ExternalOutput" if name in dbg else "Internal"
        return nc.dram_tensor(name, list(shape), dt, kind=kind).ap()

    L = 2
    din("xin", [NBC, 8, 128, T])
    din("cT", [128, 8, 3])
    din("w_mod", [L, D, 6 * D])
    din("b_modT", [L, 128, 48])
    din("n1gT", [L, 128, 8])
    din("n2gT", [L, 128, 8])
    din("fngT", [128, 8])
    din("w_mix_in", [L, D, 12 * D])
    din("decf", [L, 8])
    din("decb", [L, 8])
    din("na_bias", [L, 8, 128, 8, 512])
    din("convwT", [L, 128, 8, 4])
    din("convbT", [L, 128, 8])
    din("lru_wa", [L, 2, 8, 128, 128])
    din("lru_wx", [L, 2, 8, 128, 128])
    din("lru_baT", [L, 128, 2, 8])
    din("lru_bxT", [L, 128, 2, 8])
    din("lru_lamT", [L, 128, 2, 8])
    din("w_branch", [L, 3, D, D])
    din("w_mix_out", [L, D, D])
    din("w_router", [L, D, NE])
    din("b_router", [L, NE])
    din("w_up", [L, NE, D, 2 * D])
    din("bupT", [L, 128, NE, 2, 8])
    din("w_down", [L, NE, D, D])
    din("b_down", [L, NE, D])
    din("ropeC", [128, T])
    din("ropeS", [128, T])
    din("ropeCk", [128, T])
    din("ropeSk", [128, T])
    din("Rp", [128, 4, 512])
    din("Rn", [128, 4, 512])
    din("EA", [128, 15])
    din("EB", [128, 14])
    din("I512", [128, 512])
    din("I511r", [128, 512])
    din("Lmat", [128, 128])
    din("pj", [128, 8])
    din("iota32", [128, NE])
    din("bup2", [L, NE, 128, 16])
    I["tokid"] = nc.dram_tensor("tokid", [128, 36, 2], I32, kind="ExternalInput").ap()
    outT = nc.dram_tensor("outT", [NBC, 8, 128, TL], F32, kind="ExternalOutput").ap()

    xres = scr("xres", [NBC, 8, 128, T], F32)
    fmaj = {}
    for nm in ("rqT", "rkT", "rgT", "nqT", "nkT", "lyT", "gaT", "gbT", "gcT", "AinT", "BinT", "CinT", "hT2"):
        fmaj[nm] = scr(nm, [NBC, 8, 128, T], BF16)
    lxT = scr("lxT", [NBC, 8, 128, T], F32)
    rv = scr("rv", [NBC, T, D], BF16)
    nv = scr("nv", [NBC, T, D], BF16)
    gTd = scr("gTd", [NE, NBC * T], F32)
    yT = scr("yT", [NBC, 8, 128, T], F32)
    h2tok = scr("h2tok", [NTOK + 128, D], BF16)
    gtab = scr("gtab", [NTOK + 128, NE], F32)
    slot_tok = scr("slot_tok", [NSLOT, 2], I32)
    ypairs = scr("ypairs", [NSLOT, D], F32)
    wupb = scr("wupb", [NE * 128, 8 * 2 * D], BF16)
    wdnb = scr("wdnb", [NE * 128, 8 * D], BF16)

    def fm(ap_b):
        return ap_b.rearrange("c p t -> p c t")

    PS = [nc.alloc_psum_tensor("ps%d" % i, [128, 512], F32).ap() for i in range(8)]
    PSB = [PS[6].bitcast(BF16), PS[7].bitcast(BF16)]

    def gsb(name, shape, dt):
        return nc.alloc_sbuf_tensor(name, list(shape), dt).ap()

    ones_f = gsb("ones_f", [128, 128], F32)
    ones_b = gsb("ones_b", [128, 128], BF16)
    ident_f = gsb("ident_f", [128, 128], F32)
    ident_b = gsb("ident_b", [128, 128], BF16)
    modT = gsb("modT", [128, 48, 3], F32)
    mul1 = gsb("mul1", [128, 8, 3], F32)
    mul2 = gsb("mul2", [128, 8, 3], F32)
    fng = gsb("fng", [128, 8], F32)
    widx = gsb("widx", [128, NTILE, 8], I32)
    bidx = gsb("bidx", [128, NTILE], I32)
    eidx = gsb("eidx", [128, NTILE], I32)
    ETf = gsb("ETf", [128, NTILE], F32)
    S4_all = gsb("S4_all", [128, 36, 4], I32)
    pidx = gsb("pidx", [128, NTILE], I32)
    fw.op("pool", lambda e: e.memset(ones_f, 1.0), writes=["ones_f"])
    fw.op("pool", lambda e: e.memset(ones_b, 1.0), writes=["ones_b"])
    fw.op("pool", lambda e: e.memset(ident_f, 0.0), writes=["ident_f"])
    fw.op("pool", lambda e: e.affine_select(out=ident_f, in_=ident_f, pattern=[[-1, 128]],
                                            compare_op=ALU.not_equal, fill=1.0, base=0, channel_multiplier=1),
          reads=["ident_f"], writes=["ident_f"])
    fw.op("dve", lambda e: e.tensor_copy(out=ident_b, in_=ident_f), reads=["ident_f"], writes=["ident_b"])
    fw.dma("sp", fng, I["fngT"], writes=["fng"])
    fw.barrier()

    class Phase:
        cnt = 0

        def __init__(self, name):
            self.name = name
            self.es = ExitStack()
            self.n = 0

        def sb(self, shape, dt):
            self.n += 1
            Phase.cnt += 1
            t = self.es.enter_context(nc.sbuf_tensor("%s_%d_%d" % (self.name, Phase.cnt, self.n), list(shape), dt))
            return t.ap()

        def end(self):
            fw.barrier()
            self.es.close()

    def mm(out, lhsT, rhs, start, stop, reads, writes):
        fw.op("pe", lambda e: e.matmul(out, lhsT=lhsT, rhs=rhs, start=start, stop=stop), reads, writes)

    def phase_mod(l):
        P = Phase("mod")
        cs = P.sb([128, 8, 4], F32)
        bm = P.sb([128, 48], F32)
        fw.op("pool", lambda e: e.memset(cs, 0.0), writes=["cs"])
        n1 = P.sb([128, 8], F32)
        n2 = P.sb([128, 8], F32)
        fw.dma("sp", cs[:, :, 0:3], I["cT"], writes=["cs"])
        fw.dma("sp", bm, I["b_modT"][l], writes=["bm"])
        fw.dma("sp", n1, I["n1gT"][l], writes=["n1"])
        fw.dma("sp", n2, I["n2gT"][l], writes=["n2"])
        fw.op("act", lambda e: e.activation(out=cs, in_=cs, func=AF.Silu), reads=["cs"], writes=["cs"])
        wbuf = [P.sb([128, 8, 1024], F32) for _ in range(2)]
        psm = PS[0]
        for s in range(6):
            wb = wbuf[s % 2]
            fw.dma("sp", wb, I["w_mod"][l, :, s * 1024:(s + 1) * 1024].rearrange("(j p) n -> p j n", p=128),
                   writes=[("wm", s % 2)])
            for j in range(8):
                n = s * 8 + j
                for k in range(8):
                    mm(psm[:, n * 4:n * 4 + 4], wb[:, k, j * 128:(j + 1) * 128], cs[:, k, :], k == 0, k == 7,
                       [("wm", s % 2), "cs"], ["psm"])
        psv = psm[:, 0:192].rearrange("p (n r) -> p n r", r=4)
        for r in range(3):
            fw.op("dve", lambda e: e.tensor_tensor(out=modT[:, :, r], in0=psv[:, :, r], in1=bm, op=ALU.add),
                  reads=["psm", "bm"], writes=["modT"])
        for r in range(3):
            fw.op("dve", lambda e: e.scalar_tensor_tensor(out=mul1[:, :, r], in0=modT[:, 8:16, r], scalar=1.0, in1=n1,
                                                          op0=ALU.add, op1=ALU.mult),
                  reads=["modT", "n1"], writes=["mul1"])
            fw.op("dve", lambda e: e.scalar_tensor_tensor(out=mul2[:, :, r], in0=modT[:, 32:40, r], scalar=1.0, in1=n2,
                                                          op0=ALU.add, op1=ALU.mult),
                  reads=["modT", "n2"], writes=["mul2"])
        P.end()

    def emit_norm(xt, N, xkey, sq, rstd, mulT, r, out_fn):
        fw.op("act", lambda e: e.activation(out=sq[:, :, :N], in_=xt[:, :, :N], func=AF.Square),
              reads=[xkey], writes=["sq"])
        for j in range(8):
            mm(PS[0][:, :N], ones_f, sq[:, j, :N], j == 0, j == 7, ["sq", "ones_f"], ["ps0"])
        fw.op("act", lambda e: e.activation(out=rstd[:, :N], in_=PS[0][:, :N], func=AF.Sqrt, scale=1.0 / D, bias=EPS),
              reads=["ps0"], writes=["rstd"])
        fw.op("dve", lambda e: e.reciprocal(out=rstd[:, :N], in_=rstd[:, :N]), reads=["rstd"], writes=["rstd"])
        for j in range(8):
            out_fn(j)

    def phase_mixin(l, xsrc):
        es_h = ExitStack()
        hT = es_h.enter_context(nc.sbuf_tensor("hT_%d" % l, [128, 8, NBC * T], BF16)).ap()
        P = Phase("n1")
        xt = [P.sb([128, 8, 512], F32) for _ in range(2)]
        sq = P.sb([128, 8, 512], F32)
        rstd = P.sb([128, 512], F32)
        tmp = [P.sb([128, 512], F32) for _ in range(2)]
        i = 0
        for b in range(NBC):
            for (t0, N) in BLOCKS:
                x_ = xt[i % 2]
                xk = ("xt", i % 2)
                fw.dma("sp", x_[:, :, :N], fm(xsrc[b])[:, :, t0:t0 + N], writes=[xk])
                r = 2 if t0 < TC else b

                def out_fn(j, x_=x_, xk=xk, N=N, r=r, b=b, t0=t0):
                    tm = tmp[j % 2]
                    fw.op("dve", lambda e: e.scalar_tensor_tensor(out=tm[:, :N], in0=x_[:, j, :N], scalar=mul1[:, j, r:r + 1],
                                                                  in1=rstd[:, :N], op0=ALU.mult, op1=ALU.mult),
                          reads=[xk, "rstd", "mul1"], writes=[("tmp", j % 2)])
                    fw.op("act", lambda e: e.activation(out=hT[:, j, b * T + t0:b * T + t0 + N], in_=tm[:, :N],
                                                        func=AF.Identity, bias=modT[:, j, r:r + 1]),
                          reads=[("tmp", j % 2), "modT"], writes=["hT"])
                emit_norm(x_, N, xk, sq, rstd, mul1, r, out_fn)
                i += 1
        P.end()

        P = Phase("mix")
        wb = [P.sb([128, 8, 1024], BF16) for _ in range(2)]
        wp = P.sb([128, 8, 1024], BF16)
        stage = [P.sb([128, 8, 512], BF16) for _ in range(2)]
        stage32 = P.sb([128, 8, 512], F32)
        stT = [P.sb([128, 1024], BF16) for _ in range(2)]
        rC = P.sb([128, T], F32)
        rS = P.sb([128, T], F32)
        rt = [P.sb([128, 512], F32) for _ in range(4)]
        names = ["rqT", "rkT", None, "rgT", "nqT", "nkT", None, None, "lyT", "gaT", "gbT", "gcT"]

        def loadw(s):
            fw.dma("pool", wb[s % 2], I["w_mix_in"][l, :, s * 1024:(s + 1) * 1024].rearrange("(j p) n -> p j n", p=128),
                   writes=[("wb", s % 2)])
        loadw(0)
        psi = 0
        sti = 0
        ev = 0
        for s in range(DBG_NSEC):
            if s + 1 < DBG_NSEC:
                loadw(s + 1)
            w = wb[s % 2]
            wk = ("wb", s % 2)
            if s < 2:
                Wv = w.rearrange("p j (h q r) -> p (j h) q r", q=4, r=32)
                Pv = wp.rearrange("p j (h q r) -> p (j h) q r", q=4, r=32)
                for q in range(4):
                    eng = "dve" if q % 2 == 0 else "act"
                    if eng == "dve":
                        fw.op("dve", lambda e: e.tensor_copy(out=Pv[:, :, q, :], in_=Wv[:, :, q ^ 1, :]), reads=[wk], writes=["wp"])
                    else:
                        fw.op("act", lambda e: e.copy(out=Pv[:, :, q, :], in_=Wv[:, :, q ^ 1, :]), reads=[wk], writes=["wp"])
                fw.dma("sp", rC, I["ropeC" if s == 0 else "ropeCk"], writes=["rC"])
                fw.dma("sp", rS, I["ropeS" if s == 0 else "ropeSk"], writes=["rS"])
            if s in (2, 6):
                dst = rv if s == 2 else nv
                for b in range(NBC):
                    for tt in range(18):
                        st = stT[sti % 2]
                        sk = ("stT", sti % 2)
                        for half in range(2):
                            ps = PS[psi % 4]
                            pk = ("ps", psi % 4)
                            psi += 1
                            for k in range(8):
                                mm(ps, hT[:, k, b * T + tt * 128:b * T + (tt + 1) * 128], w[:, k, half * 512:(half + 1) * 512],
                                   k == 0, k == 7, ["hT", wk], [pk])
                            if ev % 2 == 0:
                                fw.op("act", lambda e: e.copy(out=st[:, half * 512:(half + 1) * 512], in_=ps), reads=[pk], writes=[sk])
                            else:
                                fw.op("dve", lambda e: e.tensor_copy(out=st[:, half * 512:(half + 1) * 512], in_=ps), reads=[pk], writes=[sk])
                            ev += 1
                        fw.dma("sp", dst[b, tt * 128:(tt + 1) * 128, :], st, reads=[sk], writes=[("dst", s, b, tt)])
                        sti += 1
                continue
            for b in range(NBC):
                for (t0, N) in BLOCKS:
                    if s == 7:
                        st = stage32
                        sk = "stage32"
                    else:
                        st = stage[sti % 2]
                        sk = ("stage", sti % 2)
                        sti += 1
                    hsl = slice(b * T + t0, b * T + t0 + N)
                    for c in range(8):
                        ps = PS[psi % 4]
                        pk = ("ps", psi % 4)
                        psi += 1
                        for k in range(8):
                            mm(ps[:, :N], w[:, k, c * 128:(c + 1) * 128], hT[:, k, hsl], k == 0, k == 7, ["hT", wk], [pk])
                        o = st[:, c, :N]
                        if s < 2:
                            ps2 = PS[4 + psi % 2]
                            pk2 = ("ps", 4 + psi % 2)
                            for k in range(8):
                                mm(ps2[:, :N], wp[:, k, c * 128:(c + 1) * 128], hT[:, k, hsl], k == 0, k == 7, ["hT", "wp"], [pk2])
                            ta = rt[(psi % 2) * 2]
                            tb = rt[(psi % 2) * 2 + 1]
                            ka = ("rt", (psi % 2) * 2)
                            kb = ("rt", (psi % 2) * 2 + 1)
                            fw.op("dve", lambda e: e.tensor_tensor(out=ta[:, :N], in0=ps[:, :N], in1=rC[:, t0:t0 + N], op=ALU.mult),
                                  reads=[pk, "rC"], writes=[ka])
                            fw.op("dve", lambda e: e.tensor_tensor(out=tb[:, :N], in0=ps2[:, :N], in1=rS[:, t0:t0 + N], op=ALU.mult),
                                  reads=[pk2, "rS"], writes=[kb])
                            fw.op("pool", lambda e: e.tensor_tensor(out=o, in0=ta[:, :N], in1=tb[:, :N], op=ALU.add),
                                  reads=[ka, kb], writes=[sk])
                        elif s == 3:
                            fw.op("act", lambda e: e.activation(out=o, in_=ps[:, :N], func=AF.Silu), reads=[pk], writes=[sk])
                        elif s == 8:
                            fw.op("act", lambda e: e.activation(out=o, in_=ps[:, :N], func=AF.Gelu), reads=[pk], writes=[sk])
                        elif s >= 9:
                            fw.op("act", lambda e: e.activation(out=o, in_=ps[:, :N], func=AF.Sigmoid), reads=[pk], writes=[sk])
                        elif s == 4:
                            if ev % 2 == 0:
                                fw.op("act", lambda e: e.activation(out=o, in_=ps[:, :N], func=AF.Copy, scale=0.125), reads=[pk], writes=[sk])
                            else:
                                fw.op("dve", lambda e: e.tensor_scalar(out=o, in0=ps[:, :N], scalar1=0.125, scalar2=None, op0=ALU.mult),
                                      reads=[pk], writes=[sk])
                            ev += 1
                        else:
                            if ev % 2 == 0:
                                fw.op("act", lambda e: e.copy(out=o, in_=ps[:, :N]), reads=[pk], writes=[sk])
                            else:
                                fw.op("dve", lambda e: e.tensor_copy(out=o, in_=ps[:, :N]), reads=[pk], writes=[sk])
                            ev += 1
                    dst = lxT if s == 7 else fmaj[names[s]]
                    fw.dma("sp", fm(dst[b])[:, :, t0:t0 + N], st[:, :, :N], reads=[sk], writes=[("dst", s, b, t0)])
        P.end()
        es_h.close()

    def phase_ret(l, last=False):
        P = Phase("ret")
        lg = P.sb([128, 16], F32)
        fw.dma("sp", lg[:, 0:8], I["decf"][l].partition_broadcast(128), writes=["lg"])
        fw.dma("sp", lg[:, 8:16], I["decb"][l].partition_broadcast(128), writes=["lg"])
        fw.op("act", lambda e: e.activation(out=lg, in_=lg, func=AF.Exp, scale=-1.0), reads=["lg"], writes=["lg"])
        fw.op("act", lambda e: e.activation(out=lg, in_=lg, func=AF.Ln, bias=1.0), reads=["lg"], writes=["lg"])
        fw.op("dve", lambda e: e.tensor_scalar(out=lg, in0=lg, scalar1=-1.0, scalar2=None, op0=ALU.mult), reads=["lg"], writes=["lg"])
        Rp = P.sb([128, 4, 512], F32)
        Rn = P.sb([128, 4, 512], F32)
        EA = P.sb([128, 15], F32)
        EB = P.sb([128, 14], F32)
        I512 = P.sb([128, 512], F32)
        I511r = P.sb([128, 512], F32)
        for nm, t in (("Rp", Rp), ("Rn", Rn), ("EA", EA), ("EB", EB), ("I512", I512), ("I511r", I511r)):
            fw.dma("sp", t, I[nm], writes=[nm])
        Dm = P.sb([128, 8, 4, 512], BF16)
        Af = P.sb([128, 8, 15], F32)
        Ab = P.sb([128, 8, 14], F32)
        bf = P.sb([128, 8, 512], F32)
        bb = P.sb([128, 8, 512], F32)
        t1 = P.sb([128, 512], F32)
        for h in range(8):
            for rel in range(4):
                fw.op("dve", lambda e: e.tensor_scalar(out=t1, in0=Rp[:, rel, :], scalar1=lg[:, h:h + 1], scalar2=None, op0=ALU.mult),
                      reads=["Rp", "lg"], writes=["t1"])
                fw.op("dve", lambda e: e.scalar_tensor_tensor(out=t1, in0=Rn[:, rel, :], scalar=lg[:, 8 + h:9 + h], in1=t1,
                                                              op0=ALU.mult, op1=ALU.add),
                      reads=["Rn", "lg", "t1"], writes=["t1"])
                fw.op("act", lambda e: e.activation(out=Dm[:, h, rel, :], in_=t1, func=AF.Exp), reads=["t1"], writes=["Dm"])
            fw.op("act", lambda e: e.activation(out=Af[:, h, :], in_=EA, func=AF.Exp, scale=lg[:, h:h + 1]), reads=["EA", "lg"], writes=["Af"])
            fw.op("act", lambda e: e.activation(out=Ab[:, h, :], in_=EB, func=AF.Exp, scale=lg[:, 8 + h:9 + h]), reads=["EB", "lg"], writes=["Ab"])
            fw.op("act", lambda e: e.activation(out=bf[:, h, :], in_=I512, func=AF.Exp, scale=lg[:, h:h + 1]), reads=["I512", "lg"], writes=["bf"])
            fw.op("act", lambda e: e.activation(out=bb[:, h, :], in_=I511r, func=AF.Exp, scale=lg[:, 8 + h:9 + h]), reads=["I511r", "lg"], writes=["bb"])
        qT = [P.sb([128, T], BF16) for _ in range(2)]
        kT = [P.sb([128, T], BF16) for _ in range(2)]
        V = [P.sb([128, 18, 128], BF16) for _ in range(2)]
        sg = [P.sb([128, T], BF16) for _ in range(2)]
        ost = [P.sb([128, T], BF16) for _ in range(2)]
        pb = [P.sb([128, 512], BF16) for _ in range(4)]
        y32 = P.sb([128, 512], F32)
        e1 = P.sb([128, 512], F32)
        e2 = P.sb([128, 512], F32)
        sqy = P.sb([128, 512], F32)
        rsy = P.sb([128, 512], F32)
        it = 0
        pbi = 0
        evc = 0
        for b in range(NBC):
            for h in range(8):
                if it >= DBG_RET:
                    continue
                p = it % 2
                it += 1
                q_, k_, v_, s_, o_ = qT[p], kT[p], V[p], sg[p], ost[p]
                fw.dma("sp", q_, fmaj["rqT"][b, h], writes=[("q", p)])
                fw.dma("sp", k_, fmaj["rkT"][b, h], writes=[("k", p)])
                fw.dma("sp", v_, rv[b].rearrange("(t p) (h d) -> h p t d", p=128, d=128)[h], writes=[("v", p)])
                fw.dma("sp", s_, fmaj["rgT"][b, h], writes=[("sg", p)])
                SBK = [0, 1, 6, 7]
                steps = []
                blkinfo = {}
                for ib in range(0 if last else -1, DBG_IB):
                    if ib < 0:
                        q0, N = 0, 256
                        kks = [(kk, [("d", kk)]) for kk in range(2)]
                    else:
                        q0, N = TC + 512 * ib, 512
                        kks = []
                        for kk in range(18):
                            if kk < 2:
                                kks.append((kk, [("f", 2 + 4 * ib - kk), ("b", 12 + kk - 4 * ib)]))
                            else:
                                rel = kk - 2 - 4 * ib
                                if rel < 0:
                                    kks.append((kk, [("f", -rel)]))
                                elif rel < 4:
                                    kks.append((kk, [("d", rel)]))
                                else:
                                    kks.append((kk, [("b", rel - 4)]))
                    tot = {"d": 0, "f": 0, "b": 0}
                    for (_, its) in kks:
                        for (ty, _) in its:
                            tot[ty] += 1
                    blkinfo[ib] = (q0, N, tot)
                    for j, (kk, its) in enumerate(kks):
                        steps.append((ib, kk, its, j == 0, j == len(kks) - 1))

                def emitS(i):
                    ib, kk, _, _, _ = steps[i]
                    q0, N, _ = blkinfo[ib]
                    bk = SBK[i % 4]
                    mm(PS[bk][:, :N], k_[:, kk * 128:(kk + 1) * 128], q_[:, q0:q0 + N], True, True, [("q", p), ("k", p)], [("ps", bk)])

                def epilogue(ib):
                    q0, N, _ = blkinfo[ib]
                    if ib < 0:
                        fw.op("dve", lambda e: e.tensor_copy(out=y32[:, :N], in_=PS[2][:, :N]), reads=[("ps", 2)], writes=["y32"])
                    else:
                        fw.op("dve", lambda e: e.tensor_tensor(out=e1, in0=PS[3], in1=bf[:, h, :], op=ALU.mult), reads=[("ps", 3), "bf"], writes=["e1"])
                        fw.op("dve", lambda e: e.tensor_tensor(out=e2, in0=PS[4], in1=bb[:, h, :], op=ALU.mult), reads=[("ps", 4), "bb"], writes=["e2"])
                        fw.op("pool", lambda e: e.tensor_tensor(out=e1, in0=e1, in1=e2, op=ALU.add), reads=["e1", "e2"], writes=["e1"])
                        fw.op("dve", lambda e: e.tensor_tensor(out=y32, in0=PS[2], in1=e1, op=ALU.add), reads=[("ps", 2), "e1"], writes=["y32"])
                    fw.op("act", lambda e: e.activation(out=sqy[:, :N], in_=y32[:, :N], func=AF.Square), reads=["y32"], writes=["sqy"])
                    mm(PS[5][:, :N], ones_f, sqy[:, :N], True, True, ["sqy", "ones_f"], [("ps", 5)])
                    fw.op("act", lambda e: e.activation(out=rsy[:, :N], in_=PS[5][:, :N], func=AF.Sqrt, scale=1.0 / 128, bias=EPS),
                          reads=[("ps", 5)], writes=["rsy"])
                    fw.op("dve", lambda e: e.reciprocal(out=rsy[:, :N], in_=rsy[:, :N]), reads=["rsy"], writes=["rsy"])
                    fw.op("pool", lambda e: e.tensor_tensor(out=y32[:, :N], in0=y32[:, :N], in1=rsy[:, :N], op=ALU.mult),
                          reads=["y32", "rsy"], writes=["y32"])
                    fw.op("dve", lambda e: e.tensor_tensor(out=o_[:, q0:q0 + N], in0=y32[:, :N], in1=s_[:, q0:q0 + N], op=ALU.mult),
                          reads=["y32", ("sg", p)], writes=[("ost", p)])

                LA = 3
                for i in range(min(LA, len(steps))):
                    emitS(i)
                accb = {"d": 2, "f": 3, "b": 4}
                cnts = None
                for i, (ib, kk, its, first, last) in enumerate(steps):
                    q0, N, tot = blkinfo[ib]
                    if first:
                        cnts = {"d": 0, "f": 0, "b": 0}
                    bk = SBK[i % 4]
                    sps = PS[bk]
                    spk = ("ps", bk)
                    for (ty, idx) in its:
                        pt = pb[pbi % 4]
                        pk = ("pb", pbi % 4)
                        pbi += 1
                        if ty == "d":
                            fw.op("dve", lambda e: e.tensor_tensor(out=pt[:, :N], in0=sps[:, :N], in1=Dm[:, h, idx, :N], op=ALU.mult),
                                  reads=[spk, "Dm"], writes=[pk])
                        else:
                            sc = Af[:, h, idx:idx + 1] if ty == "f" else Ab[:, h, idx:idx + 1]
                            if evc % 3 != 0:
                                fw.op("act", lambda e: e.activation(out=pt[:, :N], in_=sps[:, :N], func=AF.Identity, scale=sc),
                                      reads=[spk, "Af", "Ab"], writes=[pk])
                            else:
                                fw.op("dve", lambda e: e.tensor_scalar(out=pt[:, :N], in0=sps[:, :N], scalar1=sc, scalar2=None, op0=ALU.mult),
                                      reads=[spk, "Af", "Ab"], writes=[pk])
                            evc += 1
                        ab = accb[ty]
                        mm(PS[ab][:, :N], v_[:, kk, :], pt[:, :N], cnts[ty] == 0, cnts[ty] == tot[ty] - 1, [("v", p), pk], [("ps", ab)])
                        cnts[ty] += 1
                    if i + LA < len(steps):
                        emitS(i + LA)
                    if last:
                        epilogue(ib)
                fw.dma("pool", fmaj["AinT"][b, h], o_, reads=[("ost", p)], writes=[("Ain", b, h)])
        P.end()

    def phase_na(l, last=False):
        P = Phase("na")
        qbd = [P.sb([128, 36, 128], BF16) for _ in range(2)]
        kT = [P.sb([128, T], BF16) for _ in range(2)]
        Ve = [P.sb([128, 18, 128], BF16) for _ in range(2)]
        Vo = [P.sb([128, 15, 128], BF16) for _ in range(2)]
        Bt = [P.sb([128, 8, 512], F32) for _ in range(2)]
        Ssb = [P.sb([128, 768], F32) for _ in range(3)]
        Pb = [P.sb([128, 768], BF16) for _ in range(3)]
        PT = [P.sb([128, 6, 128], BF16) for _ in range(3)]
        mx = [P.sb([128, 1], F32) for _ in range(3)]
        rec = [P.sb([128, 128], F32) for _ in range(2)]
        ost = [P.sb([128, T], BF16) for _ in range(2)]
        for i in range(2):
            fw.op("pool", lambda e: e.memset(qbd[i], 0.0), writes=[("qbd", i)])
        wcb = [P.sb([128, 8, 1024], BF16) for _ in range(2)]
        wcu = [0]

        def wc_src_dst(u):
            ex, k = u // 3, u % 3
            if k < 2:
                src = I["w_up"][l, ex][:, k * 1024:(k + 1) * 1024].rearrange("(j p) n -> p j n", p=128)
                dst = wupb[ex * 128:(ex + 1) * 128, :].rearrange("p (j n) -> p j n", j=8)[:, :, k * 1024:(k + 1) * 1024]
            else:
                src = I["w_down"][l, ex].rearrange("(j p) n -> p j n", p=128)
                dst = wdnb[ex * 128:(ex + 1) * 128, :].rearrange("p (j n) -> p j n", j=8)
            return src, dst

        def wc_load(u):
            src, _ = wc_src_dst(u)
            fw.dma("pool", wcb[u % 2], src, writes=[("wcb", u % 2)])

        def wc_store(u):
            _, dst = wc_src_dst(u)
            fw.dma("sp", dst, wcb[u % 2], reads=[("wcb", u % 2)], writes=[("wcd", u)])
        it = 0
        ri = 0
        for b in range(NBC):
            nvv = nv[b].rearrange("(t p) (g d) -> g p t d", p=128, d=128)
            nvo = nv[b, TC + 64:TC + 64 + 15 * 128, :].rearrange("(t p) (g d) -> g p t d", p=128, d=128)
            for g in range(8):
                p = it % 2
                it += 1
                qb, k_, ve, vo, bt, o_ = qbd[p], kT[p], Ve[p], Vo[p], Bt[p], ost[p]
                nq = fmaj["nqT"][b, g]
                fw.dma("sp", qb[0:64, :, 0:64], nq[0:64, :].rearrange("p (r w) -> p r w", w=64), writes=[("qbd", p)])
                fw.dma("sp", qb[64:128, :, 64:128], nq[64:128, :].rearrange("p (r w) -> p r w", w=64), writes=[("qbd", p)])
                fw.dma("sp", k_, fmaj["nkT"][b, g], writes=[("k", p)])
                fw.dma("sp", ve, nvv[g], writes=[("ve", p)])
                fw.dma("sp", vo, nvo[g], writes=[("vo", p)])
                fw.dma("sp", bt, I["na_bias"][l, g], writes=[("bt", p)])
                def rowinfo(rr):
                    if rr < 4:
                        return True, 256, 2, 0, 0
                    r = rr - 4
                    r0 = min(max(r - 4, 0), 24)
                    return False, 768, 6, r0, r - r0

                def stA(rr):
                    ctxrow, W, nkt, r0, dl = rowinfo(rr)
                    a = rr % 2
                    a3 = rr % 3
                    X = PS[2 * a]
                    Y = PS[2 * a + 1]
                    xk = ("ps", 2 * a)
                    yk = ("ps", 2 * a + 1)
                    S_ = Ssb[a3]
                    sk = ("Ssb", a3)
                    mm(Y[:, :256], qb[:, rr, :], k_[:, 0:256], True, True, [("qbd", p), ("k", p)], [yk])
                    if ctxrow:
                        fw.op("act", lambda e: e.copy(out=S_[:, 0:256], in_=Y[:, :256]), reads=[yk], writes=[sk])
                    else:
                        mm(X, qb[:, rr, :], k_[:, TC + r0 * 64:TC + r0 * 64 + 512], True, True, [("qbd", p), ("k", p)], [xk])
                        fw.op("dve", lambda e: e.tensor_tensor(out=S_[:, 0:512], in0=X, in1=bt[:, dl, :], op=ALU.add),
                              reads=[xk, ("bt", p)], writes=[sk])
                        fw.op("act", lambda e: e.copy(out=S_[:, 512:768], in_=Y[:, :256]), reads=[yk], writes=[sk])
                    fw.op("dve", lambda e: e.tensor_reduce(out=mx[a3], in_=S_[:, :W], axis=AX.X, op=ALU.max, negate=True),
                          reads=[sk], writes=[("mx", a3)])
                    fw.op("act", lambda e: e.activation(out=Pb[a3][:, :W], in_=S_[:, :W], func=AF.Exp, bias=mx[a3]),
                          reads=[sk, ("mx", a3)], writes=[("Pb", a3)])

                def stB(rr):
                    ctxrow, W, nkt, r0, dl = rowinfo(rr)
                    a = rr % 2
                    a3 = rr % 3
                    psb = PSB[a]
                    pbk = ("psb", a)
                    for kt in range(nkt):
                        fw.op("pe", lambda e: e.transpose(out=psb[:, kt * 128:(kt + 1) * 128], in_=Pb[a3][:, kt * 128:(kt + 1) * 128], identity=ident_b),
                              reads=[("Pb", a3), "ident_b"], writes=[pbk])
                    ptv = PT[a3].rearrange("p k q -> p (k q)")
                    ptk = ("PT", a3)
                    if rr % 2 == 0:
                        fw.op("act", lambda e: e.copy(out=ptv[:, :nkt * 128], in_=psb[:, :nkt * 128]), reads=[pbk], writes=[ptk])
                    else:
                        fw.op("dve", lambda e: e.tensor_copy(out=ptv[:, :nkt * 128], in_=psb[:, :nkt * 128]), reads=[pbk], writes=[ptk])

                def stC(rr):
                    ctxrow, W, nkt, r0, dl = rowinfo(rr)
                    a = rr % 2
                    a3 = rr % 3
                    pt = PT[a3]
                    ptk = ("PT", a3)
                    if ctxrow:
                        vts = [ve[:, 0, :], ve[:, 1, :]]
                        vks = [("ve", p)]
                    else:
                        if r0 % 2 == 0:
                            vts = [ve[:, 2 + r0 // 2 + kt, :] for kt in range(4)]
                        else:
                            vts = [vo[:, (r0 - 1) // 2 + kt, :] for kt in range(4)]
                        vts += [ve[:, 0, :], ve[:, 1, :]]
                        vks = [("ve", p), ("vo", p)]
                    O = PS[4 + a][:, 0:128]
                    Dn = PS[4 + a][:, 128:256]
                    ok = ("O", a)
                    for kt in range(nkt):
                        mm(O, vts[kt], pt[:, kt, :], kt == 0, kt == nkt - 1, vks + [ptk], [ok])
                    for kt in range(nkt):
                        mm(Dn, ones_b, pt[:, kt, :], kt == 0, kt == nkt - 1, ["ones_b", ptk], [ok])
                    fw.op("dve", lambda e: e.reciprocal(out=rec[a], in_=Dn), reads=[ok], writes=[("rec", a)])
                    fw.op("dve", lambda e: e.tensor_tensor(out=o_[0:64, rr * 64:(rr + 1) * 64], in0=O[0:64, 0:64], in1=rec[a][0:64, 0:64], op=ALU.mult),
                          reads=[ok, ("rec", a)], writes=[("ost", p)])
                    fw.op("dve", lambda e: e.tensor_tensor(out=o_[64:128, rr * 64:(rr + 1) * 64], in0=O[64:128, 64:128], in1=rec[a][64:128, 64:128], op=ALU.mult),
                          reads=[ok, ("rec", a)], writes=[("ost", p)])

                rows = list(range(4, 36)) if last else list(range(36))
                nr = len(rows)
                for t in range(nr + 2):
                    if SPARSE and t % 5 == 0 and t < 30:
                        wc_load(wcu[0])
                    if SPARSE and t % 5 == 2 and t < 30:
                        wc_store(wcu[0])
                        wcu[0] += 1
                    if t < nr:
                        stA(rows[t])
                    if 0 <= t - 1 < nr:
                        stB(rows[t - 1])
                    if 0 <= t - 2 < nr:
                        stC(rows[t - 2])
                fw.dma("pool", fmaj["BinT"][b, g], o_, reads=[("ost", p)], writes=[("Bin", b, g)])
        P.end()

    def phase_lru(l):
        P = Phase("lru")
        wg = P.sb([128, 32, 128], BF16)
        fw.dma("pool", wg[:, 0:16, :], I["lru_wa"][l].rearrange("r k c d -> c (r k) d"), writes=["wg"])
        fw.dma("pool", wg[:, 16:32, :], I["lru_wx"][l].rearrange("r k c d -> c (r k) d"), writes=["wg"])
        cw = P.sb([128, 8, 4], F32)
        cb = P.sb([128, 8], F32)
        ba = P.sb([128, 2, 8], F32)
        bx = P.sb([128, 2, 8], F32)
        lam = P.sb([128, 2, 8], F32)
        fw.dma("sp", cw, I["convwT"][l], writes=["cw"])
        fw.dma("sp", cb, I["convbT"][l], writes=["cb"])
        fw.dma("sp", ba, I["lru_baT"][l], writes=["ba"])
        fw.dma("sp", bx, I["lru_bxT"][l], writes=["bx"])
        fw.dma("sp", lam, I["lru_lamT"][l], writes=["lam"])
        fw.op("act", lambda e: e.activation(out=lam, in_=lam, func=AF.Exp, scale=-1.0), reads=["lam"], writes=["lam"])
        fw.op("act", lambda e: e.activation(out=lam, in_=lam, func=AF.Ln, bias=1.0), reads=["lam"], writes=["lam"])
        fw.op("dve", lambda e: e.tensor_scalar(out=lam, in0=lam, scalar1=-8.0, scalar2=None, op0=ALU.mult), reads=["lam"], writes=["lam"])
        xs = [P.sb([128, T], F32) for _ in range(2)]
        gy = [P.sb([128, T], BF16) for _ in range(2)]
        u = P.sb([128, T], F32)
        ub = P.sb([128, T], BF16)
        r_ = P.sb([128, T], F32)
        i_ = P.sb([128, T], F32)
        a_ = P.sb([128, T], F32)
        s_ = P.sb([128, T], F32)
        hf = P.sb([128, T], F32)
        hb = P.sb([128, T], F32)
        oc = [P.sb([128, T], BF16) for _ in range(2)]
        it = 0
        psi = 0
        segs = [(0, TC), (TC, T)]
        for b in range(NBC):
            for k in range(8):
                p = it % 2
                it += 1
                x_ = xs[p]
                fw.dma("sp", x_, lxT[b, k], writes=[("x", p)])
                fw.dma("sp", gy[p], fmaj["lyT"][b, k], writes=[("gy", p)])
                fw.op("dve", lambda e: e.tensor_scalar(out=u, in0=x_, scalar1=cw[:, k, 1:2], scalar2=cb[:, k:k + 1], op0=ALU.mult, op1=ALU.add),
                      reads=[("x", p), "cw", "cb"], writes=["u"])
                for (s0, s1) in segs:
                    fw.op("dve", lambda e: e.scalar_tensor_tensor(out=u[:, s0 + 1:s1], in0=x_[:, s0:s1 - 1], scalar=cw[:, k, 0:1], in1=u[:, s0 + 1:s1],
                                                                  op0=ALU.mult, op1=ALU.add), reads=[("x", p), "u", "cw"], writes=["u"])
                    fw.op("dve", lambda e: e.scalar_tensor_tensor(out=u[:, s0:s1 - 1], in0=x_[:, s0 + 1:s1], scalar=cw[:, k, 2:3], in1=u[:, s0:s1 - 1],
                                                                  op0=ALU.mult, op1=ALU.add), reads=[("x", p), "u", "cw"], writes=["u"])
                    fw.op("dve", lambda e: e.scalar_tensor_tensor(out=u[:, s0:s1 - 2], in0=x_[:, s0 + 2:s1], scalar=cw[:, k, 3:4], in1=u[:, s0:s1 - 2],
                                                                  op0=ALU.mult, op1=ALU.add), reads=[("x", p), "u", "cw"], writes=["u"])
                fw.op("act", lambda e: e.copy(out=ub, in_=u), reads=["u"], writes=["ub"])
                for dr in range(2):
                    for (t0, N) in BLOCKS:
                        ps = PS[psi % 4]
                        pk = ("ps", psi % 4)
                        psi += 1
                        mm(ps[:, :N], wg[:, dr * 8 + k, :], ub[:, t0:t0 + N], True, True, ["wg", "ub"], [pk])
                        fw.op("act", lambda e: e.activation(out=r_[:, t0:t0 + N], in_=ps[:, :N], func=AF.Sigmoid, bias=ba[:, dr, k:k + 1]),
                              reads=[pk, "ba"], writes=["r_"])
                        ps = PS[psi % 4]
                        pk = ("ps", psi % 4)
                        psi += 1
                        mm(ps[:, :N], wg[:, 16 + dr * 8 + k, :], ub[:, t0:t0 + N], True, True, ["wg", "ub"], [pk])
                        fw.op("act", lambda e: e.activation(out=i_[:, t0:t0 + N], in_=ps[:, :N], func=AF.Sigmoid, bias=bx[:, dr, k:k + 1]),
                              reads=[pk, "bx"], writes=["i_"])
                    fw.op("act", lambda e: e.activation(out=a_, in_=r_, func=AF.Exp, scale=lam[:, dr, k:k + 1]), reads=["r_", "lam"], writes=["a_"])
                    fw.op("act", lambda e: e.activation(out=s_, in_=a_, func=AF.Square), reads=["a_"], writes=["s_"])
                    fw.op("act", lambda e: e.activation(out=s_, in_=s_, func=AF.Sqrt, scale=-1.0, bias=1.0), reads=["s_"], writes=["s_"])
                    fw.op("pool", lambda e: e.tensor_tensor(out=i_, in0=i_, in1=u, op=ALU.mult), reads=["i_", "u"], writes=["i_"])
                    fw.op("dve", lambda e: e.tensor_tensor(out=s_, in0=s_, in1=i_, op=ALU.mult), reads=["s_", "i_"], writes=["s_"])
                    if dr == 0:
                        fw.op("dve", lambda e: e.tensor_tensor_scan(out=hf, data0=a_, data1=s_, initial=0.0, op0=ALU.mult, op1=ALU.add),
                              reads=["a_", "s_"], writes=["hf"])
                    else:
                        fw.op("dve", lambda e: e.tensor_tensor_scan(out=hb[:, 0:TC][:, ::-1], data0=a_[:, 0:TC][:, ::-1], data1=s_[:, 0:TC][:, ::-1],
                                                                    initial=0.0, op0=ALU.mult, op1=ALU.add),
                              reads=["a_", "s_"], writes=["hb"])
                        fw.op("dve", lambda e: e.tensor_tensor_scan(out=hb[:, TC:T][:, ::-1], data0=a_[:, TC:T][:, ::-1], data1=s_[:, TC:T][:, ::-1],
                                                                    initial=hb[:, 0:1], op0=ALU.mult, op1=ALU.add),
                              reads=["a_", "s_", "hb"], writes=["hb"])
                fw.op("pool", lambda e: e.tensor_tensor(out=hf, in0=hf, in1=hb, op=ALU.add), reads=["hf", "hb"], writes=["hf"])
                fw.op("dve", lambda e: e.tensor_tensor(out=oc[p], in0=hf, in1=gy[p], op=ALU.mult), reads=["hf", ("gy", p)], writes=[("oc", p)])
                fw.dma("pool", fmaj["CinT"][b, k], oc[p], reads=[("oc", p)], writes=[("Cin", b, k)])
        P.end()

    def phase_merge(l, xsrc, last=False):
        P = Phase("mrg")
        wbr = P.sb([128, 3, 8, 1024], BF16)
        wmo = P.sb([128, 8, 1024], BF16)
        for x in range(3):
            fw.dma("pool", wbr[:, x], I["w_branch"][l, x].rearrange("(j p) n -> p j n", p=128), writes=["wbr"])
        fw.dma("pool", wmo, I["w_mix_out"][l].rearrange("(j p) n -> p j n", p=128), writes=["wmo"])
        NN = 256
        ins = [[P.sb([128, 8, NN], BF16) for _ in range(6)] for _ in range(2)]
        xt = [P.sb([128, 8, NN], F32) for _ in range(2)]
        mixed = P.sb([128, 8, NN], BF16)
        xo = [P.sb([128, 8, NN], F32) for _ in range(2)]
        tA = [P.sb([128, NN], F32) for _ in range(2)]
        tB = [P.sb([128, NN], F32) for _ in range(2)]
        tC = [P.sb([128, NN], F32) for _ in range(2)]
        srcs = ["AinT", "BinT", "CinT", "gaT", "gbT", "gcT"]
        it = 0
        for b in range(NBC):
            for (t0, N) in BLOCKS256:
                if last and t0 < TC:
                    continue
                p = it % 2
                it += 1
                r = 2 if t0 < TC else b
                for si, nm in enumerate(srcs):
                    fw.dma("sp", ins[p][si], fm(fmaj[nm][b])[:, :, t0:t0 + N], writes=[("in", p, si)])
                fw.dma("sp", xt[p], fm(xsrc[b])[:, :, t0:t0 + N], writes=[("xt", p)])
                for c in range(8):
                    q = c % 2
                    pss = [PS[3 * q + x] for x in range(3)]
                    for x in range(3):
                        for k in range(8):
                            mm(pss[x][:, :N], wbr[:, x, k, c * 128:(c + 1) * 128], ins[p][x][:, k, :], k == 0, k == 7,
                               ["wbr", ("in", p, x)], [("ps", 3 * q + x)])
                    tt = [tA[q], tB[q], tC[q]]
                    for x in range(3):
                        fw.op("dve", lambda e: e.tensor_tensor(out=tt[x], in0=pss[x][:, :N], in1=ins[p][3 + x][:, c, :], op=ALU.mult),
                              reads=[("ps", 3 * q + x), ("in", p, 3 + x)], writes=[("tt", q, x)])
                    fw.op("pool", lambda e: e.tensor_tensor(out=tt[0], in0=tt[0], in1=tt[1], op=ALU.add),
                          reads=[("tt", q, 0), ("tt", q, 1)], writes=[("tt", q, 0)])
                    fw.op("pool", lambda e: e.tensor_tensor(out=mixed[:, c, :], in0=tt[0], in1=tt[2], op=ALU.add),
                          reads=[("tt", q, 0), ("tt", q, 2)], writes=["mixed"])
                for c in range(8):
                    ps = PS[6 + c % 2]
                    pk = ("ps", 6 + c % 2)
                    for k in range(8):
                        mm(ps[:, :N], wmo[:, k, c * 128:(c + 1) * 128], mixed[:, k, :], k == 0, k == 7, ["wmo", "mixed"], [pk])
                    fw.op("dve", lambda e: e.scalar_tensor_tensor(out=xo[p][:, c, :], in0=ps[:, :N], scalar=modT[:, 16 + c, r:r + 1], in1=xt[p][:, c, :],
                                                                  op0=ALU.mult, op1=ALU.add),
                          reads=[pk, "modT", ("xt", p)], writes=[("xo", p)])
                fw.dma("pool", fm(xres[b])[:, :, t0:t0 + N], xo[p], reads=[("xo", p)], writes=[("xres", b, t0)])
        P.end()

    def phase_moepre(l):
        P = Phase("mpre")
        xt = [P.sb([128, 8, 512], F32) for _ in range(2)]
        sq = P.sb([128, 8, 512], F32)
        rstd = P.sb([128, 512], F32)
        tmp = [P.sb([128, 512], F32) for _ in range(2)]
        hf = P.sb([128, 8, 512], F32)
        hb = [P.sb([128, 8, 512], BF16) for _ in range(2)]
        wr = P.sb([128, 8, NE], F32)
        brt = P.sb([128, NE], F32)
        fw.dma("sp", wr, I["w_router"][l].rearrange("(j p) n -> p j n", p=128), writes=["wr"])
        fw.dma("sp", brt, I["b_router"][l].partition_broadcast(128), writes=["brt"])
        lgs = P.sb([128, NE], F32)
        top8 = P.sb([128, 8], F32)
        msk = P.sb([128, NE], F32)
        nmx = P.sb([128, 1], F32)
        ex = P.sb([128, NE], F32)
        ssum = P.sb([128, 1], F32)
        gts = P.sb([128, NE], F32)
        gTs = [P.sb([NE, 512], F32) for _ in range(2)]
        i = 0
        for b in range(NBC):
            for (t0, N) in BLOCKS:
                x_ = xt[i % 2]
                xk = ("xt", i % 2)
                hb_ = hb[i % 2]
                hbk = ("hb", i % 2)
                gt_ = gTs[i % 2]
                gtk = ("gTs", i % 2)
                i += 1
                fw.dma("sp", x_[:, :, :N], fm(xres[b])[:, :, t0:t0 + N], writes=[xk])
                r = 2 if t0 < TC else b

                def out_fn(j, x_=x_, xk=xk, N=N, r=r):
                    tm = tmp[j % 2]
                    fw.op("dve", lambda e: e.scalar_tensor_tensor(out=tm[:, :N], in0=x_[:, j, :N], scalar=mul2[:, j, r:r + 1],
                                                                  in1=rstd[:, :N], op0=ALU.mult, op1=ALU.mult),
                          reads=[xk, "rstd", "mul2"], writes=[("tmp", j % 2)])
                    fw.op("act", lambda e: e.activation(out=hf[:, j, :N], in_=tm[:, :N], func=AF.Identity, bias=modT[:, 24 + j, r:r + 1]),
                          reads=[("tmp", j % 2), "modT"], writes=["hf"])
                emit_norm(x_, N, xk, sq, rstd, mul2, r, out_fn)
                fw.op("pool", lambda e: e.tensor_copy(out=hb_[:, :, :N], in_=hf[:, :, :N]), reads=["hf"], writes=[hbk])
                fw.dma("sp", fm(fmaj["hT2"][b])[:, :, t0:t0 + N], hb_[:, :, :N], reads=[hbk], writes=[("hT2", b, t0)])
                for tt in range(N // 128):
                    lp = PS[1][:, tt * 32:(tt + 1) * 32]
                    for j in range(8):
                        mm(lp, hf[:, j, tt * 128:(tt + 1) * 128], wr[:, j, :], j == 0, j == 7, ["hf", "wr"], [("ps", 1)])
                    fw.op("dve", lambda e: e.tensor_tensor(out=lgs, in0=lp, in1=brt, op=ALU.add), reads=[("ps", 1), "brt"], writes=["lgs"])
                    fw.op("dve", lambda e: e.max(out=top8, in_=lgs), reads=["lgs"], writes=["top8"])
                    fw.op("dve", lambda e: e.tensor_scalar(out=msk, in0=lgs, scalar1=top8[:, 3:4], scalar2=None, op0=ALU.is_ge),
                          reads=["lgs", "top8"], writes=["msk"])
                    fw.op("dve", lambda e: e.tensor_scalar(out=nmx, in0=top8[:, 0:1], scalar1=-1.0, scalar2=None, op0=ALU.mult),
                          reads=["top8"], writes=["nmx"])
                    fw.op("act", lambda e: e.activation(out=ex, in_=lgs, func=AF.Exp, bias=nmx), reads=["lgs", "nmx"], writes=["ex"])
                    fw.op("dve", lambda e: e.tensor_tensor(out=ex, in0=ex, in1=msk, op=ALU.mult), reads=["ex", "msk"], writes=["ex"])
                    fw.op("dve", lambda e: e.tensor_reduce(out=ssum, in_=ex, axis=AX.X, op=ALU.add), reads=["ex"], writes=["ssum"])
                    fw.op("dve", lambda e: e.reciprocal(out=ssum, in_=ssum), reads=["ssum"], writes=["ssum"])
                    fw.op("dve", lambda e: e.tensor_scalar(out=gts, in0=ex, scalar1=ssum, scalar2=None, op0=ALU.mult),
                          reads=["ex", "ssum"], writes=["gts"])
                    fw.op("pe", lambda e: e.transpose(out=PS[2][0:NE, tt * 128:(tt + 1) * 128], in_=gts, identity=ident_f),
                          reads=["gts", "ident_f"], writes=[("ps", 2)])
                fw.op("act", lambda e: e.copy(out=gt_[:, :N], in_=PS[2][0:NE, :N]), reads=[("ps", 2)], writes=[gtk])
                fw.dma("sp", gTd[:, b * T + t0:b * T + t0 + N], gt_[:, :N], reads=[gtk], writes=[("gTd", b, t0)])
        P.end()

    def phase_moe(l):
        P = Phase("moe")
        wup = [P.sb([128, 8, 2048], BF16) for _ in range(2)]
        wdn = [P.sb([128, 8, 1024], BF16) for _ in range(2)]
        hT2 = P.sb([128, 8, GRP], BF16)
        yacc = P.sb([128, 8, GRP], F32)
        gTs = P.sb([NE, GRP], F32)
        sel = P.sb([NE, NE, 128], F32)
        bup = P.sb([128, NE, 2, 8], F32)
        bdn = P.sb([NE, D], F32)
        Ge = [P.sb([128, MB], F32) for _ in range(2)]
        actT = [P.sb([128, 8, MB], BF16) for _ in range(2)]
        tg = [P.sb([128, MB], F32) for _ in range(2)]
        tsg = [P.sb([128, MB], F32) for _ in range(2)]
        tl = [P.sb([128, MB], F32) for _ in range(2)]
        fw.dma("sp", bup, I["bupT"][l], writes=["bup"])
        fw.dma("sp", bdn, I["b_down"][l], writes=["bdn"])
        fw.op("dve", lambda e: e.tensor_scalar(out=bup[:, :, 1, :], in0=bup[:, :, 1, :], scalar1=1.0, scalar2=None, op0=ALU.add),
              reads=["bup"], writes=["bup"])
        fw.op("pool", lambda e: e.memset(sel, 0.0), writes=["sel"])
        fw.op("pool", lambda e: e.affine_select(out=sel, in_=sel, pattern=[[-1, NE], [0, 128]],
                                                compare_op=ALU.not_equal, fill=1.0, base=0, channel_multiplier=1),
              reads=["sel"], writes=["sel"])
        ngrp = NBC * T // GRP
        nblk = GRP // MB

        def loadw(i):
            e_ = i % NE
            fw.dma("pool", wup[i % 2], I["w_up"][l, e_].rearrange("(j p) n -> p j n", p=128), writes=[("wup", i % 2)])

        def loadwd(i):
            e_ = i % NE
            fw.dma("pool", wdn[i % 2], I["w_down"][l, e_].rearrange("(j p) n -> p j n", p=128), writes=[("wdn", i % 2)])
        loadw(0)
        loadwd(0)
        ci = 0
        for g in range(ngrp):
            b = g // 2
            g0 = (g % 2) * GRP
            fw.dma("sp", hT2, fm(fmaj["hT2"][b])[:, :, g0:g0 + GRP], writes=["hT2"])
            fw.dma("sp", gTs, gTd[:, b * T + g0:b * T + g0 + GRP], writes=["gTs"])
            for blk in range(nblk):
                for co in range(8):
                    ps = PS[4 + co % 2]
                    pk = ("ps", 4 + co % 2)
                    mm(ps[:, :MB], bdn[:, co * 128:(co + 1) * 128], gTs[:, blk * MB:(blk + 1) * MB], True, True, ["bdn", "gTs"], [pk])
                    fw.op("act", lambda e: e.copy(out=yacc[:, co, blk * MB:(blk + 1) * MB], in_=ps[:, :MB]), reads=[pk], writes=[("yacc", blk)])
            tasks = [(ex, blk) for ex in range(NE) for blk in range(nblk)]

            def up(ti):
                nonlocal ci
                ex, blk = tasks[ti]
                wix = wbase + ex
                wu = wup[wix % 2]
                wuk = ("wup", wix % 2)
                if blk == 0 and wix + 1 < ngrp * NE:
                    loadw(wix + 1)
                tsl = slice(blk * MB, (blk + 1) * MB)
                ge = Ge[ti % 2]
                gek = ("Ge", ti % 2)
                at = actT[ti % 2]
                atk = ("actT", ti % 2)
                mm(PS[6][:, :MB], sel[:, ex, :], gTs[:, tsl], True, True, ["sel", "gTs"], [("ps", 6)])
                fw.op("act", lambda e: e.copy(out=ge, in_=PS[6][:, :MB]), reads=[("ps", 6)], writes=[gek])
                for c in range(8):
                    q = ci % 2
                    ci += 1
                    pg = PS[2 * q]
                    pl = PS[2 * q + 1]
                    pgk = ("ps", 2 * q)
                    plk = ("ps", 2 * q + 1)
                    for k in range(8):
                        mm(pg[:, :MB], wu[:, k, c * 256:(c + 1) * 256:2], hT2[:, k, tsl], k == 0, k == 7, [wuk, "hT2"], [pgk])
                    for k in range(8):
                        mm(pl[:, :MB], wu[:, k, c * 256 + 1:(c + 1) * 256:2], hT2[:, k, tsl], k == 0, k == 7, [wuk, "hT2"], [plk])
                    g_ = tg[q]
                    s_ = tsg[q]
                    l_ = tl[q]
                    fw.op("dve", lambda e: e.tensor_scalar(out=g_, in0=pg[:, :MB], scalar1=bup[:, ex, 0, c:c + 1], scalar2=7.0, op0=ALU.add, op1=ALU.min),
                          reads=[pgk, "bup"], writes=[("tg", q)])
                    fw.op("act", lambda e: e.activation(out=s_, in_=g_, func=AF.Sigmoid, scale=1.702), reads=[("tg", q)], writes=[("tsg", q)])
                    fw.op("dve", lambda e: e.tensor_scalar(out=l_, in0=pl[:, :MB], scalar1=bup[:, ex, 1, c:c + 1], scalar2=8.0, op0=ALU.add, op1=ALU.min),
                          reads=[plk, "bup"], writes=[("tl", q)])
                    fw.op("dve", lambda e: e.scalar_tensor_tensor(out=l_, in0=l_, scalar=-6.0, in1=ge, op0=ALU.max, op1=ALU.mult),
                          reads=[("tl", q), gek], writes=[("tl", q)])
                    fw.op("pool", lambda e: e.tensor_tensor(out=g_, in0=g_, in1=s_, op=ALU.mult), reads=[("tg", q), ("tsg", q)], writes=[("tg", q)])
                    fw.op("pool", lambda e: e.tensor_tensor(out=at[:, c, :], in0=g_, in1=l_, op=ALU.mult), reads=[("tg", q), ("tl", q)], writes=[atk])

            def down(ti):
                ex, blk = tasks[ti]
                wix = wbase + ex
                wd = wdn[wix % 2]
                wdk = ("wdn", wix % 2)
                if blk == 0 and wix + 1 < ngrp * NE:
                    loadwd(wix + 1)
                tsl = slice(blk * MB, (blk + 1) * MB)
                at = actT[ti % 2]
                atk = ("actT", ti % 2)
                for co in range(8):
                    ps = PS[4 + co % 2]
                    pk = ("ps", 4 + co % 2)
                    for c in range(8):
                        mm(ps[:, :MB], wd[:, c, co * 128:(co + 1) * 128], at[:, c, :], c == 0, c == 7, [wdk, atk], [pk])
                    fw.op("dve", lambda e: e.tensor_tensor(out=yacc[:, co, tsl], in0=ps[:, :MB], in1=yacc[:, co, tsl], op=ALU.add),
                          reads=[pk, ("yacc", blk)], writes=[("yacc", blk)])

            wbase = g * NE
            up(0)
            for ti in range(len(tasks)):
                if ti + 1 < len(tasks):
                    up(ti + 1)
                down(ti)
            fw.dma("sp", fm(yT[b])[:, :, g0:g0 + GRP], yacc, reads=[("yacc", blk) for blk in range(nblk)], writes=[("yT", g)])
        P.end()

    def phase_moepost(l, last):
        P = Phase("mpost")
        xt = [P.sb([128, 8, 512], F32) for _ in range(2)]
        yt = [P.sb([128, 8, 512], F32) for _ in range(2)]
        sq = P.sb([128, 8, 512], F32)
        rstd = P.sb([128, 512], F32)
        oo = [P.sb([128, 8, 512], F32) for _ in range(2)]
        i = 0
        for b in range(NBC):
            for (t0, N) in BLOCKS:
                if last and t0 < TC:
                    continue
                p = i % 2
                i += 1
                r = 2 if t0 < TC else b
                fw.dma("sp", xt[p][:, :, :N], fm(xres[b])[:, :, t0:t0 + N], writes=[("xt", p)])
                fw.dma("sp", yt[p][:, :, :N], fm(yT[b])[:, :, t0:t0 + N], writes=[("yt", p)])
                for c in range(8):
                    fw.op("dve", lambda e: e.scalar_tensor_tensor(out=xt[p][:, c, :N], in0=yt[p][:, c, :N], scalar=modT[:, 40 + c, r:r + 1],
                                                                  in1=xt[p][:, c, :N], op0=ALU.mult, op1=ALU.add),
                          reads=[("xt", p), ("yt", p), "modT"], writes=[("xt", p)])
                if not last:
                    fw.dma("sp", fm(xres[b])[:, :, t0:t0 + N], xt[p][:, :, :N], reads=[("xt", p)], writes=[("xres", b, t0)])
                else:
                    def out_fn(j, p=p, N=N):
                        fw.op("dve", lambda e: e.scalar_tensor_tensor(out=oo[p][:, j, :N], in0=xt[p][:, j, :N], scalar=fng[:, j:j + 1],
                                                                      in1=rstd[:, :N], op0=ALU.mult, op1=ALU.mult),
                              reads=[("xt", p), "rstd", "fng"], writes=[("oo", p)])
                    emit_norm(xt[p], N, ("xt", p), sq, rstd, None, r, out_fn)
                    fw.dma("sp", fm(outT[b])[:, :, t0 - TC:t0 - TC + N], oo[p][:, :, :N], reads=[("oo", p)], writes=[("out", b, t0)])
        P.end()


    IOA = bass.IndirectOffsetOnAxis

    def phase_moepre_sparse(l):
        P = Phase("spre")
        xt = [P.sb([128, 8, 512], F32) for _ in range(2)]
        sq = P.sb([128, 8, 512], F32)
        rstd = P.sb([128, 512], F32)
        tmp = [P.sb([128, 512], F32) for _ in range(2)]
        hf = P.sb([128, 8, 512], F32)
        hb = P.sb([128, 8, 512], BF16)
        htk = [P.sb([128, 1024], BF16) for _ in range(2)]
        wr = P.sb([128, 8, NE], F32)
        brt = P.sb([128, NE], F32)
        fw.dma("sp", wr, I["w_router"][l].rearrange("(j p) n -> p j n", p=128), writes=["wr"])
        fw.dma("sp", brt, I["b_router"][l].partition_broadcast(128), writes=["brt"])
        G_all = P.sb([128, 36, NE], F32)
        M_all = P.sb([128, 36, NE], F32)
        lgs = P.sb([128, NE], F32)
        top8 = P.sb([128, 8], F32)
        nmx = P.sb([128, 1], F32)
        ex = P.sb([128, NE], F32)
        ssum = P.sb([128, 1], F32)
        zt = P.sb([128, 1024], BF16)
        zf = P.sb([128, NE], F32)
        fill = P.sb([128, NSLOT * 2 // 128], I32)
        Lm = P.sb([128, 128], F32)
        pj = P.sb([128, 8], F32)
        tokid = P.sb([128, 36, 2], I32)
        fw.dma("sp", Lm, I["Lmat"], writes=["Lm"])
        fw.dma("sp", pj, I["pj"], writes=["pj"])
        fw.dma("sp", tokid, I["tokid"], writes=["tokid"])
        fw.op("pool", lambda e: e.memset(zt, 0.0), writes=["zt"])
        fw.op("pool", lambda e: e.memset(zf, 0.0), writes=["zf"])
        fw.op("pool", lambda e: e.memset(fill, NTOK), writes=["fill"])
        fw.dma("sp", h2tok[NTOK:NTOK + 128, :], zt, reads=["zt"], writes=["h2z"])
        fw.dma("sp", gtab[NTOK:NTOK + 128, :], zf, reads=["zf"], writes=["gtz"])
        fw.dma("sp", slot_tok.rearrange("(p a) c -> p (a c)", p=128), fill, reads=["fill"], writes=["stfill"])
        i = 0
        for b in range(NBC):
            for (t0, N) in BLOCKS:
                x_ = xt[i % 2]
                xk = ("xt", i % 2)
                i += 1
                fw.dma("sp", x_[:, :, :N], fm(xres[b])[:, :, t0:t0 + N], writes=[xk])
                r = 2 if t0 < TC else b

                def out_fn(j, x_=x_, xk=xk, N=N, r=r):
                    tm = tmp[j % 2]
                    fw.op("dve", lambda e: e.scalar_tensor_tensor(out=tm[:, :N], in0=x_[:, j, :N], scalar=mul2[:, j, r:r + 1],
                                                                  in1=rstd[:, :N], op0=ALU.mult, op1=ALU.mult),
                          reads=[xk, "rstd", "mul2"], writes=[("tmp", j % 2)])
                    fw.op("act", lambda e: e.activation(out=hf[:, j, :N], in_=tm[:, :N], func=AF.Identity, bias=modT[:, 24 + j, r:r + 1]),
                          reads=[("tmp", j % 2), "modT"], writes=["hf"])
                emit_norm(x_, N, xk, sq, rstd, mul2, r, out_fn)
                fw.op("pool", lambda e: e.tensor_copy(out=hb[:, :, :N], in_=hf[:, :, :N]), reads=["hf"], writes=["hb"])
                for tt in range(N // 128):
                    gi = b * 18 + t0 // 128 + tt
                    a = gi % 2
                    for j in range(8):
                        fw.op("pe", lambda e: e.transpose(out=PSB[a][:, j * 128:(j + 1) * 128], in_=hb[:, j, tt * 128:(tt + 1) * 128], identity=ident_b),
                              reads=["hb", "ident_b"], writes=[("psb", a)])
                    fw.op("act", lambda e: e.copy(out=htk[a], in_=PSB[a]), reads=[("psb", a)], writes=[("htk", a)])
                    fw.dma("pool", h2tok[gi * 128:(gi + 1) * 128, :], htk[a], reads=[("htk", a)], writes=[("h2tok", gi)])
                    lp = PS[1][:, (tt % 4) * 32:(tt % 4) * 32 + 32]
                    for j in range(8):
                        mm(lp, hf[:, j, tt * 128:(tt + 1) * 128], wr[:, j, :], j == 0, j == 7, ["hf", "wr"], [("ps", 1)])
                    fw.op("dve", lambda e: e.tensor_tensor(out=lgs, in0=lp, in1=brt, op=ALU.add), reads=[("ps", 1), "brt"], writes=["lgs"])
                    fw.op("dve", lambda e: e.max(out=top8, in_=lgs), reads=["lgs"], writes=["top8"])
                    fw.op("dve", lambda e: e.tensor_scalar(out=M_all[:, gi, :], in0=lgs, scalar1=top8[:, 3:4], scalar2=None, op0=ALU.is_ge),
                          reads=["lgs", "top8"], writes=[("M", gi)])
                    fw.op("dve", lambda e: e.tensor_scalar(out=nmx, in0=top8[:, 0:1], scalar1=-1.0, scalar2=None, op0=ALU.mult),
                          reads=["top8"], writes=["nmx"])
                    fw.op("act", lambda e: e.activation(out=ex, in_=lgs, func=AF.Exp, bias=nmx), reads=["lgs", "nmx"], writes=["ex"])
                    fw.op("dve", lambda e: e.tensor_tensor(out=ex, in0=ex, in1=M_all[:, gi, :], op=ALU.mult), reads=["ex", ("M", gi)], writes=["ex"])
                    fw.op("dve", lambda e: e.tensor_reduce(out=ssum, in_=ex, axis=AX.X, op=ALU.add), reads=["ex"], writes=["ssum"])
                    fw.op("dve", lambda e: e.reciprocal(out=ssum, in_=ssum), reads=["ssum"], writes=["ssum"])
                    fw.op("dve", lambda e: e.tensor_scalar(out=G_all[:, gi, :], in0=ex, scalar1=ssum, scalar2=None, op0=ALU.mult),
                          reads=["ex", "ssum"], writes=[("G", gi)])
                    fw.dma("pool", gtab[gi * 128:(gi + 1) * 128, :], G_all[:, gi, :], reads=[("G", gi)], writes=[("gtab", gi)])
        cnt = P.sb([128, NE], F32)
        ntl = P.sb([128, NE], F32)
        cume = P.sb([128, NE], F32)
        cb = P.sb([128, NE], F32)
        sf = P.sb([128, NE], F32)
        t8 = P.sb([128, 8], F32)
        cm = P.sb([128, NE], F32)
        wf = P.sb([128, NTILE, 8], F32)
        bf_ = P.sb([128, NTILE], F32)
        for gi in range(36):
            mm(PS[3][:, 0:NE], ones_f, M_all[:, gi, :], gi == 0, gi == 35, ["ones_f", ("M", gi)], [("ps", 3)])
        fw.op("dve", lambda e: e.tensor_copy(out=cnt, in_=PS[3][:, 0:NE]), reads=[("ps", 3)], writes=["cnt"])
        fw.op("dve", lambda e: e.tensor_scalar(out=ntl, in0=cnt, scalar1=0.0, scalar2=None, op0=ALU.is_gt), reads=["cnt"], writes=["ntl"])
        for m in range(1, NTOK // MT + 1):
            fw.op("dve", lambda e: e.scalar_tensor_tensor(out=ntl, in0=cnt, scalar=float(m * MT), in1=ntl, op0=ALU.is_gt, op1=ALU.add),
                  reads=["cnt", "ntl"], writes=["ntl"])
        fw.op("dve", lambda e: e.tensor_tensor_scan(out=cume, data0=ones_f[:, 0:NE], data1=ntl, initial=0.0, op0=ALU.mult, op1=ALU.add),
              reads=["ntl", "ones_f"], writes=["cume"])
        fw.op("dve", lambda e: e.tensor_tensor(out=cb, in0=cume, in1=ntl, op=ALU.subtract), reads=["cume", "ntl"], writes=["cb"])
        fw.op("dve", lambda e: e.tensor_scalar(out=cb, in0=cb, scalar1=float(MT), scalar2=None, op0=ALU.mult), reads=["cb"], writes=["cb"])
        for gi in range(36):
            mm(PS[4][:, 0:NE], Lm, M_all[:, gi, :], True, True, ["Lm", ("M", gi)], [("ps", 4)])
            mm(PS[5][:, 0:NE], ones_f, M_all[:, gi, :], True, True, ["ones_f", ("M", gi)], [("ps", 5)])
            fw.op("dve", lambda e: e.tensor_tensor(out=sf, in0=PS[4][:, 0:NE], in1=cb, op=ALU.add), reads=[("ps", 4), "cb"], writes=["sf"])
            fw.op("dve", lambda e: e.scalar_tensor_tensor(out=sf, in0=sf, scalar=1.0, in1=M_all[:, gi, :], op0=ALU.add, op1=ALU.mult),
                  reads=["sf", ("M", gi)], writes=["sf"])
            fw.op("dve", lambda e: e.tensor_scalar(out=sf, in0=sf, scalar1=-1.0, scalar2=None, op0=ALU.add), reads=["sf"], writes=["sf"])
            fw.op("dve", lambda e: e.max(out=t8, in_=sf), reads=["sf"], writes=["t8"])
            fw.op("dve", lambda e: e.tensor_copy(out=S4_all[:, gi, :], in_=t8[:, 0:4]), reads=["t8"], writes=[("S4", gi)])
            fw.op("dve", lambda e: e.tensor_tensor(out=cb, in0=cb, in1=PS[5][:, 0:NE], op=ALU.add), reads=["cb", ("ps", 5)], writes=["cb"])
            for k in range(4):
                fw.idma(slot_tok, IOA(ap=S4_all[:, gi, k:k + 1], axis=0), tokid[:, gi, :], None,
                        reads=[("S4", gi), "tokid", "stfill"], writes=[("st", gi, k)])
        for t in range(NTILE):
            fw.op("dve", lambda e: e.tensor_scalar(out=cm, in0=cume, scalar1=float(t), scalar2=None, op0=ALU.is_le), reads=["cume"], writes=["cm"])
            fw.op("dve", lambda e: e.tensor_reduce(out=ETf[:, t:t + 1], in_=cm, axis=AX.X, op=ALU.add), reads=["cm"], writes=["ETf"])
        fw.op("dve", lambda e: e.tensor_scalar(out=ETf, in0=ETf, scalar1=float(NE - 1), scalar2=None, op0=ALU.min), reads=["ETf"], writes=["ETf"])
        fw.op("dve", lambda e: e.tensor_scalar(out=bf_, in0=ETf, scalar1=float(l * NE), scalar2=None, op0=ALU.add), reads=["ETf"], writes=["bf_"])
        fw.op("dve", lambda e: e.tensor_copy(out=eidx, in_=bf_), reads=["bf_"], writes=["eidx"])
        fw.op("dve", lambda e: e.tensor_scalar(out=bf_, in0=ETf, scalar1=128.0, scalar2=pj[:, 0:1], op0=ALU.mult, op1=ALU.add),
              reads=["ETf", "pj"], writes=["bf_"])
        fw.op("dve", lambda e: e.tensor_copy(out=pidx, in_=bf_), reads=["bf_"], writes=["pidx"])
        fw.op("dve", lambda e: e.tensor_scalar(out=bf_, in0=bf_, scalar1=float(l * NE * 128), scalar2=None, op0=ALU.add), reads=["bf_"], writes=["bf_"])
        fw.op("dve", lambda e: e.tensor_copy(out=bidx, in_=bf_), reads=["bf_"], writes=["bidx"])
        for j in range(8):
            fw.op("dve", lambda e: e.tensor_scalar(out=wf[:, :, j], in0=ETf, scalar1=1024.0, scalar2=pj[:, j:j + 1], op0=ALU.mult, op1=ALU.add),
                  reads=["ETf", "pj"], writes=["wf"])
        fw.op("dve", lambda e: e.tensor_scalar(out=wf, in0=wf, scalar1=float(l * NE * 1024), scalar2=None, op0=ALU.add), reads=["wf"], writes=["wf"])
        fw.op("dve", lambda e: e.tensor_copy(out=widx, in_=wf), reads=["wf"], writes=["widx"])
        P.end()

    def phase_wcast(l):
        P = Phase("wcast")
        bu = [P.sb([128, 8, 2048], BF16) for _ in range(3)]
        bd = [P.sb([128, 8, 1024], BF16) for _ in range(3)]
        for ex in range(NE):
            s_ = ex % 3
            fw.dma("pool", bu[s_], I["w_up"][l, ex].rearrange("(j p) n -> p j n", p=128), writes=[("bu", s_)])
            fw.dma("sp", wupb[ex * 128:(ex + 1) * 128, :], bu[s_].rearrange("p j n -> p (j n)"), reads=[("bu", s_)], writes=[("wupb", ex)])
            fw.dma("pool", bd[s_], I["w_down"][l, ex].rearrange("(j p) n -> p j n", p=128), writes=[("bd", s_)])
            fw.dma("sp", wdnb[ex * 128:(ex + 1) * 128, :], bd[s_].rearrange("p j n -> p (j n)"), reads=[("bd", s_)], writes=[("wdnb", ex)])
        P.end()

    def phase_moe_sparse(l):
        P = Phase("smoe")
        wup = [P.sb([128, 8, 2048], BF16) for _ in range(2)]
        wdn = [P.sb([128, 8, 1024], BF16) for _ in range(2)]
        bupg = [P.sb([128, 16], F32) for _ in range(2)]
        bdng = [P.sb([128, 1024], F32) for _ in range(2)]
        stok = [P.sb([128, 4, 2], I32) for _ in range(2)]
        htk = [[P.sb([128, 1024], BF16) for _ in range(4)] for _ in range(2)]
        grow = [P.sb([128, 4, NE], F32) for _ in range(2)]
        hsT = [P.sb([128, 8, MT], BF16) for _ in range(2)]
        at = [P.sb([128, 8, MT], BF16) for _ in range(2)]
        oh = P.sb([128, NE], F32)
        gtmp = P.sb([128, 4, NE], F32)
        gsl = [P.sb([128, 4], F32) for _ in range(2)]
        tg = [P.sb([128, MT], F32) for _ in range(2)]
        tsg = [P.sb([128, MT], F32) for _ in range(2)]
        tl = [P.sb([128, MT], F32) for _ in range(2)]
        ytmp = [P.sb([128, 512], F32) for _ in range(2)]
        yrow = [P.sb([128, 1024], F32) for _ in range(2)]
        iota = P.sb([128, NE], F32)
        fw.dma("sp", iota, I["iota32"], writes=["iota"])
        wv = I["w_up"].rearrange("l e r n -> (l e r) n")
        wdv = I["w_down"].rearrange("l e r n -> (l e r) n")
        bupv = I["bup2"].rearrange("l e p n -> (l e p) n")
        bdv = I["b_down"].rearrange("l e n -> (l e) n")

        def gather(t):
            s_ = t % 2
            fw.dma("sp", stok[s_], slot_tok[t * MT:(t + 1) * MT, :].rearrange("(a p) c -> p a c", p=128), writes=[("stok", s_)])
            for a in range(4):
                fw.idma(htk[s_][a], None, h2tok, IOA(ap=stok[s_][:, a, 0:1], axis=0), reads=[("stok", s_)], writes=[("htk", s_, a)])
            for a in range(4):
                fw.idma(grow[s_][:, a, :], None, gtab, IOA(ap=stok[s_][:, a, 0:1], axis=0), reads=[("stok", s_)], writes=[("grow", s_)])
            fw.idma(bupg[s_], None, bupv, IOA(ap=bidx[:, t:t + 1], axis=0), writes=[("bupg", s_)])
            fw.idma(bdng[s_], None, bdv, IOA(ap=eidx[:, t:t + 1], axis=0), writes=[("bdng", s_)])
            fw.idma(wup[s_].rearrange("p j n -> p (j n)"), None, wupb, IOA(ap=pidx[:, t:t + 1], axis=0), writes=[("wup", s_)])
            fw.idma(wdn[s_].rearrange("p j n -> p (j n)"), None, wdnb, IOA(ap=pidx[:, t:t + 1], axis=0), writes=[("wdn", s_)])

        gather(0)
        ci = 0
        yi = 0
        ev = 0
        for t in range(NTILE):
            s_ = t % 2
            if t + 1 < NTILE:
                gather(t + 1)
            fw.op("dve", lambda e: e.tensor_scalar(out=oh, in0=iota, scalar1=ETf[:, t:t + 1], scalar2=None, op0=ALU.is_equal),
                  reads=["iota"], writes=["oh"])
            for a in range(4):
                fw.op("dve", lambda e: e.tensor_tensor(out=gtmp[:, a, :], in0=grow[s_][:, a, :], in1=oh, op=ALU.mult),
                      reads=[("grow", s_), "oh"], writes=["gtmp"])
            fw.op("dve", lambda e: e.tensor_reduce(out=gsl[s_], in_=gtmp, axis=AX.X, op=ALU.add), reads=["gtmp"], writes=[("gsl", s_)])
            fw.op("dve", lambda e: e.tensor_scalar(out=bupg[s_][:, 8:16], in0=bupg[s_][:, 8:16], scalar1=1.0, scalar2=None, op0=ALU.add),
                  reads=[("bupg", s_)], writes=[("bupg", s_)])
            for jp in range(4):
                bank = PSB[jp % 2]
                bk = ("psb", jp % 2)
                for jj in range(2):
                    j = 2 * jp + jj
                    for a in range(4):
                        fw.op("pe", lambda e: e.transpose(out=bank[:, jj * 512 + a * 128:jj * 512 + (a + 1) * 128],
                                                          in_=htk[s_][a][:, j * 128:(j + 1) * 128], identity=ident_b),
                              reads=[("htk", s_, a), "ident_b"], writes=[bk])
                dst = hsT[s_][:, 2 * jp:2 * jp + 2, :].rearrange("p j n -> p (j n)")
                if jp % 2 == 0:
                    fw.op("act", lambda e: e.copy(out=dst, in_=bank), reads=[bk], writes=[("hsT", s_)])
                else:
                    fw.op("dve", lambda e: e.tensor_copy(out=dst, in_=bank), reads=[bk], writes=[("hsT", s_)])
            wu = wup[s_]
            wuk = ("wup", s_)
            for c in range(8):
                q = ci % 2
                ci += 1
                pg = PS[2 * q]
                pl = PS[2 * q + 1]
                pgk = ("ps", 2 * q)
                plk = ("ps", 2 * q + 1)
                for k in range(8):
                    mm(pg, wu[:, k, c * 256:(c + 1) * 256:2], hsT[s_][:, k, :], k == 0, k == 7, [wuk, ("hsT", s_)], [pgk])
                for k in range(8):
                    mm(pl, wu[:, k, c * 256 + 1:(c + 1) * 256:2], hsT[s_][:, k, :], k == 0, k == 7, [wuk, ("hsT", s_)], [plk])
                g_ = tg[q]
                sg_ = tsg[q]
                l_ = tl[q]
                fw.op("dve", lambda e: e.tensor_scalar(out=g_, in0=pg, scalar1=bupg[s_][:, c:c + 1], scalar2=7.0, op0=ALU.add, op1=ALU.min),
                      reads=[pgk, ("bupg", s_)], writes=[("tg", q)])
                fw.op("act", lambda e: e.activation(out=sg_, in_=g_, func=AF.Sigmoid, scale=1.702), reads=[("tg", q)], writes=[("tsg", q)])
                fw.op("act", lambda e: e.activation(out=l_, in_=pl, func=AF.Identity, bias=bupg[s_][:, 8 + c:9 + c]),
                      reads=[plk, ("bupg", s_)], writes=[("tl", q)])
                fw.op("dve", lambda e: e.tensor_scalar(out=l_, in0=l_, scalar1=8.0, scalar2=-6.0, op0=ALU.min, op1=ALU.max),
                      reads=[("tl", q)], writes=[("tl", q)])
                fw.op("dve", lambda e: e.tensor_tensor(out=g_, in0=g_, in1=sg_, op=ALU.mult), reads=[("tg", q), ("tsg", q)], writes=[("tg", q)])
                fw.op("dve", lambda e: e.tensor_tensor(out=at[s_][:, c, :], in0=g_, in1=l_, op=ALU.mult), reads=[("tg", q), ("tl", q)], writes=[("at", s_)])
            wd = wdn[s_]
            wdk = ("wdn", s_)
            for a in range(4):
                yr = yrow[yi % 2]
                yk = ("yrow", yi % 2)
                yi += 1
                for half in range(2):
                    ps = PS[4 + half]
                    pk = ("ps", 4 + half)
                    for c in range(8):
                        mm(ps, at[s_][:, c, a * 128:(a + 1) * 128], wd[:, c, half * 512:(half + 1) * 512], c == 0, c == 7, [("at", s_), wdk], [pk])
                    yt_ = ytmp[half]
                    fw.op("dve", lambda e: e.tensor_tensor(out=yt_, in0=ps, in1=bdng[s_][:, half * 512:(half + 1) * 512], op=ALU.add),
                          reads=[pk, ("bdng", s_)], writes=[("ytmp", half)])
                    fw.op("act", lambda e: e.activation(out=yr[:, half * 512:(half + 1) * 512], in_=yt_, func=AF.Identity, scale=gsl[s_][:, a:a + 1]),
                          reads=[("ytmp", half), ("gsl", s_)], writes=[yk])
                fw.dma("sp", ypairs[t * MT + a * 128:t * MT + (a + 1) * 128, :], yr, reads=[yk], writes=[("yp", t, a)])
        P.end()

    def phase_moepost_sparse(l, last):
        P = Phase("spost")
        xt = [P.sb([128, 8, 512], F32) for _ in range(2)]
        sq = P.sb([128, 8, 512], F32)
        rstd = P.sb([128, 512], F32)
        oo = [P.sb([128, 8, 512], F32) for _ in range(2)]
        yk = [[P.sb([128, 1024], F32) for _ in range(4)] for _ in range(2)]
        i = 0
        si = 0
        for b in range(NBC):
            for (t0, N) in BLOCKS:
                if last and t0 < TC:
                    continue
                p = i % 2
                i += 1
                r = 2 if t0 < TC else b
                fw.dma("sp", xt[p][:, :, :N], fm(xres[b])[:, :, t0:t0 + N], writes=[("xt", p)])
                for sub in range(N // 128):
                    gi = b * 18 + t0 // 128 + sub
                    u = si % 2
                    si += 1
                    for k in range(4):
                        fw.idma(yk[u][k], None, ypairs, IOA(ap=S4_all[:, gi, k:k + 1], axis=0), writes=[("yk", u, k)])
                    fw.op("dve", lambda e: e.tensor_tensor(out=yk[u][0], in0=yk[u][0], in1=yk[u][1], op=ALU.add),
                          reads=[("yk", u, 0), ("yk", u, 1)], writes=[("yk", u, 0)])
                    fw.op("pool", lambda e: e.tensor_tensor(out=yk[u][2], in0=yk[u][2], in1=yk[u][3], op=ALU.add),
                          reads=[("yk", u, 2), ("yk", u, 3)], writes=[("yk", u, 2)])
                    fw.op("dve", lambda e: e.tensor_tensor(out=yk[u][0], in0=yk[u][0], in1=yk[u][2], op=ALU.add),
                          reads=[("yk", u, 0), ("yk", u, 2)], writes=[("yk", u, 0)])
                    for c in range(8):
                        bank = PS[1 + 2 * u + c // 4]
                        fw.op("pe", lambda e: e.transpose(out=bank[:, (c % 4) * 128:(c % 4 + 1) * 128], in_=yk[u][0][:, c * 128:(c + 1) * 128], identity=ident_f),
                              reads=[("yk", u, 0), "ident_f"], writes=[("ps", 1 + 2 * u + c // 4)])
                    for c in range(8):
                        bank = PS[1 + 2 * u + c // 4]
                        fw.op("dve", lambda e: e.scalar_tensor_tensor(out=xt[p][:, c, sub * 128:(sub + 1) * 128], in0=bank[:, (c % 4) * 128:(c % 4 + 1) * 128],
                                                                      scalar=modT[:, 40 + c, r:r + 1], in1=xt[p][:, c, sub * 128:(sub + 1) * 128],
                                                                      op0=ALU.mult, op1=ALU.add),
                              reads=[("ps", 1 + 2 * u + c // 4), ("xt", p), "modT"], writes=[("xt", p)])
                if not last:
                    fw.dma("sp", fm(xres[b])[:, :, t0:t0 + N], xt[p][:, :, :N], reads=[("xt", p)], writes=[("xres", b, t0)])
                else:
                    def out_fn(j, p=p, N=N):
                        fw.op("dve", lambda e: e.scalar_tensor_tensor(out=oo[p][:, j, :N], in0=xt[p][:, j, :N], scalar=fng[:, j:j + 1],
                                                                      in1=rstd[:, :N], op0=ALU.mult, op1=ALU.mult),
                              reads=[("xt", p), "rstd", "fng"], writes=[("oo", p)])
                    emit_norm(xt[p], N, ("xt", p), sq, rstd, None, r, out_fn)
                    fw.dma("sp", fm(outT[b])[:, :, t0 - TC:t0 - TC + N], oo[p][:, :, :N], reads=[("oo", p)], writes=[("out", b, t0)])
        P.end()

    seq = []
    for l in range(nlayers):
        xsrc = I["xin"] if l == 0 else xres
        last = (l == nlayers - 1)
        seq += [("mod", lambda l=l: phase_mod(l)),
                ("mixin", lambda l=l, xsrc=xsrc: phase_mixin(l, xsrc)),
                ("ret", lambda l=l, last=last: phase_ret(l, last)),
                ("na", lambda l=l, last=last: phase_na(l, last)),
                ("lru", lambda l=l: phase_lru(l)),
                ("merge", lambda l=l, xsrc=xsrc, last=last: phase_merge(l, xsrc, last)),
                ("moepre", lambda l=l: (phase_moepre_sparse(l) if SPARSE else phase_moepre(l))),
                ("moe", lambda l=l: (phase_moe_sparse(l) if SPARSE else phase_moe(l))),
                ("moepost", lambda l=l, last=last: (phase_moepost_sparse(l, last) if SPARSE else phase_moepost(l, last)))]
    for name, fn in seq:
        fn()
        if stop_after is not None and name == stop_after:
            break
    fw.barrier()
    return nc, fw


def _consts():
    c = {}
    inv = (10000.0 ** (-np.arange(32, dtype=np.float32) / 32)).astype(np.float32)
    tok = np.arange(TL)
    rows = (tok // 64).astype(np.float32)
    cols = (tok % 64).astype(np.float32)
    ar = rows[None, :] * inv[:, None]
    ac = cols[None, :] * inv[:, None]
    C = np.ones((128, T), np.float32)
    S = np.zeros((128, T), np.float32)
    C[0:32, TC:] = np.cos(ar); C[32:64, TC:] = np.cos(ar); C[64:96, TC:] = np.cos(ac); C[96:128, TC:] = np.cos(ac)
    S[0:32, TC:] = -np.sin(ar); S[32:64, TC:] = np.sin(ar); S[64:96, TC:] = -np.sin(ac); S[96:128, TC:] = np.sin(ac)
    ks = np.float32(128.0 ** -0.5)
    c["ropeC"] = C; c["ropeS"] = S; c["ropeCk"] = C * ks; c["ropeSk"] = S * ks
    j = np.arange(128)[:, None, None]
    rel = np.arange(4)[None, :, None]
    i = np.arange(512)[None, None, :]
    diff = (i - (rel * 128 + j)).astype(np.float32)
    c["Rp"] = np.maximum(diff, 0).astype(np.float32)
    c["Rn"] = np.maximum(-diff, 0).astype(np.float32)
    jj = np.arange(128)[:, None]
    c["EA"] = (128 * np.arange(15)[None, :] - jj).astype(np.float32)
    c["EB"] = (128 * np.arange(14)[None, :] + jj + 1).astype(np.float32)
    c["I512"] = np.broadcast_to(np.arange(512, dtype=np.float32)[None, :], (128, 512)).copy()
    c["I511r"] = np.broadcast_to((511 - np.arange(512)).astype(np.float32)[None, :], (128, 512)).copy()
    c["Lmat"] = (np.arange(128)[:, None] < np.arange(128)[None, :]).astype(np.float32)
    c["pj"] = (np.arange(8)[None, :] * 128 + np.arange(128)[:, None]).astype(np.float32)
    c["iota32"] = np.broadcast_to(np.arange(NE, dtype=np.float32)[None, :], (128, NE)).copy()
    tk = (np.arange(36)[None, :] * 128 + np.arange(128)[:, None]).astype(np.int32)
    c["tokid"] = np.ascontiguousarray(np.stack([tk, tk], axis=-1))
    return c


def _na_bias_gather(rpb):
    L_ = rpb.shape[0]
    w = np.arange(64)[:, None, None]
    rr = np.arange(8)[None, :, None]
    kc = np.arange(64)[None, None, :]
    c0 = np.clip(w - 8, 0, 48)
    valid = (kc >= c0) & (kc < c0 + 16)
    dc = np.clip(kc - w + 15, 0, 30)
    out = np.empty((L_, 8, 128, 8, 512), np.float32)
    for dl in range(8):
        dr = np.clip(rr + 7 - dl, 0, 14)
        drb = np.broadcast_to(dr, (64, 8, 64))
        dcb = np.broadcast_to(dc, (64, 8, 64))
        vb = np.broadcast_to(valid, (64, 8, 64))
        g = rpb[:, :, drb, dcb]
        g = np.where(vb[None, None], g, np.float32(-30000.0)).reshape(L_, 8, 2, 64, 512)
        out[:, :, :, dl, :] = g.reshape(L_, 8, 128, 512)
    return out


def _pT(a):
    sh = a.shape[:-1]
    return np.ascontiguousarray(np.swapaxes(a.reshape(sh + (8, 128)), -1, -2))


def prep_inputs(inp):
    f = lambda k: np.asarray(inp[k], dtype=np.float32)
    x, c, ctx, c_ctx = f("x"), f("c"), f("ctx"), f("c_ctx")
    shared = {}
    shared["w_mod"] = f("w_mod")
    shared["b_modT"] = np.ascontiguousarray(f("b_mod").reshape(2, 48, 128).transpose(0, 2, 1))
    shared["n1gT"] = _pT(f("norm1_g"))
    shared["n2gT"] = _pT(f("norm2_g"))
    shared["fngT"] = _pT(f("final_norm_g"))
    shared["w_mix_in"] = f("w_mix_in")
    shared["decf"] = f("ret_decay_fwd")
    shared["decb"] = f("ret_decay_bwd")
    shared["na_bias"] = _na_bias_gather(f("na_rel_bias"))
    shared["convwT"] = np.ascontiguousarray(f("lru_conv_w").reshape(2, 4, 8, 128).transpose(0, 3, 2, 1))
    shared["convbT"] = _pT(f("lru_conv_b"))
    shared["lru_wa"] = f("lru_gate_a_w")
    shared["lru_wx"] = f("lru_gate_x_w")
    shared["lru_baT"] = np.ascontiguousarray(f("lru_gate_a_b").reshape(2, 2, 8, 128).transpose(0, 3, 1, 2))
    shared["lru_bxT"] = np.ascontiguousarray(f("lru_gate_x_b").reshape(2, 2, 8, 128).transpose(0, 3, 1, 2))
    shared["lru_lamT"] = np.ascontiguousarray(f("lru_lambda").reshape(2, 2, 8, 128).transpose(0, 3, 1, 2))
    shared["w_branch"] = f("w_branch")
    shared["w_mix_out"] = f("w_mix_out")
    shared["w_router"] = f("w_router")
    shared["b_router"] = f("b_router")
    shared["w_up"] = f("w_expert_up")
    shared["bupT"] = np.ascontiguousarray(f("b_expert_up").reshape(2, NE, 8, 128, 2).transpose(0, 3, 1, 4, 2))
    shared["bup2"] = np.ascontiguousarray(f("b_expert_up").reshape(2, NE, 8, 128, 2).transpose(0, 1, 3, 4, 2).reshape(2, NE, 128, 16))
    shared["w_down"] = f("w_expert_down")
    shared["b_down"] = f("b_expert_down")
    shared.update(_consts())
    in_maps = []
    for core in range(NCORES):
        bs = slice(core * NBC, (core + 1) * NBC)
        xa = np.concatenate([ctx[bs], x[bs]], axis=1)
        xin = np.ascontiguousarray(xa.transpose(0, 2, 1).reshape(NBC, 8, 128, T))
        crow = np.stack([c[core * NBC], c[core * NBC + 1], c_ctx], axis=0)
        cT = np.ascontiguousarray(crow.reshape(3, 8, 128).transpose(2, 1, 0))
        m = dict(shared)
        m["xin"] = xin
        m["cT"] = cT
        in_maps.append(m)
    return in_maps


def kernel(**inputs):
    in_maps = prep_inputs(inputs)
    nc, fw = build()
    res = run_bass_kernel_spmd(nc, in_maps, core_ids=list(range(NCORES)))
    outs = []
    for core in range(NCORES):
        o = res.results[core]["outT"]
        outs.append(o.reshape(NBC, D, TL).transpose(0, 2, 1))
    return np.ascontiguousarray(np.concatenate(outs, axis=0)).astype(np.float32)
```
